# Optimizing a Trainium2 kernel written in Bass

```python
import math
import jax, jax.numpy as jnp
from jax import lax
import numpy as np

D_MODEL = 1024
BATCH = 8
SEQ = 4096
DEPTH = 2

HEAD_DIM = 64
N_MOBA_HEADS = D_MODEL // (2 * HEAD_DIM)
N_DSA_HEADS = D_MODEL // (2 * HEAD_DIM)
MOBA_WIDTH = N_MOBA_HEADS * HEAD_DIM
DSA_WIDTH = N_DSA_HEADS * HEAD_DIM
MIX_WIDTH = MOBA_WIDTH + DSA_WIDTH
ROPE_THETA = 500000.0
ROPE_DIM = HEAD_DIM // 4
MOBA_BLOCK = 256
MOBA_TOPK = 3
MOBA_Q_CHUNK = 32
N_IDX_HEADS = 8
IDX_DIM = 64
DSA_TOPK = 256
DSA_Q_CHUNK = 64
D_FF = 2816
CONV_WIDTH = 3
NORM_EPS = 1e-6
N_MOD = 6
PROJ_WIDTH = 3 * MOBA_WIDTH + 3 * DSA_WIDTH + N_IDX_HEADS * IDX_DIM + IDX_DIM + N_IDX_HEADS

kernel_name = "moba_dsa_hymba_hybrid_block"


def rms_norm(x, g):
    xf = x.astype(jnp.float32)
    y = xf * lax.rsqrt(jnp.mean(xf * xf, axis=-1, keepdims=True) + NORM_EPS)
    return (y * g.astype(jnp.float32)).astype(x.dtype)


def rope_tables(seq):
    pos = jnp.arange(seq, dtype=jnp.float32)
    inv_freq = ROPE_THETA ** (-jnp.arange(0, ROPE_DIM, 2, dtype=jnp.float32) / ROPE_DIM)
    ang = pos[:, None] * inv_freq[None, :]
    return jnp.cos(ang), jnp.sin(ang)


def partial_rope(x, cos, sin):
    xf = x.astype(jnp.float32)
    half = ROPE_DIM // 2
    x1, x2, xp = xf[..., :half], xf[..., half:ROPE_DIM], xf[..., ROPE_DIM:]
    c = cos[None, :, None, :]
    s = sin[None, :, None, :]
    out = jnp.concatenate([x1 * c - x2 * s, x2 * c + x1 * s, xp], axis=-1)
    return out.astype(x.dtype)


def moba_attention(q, k, v):
    B, S, H, Dh = q.shape
    nb = -(-S // MOBA_BLOCK)
    pad = nb * MOBA_BLOCK - S
    kp = jnp.pad(k, ((0, 0), (0, pad), (0, 0), (0, 0)))
    vp = jnp.pad(v, ((0, 0), (0, pad), (0, 0), (0, 0)))
    kb = kp.reshape(B, nb, MOBA_BLOCK, H, Dh)
    vb = vp.reshape(B, nb, MOBA_BLOCK, H, Dh)
    k_mean = jnp.mean(kb.astype(jnp.float32), axis=2)
    kb_g = kb.transpose(0, 3, 1, 2, 4).reshape(B, H, nb, MOBA_BLOCK * Dh)
    vb_g = vb.transpose(0, 3, 1, 2, 4).reshape(B, H, nb, MOBA_BLOCK * Dh)
    n_sel = min(MOBA_TOPK, nb)
    C = MOBA_Q_CHUNK
    n_chunks = S // C
    scale = HEAD_DIM ** -0.5
    qc = q.reshape(B, n_chunks, C, H, Dh).swapaxes(0, 1)
    blk_ids = jnp.arange(nb)

    def chunk(args):
        ci, qb = args
        t0 = ci * C
        cur = t0 // MOBA_BLOCK
        tq = t0 + jnp.arange(C)
        qf = qb.astype(jnp.float32)
        gate = jnp.einsum('bchd,bnhd->bhcn', qf, k_mean)
        gate = jnp.where(blk_ids < cur, gate, -jnp.inf)
        _, sel = lax.top_k(gate, n_sel)
        sel_ok = jnp.repeat(sel < cur, MOBA_BLOCK, axis=-1)
        gidx = sel.reshape(B, H, C * n_sel)[..., None]
        kg = jnp.take_along_axis(kb_g, gidx, axis=2).reshape(B, H, C, n_sel * MOBA_BLOCK, Dh)
        vg = jnp.take_along_axis(vb_g, gidx, axis=2).reshape(B, H, C, n_sel * MOBA_BLOCK, Dh)
        k_own = lax.dynamic_slice_in_dim(kp, cur * MOBA_BLOCK, MOBA_BLOCK, axis=1)
        v_own = lax.dynamic_slice_in_dim(vp, cur * MOBA_BLOCK, MOBA_BLOCK, axis=1)
        own_pos = cur * MOBA_BLOCK + jnp.arange(MOBA_BLOCK)
        causal = own_pos[None, :] <= tq[:, None]
        s_sel = jnp.einsum('bchd,bhckd->bhck', qb, kg).astype(jnp.float32) * scale
        s_own = jnp.einsum('bchd,bkhd->bhck', qb, k_own).astype(jnp.float32) * scale
        s_sel = jnp.where(sel_ok, s_sel, -jnp.inf)
        s_own = jnp.where(causal[None, None], s_own, -jnp.inf)
        p = jax.nn.softmax(jnp.concatenate([s_sel, s_own], axis=-1), axis=-1).astype(v.dtype)
        p_sel, p_own = p[..., : n_sel * MOBA_BLOCK], p[..., n_sel * MOBA_BLOCK:]
        return (jnp.einsum('bhck,bhckd->bchd', p_sel, vg)
                + jnp.einsum('bhck,bkhd->bchd', p_own, v_own))

    out = lax.map(chunk, (jnp.arange(n_chunks), qc))
    return out.swapaxes(0, 1).reshape(B, S, H * Dh)


def dsa_attention(q, k, v, q_idx, k_idx, w_idx):
    B, S, H, Dh = q.shape
    n_keep = min(DSA_TOPK, S // 4)
    C = DSA_Q_CHUNK
    n_chunks = S // C
    scale = HEAD_DIM ** -0.5
    idx_scale = IDX_DIM ** -0.5
    w_scale = N_IDX_HEADS ** -0.5
    kf = k.reshape(B, S, H * Dh)
    vf = v.reshape(B, S, H * Dh)
    kif = k_idx.astype(jnp.float32)
    key_pos = jnp.arange(S)
    qc = q.reshape(B, n_chunks, C, H, Dh).swapaxes(0, 1)
    qic = q_idx.reshape(B, n_chunks, C, N_IDX_HEADS, IDX_DIM).swapaxes(0, 1)
    wc = w_idx.reshape(B, n_chunks, C, N_IDX_HEADS).swapaxes(0, 1)

    def chunk(args):
        ci, qb, qib, wb = args
        tq = ci * C + jnp.arange(C)
        logits = jnp.einsum('bchd,bsd->bchs', qib.astype(jnp.float32), kif) * idx_scale
        score = jnp.einsum('bch,bchs->bcs', wb.astype(jnp.float32) * w_scale, jax.nn.relu(logits))
        admissible = key_pos[None, :] <= tq[:, None]
        score = jnp.where(admissible[None], score, -jnp.inf)
        _, sel = lax.top_k(score, n_keep)
        sel_ok = sel <= tq[None, :, None]
        gidx = sel.reshape(B, C * n_keep)[..., None]
        kg = jnp.take_along_axis(kf, gidx, axis=1).reshape(B, C, n_keep, H, Dh)
        vg = jnp.take_along_axis(vf, gidx, axis=1).reshape(B, C, n_keep, H, Dh)
        s = jnp.einsum('bchd,bckhd->bhck', qb, kg).astype(jnp.float32) * scale
        s = jnp.where(sel_ok[:, None], s, -jnp.inf)
        p = jax.nn.softmax(s, axis=-1).astype(v.dtype)
        return jnp.einsum('bhck,bckhd->bchd', p, vg)

    out = lax.map(chunk, (jnp.arange(n_chunks), qc, qic, wc))
    return out.swapaxes(0, 1).reshape(B, S, H * Dh)


def token_mixer(h, w_in, g_moba_out, g_dsa_out, w_out, cos, sin):
    B, S, _ = h.shape
    proj = h @ w_in
    sizes = [MOBA_WIDTH] * 3 + [DSA_WIDTH] * 3 + [N_IDX_HEADS * IDX_DIM, IDX_DIM, N_IDX_HEADS]
    offs = []
    acc = 0
    for sz in sizes[:-1]:
        acc += sz
        offs.append(acc)
    mq, mk, mv, dq, dk, dv, qi, ki, wi = jnp.split(proj, offs, axis=-1)
    mq = partial_rope(mq.reshape(B, S, N_MOBA_HEADS, HEAD_DIM), cos, sin)
    mk = partial_rope(mk.reshape(B, S, N_MOBA_HEADS, HEAD_DIM), cos, sin)
    mv = mv.reshape(B, S, N_MOBA_HEADS, HEAD_DIM)
    dq = partial_rope(dq.reshape(B, S, N_DSA_HEADS, HEAD_DIM), cos, sin)
    dk = partial_rope(dk.reshape(B, S, N_DSA_HEADS, HEAD_DIM), cos, sin)
    dv = dv.reshape(B, S, N_DSA_HEADS, HEAD_DIM)
    qi = partial_rope(qi.reshape(B, S, N_IDX_HEADS, IDX_DIM), cos, sin)
    ki = partial_rope(ki[:, :, None, :], cos, sin)[:, :, 0, :]
    o_moba = moba_attention(mq, mk, mv)
    o_dsa = dsa_attention(dq, dk, dv, qi, ki, wi)
    o = jnp.concatenate([rms_norm(o_moba, g_moba_out), rms_norm(o_dsa, g_dsa_out)], axis=-1)
    return o @ w_out


def causal_depthwise_conv(u, w, b):
    out = lax.conv_general_dilated(
        u, w[:, None, :].astype(u.dtype), window_strides=(1,),
        padding=[(CONV_WIDTH - 1, 0)], dimension_numbers=('NWC', 'WIO', 'NWC'),
        feature_group_count=u.shape[-1])
    return out + b.astype(u.dtype)


def conv_glu_ffn(h, w_up_act, w_up_lin, w_conv, b_conv, w_down):
    a = causal_depthwise_conv(h @ w_up_act, w_conv, b_conv)
    return (jax.nn.gelu(a) * (h @ w_up_lin)) @ w_down


def setup_inputs(seed: int = 0) -> dict:
    key = jax.random.key(seed)
    ks = jax.random.split(key, 20)
    f32 = jnp.float32
    nrm = lambda k, shape, s: jax.random.normal(k, shape, f32) * s
    return {
        "x": nrm(ks[0], (BATCH, SEQ, D_MODEL), 1.0),
        "c": nrm(ks[1], (BATCH, D_MODEL), 1.0),
        "w_ada": nrm(ks[2], (DEPTH, D_MODEL, N_MOD * D_MODEL), 0.5 * D_MODEL ** -0.5),
        "b_ada": nrm(ks[3], (DEPTH, N_MOD * D_MODEL), 0.02),
        "g_pre_mix": 1.0 + nrm(ks[4], (DEPTH, D_MODEL), 0.02),
        "w_in": nrm(ks[5], (DEPTH, D_MODEL, PROJ_WIDTH), D_MODEL ** -0.5),
        "g_moba_out": 1.0 + nrm(ks[6], (DEPTH, MOBA_WIDTH), 0.1),
        "g_dsa_out": 1.0 + nrm(ks[7], (DEPTH, DSA_WIDTH), 0.1),
        "w_out": nrm(ks[8], (DEPTH, MIX_WIDTH, D_MODEL), MIX_WIDTH ** -0.5),
        "g_post_mix": 1.0 + nrm(ks[9], (DEPTH, D_MODEL), 0.02),
        "g_pre_ffn": 1.0 + nrm(ks[10], (DEPTH, D_MODEL), 0.02),
        "w_up_act": nrm(ks[11], (DEPTH, D_MODEL, D_FF), D_MODEL ** -0.5),
        "w_up_lin": nrm(ks[12], (DEPTH, D_MODEL, D_FF), D_MODEL ** -0.5),
        "w_conv": nrm(ks[13], (DEPTH, CONV_WIDTH, D_FF), CONV_WIDTH ** -0.5),
        "b_conv": nrm(ks[14], (DEPTH, D_FF), 0.02),
        "w_down": nrm(ks[15], (DEPTH, D_FF, D_MODEL), D_FF ** -0.5),
        "g_post_ffn": 1.0 + nrm(ks[16], (DEPTH, D_MODEL), 0.02),
    }


def reference(x, c, w_ada, b_ada, g_pre_mix, w_in, g_moba_out, g_dsa_out, w_out, g_post_mix,
              g_pre_ffn, w_up_act, w_up_lin, w_conv, b_conv, w_down, g_post_ffn):
    cos, sin = rope_tables(x.shape[1])
    c_act = jax.nn.silu(c)
    for l in range(DEPTH):
        mod = c_act @ w_ada[l] + b_ada[l]
        sh_m, sc_m, gt_m, sh_f, sc_f, gt_f = jnp.split(mod[:, None, :], N_MOD, axis=-1)
        h = rms_norm(x, g_pre_mix[l]) * (1.0 + sc_m) + sh_m
        y = token_mixer(h, w_in[l], g_moba_out[l], g_dsa_out[l], w_out[l], cos, sin)
        x = x + gt_m * rms_norm(y, g_post_mix[l])
        h = rms_norm(x, g_pre_ffn[l]) * (1.0 + sc_f) + sh_f
        y = conv_glu_ffn(h, w_up_act[l], w_up_lin[l], w_conv[l], b_conv[l], w_down[l])
        x = x + gt_f * rms_norm(y, g_post_ffn[l])
    return x
```

```python
import numpy as np
import ml_dtypes
from contextlib import ExitStack
import concourse.bass as bass
import concourse.mybir as mybir
from concourse.bass_utils import run_bass_kernel_spmd

F32 = mybir.dt.float32
BF16 = mybir.dt.bfloat16
ALU = mybir.AluOpType
AF = mybir.ActivationFunctionType
AX = mybir.AxisListType


class DSem:
    __slots__ = ("idx", "cnt")

    def __init__(self, idx):
        self.idx = idx
        self.cnt = 0


class Buf:
    __slots__ = ("name", "t", "w", "r", "dsem")

    def __init__(self, name, t=None):
        self.name = name
        self.t = t
        self.w = {}
        self.r = {}
        self.dsem = None


class _Scope:
    def __init__(self, fw):
        self.fw = fw

    def __enter__(self):
        fw = self.fw
        self.prev = (fw.es, fw.scope_dsems)
        self.stack = ExitStack()
        self.stack.__enter__()
        fw.es = self.stack
        fw.scope_dsems = []
        return self

    def __exit__(self, *a):
        fw = self.fw
        fw.barrier()
        fw.dpool.extend(fw.scope_dsems)
        fw.es, fw.scope_dsems = self.prev
        return self.stack.__exit__(*a)


class FW:
    SEM_MAX = 30000

    def __init__(self, nc, es):
        self.nc = nc
        self.es = es
        self.sem_es = es
        self.eng = {"pe": nc.tensor, "act": nc.scalar, "dve": nc.vector, "pool": nc.gpsimd, "sp": nc.sync}
        self.sems = []
        self.cur = {}
        self.own = {e: set() for e in self.eng}
        self.known = {e: {} for e in self.eng}
        self.issued = {}
        self.dpool = []
        self.scope_dsems = []
        self.nwaits = 0
        self.nops = 0
        for e in self.eng:
            self._newsem(e)

    def _alloc_sem(self, name):
        s = self.sem_es.enter_context(self.nc.semaphore(name))
        self.sems.append(s)
        return len(self.sems) - 1

    def _newsem(self, e):
        i = self._alloc_sem(f"s_{e}_{len(self.sems)}")
        self.cur[e] = [i, 0]
        self.own[e].add(i)

    def _get_dsem(self):
        if self.dpool:
            d = self.dpool.pop()
        else:
            d = DSem(self._alloc_sem(f"d_{len(self.sems)}"))
        self.scope_dsems.append(d)
        return d

    def scope(self):
        return _Scope(self)

    def sb(self, name, shape, dtype, dma=False):
        self.nuniq = getattr(self, "nuniq", 0) + 1
        t = self.es.enter_context(self.nc.sbuf_tensor(f"{name}_u{self.nuniq}", shape, dtype))
        b = Buf(name, t)
        if dma:
            b.dsem = self._get_dsem()
        return b

    def ps(self, name, shape, dtype):
        self.nuniq = getattr(self, "nuniq", 0) + 1
        t = self.es.enter_context(self.nc.psum_tensor(f"{name}_u{self.nuniq}", shape, dtype))
        return Buf(name, t)

    def view(self, name, t, dma=False):
        b = Buf(name, t)
        if dma:
            b.dsem = self._get_dsem()
        return b

    def _need(self, reads, writes):
        need = {}
        for b in reads:
            for s, v in b.w.items():
                if need.get(s, 0) < v:
                    need[s] = v
        for b in writes:
            for s, v in b.w.items():
                if need.get(s, 0) < v:
                    need[s] = v
            for s, v in b.r.items():
                if need.get(s, 0) < v:
                    need[s] = v
        return need

    def _waits(self, e, need, skip_own=False):
        k = self.known[e]
        eng = self.eng[e]
        for s, v in need.items():
            if skip_own and s in self.own[e]:
                continue
            if k.get(s, 0) >= v:
                continue
            eng.wait_ge(self.sems[s], v)
            self.nwaits += 1
            k[s] = v

    def _record(self, t, reads, writes):
        s, v = t
        self.issued[s] = v
        for b in reads:
            if b.r.get(s, 0) < v:
                b.r[s] = v
        for b in writes:
            b.w = {s: v}
            b.r = {}

    def op(self, e, fn, reads=(), writes=(), same=None):
        if same is None:
            same = (e != "pe")
        need = self._need(reads, writes)
        self._waits(e, need, skip_own=not same)
        ins = fn(self.eng[e])
        c = self.cur[e]
        c[1] += 1
        ins.then_inc(self.sems[c[0]], 1)
        self._record((c[0], c[1]), reads, writes)
        self.nops += 1
        if c[1] >= self.SEM_MAX:
            self._newsem(e)
        return ins

    def dma(self, q, out, in_, reads=(), writes=(), sem=None, **kw):
        return self.dma_group(q, [(out, in_)], reads, writes, sem, **kw)

    def dma_group(self, q, pairs, reads=(), writes=(), sem=None, **kw):
        d = sem.dsem
        need = self._need(reads, writes)
        if d.cnt:
            v = 16 * d.cnt
            if need.get(d.idx, 0) < v:
                need[d.idx] = v
        self._waits(q, need)
        for (out, in_) in pairs:
            ins = self.eng[q].dma_start(out=out, in_=in_, **kw)
            d.cnt += 1
            ins.then_inc(self.sems[d.idx], 16)
            self.nops += 1
        self._record((d.idx, 16 * d.cnt), reads, writes)

    def barrier(self):
        need = dict(self.issued)
        for e in self.eng:
            self._waits(e, need)


S = 4096
D = 1024
NT = 32
NCH = 8
DFF = 2816
NFC = 22
NCOLP = 6280
C_MV, C_DV, C_WI = 5248, 5760, 6272
BIG = 30000.0
KBIS = 24
EPS = 1e-6


class P:
    pass


def _rms_rstd(fw, ssb, rstd, nh, n):
    (ss_buf, ss_ap), (r_buf, r_ap) = ssb, rstd
    fw.op('dve', lambda e: e.tensor_scalar(out=ss_ap, in0=ss_ap, scalar1=1.0 / n, scalar2=EPS, op0=ALU.mult, op1=ALU.add),
          reads=[ss_buf], writes=[ss_buf])
    fw.op('pool', lambda e: e.tensor_tensor(out=r_ap, in0=ss_ap, in1=nh.t[:, 0:1], op=ALU.pow), reads=[ss_buf, nh], writes=[r_buf])


def build_nc(nlayers=2, phases="ABCDEF", dbg=False):
    nc = bass.Bass("TRN2", target_bir_lowering=False)
    p = P()

    def din(name, shape, dt=F32):
        return nc.dram_tensor(name, shape, dt, kind="ExternalInput").ap()

    def dscr(name, shape, dt):
        return nc.dram_tensor(name, shape, dt, kind=("ExternalOutput" if dbg else "Internal")).ap()

    x = din("x", [S, D])
    cT = din("cT", [128, 8])
    w_ada = din("w_ada", [2, D, 6 * D])
    b_ada = din("b_ada", [2, 6 * D])
    b_ada_col = din("b_ada_col", [2, 128, 48])
    g_pre_mix_col = din("g_pre_mix_col", [2, 128, 8])
    g_pre_ffn_col = din("g_pre_ffn_col", [2, 128, 8])
    g_post_mix = din("g_post_mix", [2, D])
    g_post_ffn = din("g_post_ffn", [2, D])
    g_moba_out = din("g_moba_out", [2, 512])
    g_dsa_out = din("g_dsa_out", [2, 512])
    w_in_p = din("w_in_p", [2, D, NCOLP])
    w_out = din("w_out", [2, D, D])
    w_up_act = din("w_up_act", [2, D, DFF])
    w_up_lin = din("w_up_lin", [2, D, DFF])
    w_conv_col = din("w_conv_col", [2, 128, NFC, 3])
    b_conv_col = din("b_conv_col", [2, 128, NFC])
    w_down = din("w_down", [2, DFF, D])
    ropeC = din("ropeC", [128, S])
    ropeS = din("ropeS", [128, S])
    ident_d = din("ident", [128, 128], BF16)
    tri_d = din("tri", [128, 128], BF16)
    negtri_d = din("negtri", [128, 128])
    onehot_d = din("onehot", [16, S], BF16)
    cbsel_d = din("cbsel", [128, 512])
    cblt_d = din("cblt", [128, 512])
    cbfin_d = din("cbfin", [128, 512])
    cpow_d = din("cpow", [128, KBIS])
    out = nc.dram_tensor("out", [S, D], F32, kind="ExternalOutput").ap()

    scrT = [dscr(n, [512, S], BF16) for n in ("mqT", "mkT", "dqT", "dkT", "qiT")]
    kiT_d = dscr("kiT", [64, S], BF16)
    mva = dscr("mva", [S, 520], BF16)
    dva = dscr("dva", [S, 520], BF16)
    om_d = dscr("om", [S, 512], F32)
    od_d = dscr("od", [S, 512], F32)
    x1_d = dscr("x1", [S, D], F32)
    x2_d = dscr("x2", [S, D], F32)
    gbc_d = dscr("gbc", [2, 2, 128, D], F32)

    with ExitStack() as es:
        fw = FW(nc, es)
        AB = fw.sb("AB", [128, 2, 4, 8], F32)
        WI = fw.sb("WI", [128, NT, 8], F32)
        ksum = fw.sb("ksum", [128, 4, 16], F32)
        ident = fw.sb("identb", [128, 128], BF16, dma=True)
        nh = fw.sb("neghalf", [128, 1], F32)
        fw.dma('sp', ident.t[:], ident_d[:, :], writes=[ident], sem=ident)
        fw.op('dve', lambda e: e.memset(nh.t[:], -0.5), writes=[nh])

        if "A" in phases:
          with fw.scope():
            ct = fw.sb("ct", [128, 8], F32, dma=True)
            sc = fw.sb("sc", [128, 8], F32)
            screp = fw.sb("screp", [128, 8, 128], F32)
            ones1 = fw.sb("ones1", [1, 128], F32)
            wa = [fw.sb(f"wa{i}", [128, 8, 1024], F32, dma=True) for i in range(2)]
            brow = [fw.sb(f"brow{i}", [1, 1024], F32, dma=True) for i in range(2)]
            bcol = fw.sb("bcol", [128, 2, 48], F32, dma=True)
            gcol = fw.sb("gcol", [128, 2, 2, 8], F32, dma=True)
            gpb = [fw.sb(f"gpb{i}", [128, 1024], F32, dma=True) for i in range(2)]
            colps = fw.ps("colps", [128, 512], F32)
            bcps = [fw.ps(f"bcps{i}", [128, 512], F32) for i in range(2)]
            mcol = fw.sb("mcol", [128, 8], F32)
            Gt = [fw.sb(f"Gt{i}", [128, D], F32, dma=True) for i in range(2)]
            fw.dma('sp', ct.t[:], cT[:, :], writes=[ct], sem=ct)
            fw.dma_group('sp', [(bcol.t[:, l, :], b_ada_col[l]) for l in range(2)], writes=[bcol], sem=bcol)
            fw.dma_group('sp', [(gcol.t[:, l, 0, :], g_pre_mix_col[l]) for l in range(2)]
                         + [(gcol.t[:, l, 1, :], g_pre_ffn_col[l]) for l in range(2)], writes=[gcol], sem=gcol)
            fw.op('act', lambda e: e.activation(out=sc.t[:], in_=ct.t[:], func=AF.Silu), reads=[ct], writes=[sc])
            fw.op('dve', lambda e: e.tensor_copy(out=screp.t[:], in_=sc.t[:, :].unsqueeze(2).to_broadcast([128, 8, 128])),
                  reads=[sc], writes=[screp])
            fw.op('dve', lambda e: e.memset(ones1.t[:], 1.0), writes=[ones1])
            pi = 0
            for l in range(nlayers):
                wv = w_ada[l].rearrange("(k p) n -> p k n", p=128)
                for piece in range(6):
                    w = wa[pi % 2]
                    pi += 1
                    fw.dma_group('sp', [(w.t[:, k, :], wv[:, k, piece * 1024:(piece + 1) * 1024]) for k in range(8)],
                                 writes=[w], sem=w)
                    if piece in (2, 5):
                        j = 0 if piece == 2 else 1
                        br = brow[j]
                        gp = gpb[j]
                        fw.dma('sp', br.t[:], b_ada[l:l + 1, piece * 1024:(piece + 1) * 1024], writes=[br], sem=br)
                        fw.dma('sp', gp.t[:], (g_post_mix if j == 0 else g_post_ffn)[l, :].partition_broadcast(128),
                               writes=[gp], sem=gp)
                        for nhf in range(2):
                            ps = bcps[nhf]
                            for k in range(8):
                                fw.op('pe', lambda e: e.matmul(ps.t[:], lhsT=screp.t[:, k, :], rhs=w.t[:, k, nhf * 512:(nhf + 1) * 512],
                                                               start=(k == 0), stop=False), reads=[screp, w], writes=[ps])
                            fw.op('pe', lambda e: e.matmul(ps.t[:], lhsT=ones1.t[0:1, :], rhs=br.t[0:1, nhf * 512:(nhf + 1) * 512],
                                                           start=False, stop=True), reads=[ones1, br], writes=[ps])
                            G = Gt[j]
                            fw.op('dve', lambda e: e.tensor_tensor(out=G.t[:, nhf * 512:(nhf + 1) * 512], in0=ps.t[:],
                                                                   in1=gp.t[:, nhf * 512:(nhf + 1) * 512], op=ALU.mult),
                                  reads=[ps, gp], writes=[G])
                        fw.dma('sp', gbc_d[l, j], Gt[j].t[:], reads=[Gt[j]], sem=Gt[j])
                    else:
                        for jj in range(8):
                            for k in range(8):
                                fw.op('pe', lambda e: e.matmul(colps.t[:, jj:jj + 1], lhsT=w.t[:, k, jj * 128:(jj + 1) * 128],
                                                               rhs=sc.t[:, k:k + 1], start=(k == 0), stop=(k == 7)),
                                      reads=[sc, w], writes=[colps])
                        fw.op('dve', lambda e: e.tensor_tensor(out=mcol.t[:], in0=colps.t[:, 0:8],
                                                               in1=bcol.t[:, l, piece * 8:(piece + 1) * 8], op=ALU.add),
                              reads=[colps, bcol], writes=[mcol])
                        if piece in (0, 3):
                            slot = 1 if piece == 0 else 3
                            fw.op('dve', lambda e: e.tensor_copy(out=AB.t[:, l, slot, :], in_=mcol.t[:]), reads=[mcol], writes=[AB])
                        else:
                            slot = 0 if piece == 1 else 2
                            gi = 0 if piece == 1 else 1
                            fw.op('dve', lambda e: e.scalar_tensor_tensor(out=AB.t[:, l, slot, :], in0=mcol.t[:], scalar=1.0,
                                                                          in1=gcol.t[:, l, gi, :], op0=ALU.add, op1=ALU.mult),
                                  reads=[mcol, gcol], writes=[AB])

        def norm_transpose(xb, l, slot, tp, hbuf, hview, sq, ss, rstd, xn):
            fw.op('dve', lambda e: e.memset(ss.t[:], 0.0), writes=[ss])
            fw.op('act', lambda e: e.activation(out=sq.t[:], in_=xb.t[:], func=AF.Square, accum_out=ss.t[:, 0:1]),
                  reads=[xb], writes=[sq, ss])
            _rms_rstd(fw, (ss, ss.t[:, 0:1]), (rstd, rstd.t[:, 0:1]), nh, D)
            fw.op('dve', lambda e: e.tensor_scalar(out=xn.t[:], in0=xb.t[:], scalar1=rstd.t[:, 0:1], scalar2=None, op0=ALU.mult),
                  reads=[xb, rstd], writes=[xn])
            for k in range(8):
                fw.op('pe', lambda e: e.transpose(out=tp.t[:, k, :], in_=xn.t[:, k * 128:(k + 1) * 128], identity=ident.t[:]),
                      reads=[xn, ident], writes=[tp])
            for k in range(8):
                fw.op('act', lambda e: e.activation(out=hview(k), in_=tp.t[:, k, :], func=AF.Identity,
                                                    scale=AB.t[:, l, slot, k:k + 1], bias=AB.t[:, l, slot + 1, k:k + 1]),
                      reads=[tp, AB], writes=[hbuf])

        for l in range(nlayers):
            xin = x if l == 0 else x2_d
            xout = out if l == nlayers - 1 else x2_d
            if "B" in phases:
              with fw.scope():
                W = fw.sb("winb", [128, 8, NCOLP], BF16, dma=True)
                fw.dma_group('pool', [(W.t[:, k, :], w_in_p[l, k * 128:(k + 1) * 128, :]) for k in range(8)], writes=[W], sem=W)
                rC = fw.sb("rC", [128, S], F32, dma=True)
                rS = fw.sb("rS", [128, S], F32, dma=True)
                fw.dma('sp', rC.t[:], ropeC[:, :], writes=[rC], sem=rC)
                fw.dma('sp', rS.t[:], ropeS[:, :], writes=[rS], sem=rS)
                xt = [fw.sb(f"xt{i}", [128, D], F32, dma=True) for i in range(2)]
                sq = fw.sb("sq", [128, D], BF16)
                ss = [fw.sb(f"ss{i}", [128, 1], F32) for i in range(2)]
                rstd = [fw.sb(f"rstd{i}", [128, 1], F32) for i in range(2)]
                xn = [fw.sb(f"xn{i}", [128, D], BF16) for i in range(2)]
                tps = [fw.ps(f"tp{i}", [128, 8, 128], BF16) for i in range(2)]
                hT = [fw.sb(f"hT{i}", [128, 8, 512], BF16) for i in range(2)]
                pm = [fw.ps(f"pm{i}", [128, 512], F32) for i in range(2)]
                pp = [fw.ps(f"pp{i}", [128, 512], F32) for i in range(2)]
                pv = [fw.ps(f"pv{i}", [128, 512], F32) for i in range(2)]
                t1 = [fw.sb(f"t1_{i}", [128, 512], F32) for i in range(2)]
                t2 = [fw.sb(f"t2_{i}", [128, 512], F32) for i in range(2)]
                t3 = [fw.sb(f"t3_{i}", [128, 512], F32) for i in range(2)]
                ob = [fw.sb(f"ob{i}", [128, 512], BF16, dma=True) for i in range(3)]
                vt = [fw.sb(f"vt{i}", [128, 8, 65], BF16, dma=True) for i in range(2)]
                for v in vt:
                    fw.op('dve', lambda e: e.memset(v.t[:], 1.0), writes=[v])
                fj = 0
                oj = 0
                vj = 0
                for c in range(NCH):
                    h = hT[c % 2]
                    cs = slice(c * 512, (c + 1) * 512)
                    for i in range(4):
                        tt = c * 4 + i
                        xb = xt[tt % 2]
                        fw.dma('sp', xb.t[:], xin[tt * 128:(tt + 1) * 128, :], writes=[xb], sem=xb)
                        norm_transpose(xb, l, 0, tps[tt % 2], h, lambda k: h.t[:, k, i * 128:(i + 1) * 128],
                                       sq, ss[tt % 2], rstd[tt % 2], xn[tt % 2])
                    tiles = [(g, ft, 128) for g in range(5) for ft in range(4)] + [(5, 0, 64)]
                    for (g, ft, M) in tiles:
                        cm = g * 1024 + ft * 128 if g < 5 else 5120
                        cp = cm + 512 if g < 5 else 5184
                        pmain, ppart = pm[fj % 2], pp[fj % 2]
                        a1, a2, a3 = t1[fj % 2], t2[fj % 2], t3[fj % 2]
                        fj += 1
                        for k in range(8):
                            fw.op('pe', lambda e: e.matmul(pmain.t[0:M, :], lhsT=W.t[:, k, cm:cm + M], rhs=h.t[:, k, :],
                                                           start=(k == 0), stop=(k == 7)), reads=[W, h], writes=[pmain])
                        for k in range(8):
                            fw.op('pe', lambda e: e.matmul(ppart.t[0:M, :], lhsT=W.t[:, k, cp:cp + M], rhs=h.t[:, k, :],
                                                           start=(k == 0), stop=(k == 7)), reads=[W, h], writes=[ppart])
                        fw.op('dve', lambda e: e.tensor_tensor(out=a1.t[0:M, :], in0=ppart.t[0:M, :], in1=rS.t[0:M, cs], op=ALU.mult),
                              reads=[ppart, rS], writes=[a1])
                        fw.op('dve', lambda e: e.tensor_tensor(out=a2.t[0:M, :], in0=pmain.t[0:M, :], in1=rC.t[0:M, cs], op=ALU.mult),
                              reads=[pmain, rC], writes=[a2])
                        o = ob[oj % 3]
                        oj += 1
                        if g == 1:
                            fw.op('pool', lambda e: e.tensor_tensor(out=a3.t[:], in0=a1.t[:], in1=a2.t[:], op=ALU.add),
                                  reads=[a1, a2], writes=[a3])
                            fw.op('act', lambda e: e.copy(out=o.t[:], in_=a3.t[:]), reads=[a3], writes=[o])
                            fw.op('dve', lambda e: e.reduce_sum(out=ksum.t[:, ft, 2 * c:2 * c + 2],
                                                                in_=a3.t[:, :].rearrange("p (b s) -> p b s", b=2), axis=AX.X),
                                  reads=[a3], writes=[ksum])
                        else:
                            fw.op('pool', lambda e: e.tensor_tensor(out=o.t[0:M, :], in0=a1.t[0:M, :], in1=a2.t[0:M, :], op=ALU.add),
                                  reads=[a1, a2], writes=[o])
                        dst = scrT[g][ft * 128:(ft + 1) * 128, cs] if g < 5 else kiT_d[:, cs]
                        fw.dma('sp', dst, o.t[0:M, :], reads=[o], sem=o)
                    for i in range(4):
                        tt = c * 4 + i
                        for (dst, c0) in ((mva, C_MV), (dva, C_DV)):
                            ps = pv[vj % 2]
                            v = vt[vj % 2]
                            vj += 1
                            for k in range(8):
                                fw.op('pe', lambda e: e.matmul(ps.t[:], lhsT=h.t[:, k, i * 128:(i + 1) * 128], rhs=W.t[:, k, c0:c0 + 512],
                                                               start=(k == 0), stop=(k == 7)), reads=[W, h], writes=[ps])
                            fw.op('act', lambda e: e.copy(out=v.t[:, :, 0:64], in_=ps.t[:, :].rearrange("p (h d) -> p h d", h=8)),
                                  reads=[ps], writes=[v])
                            fw.dma('sp', dst[tt * 128:(tt + 1) * 128, :], v.t[:, :, :].rearrange("p h d -> p (h d)"), reads=[v], sem=v)
                        ps = pv[vj % 2]
                        vj += 1
                        for k in range(8):
                            fw.op('pe', lambda e: e.matmul(ps.t[:, 0:8], lhsT=h.t[:, k, i * 128:(i + 1) * 128], rhs=W.t[:, k, C_WI:C_WI + 8],
                                                           start=(k == 0), stop=(k == 7)), reads=[W, h], writes=[ps])
                        fw.op('act', lambda e: e.copy(out=WI.t[:, tt, :], in_=ps.t[:, 0:8]), reads=[ps], writes=[WI])
            if "C" in phases:
              with fw.scope():
                V = fw.sb("mV", [128, NT, 520], BF16, dma=True)
                fw.dma('sp', V.t[:], mva.rearrange("(n p) c -> p n c", p=128), writes=[V], sem=V)
                Qt = [fw.sb(f"mQ{i}", [80, S], BF16) for i in range(2)]
                Kt = [fw.sb(f"mK{i}", [80, S], BF16) for i in range(2)]
                Qm = [fw.view(f"mQm{i}", Qt[i].t, dma=True) for i in range(2)]
                Qb = [fw.view(f"mQb{i}", Qt[i].t) for i in range(2)]
                Km = [fw.view(f"mKm{i}", Kt[i].t, dma=True) for i in range(2)]
                Kc = [fw.view(f"mKc{i}", Kt[i].t, dma=True) for i in range(2)]
                for i in range(2):
                    fw.dma('sp', Kt[i].t[64:80, :], onehot_d[:, :], writes=[Kc[i]], sem=Kc[i])
                kmb = fw.sb("kmb", [64, 8, 16], BF16)
                for h in range(8):
                    e_, ft = h % 2, h // 2
                    fw.op('act', lambda e: e.mul(out=kmb.t[0:64, h, :], in_=ksum.t[e_ * 64:(e_ + 1) * 64, ft, :], mul=1.0 / 256.0),
                          reads=[ksum], writes=[kmb])
                tri = fw.sb("tri", [128, 128], BF16, dma=True)
                fw.dma('sp', tri.t[:], tri_d[:, :], writes=[tri], sem=tri)
                cbs = fw.sb("cbs", [128, 3, 512], F32, dma=True)
                fw.dma_group('sp', [(cbs.t[:, 0, :], cbsel_d[:, :]), (cbs.t[:, 1, :], cblt_d[:, :]), (cbs.t[:, 2, :], cbfin_d[:, :])],
                             writes=[cbs], sem=cbs)
                gps = fw.ps("gps", [128, 512], F32)
                tb = fw.ps("tb", [128, 1024], BF16)
                sps = [fw.ps(f"sp{i}", [128, 512], F32) for i in range(3)]
                ops_ = [fw.ps(f"op{i}", [128, 512], F32) for i in range(2)]
                pt = [fw.sb(f"pt{i}", [128, 512], BF16) for i in range(3)]
                gm = fw.sb("gm", [128, 512], F32)
                ee = fw.sb("ee", [128, 512], F32)
                g2 = fw.sb("g2", [128, 512], F32)
                g3 = fw.sb("g3", [128, 512], F32)
                mx = fw.sb("mx", [128, 3, 32], F32)
                bt = fw.sb("bt", [128, 512], BF16)
                rec = [fw.sb(f"rec{i}", [128, 4, 1], F32) for i in range(2)]
                osb = [fw.sb(f"osb{i}", [128, 4, 64], F32, dma=True) for i in range(2)]
                v3 = lambda t: t[:, :].rearrange("p (a n) -> p a n", n=16)
                bc3 = lambda j: mx.t[:, j, :].unsqueeze(2).to_broadcast([128, 32, 16])
                si = 0
                oi = 0
                for h in range(8):
                    b = h % 2
                    fw.dma('sp', Qt[b].t[0:64, :], scrT[0][h * 64:(h + 1) * 64, :], writes=[Qm[b]], sem=Qm[b])
                    fw.dma('sp', Kt[b].t[0:64, :], scrT[1][h * 64:(h + 1) * 64, :], writes=[Km[b]], sem=Km[b])
                    for tt in range(NT):
                        fw.op('pe', lambda e: e.matmul(gps.t[:, tt * 16:(tt + 1) * 16], lhsT=Qt[b].t[0:64, tt * 128:(tt + 1) * 128],
                                                       rhs=kmb.t[0:64, h, :], start=True, stop=True), reads=[Qm[b], kmb], writes=[gps])
                    fw.op('dve', lambda e: e.tensor_tensor(out=gm.t[:], in0=gps.t[:], in1=cbs.t[:, 0, :], op=ALU.add),
                          reads=[gps, cbs], writes=[gm])
                    fw.op('dve', lambda e: e.reduce_max(out=mx.t[:, 0, :], in_=v3(gm.t), axis=AX.X), reads=[gm], writes=[mx])
                    fw.op('dve', lambda e: e.tensor_tensor(out=v3(ee.t), in0=v3(gm.t), in1=bc3(0), op=ALU.is_equal),
                          reads=[gm, mx], writes=[ee])
                    fw.op('dve', lambda e: e.scalar_tensor_tensor(out=g2.t[:], in0=ee.t[:], scalar=-1e9, in1=gm.t[:], op0=ALU.mult, op1=ALU.add),
                          reads=[ee, gm], writes=[g2])
                    fw.op('dve', lambda e: e.reduce_max(out=mx.t[:, 1, :], in_=v3(g2.t), axis=AX.X), reads=[g2], writes=[mx])
                    fw.op('dve', lambda e: e.tensor_tensor(out=v3(ee.t), in0=v3(g2.t), in1=bc3(1), op=ALU.is_equal),
                          reads=[g2, mx], writes=[ee])
                    fw.op('dve', lambda e: e.scalar_tensor_tensor(out=g3.t[:], in0=ee.t[:], scalar=-1e9, in1=g2.t[:], op0=ALU.mult, op1=ALU.add),
                          reads=[ee, g2], writes=[g3])
                    fw.op('dve', lambda e: e.reduce_max(out=mx.t[:, 2, :], in_=v3(g3.t), axis=AX.X), reads=[g3], writes=[mx])
                    fw.op('dve', lambda e: e.tensor_tensor(out=v3(ee.t), in0=v3(gm.t), in1=bc3(2), op=ALU.is_lt),
                          reads=[gm, mx], writes=[ee])
                    fw.op('dve', lambda e: e.tensor_tensor(out=g2.t[:], in0=ee.t[:], in1=cbs.t[:, 1, :], op=ALU.mult),
                          reads=[ee, cbs], writes=[g2])
                    fw.op('dve', lambda e: e.tensor_tensor(out=bt.t[:], in0=g2.t[:], in1=cbs.t[:, 2, :], op=ALU.add),
                          reads=[g2, cbs], writes=[bt])
                    for tt in range(NT):
                        fw.op('pe', lambda e: e.transpose(out=tb.t[0:16, (tt % 4) * 128:(tt % 4 + 1) * 128], in_=bt.t[:, tt * 16:(tt + 1) * 16],
                                                          identity=ident.t[:]), reads=[bt, ident], writes=[tb])
                        if tt % 4 == 3:
                            q4 = tt // 4
                            fw.op('act', lambda e: e.copy(out=Qt[b].t[64:80, q4 * 512:(q4 + 1) * 512], in_=tb.t[0:16, 0:512]),
                                  reads=[tb], writes=[Qb[b]])
                    for qc in range(NCH):
                        O = ops_[oi % 2]
                        rc = rec[oi % 2]
                        ob_ = osb[oi % 2]
                        oi += 1
                        first = True
                        nst = 4 * qc + 4
                        for st in range(nst):
                            sp_ = sps[si % 3]
                            Pt = pt[si % 3]
                            si += 1
                            fw.op('pe', lambda e: e.matmul(sp_.t[:], lhsT=Kt[b].t[0:80, st * 128:(st + 1) * 128],
                                                           rhs=Qt[b].t[0:80, qc * 512:(qc + 1) * 512], start=True, stop=True),
                                  reads=[Km[b], Kc[b], Qm[b], Qb[b]], writes=[sp_])
                            fw.op('act', lambda e: e.activation(out=Pt.t[:], in_=sp_.t[:], func=AF.Exp, scale=0.125),
                                  reads=[sp_], writes=[Pt])
                            j = st - 4 * qc
                            if j >= 0:
                                fw.op('pool', lambda e: e.tensor_tensor(out=Pt.t[:, j * 128:(j + 1) * 128], in0=Pt.t[:, j * 128:(j + 1) * 128],
                                                                        in1=tri.t[:], op=ALU.mult), reads=[Pt, tri], writes=[Pt])
                            for i in range(max(j, 0), 4):
                                fw.op('pe', lambda e: e.matmul(O.t[:, i * 65:(i + 1) * 65], lhsT=Pt.t[:, i * 128:(i + 1) * 128],
                                                               rhs=V.t[:, st, h * 65:(h + 1) * 65], start=first, stop=(st == 4 * qc + i)),
                                      reads=[Pt, V], writes=[O])
                                first = False
                        Ov = O.t[:, 0:260].rearrange("p (i d) -> p i d", d=65)
                        fw.op('dve', lambda e: e.reciprocal(out=rc.t[:], in_=Ov[:, :, 64:65]), reads=[O], writes=[rc])
                        fw.op('dve', lambda e: e.tensor_tensor(out=ob_.t[:], in0=Ov[:, :, 0:64], in1=rc.t[:, :, :].to_broadcast([128, 4, 64]),
                                                               op=ALU.mult), reads=[O, rc], writes=[ob_])
                        fw.dma('sp', om_d[qc * 512:(qc + 1) * 512, h * 64:(h + 1) * 64].rearrange("(i p) d -> p i d", p=128), ob_.t[:],
                               reads=[ob_], sem=ob_)

            if "D" in phases:
              with fw.scope():
                V = fw.sb("dV", [128, NT, 520], BF16, dma=True)
                fw.dma('sp', V.t[:], dva.rearrange("(n p) c -> p n c", p=128), writes=[V], sem=V)
                K2 = fw.sb("K2", [128, 4, S], BF16, dma=True)
                fw.dma('sp', K2.t[:], scrT[3].rearrange("(hp two d) t -> (two d) hp t", two=2, d=64), writes=[K2], sem=K2)
                ki = fw.sb("kiT", [64, S], BF16, dma=True)
                fw.dma('sp', ki.t[:], kiT_d[:, :], writes=[ki], sem=ki)
                negtri = fw.sb("negtri", [128, 128], F32, dma=True)
                fw.dma('sp', negtri.t[:], negtri_d[:, :], writes=[negtri], sem=negtri)
                cpow = fw.sb("cpow", [128, KBIS], F32, dma=True)
                fw.dma('sp', cpow.t[:], cpow_d[:, :], writes=[cpow], sem=cpow)
                QI = [fw.sb(f"QI{i}", [64, 8, 128], BF16, dma=True) for i in range(2)]
                Q2 = [fw.sb(f"Q2{i}", [128, 4, 128], BF16, dma=True) for i in range(2)]
                score = [fw.sb(f"score{i}", [128, S], F32) for i in range(2)]
                junk = fw.sb("junk", [128, S], BF16)
                mb = [fw.sb(f"mb{i}", [128, S], BF16) for i in range(2)]
                tmp = [fw.sb(f"rl{i}", [128, 512], F32) for i in range(3)]
                lps = [fw.ps(f"lps{i}", [128, 512], F32) for i in range(2)]
                spsd = [[fw.ps(f"dsp{i}{e_}", [128, 512], F32) for e_ in range(2)] for i in range(2)]
                opsd = [fw.ps(f"dop{e_}", [128, 512], F32) for e_ in range(2)]
                ptd = [fw.sb(f"dpt{i}", [128, 2, 512], BF16) for i in range(2)]
                sm = [fw.sb(f"sm{i}", [128, 8], F32) for i in range(2)]
                dkt = [fw.sb(f"dk{i}", [128, KBIS], F32) for i in range(2)]
                recd = [fw.sb(f"drec{e_}", [128, 4, 1], F32) for e_ in range(2)]
                osbd = [fw.sb(f"dosb{i}", [128, 4, 2, 64], F32, dma=True) for i in range(2)]
                qiv = scrT[4].rearrange("(h d) t -> d h t", d=64)
                q2v = scrT[2].rearrange("(hp two d) t -> (two d) hp t", two=2, d=64)
                cnt_ = {"li": 0, "ti": 0, "ai": 0}

                def indexer(qt):
                    L = (qt + 1) * 128
                    sc_ = score[qt % 2]
                    qi_ = QI[qt % 2]
                    fw.dma('sp', qi_.t[:], qiv[:, :, qt * 128:(qt + 1) * 128], writes=[qi_], sem=qi_)
                    fw.dma('sp', Q2[qt % 2].t[:], q2v[:, :, qt * 128:(qt + 1) * 128], writes=[Q2[qt % 2]], sem=Q2[qt % 2])
                    nch = (L + 511) // 512
                    for ch in range(nch):
                        ncol = min(512, L - ch * 512)
                        cs = slice(ch * 512, ch * 512 + ncol)
                        for h in range(8):
                            lp = lps[cnt_["li"] % 2]
                            cnt_["li"] += 1
                            fw.op('pe', lambda e: e.matmul(lp.t[:, 0:ncol], lhsT=qi_.t[0:64, h, :], rhs=ki.t[0:64, cs], start=True, stop=True),
                                  reads=[qi_, ki], writes=[lp])
                            if h == 0:
                                fw.op('dve', lambda e: e.tensor_scalar(out=sc_.t[:, cs], in0=lp.t[:, 0:ncol], scalar1=0.0,
                                                                       scalar2=WI.t[:, qt, h:h + 1], op0=ALU.max, op1=ALU.mult),
                                      reads=[lp, WI], writes=[sc_])
                            else:
                                tm = tmp[cnt_["ti"] % 3]
                                cnt_["ti"] += 1
                                fw.op('dve', lambda e: e.tensor_scalar(out=tm.t[:, 0:ncol], in0=lp.t[:, 0:ncol], scalar1=0.0,
                                                                       scalar2=WI.t[:, qt, h:h + 1], op0=ALU.max, op1=ALU.mult),
                                      reads=[lp, WI], writes=[tm])
                                fw.op('pool', lambda e: e.tensor_tensor(out=sc_.t[:, cs], in0=sc_.t[:, cs], in1=tm.t[:, 0:ncol], op=ALU.add),
                                      reads=[sc_, tm], writes=[sc_])

                def select(qt):
                    L = (qt + 1) * 128
                    sc_ = score[qt % 2]
                    s_ = sm[qt % 2]
                    dk_ = dkt[qt % 2]
                    mb_ = mb[qt % 2]
                    if qt >= 2:
                        fw.op('dve', lambda e: e.reduce_max(out=s_.t[:, 0:1], in_=sc_.t[:, 0:L], axis=AX.X), reads=[sc_], writes=[s_])
                        fw.op('dve', lambda e: e.tensor_reduce(out=s_.t[:, 1:2], in_=sc_.t[:, 0:L], axis=AX.X, op=ALU.min), reads=[sc_], writes=[s_])
                        fw.op('dve', lambda e: e.scalar_tensor_tensor(out=s_.t[:, 2:3], in0=s_.t[:, 1:2], scalar=-1.0, in1=s_.t[:, 0:1],
                                                                      op0=ALU.mult, op1=ALU.max), reads=[s_], writes=[s_])
                    fw.op('pool', lambda e: e.tensor_tensor(out=sc_.t[:, qt * 128:L], in0=sc_.t[:, qt * 128:L], in1=negtri.t[:], op=ALU.add),
                          reads=[sc_, negtri], writes=[sc_])
                    if qt >= 2:
                        fw.op('dve', lambda e: e.tensor_scalar(out=dk_.t[:], in0=cpow.t[:], scalar1=s_.t[:, 2:3], scalar2=None, op0=ALU.mult),
                              reads=[cpow, s_], writes=[dk_])
                        fw.op('dve', lambda e: e.memset(s_.t[:, 3:4], 0.0), writes=[s_])
                        for k in range(KBIS):
                            fw.op('dve', lambda e: e.tensor_scalar(out=junk.t[:, 0:L], in0=sc_.t[:, 0:L], scalar1=s_.t[:, 3:4], scalar2=0.0,
                                                                   op0=ALU.is_ge, op1=ALU.add, accum_out=s_.t[:, 4:5]),
                                  reads=[sc_, s_], writes=[junk, s_])
                            last = (k == KBIS - 1)
                            fw.op('dve', lambda e: e.tensor_scalar(out=s_.t[:, 5:6], in0=s_.t[:, 4:5], scalar1=255.5,
                                                                   scalar2=(1.0 if last else 0.5), op0=ALU.is_ge, op1=ALU.subtract),
                                  reads=[s_], writes=[s_])
                            dst = s_.t[:, 6:7] if last else s_.t[:, 3:4]
                            fw.op('dve', lambda e: e.scalar_tensor_tensor(out=dst, in0=s_.t[:, 5:6], scalar=dk_.t[:, k:k + 1], in1=s_.t[:, 3:4],
                                                                          op0=ALU.mult, op1=ALU.add), reads=[s_, dk_], writes=[s_])
                        fw.op('dve', lambda e: e.tensor_scalar(out=mb_.t[:, 0:L], in0=sc_.t[:, 0:L], scalar1=s_.t[:, 6:7], scalar2=-BIG,
                                                               op0=ALU.is_lt, op1=ALU.mult), reads=[sc_, s_], writes=[mb_])
                    else:
                        fw.op('dve', lambda e: e.tensor_scalar(out=mb_.t[:, 0:L], in0=sc_.t[:, 0:L], scalar1=-1e29, scalar2=-BIG,
                                                               op0=ALU.is_lt, op1=ALU.mult), reads=[sc_], writes=[mb_])

                def attend(qt):
                    mb_ = mb[qt % 2]
                    q2_ = Q2[qt % 2]
                    for st in range(qt + 1):
                        SP = spsd[cnt_["ai"] % 2]
                        PT = ptd[cnt_["ai"] % 2]
                        cnt_["ai"] += 1
                        for e_ in range(2):
                            bank = SP[e_]
                            for hp in range(4):
                                fw.op('pe', lambda e: e.matmul(bank.t[:, hp * 128:(hp + 1) * 128],
                                                               lhsT=K2.t[64 * e_:64 * e_ + 64, hp, st * 128:(st + 1) * 128],
                                                               rhs=q2_.t[64 * e_:64 * e_ + 64, hp, :], start=True, stop=False),
                                      reads=[K2, q2_], writes=[bank])
                                fw.op('pe', lambda e: e.matmul(bank.t[:, hp * 128:(hp + 1) * 128], lhsT=mb_.t[:, st * 128:(st + 1) * 128],
                                                               rhs=ident.t[:], start=False, stop=True), reads=[mb_, ident], writes=[bank])
                            fw.op('act', lambda e: e.activation(out=PT.t[:, e_, :], in_=bank.t[:], func=AF.Exp, scale=0.125),
                                  reads=[bank], writes=[PT])
                        for e_ in range(2):
                            for hp in range(4):
                                hh = 2 * hp + e_
                                fw.op('pe', lambda e: e.matmul(opsd[e_].t[:, hp * 65:(hp + 1) * 65], lhsT=PT.t[:, e_, hp * 128:(hp + 1) * 128],
                                                               rhs=V.t[:, st, hh * 65:(hh + 1) * 65], start=(st == 0 and hp == 0), stop=(st == qt)),
                                      reads=[PT, V], writes=[opsd[e_]])
                    ob_ = osbd[qt % 2]
                    for e_ in range(2):
                        Ov = opsd[e_].t[:, 0:260].rearrange("p (i d) -> p i d", d=65)
                        fw.op('dve', lambda e: e.reciprocal(out=recd[e_].t[:], in_=Ov[:, :, 64:65]), reads=[opsd[e_]], writes=[recd[e_]])
                        fw.op('dve', lambda e: e.tensor_tensor(out=ob_.t[:, :, e_, :], in0=Ov[:, :, 0:64],
                                                               in1=recd[e_].t[:, :, :].to_broadcast([128, 4, 64]), op=ALU.mult),
                              reads=[opsd[e_], recd[e_]], writes=[ob_])
                    fw.dma('sp', od_d[qt * 128:(qt + 1) * 128, :], ob_.t[:, :, :, :].rearrange("p a e d -> p (a e d)"), reads=[ob_], sem=ob_)

                indexer(0)
                for qt in range(NT):
                    if qt + 1 < NT:
                        indexer(qt + 1)
                    select(qt)
                    attend(qt)

            if "E" in phases:
              with fw.scope():
                Wo = fw.sb("Wo", [128, 8, D], BF16, dma=True)
                fw.dma_group('pool', [(Wo.t[:, k, :], w_out[l, k * 128:(k + 1) * 128, :]) for k in range(8)], writes=[Wo], sem=Wo)
                gmo = fw.sb("gmo", [128, D], F32, dma=True)
                fw.dma_group('sp', [(gmo.t[:, 0:512], g_moba_out[l, :].partition_broadcast(128)),
                                    (gmo.t[:, 512:1024], g_dsa_out[l, :].partition_broadcast(128))], writes=[gmo], sem=gmo)
                G = fw.sb("Gm", [128, D], F32, dma=True)
                fw.dma('sp', G.t[:], gbc_d[l, 0], writes=[G], sem=G)
                ot = [fw.sb(f"ot{i}", [128, D], F32, dma=True) for i in range(2)]
                xt = [fw.sb(f"ext{i}", [128, D], F32, dma=True) for i in range(2)]
                sq = fw.sb("esq", [128, D], BF16)
                ss = [fw.sb(f"ess{i}", [128, 4], F32) for i in range(2)]
                rs = [fw.sb(f"ers{i}", [128, 4], F32) for i in range(2)]
                on = [fw.sb(f"on{i}", [128, D], BF16) for i in range(2)]
                tps = [fw.ps(f"etp{i}", [128, 8, 128], BF16) for i in range(2)]
                oT = [fw.sb(f"oT{i}", [128, 8, 128], BF16) for i in range(2)]
                yps = [[fw.ps(f"yps{i}{j}", [128, 512], F32) for j in range(2)] for i in range(2)]
                ysb = [fw.sb(f"ysb{i}", [128, D], F32, dma=True) for i in range(2)]
                for tt in range(NT):
                    o_, x_, s_, r_, n_, tp, oT_, yp, y_ = ot[tt % 2], xt[tt % 2], ss[tt % 2], rs[tt % 2], on[tt % 2], tps[tt % 2], oT[tt % 2], yps[tt % 2], ysb[tt % 2]
                    rows = slice(tt * 128, (tt + 1) * 128)
                    fw.dma_group('sp', [(o_.t[:, 0:512], om_d[rows, :]), (o_.t[:, 512:1024], od_d[rows, :])], writes=[o_], sem=o_)
                    fw.dma('sp', x_.t[:], xin[rows, :], writes=[x_], sem=x_)
                    fw.op('dve', lambda e: e.memset(s_.t[:], 0.0), writes=[s_])
                    for j in range(2):
                        fw.op('act', lambda e: e.activation(out=sq.t[:, 0:512], in_=o_.t[:, j * 512:(j + 1) * 512], func=AF.Square,
                                                            accum_out=s_.t[:, j:j + 1]), reads=[o_], writes=[sq, s_])
                    fw.op('dve', lambda e: e.tensor_scalar(out=s_.t[:, 0:2], in0=s_.t[:, 0:2], scalar1=1.0 / 512, scalar2=EPS, op0=ALU.mult, op1=ALU.add),
                          reads=[s_], writes=[s_])
                    fw.op('pool', lambda e: e.tensor_tensor(out=r_.t[:, 0:2], in0=s_.t[:, 0:2], in1=nh.t[:, 0:1].to_broadcast([128, 2]), op=ALU.pow),
                          reads=[s_, nh], writes=[r_])
                    for j in range(2):
                        fw.op('dve', lambda e: e.scalar_tensor_tensor(out=n_.t[:, j * 512:(j + 1) * 512], in0=o_.t[:, j * 512:(j + 1) * 512],
                                                                      scalar=r_.t[:, j:j + 1], in1=gmo.t[:, j * 512:(j + 1) * 512],
                                                                      op0=ALU.mult, op1=ALU.mult), reads=[o_, r_, gmo], writes=[n_])
                    for k in range(8):
                        fw.op('pe', lambda e: e.transpose(out=tp.t[:, k, :], in_=n_.t[:, k * 128:(k + 1) * 128], identity=ident.t[:]),
                              reads=[n_, ident], writes=[tp])
                    fw.op('act', lambda e: e.copy(out=oT_.t[:], in_=tp.t[:]), reads=[tp], writes=[oT_])
                    for j in range(2):
                        for k in range(8):
                            fw.op('pe', lambda e: e.matmul(yp[j].t[:], lhsT=oT_.t[:, k, :], rhs=Wo.t[:, k, j * 512:(j + 1) * 512],
                                                           start=(k == 0), stop=(k == 7)), reads=[oT_, Wo], writes=[yp[j]])
                        fw.op('act', lambda e: e.activation(out=sq.t[:, 0:512], in_=yp[j].t[:], func=AF.Square, accum_out=s_.t[:, 2 + j:3 + j]),
                              reads=[yp[j]], writes=[sq, s_])
                    fw.op('dve', lambda e: e.tensor_tensor(out=s_.t[:, 2:3], in0=s_.t[:, 2:3], in1=s_.t[:, 3:4], op=ALU.add), reads=[s_], writes=[s_])
                    _rms_rstd(fw, (s_, s_.t[:, 2:3]), (r_, r_.t[:, 2:3]), nh, D)
                    for j in range(2):
                        fw.op('dve', lambda e: e.scalar_tensor_tensor(out=y_.t[:, j * 512:(j + 1) * 512], in0=yp[j].t[:], scalar=r_.t[:, 2:3],
                                                                      in1=G.t[:, j * 512:(j + 1) * 512], op0=ALU.mult, op1=ALU.mult),
                              reads=[yp[j], r_, G], writes=[y_])
                    fw.op('pool', lambda e: e.tensor_tensor(out=y_.t[:], in0=y_.t[:], in1=x_.t[:], op=ALU.add), reads=[y_, x_], writes=[y_])
                    fw.dma('sp', x1_d[rows, :], y_.t[:], reads=[y_], sem=y_)

            if "F" in phases:
              with fw.scope():
                Wa = fw.sb("Wa", [128, 8, DFF], BF16, dma=True)
                Wl = fw.sb("Wl", [128, 8, DFF], BF16, dma=True)
                Wd = fw.sb("Wd", [128, NFC, D], BF16, dma=True)
                fw.dma_group('pool', [(Wa.t[:, k, :], w_up_act[l, k * 128:(k + 1) * 128, :]) for k in range(8)], writes=[Wa], sem=Wa)
                fw.dma_group('pool', [(Wl.t[:, k, :], w_up_lin[l, k * 128:(k + 1) * 128, :]) for k in range(8)], writes=[Wl], sem=Wl)
                fw.dma_group('pool', [(Wd.t[:, f, :], w_down[l, f * 128:(f + 1) * 128, :]) for f in range(NFC)], writes=[Wd], sem=Wd)
                wc = fw.sb("wc", [128, NFC, 3], F32, dma=True)
                bcv = fw.sb("bcv", [128, NFC], F32, dma=True)
                fw.dma('sp', wc.t[:], w_conv_col[l], writes=[wc], sem=wc)
                fw.dma('sp', bcv.t[:], b_conv_col[l], writes=[bcv], sem=bcv)
                G = fw.sb("Gf", [128, D], F32, dma=True)
                fw.dma('sp', G.t[:], gbc_d[l, 1], writes=[G], sem=G)
                carry = fw.sb("carry", [128, NFC, 2], F32)
                fw.op('dve', lambda e: e.memset(carry.t[:], 0.0), writes=[carry])
                xt = fw.sb("fxt", [128, D], F32, dma=True)
                sq = fw.sb("fsq", [128, D], BF16)
                ss = fw.sb("fss", [128, 4], F32)
                rs = fw.sb("frs", [128, 4], F32)
                xn = fw.sb("fxn", [128, D], BF16)
                tps = [fw.ps(f"ftp{i}", [128, 8, 128], BF16) for i in range(2)]
                hT = fw.sb("fhT", [128, 8, 512], BF16)
                ups = [fw.ps(f"ups{i}", [128, 512], F32) for i in range(2)]
                lps = [fw.ps(f"flps{i}", [128, 512], F32) for i in range(2)]
                yps = [fw.ps(f"fyps{j}", [128, 512], F32) for j in range(2)]
                ubuf = [fw.sb(f"ubuf{i}", [128, 514], F32) for i in range(2)]
                av = [fw.sb(f"av{i}", [128, 512], F32) for i in range(2)]
                gT = fw.sb("gT", [128, NFC, 512], BF16)
                xr = fw.sb("xr", [128, D], F32, dma=True)
                ysb = fw.sb("fysb", [128, D], F32, dma=True)
                ssv = fw.view("fssv", ss.t)
                rsv = fw.view("frsv", rs.t)
                fi = 0
                for c in range(NCH):
                    for i in range(4):
                        tt = c * 4 + i
                        fw.dma('sp', xt.t[:], x1_d[tt * 128:(tt + 1) * 128, :], writes=[xt], sem=xt)
                        norm_transpose(xt, l, 2, tps[tt % 2], hT, lambda k: hT.t[:, k, i * 128:(i + 1) * 128], sq, ssv, rsv, xn)
                    for fc in range(NFC):
                        U, Lp, ub, a_ = ups[fi % 2], lps[fi % 2], ubuf[fi % 2], av[fi % 2]
                        fi += 1
                        fs = slice(fc * 128, (fc + 1) * 128)
                        for k in range(8):
                            fw.op('pe', lambda e: e.matmul(U.t[:], lhsT=Wa.t[:, k, fs], rhs=hT.t[:, k, :], start=(k == 0), stop=(k == 7)),
                                  reads=[Wa, hT], writes=[U])
                        for k in range(8):
                            fw.op('pe', lambda e: e.matmul(Lp.t[:], lhsT=Wl.t[:, k, fs], rhs=hT.t[:, k, :], start=(k == 0), stop=(k == 7)),
                                  reads=[Wl, hT], writes=[Lp])
                        fw.op('act', lambda e: e.copy(out=ub.t[:, 2:514], in_=U.t[:]), reads=[U], writes=[ub])
                        fw.op('act', lambda e: e.copy(out=ub.t[:, 0:2], in_=carry.t[:, fc, :]), reads=[carry], writes=[ub])
                        fw.op('dve', lambda e: e.tensor_scalar(out=a_.t[:], in0=ub.t[:, 2:514], scalar1=wc.t[:, fc, 2:3], scalar2=bcv.t[:, fc:fc + 1],
                                                               op0=ALU.mult, op1=ALU.add), reads=[ub, wc, bcv], writes=[a_])
                        fw.op('dve', lambda e: e.scalar_tensor_tensor(out=a_.t[:], in0=ub.t[:, 1:513], scalar=wc.t[:, fc, 1:2], in1=a_.t[:],
                                                                      op0=ALU.mult, op1=ALU.add), reads=[ub, wc, a_], writes=[a_])
                        fw.op('dve', lambda e: e.scalar_tensor_tensor(out=a_.t[:], in0=ub.t[:, 0:512], scalar=wc.t[:, fc, 0:1], in1=a_.t[:],
                                                                       op0=ALU.mult, op1=ALU.add), reads=[ub, wc, a_], writes=[a_])
                        fw.op('act', lambda e: e.copy(out=carry.t[:, fc, :], in_=ub.t[:, 512:514]), reads=[ub], writes=[carry])
                        fw.op('act', lambda e: e.activation(out=a_.t[:], in_=a_.t[:], func=AF.Gelu_apprx_tanh), reads=[a_], writes=[a_])
                        fw.op('dve', lambda e: e.tensor_tensor(out=gT.t[:, fc, :], in0=a_.t[:], in1=Lp.t[:], op=ALU.mult),
                              reads=[a_, Lp], writes=[gT])
                    for i in range(4):
                        tt = c * 4 + i
                        rows = slice(tt * 128, (tt + 1) * 128)
                        fw.dma('sp', xr.t[:], x1_d[rows, :], writes=[xr], sem=xr)
                        fw.op('dve', lambda e: e.memset(ss.t[:, 2:4], 0.0), writes=[ssv])
                        for j in range(2):
                            for fc in range(NFC):
                                fw.op('pe', lambda e: e.matmul(yps[j].t[:], lhsT=gT.t[:, fc, i * 128:(i + 1) * 128], rhs=Wd.t[:, fc, j * 512:(j + 1) * 512],
                                                               start=(fc == 0), stop=(fc == NFC - 1)), reads=[gT, Wd], writes=[yps[j]])
                            fw.op('act', lambda e: e.activation(out=sq.t[:, 0:512], in_=yps[j].t[:], func=AF.Square, accum_out=ss.t[:, 2 + j:3 + j]),
                                  reads=[yps[j]], writes=[sq, ssv])
                        fw.op('dve', lambda e: e.tensor_tensor(out=ss.t[:, 2:3], in0=ss.t[:, 2:3], in1=ss.t[:, 3:4], op=ALU.add), reads=[ssv], writes=[ssv])
                        _rms_rstd(fw, (ssv, ss.t[:, 2:3]), (rsv, rs.t[:, 2:3]), nh, D)
                        for j in range(2):
                            fw.op('dve', lambda e: e.scalar_tensor_tensor(out=ysb.t[:, j * 512:(j + 1) * 512], in0=yps[j].t[:], scalar=rs.t[:, 2:3],
                                                                          in1=G.t[:, j * 512:(j + 1) * 512], op0=ALU.mult, op1=ALU.mult),
                                  reads=[yps[j], rsv, G], writes=[ysb])
                        fw.op('pool', lambda e: e.tensor_tensor(out=ysb.t[:], in0=ysb.t[:], in1=xr.t[:], op=ALU.add), reads=[ysb, xr], writes=[ysb])
                        fw.dma('sp', xout[rows, :], ysb.t[:], reads=[ysb], sem=ysb)
        fw.barrier()
    return nc


def _consts():
    bf = ml_dtypes.bfloat16
    pos = np.arange(S, dtype=np.float32)
    inv = (np.float32(500000.0) ** (-np.arange(0, 16, 2, dtype=np.float32) / np.float32(16))).astype(np.float32)
    ang = (pos[None, :] * inv[:, None]).astype(np.float32)
    cos, sin = np.cos(ang).astype(np.float32), np.sin(ang).astype(np.float32)
    C = np.ones((128, S), np.float32)
    Sg = np.zeros((128, S), np.float32)
    for p_ in range(128):
        d = p_ % 64
        if d < 16:
            C[p_] = cos[d % 8]
            Sg[p_] = -sin[d % 8] if d < 8 else sin[d % 8]
    i = np.arange(128)
    tri = (i[None, :] >= i[:, None]).astype(np.float32).astype(bf)
    negtri = np.where(i[None, :] <= i[:, None], 0.0, -1e30).astype(np.float32)
    onehot = (np.arange(S)[None, :] // 256 == np.arange(16)[:, None]).astype(np.float32).astype(bf)
    tt = np.repeat(np.arange(NT), 16)
    n = np.tile(np.arange(16), NT)
    cur = tt // 2
    cbsel = np.where(n >= cur, -BIG, 0.0).astype(np.float32)
    cblt = np.where(n < cur, -BIG, 0.0).astype(np.float32)
    cbfin = np.where(n > cur, -BIG, 0.0).astype(np.float32)
    rep = lambda v: np.ascontiguousarray(np.broadcast_to(v[None, :], (128, v.shape[0])))
    cpow = (2.0 ** (-np.arange(KBIS, dtype=np.float64))).astype(np.float32)
    return {
        "ropeC": C, "ropeS": Sg, "ident": np.eye(128, dtype=np.float32).astype(bf), "tri": tri, "negtri": negtri,
        "onehot": onehot, "cbsel": rep(cbsel), "cblt": rep(cblt), "cbfin": rep(cbfin), "cpow": rep(cpow),
    }


def _perm_w_in(w_in):
    offs = {"mq": 0, "mk": 512, "mv": 1024, "dq": 1536, "dk": 2048, "dv": 2560, "qi": 3072, "ki": 3584, "wi": 3648}
    j = np.arange(64)
    perm = np.where(j < 8, j + 8, np.where(j < 16, j - 8, j))
    cols = []
    for g in ("mq", "mk", "dq", "dk", "qi"):
        base = offs[g]
        cols.append(base + np.arange(512))
        cols.append(base + (np.arange(512) // 64) * 64 + perm[np.arange(512) % 64])
    cols.append(offs["ki"] + np.arange(64))
    cols.append(offs["ki"] + perm)
    cols.append(offs["mv"] + np.arange(512))
    cols.append(offs["dv"] + np.arange(512))
    cols.append(offs["wi"] + np.arange(8))
    cols = np.concatenate(cols)
    assert cols.shape[0] == NCOLP
    return np.ascontiguousarray(w_in[:, :, cols])


def _shared_inputs(w_ada, b_ada, g_pre_mix, w_in, g_moba_out, g_dsa_out, w_out, g_post_mix, g_pre_ffn,
                   w_up_act, w_up_lin, w_conv, b_conv, w_down, g_post_ffn):
    f = lambda a: np.ascontiguousarray(np.asarray(a, dtype=np.float32))
    col8 = lambda g: np.ascontiguousarray(f(g).reshape(2, 8, 128).transpose(0, 2, 1))
    sh = {
        "w_ada": f(w_ada), "b_ada": f(b_ada),
        "b_ada_col": np.ascontiguousarray(f(b_ada).reshape(2, 48, 128).transpose(0, 2, 1)),
        "g_pre_mix_col": col8(g_pre_mix), "g_pre_ffn_col": col8(g_pre_ffn),
        "g_post_mix": f(g_post_mix), "g_post_ffn": f(g_post_ffn),
        "g_moba_out": f(g_moba_out), "g_dsa_out": f(g_dsa_out),
        "w_in_p": _perm_w_in(f(w_in)), "w_out": f(w_out), "w_up_act": f(w_up_act), "w_up_lin": f(w_up_lin),
        "w_conv_col": np.ascontiguousarray(f(w_conv).reshape(2, 3, NFC, 128).transpose(0, 3, 2, 1)),
        "b_conv_col": np.ascontiguousarray(f(b_conv).reshape(2, NFC, 128).transpose(0, 2, 1)),
        "w_down": f(w_down),
    }
    sh.update(_consts())
    return sh


def _core_inputs(x, c, b, shared):
    m = dict(shared)
    m["x"] = np.ascontiguousarray(np.asarray(x[b], dtype=np.float32))
    m["cT"] = np.ascontiguousarray(np.asarray(c[b], dtype=np.float32).reshape(8, 128).T)
    return m


def kernel(x, c, w_ada, b_ada, g_pre_mix, w_in, g_moba_out, g_dsa_out, w_out, g_post_mix,
           g_pre_ffn, w_up_act, w_up_lin, w_conv, b_conv, w_down, g_post_ffn):
    x = np.asarray(x)
    c = np.asarray(c)
    shared = _shared_inputs(w_ada, b_ada, g_pre_mix, w_in, g_moba_out, g_dsa_out, w_out, g_post_mix, g_pre_ffn,
                            w_up_act, w_up_lin, w_conv, b_conv, w_down, g_post_ffn)
    nc = build_nc()
    in_maps = [_core_inputs(x, c, b, shared) for b in range(8)]
    res = run_bass_kernel_spmd(nc, in_maps, core_ids=list(range(8)))
    return np.stack([np.asarray(r["out"], dtype=np.float32) for r in res.results], axis=0)
```

```python
import numpy as np
import ml_dtypes
from contextlib import ExitStack
import concourse.bass as bass
import concourse.mybir as mybir
from concourse.bass_utils import run_bass_kernel_spmd

F32 = mybir.dt.float32
BF16 = mybir.dt.bfloat16
ALU = mybir.AluOpType
AF = mybir.ActivationFunctionType
AX = mybir.AxisListType


class DSem:
    __slots__ = ("idx", "cnt")

    def __init__(self, idx):
        self.idx = idx
        self.cnt = 0


class Buf:
    __slots__ = ("name", "t", "w", "r", "dsem")

    def __init__(self, name, t=None):
        self.name = name
        self.t = t
        self.w = {}
        self.r = {}
        self.dsem = None


class _Scope:
    def __init__(self, fw):
        self.fw = fw

    def __enter__(self):
        fw = self.fw
        self.prev = (fw.es, fw.scope_dsems)
        self.stack = ExitStack()
        self.stack.__enter__()
        fw.es = self.stack
        fw.scope_dsems = []
        return self

    def __exit__(self, *a):
        fw = self.fw
        fw.barrier()
        fw.dpool.extend(fw.scope_dsems)
        fw.es, fw.scope_dsems = self.prev
        return self.stack.__exit__(*a)


class FW:
    SEM_MAX = 30000

    def __init__(self, nc, es):
        self.nc = nc
        self.es = es
        self.sem_es = es
        self.eng = {"pe": nc.tensor, "act": nc.scalar, "dve": nc.vector, "pool": nc.gpsimd, "sp": nc.sync}
        self.sems = []
        self.cur = {}
        self.own = {e: set() for e in self.eng}
        self.known = {e: {} for e in self.eng}
        self.issued = {}
        self.dpool = []
        self.scope_dsems = []
        self.nwaits = 0
        self.nops = 0
        for e in self.eng:
            self._newsem(e)

    def _alloc_sem(self, name):
        s = self.sem_es.enter_context(self.nc.semaphore(name))
        self.sems.append(s)
        return len(self.sems) - 1

    def _newsem(self, e):
        i = self._alloc_sem(f"s_{e}_{len(self.sems)}")
        self.cur[e] = [i, 0]
        self.own[e].add(i)

    def _get_dsem(self):
        if self.dpool:
            d = self.dpool.pop()
        else:
            d = DSem(self._alloc_sem(f"d_{len(self.sems)}"))
        self.scope_dsems.append(d)
        return d

    def scope(self):
        return _Scope(self)

    def sb(self, name, shape, dtype, dma=False):
        self.nuniq = getattr(self, "nuniq", 0) + 1
        t = self.es.enter_context(self.nc.sbuf_tensor(f"{name}_u{self.nuniq}", shape, dtype))
        b = Buf(name, t)
        if dma:
            b.dsem = self._get_dsem()
        return b

    def ps(self, name, shape, dtype):
        self.nuniq = getattr(self, "nuniq", 0) + 1
        t = self.es.enter_context(self.nc.psum_tensor(f"{name}_u{self.nuniq}", shape, dtype))
        return Buf(name, t)

    def view(self, name, t, dma=False):
        b = Buf(name, t)
        if dma:
            b.dsem = self._get_dsem()
        return b

    def _need(self, reads, writes):
        need = {}
        for b in reads:
            for s, v in b.w.items():
                if need.get(s, 0) < v:
                    need[s] = v
        for b in writes:
            for s, v in b.w.items():
                if need.get(s, 0) < v:
                    need[s] = v
            for s, v in b.r.items():
                if need.get(s, 0) < v:
                    need[s] = v
        return need

    def _waits(self, e, need, skip_own=False):
        k = self.known[e]
        eng = self.eng[e]
        for s, v in need.items():
            if skip_own and s in self.own[e]:
                continue
            if k.get(s, 0) >= v:
                continue
            eng.wait_ge(self.sems[s], v)
            self.nwaits += 1
            k[s] = v

    def _record(self, t, reads, writes):
        s, v = t
        self.issued[s] = v
        for b in reads:
            if b.r.get(s, 0) < v:
                b.r[s] = v
        for b in writes:
            b.w = {s: v}
            b.r = {}

    def op(self, e, fn, reads=(), writes=(), same=None):
        if same is None:
            same = (e != "pe")
        need = self._need(reads, writes)
        self._waits(e, need, skip_own=not same)
        ins = fn(self.eng[e])
        c = self.cur[e]
        c[1] += 1
        ins.then_inc(self.sems[c[0]], 1)
        self._record((c[0], c[1]), reads, writes)
        self.nops += 1
        if c[1] >= self.SEM_MAX:
            self._newsem(e)
        return ins

    def dma(self, q, out, in_, reads=(), writes=(), sem=None, **kw):
        return self.dma_group(q, [(out, in_)], reads, writes, sem, **kw)

    def dma_group(self, q, pairs, reads=(), writes=(), sem=None, **kw):
        d = sem.dsem
        need = self._need(reads, writes)
        if d.cnt:
            v = 16 * d.cnt
            if need.get(d.idx, 0) < v:
                need[d.idx] = v
        self._waits(q, need)
        for (out, in_) in pairs:
            ins = self.eng[q].dma_start(out=out, in_=in_, **kw)
            d.cnt += 1
            ins.then_inc(self.sems[d.idx], 16)
            self.nops += 1
        self._record((d.idx, 16 * d.cnt), reads, writes)

    def barrier(self):
        need = dict(self.issued)
        for e in self.eng:
            self._waits(e, need)


S = 4096
D = 1024
NT = 32
NCH = 8
DFF = 2816
NFC = 22
NCOLP = 6280
C_MV, C_DV, C_WI = 5248, 5760, 6272
BIG = 30000.0
KBIS = 24
EPS = 1e-6


class P:
    pass


def _rms_rstd(fw, ssb, rstd, nh, n):
    (ss_buf, ss_ap), (r_buf, r_ap) = ssb, rstd
    fw.op('dve', lambda e: e.tensor_scalar(out=ss_ap, in0=ss_ap, scalar1=1.0 / n, scalar2=EPS, op0=ALU.mult, op1=ALU.add),
          reads=[ss_buf], writes=[ss_buf])
    fw.op('pool', lambda e: e.tensor_tensor(out=r_ap, in0=ss_ap, in1=nh.t[:, 0:1], op=ALU.pow), reads=[ss_buf, nh], writes=[r_buf])


def build_nc(nlayers=2, phases="ABCDEF", dbg=False):
    nc = bass.Bass("TRN2", target_bir_lowering=False)
    p = P()

    def din(name, shape, dt=F32):
        return nc.dram_tensor(name, shape, dt, kind="ExternalInput").ap()

    def dscr(name, shape, dt):
        return nc.dram_tensor(name, shape, dt, kind=("ExternalOutput" if dbg else "Internal")).ap()

    x = din("x", [S, D])
    cT = din("cT", [128, 8])
    w_ada = din("w_ada", [2, D, 6 * D])
    b_ada = din("b_ada", [2, 6 * D])
    b_ada_col = din("b_ada_col", [2, 128, 48])
    g_pre_mix_col = din("g_pre_mix_col", [2, 128, 8])
    g_pre_ffn_col = din("g_pre_ffn_col", [2, 128, 8])
    g_post_mix = din("g_post_mix", [2, D])
    g_post_ffn = din("g_post_ffn", [2, D])
    g_moba_out = din("g_moba_out", [2, 512])
    g_dsa_out = din("g_dsa_out", [2, 512])
    w_in_p = din("w_in_p", [2, D, NCOLP])
    w_out = din("w_out", [2, D, D])
    w_up_act = din("w_up_act", [2, D, DFF])
    w_up_lin = din("w_up_lin", [2, D, DFF])
    w_conv_col = din("w_conv_col", [2, 128, NFC, 3])
    b_conv_col = din("b_conv_col", [2, 128, NFC])
    w_down = din("w_down", [2, DFF, D])
    ropeC = din("ropeC", [128, S])
    ropeS = din("ropeS", [128, S])
    ident_d = din("ident", [128, 128], BF16)
    tri_d = din("tri", [128, 128], BF16)
    negtri_d = din("negtri", [128, 128])
    onehot_d = din("onehot", [16, S], BF16)
    cbsel_d = din("cbsel", [128, 512])
    cblt_d = din("cblt", [128, 512])
    cbfin_d = din("cbfin", [128, 512])
    cpow_d = din("cpow", [128, KBIS])
    out = nc.dram_tensor("out", [S, D], F32, kind="ExternalOutput").ap()

    scrT = [dscr(n, [512, S], BF16) for n in ("mqT", "mkT", "dqT", "dkT", "qiT")]
    kiT_d = dscr("kiT", [64, S], BF16)
    mva = dscr("mva", [S, 520], BF16)
    dva = dscr("dva", [S, 520], BF16)
    om_d = dscr("om", [S, 512], F32)
    od_d = dscr("od", [S, 512], F32)
    x1_d = dscr("x1", [S, D], F32)
    x2_d = dscr("x2", [S, D], F32)
    gbc_d = dscr("gbc", [2, 2, 128, D], F32)

    with ExitStack() as es:
        fw = FW(nc, es)
        AB = fw.sb("AB", [128, 2, 4, 8], F32)
        WI = fw.sb("WI", [128, NT, 8], F32)
        ksum = fw.sb("ksum", [128, 4, 16], F32)
        ident = fw.sb("identb", [128, 128], BF16, dma=True)
        nh = fw.sb("neghalf", [128, 1], F32)
        fw.dma('sp', ident.t[:], ident_d[:, :], writes=[ident], sem=ident)
        fw.op('dve', lambda e: e.memset(nh.t[:], -0.5), writes=[nh])

        if "A" in phases:
          with fw.scope():
            ct = fw.sb("ct", [128, 8], F32, dma=True)
            sc = fw.sb("sc", [128, 8], F32)
            screp = fw.sb("screp", [128, 8, 128], F32)
            ones1 = fw.sb("ones1", [1, 128], F32)
            wa = [fw.sb(f"wa{i}", [128, 8, 1024], F32, dma=True) for i in range(2)]
            brow = [fw.sb(f"brow{i}", [1, 1024], F32, dma=True) for i in range(2)]
            bcol = fw.sb("bcol", [128, 2, 48], F32, dma=True)
            gcol = fw.sb("gcol", [128, 2, 2, 8], F32, dma=True)
            gpb = [fw.sb(f"gpb{i}", [128, 1024], F32, dma=True) for i in range(2)]
            colps = fw.ps("colps", [128, 512], F32)
            bcps = [fw.ps(f"bcps{i}", [128, 512], F32) for i in range(2)]
            mcol = fw.sb("mcol", [128, 8], F32)
            Gt = [fw.sb(f"Gt{i}", [128, D], F32, dma=True) for i in range(2)]
            fw.dma('sp', ct.t[:], cT[:, :], writes=[ct], sem=ct)
            fw.dma_group('sp', [(bcol.t[:, l, :], b_ada_col[l]) for l in range(2)], writes=[bcol], sem=bcol)
            fw.dma_group('sp', [(gcol.t[:, l, 0, :], g_pre_mix_col[l]) for l in range(2)]
                         + [(gcol.t[:, l, 1, :], g_pre_ffn_col[l]) for l in range(2)], writes=[gcol], sem=gcol)
            fw.op('act', lambda e: e.activation(out=sc.t[:], in_=ct.t[:], func=AF.Silu), reads=[ct], writes=[sc])
            fw.op('dve', lambda e: e.tensor_copy(out=screp.t[:], in_=sc.t[:, :].unsqueeze(2).to_broadcast([128, 8, 128])),
                  reads=[sc], writes=[screp])
            fw.op('dve', lambda e: e.memset(ones1.t[:], 1.0), writes=[ones1])
            pi = 0
            for l in range(nlayers):
                wv = w_ada[l].rearrange("(k p) n -> p k n", p=128)
                for piece in range(6):
                    w = wa[pi % 2]
                    pi += 1
                    fw.dma_group('sp', [(w.t[:, k, :], wv[:, k, piece * 1024:(piece + 1) * 1024]) for k in range(8)],
                                 writes=[w], sem=w)
                    if piece in (2, 5):
                        j = 0 if piece == 2 else 1
                        br = brow[j]
                        gp = gpb[j]
                        fw.dma('sp', br.t[:], b_ada[l:l + 1, piece * 1024:(piece + 1) * 1024], writes=[br], sem=br)
                        fw.dma('sp', gp.t[:], (g_post_mix if j == 0 else g_post_ffn)[l, :].partition_broadcast(128),
                               writes=[gp], sem=gp)
                        for nhf in range(2):
                            ps = bcps[nhf]
                            for k in range(8):
                                fw.op('pe', lambda e: e.matmul(ps.t[:], lhsT=screp.t[:, k, :], rhs=w.t[:, k, nhf * 512:(nhf + 1) * 512],
                                                               start=(k == 0), stop=False), reads=[screp, w], writes=[ps])
                            fw.op('pe', lambda e: e.matmul(ps.t[:], lhsT=ones1.t[0:1, :], rhs=br.t[0:1, nhf * 512:(nhf + 1) * 512],
                                                           start=False, stop=True), reads=[ones1, br], writes=[ps])
                            G = Gt[j]
                            fw.op('dve', lambda e: e.tensor_tensor(out=G.t[:, nhf * 512:(nhf + 1) * 512], in0=ps.t[:],
                                                                   in1=gp.t[:, nhf * 512:(nhf + 1) * 512], op=ALU.mult),
                                  reads=[ps, gp], writes=[G])
                        fw.dma('sp', gbc_d[l, j], Gt[j].t[:], reads=[Gt[j]], sem=Gt[j])
                    else:
                        for jj in range(8):
                            for k in range(8):
                                fw.op('pe', lambda e: e.matmul(colps.t[:, jj:jj + 1], lhsT=w.t[:, k, jj * 128:(jj + 1) * 128],
                                                               rhs=sc.t[:, k:k + 1], start=(k == 0), stop=(k == 7)),
                                      reads=[sc, w], writes=[colps])
                        fw.op('dve', lambda e: e.tensor_tensor(out=mcol.t[:], in0=colps.t[:, 0:8],
                                                               in1=bcol.t[:, l, piece * 8:(piece + 1) * 8], op=ALU.add),
                              reads=[colps, bcol], writes=[mcol])
                        if piece in (0, 3):
                            slot = 1 if piece == 0 else 3
                            fw.op('dve', lambda e: e.tensor_copy(out=AB.t[:, l, slot, :], in_=mcol.t[:]), reads=[mcol], writes=[AB])
                        else:
                            slot = 0 if piece == 1 else 2
                            gi = 0 if piece == 1 else 1
                            fw.op('dve', lambda e: e.scalar_tensor_tensor(out=AB.t[:, l, slot, :], in0=mcol.t[:], scalar=1.0,
                                                                          in1=gcol.t[:, l, gi, :], op0=ALU.add, op1=ALU.mult),
                                  reads=[mcol, gcol], writes=[AB])

        def norm_transpose(xb, l, slot, tp, hbuf, hview, sq, ss, rstd, xn):
            fw.op('dve', lambda e: e.memset(ss.t[:], 0.0), writes=[ss])
            fw.op('act', lambda e: e.activation(out=sq.t[:], in_=xb.t[:], func=AF.Square, accum_out=ss.t[:, 0:1]),
                  reads=[xb], writes=[sq, ss])
            _rms_rstd(fw, (ss, ss.t[:, 0:1]), (rstd, rstd.t[:, 0:1]), nh, D)
            fw.op('dve', lambda e: e.tensor_scalar(out=xn.t[:], in0=xb.t[:], scalar1=rstd.t[:, 0:1], scalar2=None, op0=ALU.mult),
                  reads=[xb, rstd], writes=[xn])
            for k in range(8):
                fw.op('pe', lambda e: e.transpose(out=tp.t[:, k, :], in_=xn.t[:, k * 128:(k + 1) * 128], identity=ident.t[:]),
                      reads=[xn, ident], writes=[tp])
            for k in range(8):
                fw.op('act', lambda e: e.activation(out=hview(k), in_=tp.t[:, k, :], func=AF.Identity,
                                                    scale=AB.t[:, l, slot, k:k + 1], bias=AB.t[:, l, slot + 1, k:k + 1]),
                      reads=[tp, AB], writes=[hbuf])

        for l in range(nlayers):
            xin = x if l == 0 else x2_d
            xout = out if l == nlayers - 1 else x2_d
            if "B" in phases:
              with fw.scope():
                W = fw.sb("winb", [128, 8, NCOLP], BF16, dma=True)
                fw.dma_group('pool', [(W.t[:, k, :], w_in_p[l, k * 128:(k + 1) * 128, :]) for k in range(8)], writes=[W], sem=W)
                rC = fw.sb("rC", [128, S], F32, dma=True)
                rS = fw.sb("rS", [128, S], F32, dma=True)
                fw.dma('sp', rC.t[:], ropeC[:, :], writes=[rC], sem=rC)
                fw.dma('sp', rS.t[:], ropeS[:, :], writes=[rS], sem=rS)
                xt = [fw.sb(f"xt{i}", [128, D], F32, dma=True) for i in range(2)]
                sq = fw.sb("sq", [128, D], BF16)
                ss = [fw.sb(f"ss{i}", [128, 1], F32) for i in range(2)]
                rstd = [fw.sb(f"rstd{i}", [128, 1], F32) for i in range(2)]
                xn = [fw.sb(f"xn{i}", [128, D], BF16) for i in range(2)]
                tps = [fw.ps(f"tp{i}", [128, 8, 128], BF16) for i in range(2)]
                hT = [fw.sb(f"hT{i}", [128, 8, 512], BF16) for i in range(2)]
                pm = [fw.ps(f"pm{i}", [128, 512], F32) for i in range(2)]
                pp = [fw.ps(f"pp{i}", [128, 512], F32) for i in range(2)]
                pv = [fw.ps(f"pv{i}", [128, 512], F32) for i in range(2)]
                t1 = [fw.sb(f"t1_{i}", [128, 512], F32) for i in range(2)]
                t2 = [fw.sb(f"t2_{i}", [128, 512], F32) for i in range(2)]
                t3 = [fw.sb(f"t3_{i}", [128, 512], F32) for i in range(2)]
                ob = [fw.sb(f"ob{i}", [128, 512], BF16, dma=True) for i in range(3)]
                vt = [fw.sb(f"vt{i}", [128, 8, 65], BF16, dma=True) for i in range(2)]
                for v in vt:
                    fw.op('dve', lambda e: e.memset(v.t[:], 1.0), writes=[v])
                fj = 0
                oj = 0
                vj = 0
                for c in range(NCH):
                    h = hT[c % 2]
                    cs = slice(c * 512, (c + 1) * 512)
                    for i in range(4):
                        tt = c * 4 + i
                        xb = xt[tt % 2]
                        fw.dma('sp', xb.t[:], xin[tt * 128:(tt + 1) * 128, :], writes=[xb], sem=xb)
                        norm_transpose(xb, l, 0, tps[tt % 2], h, lambda k: h.t[:, k, i * 128:(i + 1) * 128],
                                       sq, ss[tt % 2], rstd[tt % 2], xn[tt % 2])
                    tiles = [(g, ft, 128) for g in range(5) for ft in range(4)] + [(5, 0, 64)]
                    for (g, ft, M) in tiles:
                        cm = g * 1024 + ft * 128 if g < 5 else 5120
                        cp = cm + 512 if g < 5 else 5184
                        pmain, ppart = pm[fj % 2], pp[fj % 2]
                        a1, a2, a3 = t1[fj % 2], t2[fj % 2], t3[fj % 2]
                        fj += 1
                        for k in range(8):
                            fw.op('pe', lambda e: e.matmul(pmain.t[0:M, :], lhsT=W.t[:, k, cm:cm + M], rhs=h.t[:, k, :],
                                                           start=(k == 0), stop=(k == 7)), reads=[W, h], writes=[pmain])
                        for k in range(8):
                            fw.op('pe', lambda e: e.matmul(ppart.t[0:M, :], lhsT=W.t[:, k, cp:cp + M], rhs=h.t[:, k, :],
                                                           start=(k == 0), stop=(k == 7)), reads=[W, h], writes=[ppart])
                        fw.op('dve', lambda e: e.tensor_tensor(out=a1.t[0:M, :], in0=ppart.t[0:M, :], in1=rS.t[0:M, cs], op=ALU.mult),
                              reads=[ppart, rS], writes=[a1])
                        fw.op('dve', lambda e: e.tensor_tensor(out=a2.t[0:M, :], in0=pmain.t[0:M, :], in1=rC.t[0:M, cs], op=ALU.mult),
                              reads=[pmain, rC], writes=[a2])
                        o = ob[oj % 3]
                        oj += 1
                        if g == 1:
                            fw.op('pool', lambda e: e.tensor_tensor(out=a3.t[:], in0=a1.t[:], in1=a2.t[:], op=ALU.add),
                                  reads=[a1, a2], writes=[a3])
                            fw.op('act', lambda e: e.copy(out=o.t[:], in_=a3.t[:]), reads=[a3], writes=[o])
                            fw.op('dve', lambda e: e.reduce_sum(out=ksum.t[:, ft, 2 * c:2 * c + 2],
                                                                in_=a3.t[:, :].rearrange("p (b s) -> p b s", b=2), axis=AX.X),
                                  reads=[a3], writes=[ksum])
                        else:
                            fw.op('pool', lambda e: e.tensor_tensor(out=o.t[0:M, :], in0=a1.t[0:M, :], in1=a2.t[0:M, :], op=ALU.add),
                                  reads=[a1, a2], writes=[o])
                        dst = scrT[g][ft * 128:(ft + 1) * 128, cs] if g < 5 else kiT_d[:, cs]
                        fw.dma('sp', dst, o.t[0:M, :], reads=[o], sem=o)
                    for i in range(4):
                        tt = c * 4 + i
                        for (dst, c0) in ((mva, C_MV), (dva, C_DV)):
                            ps = pv[vj % 2]
                            v = vt[vj % 2]
                            vj += 1
                            for k in range(8):
                                fw.op('pe', lambda e: e.matmul(ps.t[:], lhsT=h.t[:, k, i * 128:(i + 1) * 128], rhs=W.t[:, k, c0:c0 + 512],
                                                               start=(k == 0), stop=(k == 7)), reads=[W, h], writes=[ps])
                            fw.op('act', lambda e: e.copy(out=v.t[:, :, 0:64], in_=ps.t[:, :].rearrange("p (h d) -> p h d", h=8)),
                                  reads=[ps], writes=[v])
                            fw.dma('sp', dst[tt * 128:(tt + 1) * 128, :], v.t[:, :, :].rearrange("p h d -> p (h d)"), reads=[v], sem=v)
                        ps = pv[vj % 2]
                        vj += 1
                        for k in range(8):
                            fw.op('pe', lambda e: e.matmul(ps.t[:, 0:8], lhsT=h.t[:, k, i * 128:(i + 1) * 128], rhs=W.t[:, k, C_WI:C_WI + 8],
                                                           start=(k == 0), stop=(k == 7)), reads=[W, h], writes=[ps])
                        fw.op('act', lambda e: e.copy(out=WI.t[:, tt, :], in_=ps.t[:, 0:8]), reads=[ps], writes=[WI])
            if "C" in phases:
              with fw.scope():
                V = fw.sb("mV", [128, NT, 520], BF16, dma=True)
                fw.dma('sp', V.t[:], mva.rearrange("(n p) c -> p n c", p=128), writes=[V], sem=V)
                Qt = [fw.sb(f"mQ{i}", [80, S], BF16) for i in range(2)]
                Kt = [fw.sb(f"mK{i}", [80, S], BF16) for i in range(2)]
                Qm = [fw.view(f"mQm{i}", Qt[i].t, dma=True) for i in range(2)]
                Qb = [fw.view(f"mQb{i}", Qt[i].t) for i in range(2)]
                Km = [fw.view(f"mKm{i}", Kt[i].t, dma=True) for i in range(2)]
                Kc = [fw.view(f"mKc{i}", Kt[i].t, dma=True) for i in range(2)]
                for i in range(2):
                    fw.dma('sp', Kt[i].t[64:80, :], onehot_d[:, :], writes=[Kc[i]], sem=Kc[i])
                kmb = fw.sb("kmb", [64, 8, 16], BF16)
                for h in range(8):
                    e_, ft = h % 2, h // 2
                    fw.op('act', lambda e: e.mul(out=kmb.t[0:64, h, :], in_=ksum.t[e_ * 64:(e_ + 1) * 64, ft, :], mul=1.0 / 256.0),
                          reads=[ksum], writes=[kmb])
                tri = fw.sb("tri", [128, 128], BF16, dma=True)
                fw.dma('sp', tri.t[:], tri_d[:, :], writes=[tri], sem=tri)
                cbs = fw.sb("cbs", [128, 3, 512], F32, dma=True)
                fw.dma_group('sp', [(cbs.t[:, 0, :], cbsel_d[:, :]), (cbs.t[:, 1, :], cblt_d[:, :]), (cbs.t[:, 2, :], cbfin_d[:, :])],
                             writes=[cbs], sem=cbs)
                gps = fw.ps("gps", [128, 512], F32)
                tb = fw.ps("tb", [128, 1024], BF16)
                sps = [fw.ps(f"sp{i}", [128, 512], F32) for i in range(3)]
                ops_ = [fw.ps(f"op{i}", [128, 512], F32) for i in range(2)]
                pt = [fw.sb(f"pt{i}", [128, 512], BF16) for i in range(3)]
                gm = fw.sb("gm", [128, 512], F32)
                ee = fw.sb("ee", [128, 512], F32)
                g2 = fw.sb("g2", [128, 512], F32)
                g3 = fw.sb("g3", [128, 512], F32)
                mx = fw.sb("mx", [128, 3, 32], F32)
                bt = fw.sb("bt", [128, 512], BF16)
                rec = [fw.sb(f"rec{i}", [128, 4, 1], F32) for i in range(2)]
                osb = [fw.sb(f"osb{i}", [128, 4, 64], F32, dma=True) for i in range(2)]
                v3 = lambda t: t[:, :].rearrange("p (a n) -> p a n", n=16)
                bc3 = lambda j: mx.t[:, j, :].unsqueeze(2).to_broadcast([128, 32, 16])
                si = 0
                oi = 0
                for h in range(8):
                    b = h % 2
                    fw.dma('sp', Qt[b].t[0:64, :], scrT[0][h * 64:(h + 1) * 64, :], writes=[Qm[b]], sem=Qm[b])
                    fw.dma('sp', Kt[b].t[0:64, :], scrT[1][h * 64:(h + 1) * 64, :], writes=[Km[b]], sem=Km[b])
                    for tt in range(NT):
                        fw.op('pe', lambda e: e.matmul(gps.t[:, tt * 16:(tt + 1) * 16], lhsT=Qt[b].t[0:64, tt * 128:(tt + 1) * 128],
                                                       rhs=kmb.t[0:64, h, :], start=True, stop=True), reads=[Qm[b], kmb], writes=[gps])
                    fw.op('dve', lambda e: e.tensor_tensor(out=gm.t[:], in0=gps.t[:], in1=cbs.t[:, 0, :], op=ALU.add),
                          reads=[gps, cbs], writes=[gm])
                    fw.op('dve', lambda e: e.reduce_max(out=mx.t[:, 0, :], in_=v3(gm.t), axis=AX.X), reads=[gm], writes=[mx])
                    fw.op('dve', lambda e: e.tensor_tensor(out=v3(ee.t), in0=v3(gm.t), in1=bc3(0), op=ALU.is_equal),
                          reads=[gm, mx], writes=[ee])
                    fw.op('dve', lambda e: e.scalar_tensor_tensor(out=g2.t[:], in0=ee.t[:], scalar=-1e9, in1=gm.t[:], op0=ALU.mult, op1=ALU.add),
                          reads=[ee, gm], writes=[g2])
                    fw.op('dve', lambda e: e.reduce_max(out=mx.t[:, 1, :], in_=v3(g2.t), axis=AX.X), reads=[g2], writes=[mx])
                    fw.op('dve', lambda e: e.tensor_tensor(out=v3(ee.t), in0=v3(g2.t), in1=bc3(1), op=ALU.is_equal),
                          reads=[g2, mx], writes=[ee])
                    fw.op('dve', lambda e: e.scalar_tensor_tensor(out=g3.t[:], in0=ee.t[:], scalar=-1e9, in1=g2.t[:], op0=ALU.mult, op1=ALU.add),
                          reads=[ee, g2], writes=[g3])
                    fw.op('dve', lambda e: e.reduce_max(out=mx.t[:, 2, :], in_=v3(g3.t), axis=AX.X), reads=[g3], writes=[mx])
                    fw.op('dve', lambda e: e.tensor_tensor(out=v3(ee.t), in0=v3(gm.t), in1=bc3(2), op=ALU.is_lt),
                          reads=[gm, mx], writes=[ee])
                    fw.op('dve', lambda e: e.tensor_tensor(out=g2.t[:], in0=ee.t[:], in1=cbs.t[:, 1, :], op=ALU.mult),
                          reads=[ee, cbs], writes=[g2])
                    fw.op('dve', lambda e: e.tensor_tensor(out=bt.t[:], in0=g2.t[:], in1=cbs.t[:, 2, :], op=ALU.add),
                          reads=[g2, cbs], writes=[bt])
                    for tt in range(NT):
                        fw.op('pe', lambda e: e.transpose(out=tb.t[0:16, (tt % 4) * 128:(tt % 4 + 1) * 128], in_=bt.t[:, tt * 16:(tt + 1) * 16],
                                                          identity=ident.t[:]), reads=[bt, ident], writes=[tb])
                        if tt % 4 == 3:
                            q4 = tt // 4
                            fw.op('act', lambda e: e.copy(out=Qt[b].t[64:80, q4 * 512:(q4 + 1) * 512], in_=tb.t[0:16, 0:512]),
                                  reads=[tb], writes=[Qb[b]])
                    for qc in range(NCH):
                        O = ops_[oi % 2]
                        rc = rec[oi % 2]
                        ob_ = osb[oi % 2]
                        oi += 1
                        nst = 4 * qc + 4

                        def qk(st):
                            nonlocal si
                            sp_ = sps[si % 3]
                            Pt = pt[si % 3]
                            si += 1
                            fw.op('pe', lambda e: e.matmul(sp_.t[:], lhsT=Kt[b].t[0:80, st * 128:(st + 1) * 128],
                                                           rhs=Qt[b].t[0:80, qc * 512:(qc + 1) * 512], start=True, stop=True),
                                  reads=[Km[b], Kc[b], Qm[b], Qb[b]], writes=[sp_])
                            fw.op('act', lambda e: e.activation(out=Pt.t[:], in_=sp_.t[:], func=AF.Exp, scale=0.125),
                                  reads=[sp_], writes=[Pt])
                            j = st - 4 * qc
                            if j >= 0:
                                fw.op('pool', lambda e: e.tensor_tensor(out=Pt.t[:, j * 128:(j + 1) * 128], in0=Pt.t[:, j * 128:(j + 1) * 128],
                                                                        in1=tri.t[:], op=ALU.mult), reads=[Pt, tri], writes=[Pt])
                            return Pt, j

                        pend = qk(0)
                        first = True
                        for st in range(nst):
                            nxt = qk(st + 1) if st + 1 < nst else None
                            Pt, j = pend
                            for i in range(max(j, 0), 4):
                                fw.op('pe', lambda e: e.matmul(O.t[:, i * 65:(i + 1) * 65], lhsT=Pt.t[:, i * 128:(i + 1) * 128],
                                                               rhs=V.t[:, st, h * 65:(h + 1) * 65], start=first, stop=(st == 4 * qc + i)),
                                      reads=[Pt, V], writes=[O])
                                first = False
                            pend = nxt
                        Ov = O.t[:, 0:260].rearrange("p (i d) -> p i d", d=65)
                        fw.op('dve', lambda e: e.reciprocal(out=rc.t[:], in_=Ov[:, :, 64:65]), reads=[O], writes=[rc])
                        fw.op('dve', lambda e: e.tensor_tensor(out=ob_.t[:], in0=Ov[:, :, 0:64], in1=rc.t[:, :, :].to_broadcast([128, 4, 64]),
                                                               op=ALU.mult), reads=[O, rc], writes=[ob_])
                        fw.dma('sp', om_d[qc * 512:(qc + 1) * 512, h * 64:(h + 1) * 64].rearrange("(i p) d -> p i d", p=128), ob_.t[:],
                               reads=[ob_], sem=ob_)

            if "D" in phases:
              with fw.scope():
                V = fw.sb("dV", [128, NT, 520], BF16, dma=True)
                fw.dma('sp', V.t[:], dva.rearrange("(n p) c -> p n c", p=128), writes=[V], sem=V)
                K2 = fw.sb("K2", [128, 4, S], BF16, dma=True)
                fw.dma('sp', K2.t[:], scrT[3].rearrange("(hp two d) t -> (two d) hp t", two=2, d=64), writes=[K2], sem=K2)
                ki = fw.sb("kiT", [64, S], BF16, dma=True)
                fw.dma('sp', ki.t[:], kiT_d[:, :], writes=[ki], sem=ki)
                negtri = fw.sb("negtri", [128, 128], F32, dma=True)
                fw.dma('sp', negtri.t[:], negtri_d[:, :], writes=[negtri], sem=negtri)
                cpow = fw.sb("cpow", [128, KBIS], F32, dma=True)
                fw.dma('sp', cpow.t[:], cpow_d[:, :], writes=[cpow], sem=cpow)
                QI = [fw.sb(f"QI{i}", [64, 8, 128], BF16, dma=True) for i in range(2)]
                Q2 = [fw.sb(f"Q2{i}", [128, 4, 128], BF16, dma=True) for i in range(3)]
                i4 = fw.sb("i4", [128, 4, 128], BF16)
                fw.op('dve', lambda e: e.tensor_copy(out=i4.t[:], in_=ident.t[:, :].unsqueeze(1).to_broadcast([128, 4, 128])), reads=[ident], writes=[i4])
                score = [fw.sb(f"score{i}", [128, S], F32) for i in range(2)]
                junk = fw.sb("junk", [128, S], BF16)
                mb = [fw.sb(f"mb{i}", [128, S], BF16) for i in range(2)]
                tmp = [fw.sb(f"rl{i}", [128, 512], F32) for i in range(3)]
                lps = [fw.ps(f"lps{i}", [128, 512], F32) for i in range(2)]
                spsd = [[fw.ps(f"dsp{i}{e_}", [128, 512], F32) for e_ in range(2)] for i in range(2)]
                opsd = [fw.ps(f"dop{e_}", [128, 512], F32) for e_ in range(2)]
                ptd = [fw.sb(f"dpt{i}", [128, 2, 512], BF16) for i in range(2)]
                sm = [fw.sb(f"sm{i}", [128, 8], F32) for i in range(2)]
                dkt = [fw.sb(f"dk{i}", [128, KBIS], F32) for i in range(2)]
                recd = [fw.sb(f"drec{e_}", [128, 4, 1], F32) for e_ in range(2)]
                osbd = [fw.sb(f"dosb{i}", [128, 4, 2, 64], F32, dma=True) for i in range(2)]
                qiv = scrT[4].rearrange("(h d) t -> d h t", d=64)
                q2v = scrT[2].rearrange("(hp two d) t -> (two d) hp t", two=2, d=64)
                cnt_ = {"li": 0, "ti": 0, "ai": 0}

                def indexer(qt):
                    L = (qt + 1) * 128
                    sc_ = score[qt % 2]
                    qi_ = QI[qt % 2]
                    fw.dma('sp', qi_.t[:], qiv[:, :, qt * 128:(qt + 1) * 128], writes=[qi_], sem=qi_)
                    fw.dma('sp', Q2[qt % 3].t[:], q2v[:, :, qt * 128:(qt + 1) * 128], writes=[Q2[qt % 3]], sem=Q2[qt % 3])
                    nch = (L + 511) // 512
                    for ch in range(nch):
                        ncol = min(512, L - ch * 512)
                        cs = slice(ch * 512, ch * 512 + ncol)
                        for h in range(8):
                            lp = lps[cnt_["li"] % 2]
                            cnt_["li"] += 1
                            fw.op('pe', lambda e: e.matmul(lp.t[:, 0:ncol], lhsT=qi_.t[0:64, h, :], rhs=ki.t[0:64, cs], start=True, stop=True),
                                  reads=[qi_, ki], writes=[lp])
                            if h == 0:
                                fw.op('dve', lambda e: e.tensor_scalar(out=sc_.t[:, cs], in0=lp.t[:, 0:ncol], scalar1=0.0,
                                                                       scalar2=WI.t[:, qt, h:h + 1], op0=ALU.max, op1=ALU.mult),
                                      reads=[lp, WI], writes=[sc_])
                            else:
                                tm = tmp[cnt_["ti"] % 3]
                                cnt_["ti"] += 1
                                fw.op('dve', lambda e: e.tensor_scalar(out=tm.t[:, 0:ncol], in0=lp.t[:, 0:ncol], scalar1=0.0,
                                                                       scalar2=WI.t[:, qt, h:h + 1], op0=ALU.max, op1=ALU.mult),
                                      reads=[lp, WI], writes=[tm])
                                fw.op('pool', lambda e: e.tensor_tensor(out=sc_.t[:, cs], in0=sc_.t[:, cs], in1=tm.t[:, 0:ncol], op=ALU.add),
                                      reads=[sc_, tm], writes=[sc_])

                def select(qt):
                    L = (qt + 1) * 128
                    sc_ = score[qt % 2]
                    s_ = sm[qt % 2]
                    dk_ = dkt[qt % 2]
                    mb_ = mb[qt % 2]
                    if qt >= 2:
                        fw.op('dve', lambda e: e.reduce_max(out=s_.t[:, 0:1], in_=sc_.t[:, 0:L], axis=AX.X), reads=[sc_], writes=[s_])
                        fw.op('dve', lambda e: e.tensor_reduce(out=s_.t[:, 1:2], in_=sc_.t[:, 0:L], axis=AX.X, op=ALU.min), reads=[sc_], writes=[s_])
                        fw.op('dve', lambda e: e.scalar_tensor_tensor(out=s_.t[:, 2:3], in0=s_.t[:, 1:2], scalar=-1.0, in1=s_.t[:, 0:1],
                                                                      op0=ALU.mult, op1=ALU.max), reads=[s_], writes=[s_])
                    fw.op('pool', lambda e: e.tensor_tensor(out=sc_.t[:, qt * 128:L], in0=sc_.t[:, qt * 128:L], in1=negtri.t[:], op=ALU.add),
                          reads=[sc_, negtri], writes=[sc_])
                    if qt >= 2:
                        fw.op('dve', lambda e: e.tensor_scalar(out=dk_.t[:], in0=cpow.t[:], scalar1=s_.t[:, 2:3], scalar2=None, op0=ALU.mult),
                              reads=[cpow, s_], writes=[dk_])
                        fw.op('dve', lambda e: e.memset(s_.t[:, 3:4], 0.0), writes=[s_])
                        for k in range(KBIS):
                            fw.op('dve', lambda e: e.tensor_scalar(out=junk.t[:, 0:L], in0=sc_.t[:, 0:L], scalar1=s_.t[:, 3:4], scalar2=0.0,
                                                                   op0=ALU.is_ge, op1=ALU.add, accum_out=s_.t[:, 4:5]),
                                  reads=[sc_, s_], writes=[junk, s_])
                            last = (k == KBIS - 1)
                            fw.op('dve', lambda e: e.tensor_scalar(out=s_.t[:, 5:6], in0=s_.t[:, 4:5], scalar1=255.5,
                                                                   scalar2=(1.0 if last else 0.5), op0=ALU.is_ge, op1=ALU.subtract),
                                  reads=[s_], writes=[s_])
                            dst = s_.t[:, 6:7] if last else s_.t[:, 3:4]
                            fw.op('dve', lambda e: e.scalar_tensor_tensor(out=dst, in0=s_.t[:, 5:6], scalar=dk_.t[:, k:k + 1], in1=s_.t[:, 3:4],
                                                                          op0=ALU.mult, op1=ALU.add), reads=[s_, dk_], writes=[s_])
                        fw.op('dve', lambda e: e.tensor_scalar(out=mb_.t[:, 0:L], in0=sc_.t[:, 0:L], scalar1=s_.t[:, 6:7], scalar2=-BIG,
                                                               op0=ALU.is_lt, op1=ALU.mult), reads=[sc_, s_], writes=[mb_])
                    else:
                        fw.op('dve', lambda e: e.tensor_scalar(out=mb_.t[:, 0:L], in0=sc_.t[:, 0:L], scalar1=-1e29, scalar2=-BIG,
                                                               op0=ALU.is_lt, op1=ALU.mult), reads=[sc_], writes=[mb_])

                def attend(qt):
                    mb_ = mb[qt % 2]
                    q2_ = Q2[qt % 3]
                    def qk(st):
                        SP = spsd[cnt_["ai"] % 2]
                        PT = ptd[cnt_["ai"] % 2]
                        cnt_["ai"] += 1
                        for e_ in range(2):
                            bank = SP[e_]
                            for hp in range(4):
                                fw.op('pe', lambda e: e.matmul(bank.t[:, hp * 128:(hp + 1) * 128],
                                                               lhsT=K2.t[64 * e_:64 * e_ + 64, hp, st * 128:(st + 1) * 128],
                                                               rhs=q2_.t[64 * e_:64 * e_ + 64, hp, :], start=(hp == 0), stop=False),
                                      reads=[K2, q2_], writes=[bank])
                            fw.op('pe', lambda e: e.matmul(bank.t[:], lhsT=mb_.t[:, st * 128:(st + 1) * 128],
                                                           rhs=i4.t[:, :, :].rearrange("p a t -> p (a t)"), start=False, stop=True),
                                  reads=[mb_, i4], writes=[bank])
                            fw.op('act', lambda e: e.activation(out=PT.t[:, e_, :], in_=bank.t[:], func=AF.Exp, scale=0.125),
                                  reads=[bank], writes=[PT])
                        return PT

                    pend = qk(0)
                    for st in range(qt + 1):
                        nxt = qk(st + 1) if st + 1 <= qt else None
                        PT = pend
                        for e_ in range(2):
                            for hp in range(4):
                                hh = 2 * hp + e_
                                fw.op('pe', lambda e: e.matmul(opsd[e_].t[:, hp * 65:(hp + 1) * 65], lhsT=PT.t[:, e_, hp * 128:(hp + 1) * 128],
                                                               rhs=V.t[:, st, hh * 65:(hh + 1) * 65], start=(st == 0 and hp == 0), stop=(st == qt)),
                                      reads=[PT, V], writes=[opsd[e_]])
                        pend = nxt
                    ob_ = osbd[qt % 2]
                    for e_ in range(2):
                        Ov = opsd[e_].t[:, 0:260].rearrange("p (i d) -> p i d", d=65)
                        fw.op('dve', lambda e: e.reciprocal(out=recd[e_].t[:], in_=Ov[:, :, 64:65]), reads=[opsd[e_]], writes=[recd[e_]])
                        fw.op('dve', lambda e: e.tensor_tensor(out=ob_.t[:, :, e_, :], in0=Ov[:, :, 0:64],
                                                               in1=recd[e_].t[:, :, :].to_broadcast([128, 4, 64]), op=ALU.mult),
                              reads=[opsd[e_], recd[e_]], writes=[ob_])
                    fw.dma('sp', od_d[qt * 128:(qt + 1) * 128, :], ob_.t[:, :, :, :].rearrange("p a e d -> p (a e d)"), reads=[ob_], sem=ob_)

                indexer(0)
                for qt in range(NT):
                    if qt + 1 < NT:
                        indexer(qt + 1)
                    select(qt)
                    if qt >= 1:
                        attend(qt - 1)
                attend(NT - 1)

            if "E" in phases:
              with fw.scope():
                Wo = fw.sb("Wo", [128, 8, D], BF16, dma=True)
                fw.dma_group('pool', [(Wo.t[:, k, :], w_out[l, k * 128:(k + 1) * 128, :]) for k in range(8)], writes=[Wo], sem=Wo)
                gmo = fw.sb("gmo", [128, D], F32, dma=True)
                fw.dma_group('sp', [(gmo.t[:, 0:512], g_moba_out[l, :].partition_broadcast(128)),
                                    (gmo.t[:, 512:1024], g_dsa_out[l, :].partition_broadcast(128))], writes=[gmo], sem=gmo)
                G = fw.sb("Gm", [128, D], F32, dma=True)
                fw.dma('sp', G.t[:], gbc_d[l, 0], writes=[G], sem=G)
                ot = [fw.sb(f"ot{i}", [128, D], F32, dma=True) for i in range(2)]
                xt = [fw.sb(f"ext{i}", [128, D], F32, dma=True) for i in range(2)]
                sq = fw.sb("esq", [128, D], BF16)
                ss = [fw.sb(f"ess{i}", [128, 4], F32) for i in range(2)]
                rs = [fw.sb(f"ers{i}", [128, 4], F32) for i in range(2)]
                on = [fw.sb(f"on{i}", [128, D], BF16) for i in range(2)]
                tps = [fw.ps(f"etp{i}", [128, 8, 128], BF16) for i in range(2)]
                oT = [fw.sb(f"oT{i}", [128, 8, 128], BF16) for i in range(2)]
                yps = [[fw.ps(f"yps{i}{j}", [128, 512], F32) for j in range(2)] for i in range(2)]
                ysb = [fw.sb(f"ysb{i}", [128, D], F32, dma=True) for i in range(2)]
                for tt in range(NT):
                    o_, x_, s_, r_, n_, tp, oT_, yp, y_ = ot[tt % 2], xt[tt % 2], ss[tt % 2], rs[tt % 2], on[tt % 2], tps[tt % 2], oT[tt % 2], yps[tt % 2], ysb[tt % 2]
                    rows = slice(tt * 128, (tt + 1) * 128)
                    fw.dma_group('sp', [(o_.t[:, 0:512], om_d[rows, :]), (o_.t[:, 512:1024], od_d[rows, :])], writes=[o_], sem=o_)
                    fw.dma('sp', x_.t[:], xin[rows, :], writes=[x_], sem=x_)
                    fw.op('dve', lambda e: e.memset(s_.t[:], 0.0), writes=[s_])
                    for j in range(2):
                        fw.op('act', lambda e: e.activation(out=sq.t[:, 0:512], in_=o_.t[:, j * 512:(j + 1) * 512], func=AF.Square,
                                                            accum_out=s_.t[:, j:j + 1]), reads=[o_], writes=[sq, s_])
                    fw.op('dve', lambda e: e.tensor_scalar(out=s_.t[:, 0:2], in0=s_.t[:, 0:2], scalar1=1.0 / 512, scalar2=EPS, op0=ALU.mult, op1=ALU.add),
                          reads=[s_], writes=[s_])
                    fw.op('pool', lambda e: e.tensor_tensor(out=r_.t[:, 0:2], in0=s_.t[:, 0:2], in1=nh.t[:, 0:1].to_broadcast([128, 2]), op=ALU.pow),
                          reads=[s_, nh], writes=[r_])
                    for j in range(2):
                        fw.op('dve', lambda e: e.scalar_tensor_tensor(out=n_.t[:, j * 512:(j + 1) * 512], in0=o_.t[:, j * 512:(j + 1) * 512],
                                                                      scalar=r_.t[:, j:j + 1], in1=gmo.t[:, j * 512:(j + 1) * 512],
                                                                      op0=ALU.mult, op1=ALU.mult), reads=[o_, r_, gmo], writes=[n_])
                    for k in range(8):
                        fw.op('pe', lambda e: e.transpose(out=tp.t[:, k, :], in_=n_.t[:, k * 128:(k + 1) * 128], identity=ident.t[:]),
                              reads=[n_, ident], writes=[tp])
                    fw.op('act', lambda e: e.copy(out=oT_.t[:], in_=tp.t[:]), reads=[tp], writes=[oT_])
                    for j in range(2):
                        for k in range(8):
                            fw.op('pe', lambda e: e.matmul(yp[j].t[:], lhsT=oT_.t[:, k, :], rhs=Wo.t[:, k, j * 512:(j + 1) * 512],
                                                           start=(k == 0), stop=(k == 7)), reads=[oT_, Wo], writes=[yp[j]])
                        fw.op('act', lambda e: e.activation(out=sq.t[:, 0:512], in_=yp[j].t[:], func=AF.Square, accum_out=s_.t[:, 2 + j:3 + j]),
                              reads=[yp[j]], writes=[sq, s_])
                    fw.op('dve', lambda e: e.tensor_tensor(out=s_.t[:, 2:3], in0=s_.t[:, 2:3], in1=s_.t[:, 3:4], op=ALU.add), reads=[s_], writes=[s_])
                    _rms_rstd(fw, (s_, s_.t[:, 2:3]), (r_, r_.t[:, 2:3]), nh, D)
                    for j in range(2):
                        fw.op('dve', lambda e: e.scalar_tensor_tensor(out=y_.t[:, j * 512:(j + 1) * 512], in0=yp[j].t[:], scalar=r_.t[:, 2:3],
                                                                      in1=G.t[:, j * 512:(j + 1) * 512], op0=ALU.mult, op1=ALU.mult),
                              reads=[yp[j], r_, G], writes=[y_])
                    fw.op('pool', lambda e: e.tensor_tensor(out=y_.t[:], in0=y_.t[:], in1=x_.t[:], op=ALU.add), reads=[y_, x_], writes=[y_])
                    fw.dma('sp', x1_d[rows, :], y_.t[:], reads=[y_], sem=y_)

            if "F" in phases:
              with fw.scope():
                Wa = fw.sb("Wa", [128, 8, DFF], BF16, dma=True)
                Wl = fw.sb("Wl", [128, 8, DFF], BF16, dma=True)
                Wd = fw.sb("Wd", [128, NFC, D], BF16, dma=True)
                fw.dma_group('pool', [(Wa.t[:, k, :], w_up_act[l, k * 128:(k + 1) * 128, :]) for k in range(8)], writes=[Wa], sem=Wa)
                fw.dma_group('pool', [(Wl.t[:, k, :], w_up_lin[l, k * 128:(k + 1) * 128, :]) for k in range(8)], writes=[Wl], sem=Wl)
                fw.dma_group('pool', [(Wd.t[:, f, :], w_down[l, f * 128:(f + 1) * 128, :]) for f in range(NFC)], writes=[Wd], sem=Wd)
                wc = fw.sb("wc", [128, NFC, 3], F32, dma=True)
                bcv = fw.sb("bcv", [128, NFC], F32, dma=True)
                fw.dma('sp', wc.t[:], w_conv_col[l], writes=[wc], sem=wc)
                fw.dma('sp', bcv.t[:], b_conv_col[l], writes=[bcv], sem=bcv)
                G = fw.sb("Gf", [128, D], F32, dma=True)
                fw.dma('sp', G.t[:], gbc_d[l, 1], writes=[G], sem=G)
                carry = fw.sb("carry", [128, NFC, 2], F32)
                fw.op('dve', lambda e: e.memset(carry.t[:], 0.0), writes=[carry])
                xt = fw.sb("fxt", [128, D], F32, dma=True)
                sq = fw.sb("fsq", [128, D], BF16)
                ss = fw.sb("fss", [128, 4], F32)
                rs = fw.sb("frs", [128, 4], F32)
                xn = fw.sb("fxn", [128, D], BF16)
                tps = [fw.ps(f"ftp{i}", [128, 8, 128], BF16) for i in range(2)]
                hT = fw.sb("fhT", [128, 8, 512], BF16)
                ups = [fw.ps(f"ups{i}", [128, 512], F32) for i in range(2)]
                lps = [fw.ps(f"flps{i}", [128, 512], F32) for i in range(2)]
                yps = [fw.ps(f"fyps{j}", [128, 512], F32) for j in range(2)]
                ubuf = [fw.sb(f"ubuf{i}", [128, 514], F32) for i in range(2)]
                av = [fw.sb(f"av{i}", [128, 512], F32) for i in range(2)]
                gT = fw.sb("gT", [128, NFC, 512], BF16)
                xr = fw.sb("xr", [128, D], F32, dma=True)
                ysb = fw.sb("fysb", [128, D], F32, dma=True)
                ssv = fw.view("fssv", ss.t)
                rsv = fw.view("frsv", rs.t)
                fi = 0
                for c in range(NCH):
                    for i in range(4):
                        tt = c * 4 + i
                        fw.dma('sp', xt.t[:], x1_d[tt * 128:(tt + 1) * 128, :], writes=[xt], sem=xt)
                        norm_transpose(xt, l, 2, tps[tt % 2], hT, lambda k: hT.t[:, k, i * 128:(i + 1) * 128], sq, ssv, rsv, xn)
                    for fc in range(NFC):
                        U, Lp, ub, a_ = ups[fi % 2], lps[fi % 2], ubuf[fi % 2], av[fi % 2]
                        fi += 1
                        fs = slice(fc * 128, (fc + 1) * 128)
                        for k in range(8):
                            fw.op('pe', lambda e: e.matmul(U.t[:], lhsT=Wa.t[:, k, fs], rhs=hT.t[:, k, :], start=(k == 0), stop=(k == 7)),
                                  reads=[Wa, hT], writes=[U])
                        for k in range(8):
                            fw.op('pe', lambda e: e.matmul(Lp.t[:], lhsT=Wl.t[:, k, fs], rhs=hT.t[:, k, :], start=(k == 0), stop=(k == 7)),
                                  reads=[Wl, hT], writes=[Lp])
                        fw.op('act', lambda e: e.copy(out=ub.t[:, 2:514], in_=U.t[:]), reads=[U], writes=[ub])
                        fw.op('act', lambda e: e.copy(out=ub.t[:, 0:2], in_=carry.t[:, fc, :]), reads=[carry], writes=[ub])
                        fw.op('dve', lambda e: e.tensor_scalar(out=a_.t[:], in0=ub.t[:, 2:514], scalar1=wc.t[:, fc, 2:3], scalar2=bcv.t[:, fc:fc + 1],
                                                               op0=ALU.mult, op1=ALU.add), reads=[ub, wc, bcv], writes=[a_])
                        fw.op('dve', lambda e: e.scalar_tensor_tensor(out=a_.t[:], in0=ub.t[:, 1:513], scalar=wc.t[:, fc, 1:2], in1=a_.t[:],
                                                                      op0=ALU.mult, op1=ALU.add), reads=[ub, wc, a_], writes=[a_])
                        fw.op('dve', lambda e: e.scalar_tensor_tensor(out=a_.t[:], in0=ub.t[:, 0:512], scalar=wc.t[:, fc, 0:1], in1=a_.t[:],
                                                                       op0=ALU.mult, op1=ALU.add), reads=[ub, wc, a_], writes=[a_])
                        fw.op('act', lambda e: e.copy(out=carry.t[:, fc, :], in_=ub.t[:, 512:514]), reads=[ub], writes=[carry])
                        fw.op('act', lambda e: e.activation(out=a_.t[:], in_=a_.t[:], func=AF.Gelu_apprx_tanh), reads=[a_], writes=[a_])
                        fw.op('dve', lambda e: e.tensor_tensor(out=gT.t[:, fc, :], in0=a_.t[:], in1=Lp.t[:], op=ALU.mult),
                              reads=[a_, Lp], writes=[gT])
                    for i in range(4):
                        tt = c * 4 + i
                        rows = slice(tt * 128, (tt + 1) * 128)
                        fw.dma('sp', xr.t[:], x1_d[rows, :], writes=[xr], sem=xr)
                        fw.op('dve', lambda e: e.memset(ss.t[:, 2:4], 0.0), writes=[ssv])
                        for j in range(2):
                            for fc in range(NFC):
                                fw.op('pe', lambda e: e.matmul(yps[j].t[:], lhsT=gT.t[:, fc, i * 128:(i + 1) * 128], rhs=Wd.t[:, fc, j * 512:(j + 1) * 512],
                                                               start=(fc == 0), stop=(fc == NFC - 1)), reads=[gT, Wd], writes=[yps[j]])
                            fw.op('act', lambda e: e.activation(out=sq.t[:, 0:512], in_=yps[j].t[:], func=AF.Square, accum_out=ss.t[:, 2 + j:3 + j]),
                                  reads=[yps[j]], writes=[sq, ssv])
                        fw.op('dve', lambda e: e.tensor_tensor(out=ss.t[:, 2:3], in0=ss.t[:, 2:3], in1=ss.t[:, 3:4], op=ALU.add), reads=[ssv], writes=[ssv])
                        _rms_rstd(fw, (ssv, ss.t[:, 2:3]), (rsv, rs.t[:, 2:3]), nh, D)
                        for j in range(2):
                            fw.op('dve', lambda e: e.scalar_tensor_tensor(out=ysb.t[:, j * 512:(j + 1) * 512], in0=yps[j].t[:], scalar=rs.t[:, 2:3],
                                                                          in1=G.t[:, j * 512:(j + 1) * 512], op0=ALU.mult, op1=ALU.mult),
                                  reads=[yps[j], rsv, G], writes=[ysb])
                        fw.op('pool', lambda e: e.tensor_tensor(out=ysb.t[:], in0=ysb.t[:], in1=xr.t[:], op=ALU.add), reads=[ysb, xr], writes=[ysb])
                        fw.dma('sp', xout[rows, :], ysb.t[:], reads=[ysb], sem=ysb)
        fw.barrier()
    return nc


def _consts():
    bf = ml_dtypes.bfloat16
    pos = np.arange(S, dtype=np.float32)
    inv = (np.float32(500000.0) ** (-np.arange(0, 16, 2, dtype=np.float32) / np.float32(16))).astype(np.float32)
    ang = (pos[None, :] * inv[:, None]).astype(np.float32)
    cos, sin = np.cos(ang).astype(np.float32), np.sin(ang).astype(np.float32)
    C = np.ones((128, S), np.float32)
    Sg = np.zeros((128, S), np.float32)
    for p_ in range(128):
        d = p_ % 64
        if d < 16:
            C[p_] = cos[d % 8]
            Sg[p_] = -sin[d % 8] if d < 8 else sin[d % 8]
    i = np.arange(128)
    tri = (i[None, :] >= i[:, None]).astype(np.float32).astype(bf)
    negtri = np.where(i[None, :] <= i[:, None], 0.0, -1e30).astype(np.float32)
    onehot = (np.arange(S)[None, :] // 256 == np.arange(16)[:, None]).astype(np.float32).astype(bf)
    tt = np.repeat(np.arange(NT), 16)
    n = np.tile(np.arange(16), NT)
    cur = tt // 2
    cbsel = np.where(n >= cur, -BIG, 0.0).astype(np.float32)
    cblt = np.where(n < cur, -BIG, 0.0).astype(np.float32)
    cbfin = np.where(n > cur, -BIG, 0.0).astype(np.float32)
    rep = lambda v: np.ascontiguousarray(np.broadcast_to(v[None, :], (128, v.shape[0])))
    cpow = (2.0 ** (-np.arange(KBIS, dtype=np.float64))).astype(np.float32)
    return {
        "ropeC": C, "ropeS": Sg, "ident": np.eye(128, dtype=np.float32).astype(bf), "tri": tri, "negtri": negtri,
        "onehot": onehot, "cbsel": rep(cbsel), "cblt": rep(cblt), "cbfin": rep(cbfin), "cpow": rep(cpow),
    }


def _perm_w_in(w_in):
    offs = {"mq": 0, "mk": 512, "mv": 1024, "dq": 1536, "dk": 2048, "dv": 2560, "qi": 3072, "ki": 3584, "wi": 3648}
    j = np.arange(64)
    perm = np.where(j < 8, j + 8, np.where(j < 16, j - 8, j))
    cols = []
    for g in ("mq", "mk", "dq", "dk", "qi"):
        base = offs[g]
        cols.append(base + np.arange(512))
        cols.append(base + (np.arange(512) // 64) * 64 + perm[np.arange(512) % 64])
    cols.append(offs["ki"] + np.arange(64))
    cols.append(offs["ki"] + perm)
    cols.append(offs["mv"] + np.arange(512))
    cols.append(offs["dv"] + np.arange(512))
    cols.append(offs["wi"] + np.arange(8))
    cols = np.concatenate(cols)
    assert cols.shape[0] == NCOLP
    return np.ascontiguousarray(w_in[:, :, cols])


def _shared_inputs(w_ada, b_ada, g_pre_mix, w_in, g_moba_out, g_dsa_out, w_out, g_post_mix, g_pre_ffn,
                   w_up_act, w_up_lin, w_conv, b_conv, w_down, g_post_ffn):
    f = lambda a: np.ascontiguousarray(np.asarray(a, dtype=np.float32))
    col8 = lambda g: np.ascontiguousarray(f(g).reshape(2, 8, 128).transpose(0, 2, 1))
    sh = {
        "w_ada": f(w_ada), "b_ada": f(b_ada),
        "b_ada_col": np.ascontiguousarray(f(b_ada).reshape(2, 48, 128).transpose(0, 2, 1)),
        "g_pre_mix_col": col8(g_pre_mix), "g_pre_ffn_col": col8(g_pre_ffn),
        "g_post_mix": f(g_post_mix), "g_post_ffn": f(g_post_ffn),
        "g_moba_out": f(g_moba_out), "g_dsa_out": f(g_dsa_out),
        "w_in_p": _perm_w_in(f(w_in)), "w_out": f(w_out), "w_up_act": f(w_up_act), "w_up_lin": f(w_up_lin),
        "w_conv_col": np.ascontiguousarray(f(w_conv).reshape(2, 3, NFC, 128).transpose(0, 3, 2, 1)),
        "b_conv_col": np.ascontiguousarray(f(b_conv).reshape(2, NFC, 128).transpose(0, 2, 1)),
        "w_down": f(w_down),
    }
    sh.update(_consts())
    return sh


def _core_inputs(x, c, b, shared):
    m = dict(shared)
    m["x"] = np.ascontiguousarray(np.asarray(x[b], dtype=np.float32))
    m["cT"] = np.ascontiguousarray(np.asarray(c[b], dtype=np.float32).reshape(8, 128).T)
    return m


def kernel(x, c, w_ada, b_ada, g_pre_mix, w_in, g_moba_out, g_dsa_out, w_out, g_post_mix,
           g_pre_ffn, w_up_act, w_up_lin, w_conv, b_conv, w_down, g_post_ffn):
    x = np.asarray(x)
    c = np.asarray(c)
    shared = _shared_inputs(w_ada, b_ada, g_pre_mix, w_in, g_moba_out, g_dsa_out, w_out, g_post_mix, g_pre_ffn,
                            w_up_act, w_up_lin, w_conv, b_conv, w_down, g_post_ffn)
    nc = build_nc()
    in_maps = [_core_inputs(x, c, b, shared) for b in range(8)]
    res = run_bass_kernel_spmd(nc, in_maps, core_ids=list(range(8)))
    return np.stack([np.asarray(r["out"], dtype=np.float32) for r in res.results], axis=0)
```

```python
import numpy as np
import ml_dtypes
from contextlib import ExitStack
import concourse.bass as bass
import concourse.mybir as mybir
from concourse.bass_utils import run_bass_kernel_spmd

F32 = mybir.dt.float32
BF16 = mybir.dt.bfloat16
ALU = mybir.AluOpType
AF = mybir.ActivationFunctionType
AX = mybir.AxisListType


class DSem:
    __slots__ = ("idx", "cnt")

    def __init__(self, idx):
        self.idx = idx
        self.cnt = 0


class Buf:
    __slots__ = ("name", "t", "w", "r", "dsem")

    def __init__(self, name, t=None):
        self.name = name
        self.t = t
        self.w = {}
        self.r = {}
        self.dsem = None


class _Scope:
    def __init__(self, fw):
        self.fw = fw

    def __enter__(self):
        fw = self.fw
        self.prev = (fw.es, fw.scope_dsems)
        self.stack = ExitStack()
        self.stack.__enter__()
        fw.es = self.stack
        fw.scope_dsems = []
        return self

    def __exit__(self, *a):
        fw = self.fw
        fw.barrier()
        fw.dpool.extend(fw.scope_dsems)
        fw.es, fw.scope_dsems = self.prev
        return self.stack.__exit__(*a)


class FW:
    SEM_MAX = 30000

    def __init__(self, nc, es):
        self.nc = nc
        self.es = es
        self.sem_es = es
        self.eng = {"pe": nc.tensor, "act": nc.scalar, "dve": nc.vector, "pool": nc.gpsimd, "sp": nc.sync}
        self.sems = []
        self.cur = {}
        self.own = {e: set() for e in self.eng}
        self.known = {e: {} for e in self.eng}
        self.issued = {}
        self.dpool = []
        self.scope_dsems = []
        self.nwaits = 0
        self.nops = 0
        for e in self.eng:
            self._newsem(e)

    def _alloc_sem(self, name):
        s = self.sem_es.enter_context(self.nc.semaphore(name))
        self.sems.append(s)
        return len(self.sems) - 1

    def _newsem(self, e):
        i = self._alloc_sem(f"s_{e}_{len(self.sems)}")
        self.cur[e] = [i, 0]
        self.own[e].add(i)

    def _get_dsem(self):
        if self.dpool:
            d = self.dpool.pop()
        else:
            d = DSem(self._alloc_sem(f"d_{len(self.sems)}"))
        self.scope_dsems.append(d)
        return d

    def scope(self):
        return _Scope(self)

    def sb(self, name, shape, dtype, dma=False):
        self.nuniq = getattr(self, "nuniq", 0) + 1
        t = self.es.enter_context(self.nc.sbuf_tensor(f"{name}_u{self.nuniq}", shape, dtype))
        b = Buf(name, t)
        if dma:
            b.dsem = self._get_dsem()
        return b

    def ps(self, name, shape, dtype):
        self.nuniq = getattr(self, "nuniq", 0) + 1
        t = self.es.enter_context(self.nc.psum_tensor(f"{name}_u{self.nuniq}", shape, dtype))
        return Buf(name, t)

    def view(self, name, t, dma=False):
        b = Buf(name, t)
        if dma:
            b.dsem = self._get_dsem()
        return b

    def _need(self, reads, writes):
        need = {}
        for b in reads:
            for s, v in b.w.items():
                if need.get(s, 0) < v:
                    need[s] = v
        for b in writes:
            for s, v in b.w.items():
                if need.get(s, 0) < v:
                    need[s] = v
            for s, v in b.r.items():
                if need.get(s, 0) < v:
                    need[s] = v
        return need

    def _waits(self, e, need, skip_own=False):
        k = self.known[e]
        eng = self.eng[e]
        for s, v in need.items():
            if skip_own and s in self.own[e]:
                continue
            if k.get(s, 0) >= v:
                continue
            eng.wait_ge(self.sems[s], v)
            self.nwaits += 1
            k[s] = v

    def _record(self, t, reads, writes):
        s, v = t
        self.issued[s] = v
        for b in reads:
            if b.r.get(s, 0) < v:
                b.r[s] = v
        for b in writes:
            b.w = {s: v}
            b.r = {}

    def op(self, e, fn, reads=(), writes=(), same=None):
        if same is None:
            same = (e != "pe")
        need = self._need(reads, writes)
        self._waits(e, need, skip_own=not same)
        ins = fn(self.eng[e])
        c = self.cur[e]
        c[1] += 1
        ins.then_inc(self.sems[c[0]], 1)
        self._record((c[0], c[1]), reads, writes)
        self.nops += 1
        if c[1] >= self.SEM_MAX:
            self._newsem(e)
        return ins

    def dma(self, q, out, in_, reads=(), writes=(), sem=None, **kw):
        return self.dma_group(q, [(out, in_)], reads, writes, sem, **kw)

    def dma_group(self, q, pairs, reads=(), writes=(), sem=None, **kw):
        d = sem.dsem
        need = self._need(reads, writes)
        if d.cnt:
            v = 16 * d.cnt
            if need.get(d.idx, 0) < v:
                need[d.idx] = v
        self._waits(q, need)
        for (out, in_) in pairs:
            ins = self.eng[q].dma_start(out=out, in_=in_, **kw)
            d.cnt += 1
            ins.then_inc(self.sems[d.idx], 16)
            self.nops += 1
        self._record((d.idx, 16 * d.cnt), reads, writes)

    def barrier(self):
        need = dict(self.issued)
        for e in self.eng:
            self._waits(e, need)


S = 4096
D = 1024
NT = 32
NCH = 8
DFF = 2816
NFC = 22
NCOLP = 6280
C_MV, C_DV, C_WI = 5248, 5760, 6272
BIG = 30000.0
KBIS = 24
EPS = 1e-6


class P:
    pass


def _rms_rstd(fw, ssb, rstd, nh, n):
    (ss_buf, ss_ap), (r_buf, r_ap) = ssb, rstd
    fw.op('dve', lambda e: e.tensor_scalar(out=ss_ap, in0=ss_ap, scalar1=1.0 / n, scalar2=EPS, op0=ALU.mult, op1=ALU.add),
          reads=[ss_buf], writes=[ss_buf])
    fw.op('pool', lambda e: e.tensor_tensor(out=r_ap, in0=ss_ap, in1=nh.t[:, 0:1], op=ALU.pow), reads=[ss_buf, nh], writes=[r_buf])


def build_nc(nlayers=2, phases="ABCDEF", dbg=False):
    nc = bass.Bass("TRN2", target_bir_lowering=False)
    p = P()

    def din(name, shape, dt=F32):
        return nc.dram_tensor(name, shape, dt, kind="ExternalInput").ap()

    def dscr(name, shape, dt):
        return nc.dram_tensor(name, shape, dt, kind=("ExternalOutput" if dbg else "Internal")).ap()

    x = din("x", [S, D])
    cT = din("cT", [128, 8])
    w_ada = din("w_ada", [2, D, 6 * D])
    b_ada = din("b_ada", [2, 6 * D])
    b_ada_col = din("b_ada_col", [2, 128, 48])
    g_pre_mix_col = din("g_pre_mix_col", [2, 128, 8])
    g_pre_ffn_col = din("g_pre_ffn_col", [2, 128, 8])
    g_post_mix = din("g_post_mix", [2, D])
    g_post_ffn = din("g_post_ffn", [2, D])
    g_moba_out = din("g_moba_out", [2, 512])
    g_dsa_out = din("g_dsa_out", [2, 512])
    w_in_p = din("w_in_p", [2, D, NCOLP])
    w_out = din("w_out", [2, D, D])
    w_up_act = din("w_up_act", [2, D, DFF])
    w_up_lin = din("w_up_lin", [2, D, DFF])
    w_conv_col = din("w_conv_col", [2, 128, NFC, 3])
    b_conv_col = din("b_conv_col", [2, 128, NFC])
    w_down = din("w_down", [2, DFF, D])
    ropeC = din("ropeC", [128, S])
    ropeS = din("ropeS", [128, S])
    ident_d = din("ident", [128, 128], BF16)
    tri_d = din("tri", [128, 128], BF16)
    negtri_d = din("negtri", [128, 128])
    onehot_d = din("onehot", [16, S], BF16)
    cbsel_d = din("cbsel", [128, 512])
    cblt_d = din("cblt", [128, 512])
    cbfin_d = din("cbfin", [128, 512])
    cpow_d = din("cpow", [128, KBIS])
    out = nc.dram_tensor("out", [S, D], F32, kind="ExternalOutput").ap()

    scrT = [dscr(n, [512, S], BF16) for n in ("mqT", "mkT", "dqT", "dkT", "qiT")]
    kiT_d = dscr("kiT", [64, S], BF16)
    mva = dscr("mva", [S, 520], BF16)
    dva = dscr("dva", [S, 520], BF16)
    om_d = dscr("om", [S, 512], F32)
    od_d = dscr("od", [S, 512], F32)
    x1_d = dscr("x1", [S, D], F32)
    x2_d = dscr("x2", [S, D], F32)
    gbc_d = dscr("gbc", [2, 2, 128, D], F32)

    with ExitStack() as es:
        fw = FW(nc, es)
        AB = fw.sb("AB", [128, 2, 4, 8], F32)
        WI = fw.sb("WI", [128, NT, 8], F32)
        ksum = fw.sb("ksum", [128, 4, 16], F32)
        ident = fw.sb("identb", [128, 128], BF16, dma=True)
        nh = fw.sb("neghalf", [128, 1], F32)
        fw.dma('sp', ident.t[:], ident_d[:, :], writes=[ident], sem=ident)
        fw.op('dve', lambda e: e.memset(nh.t[:], -0.5), writes=[nh])

        if "A" in phases:
          with fw.scope():
            ct = fw.sb("ct", [128, 8], F32, dma=True)
            sc = fw.sb("sc", [128, 8], F32)
            screp = fw.sb("screp", [128, 8, 128], F32)
            ones1 = fw.sb("ones1", [1, 128], F32)
            wa = [fw.sb(f"wa{i}", [128, 8, 1024], F32, dma=True) for i in range(2)]
            brow = [fw.sb(f"brow{i}", [1, 1024], F32, dma=True) for i in range(2)]
            bcol = fw.sb("bcol", [128, 2, 48], F32, dma=True)
            gcol = fw.sb("gcol", [128, 2, 2, 8], F32, dma=True)
            gpb = [fw.sb(f"gpb{i}", [128, 1024], F32, dma=True) for i in range(2)]
            colps = fw.ps("colps", [128, 512], F32)
            bcps = [fw.ps(f"bcps{i}", [128, 512], F32) for i in range(2)]
            mcol = fw.sb("mcol", [128, 8], F32)
            Gt = [fw.sb(f"Gt{i}", [128, D], F32, dma=True) for i in range(2)]
            fw.dma('sp', ct.t[:], cT[:, :], writes=[ct], sem=ct)
            fw.dma_group('sp', [(bcol.t[:, l, :], b_ada_col[l]) for l in range(2)], writes=[bcol], sem=bcol)
            fw.dma_group('sp', [(gcol.t[:, l, 0, :], g_pre_mix_col[l]) for l in range(2)]
                         + [(gcol.t[:, l, 1, :], g_pre_ffn_col[l]) for l in range(2)], writes=[gcol], sem=gcol)
            fw.op('act', lambda e: e.activation(out=sc.t[:], in_=ct.t[:], func=AF.Silu), reads=[ct], writes=[sc])
            fw.op('dve', lambda e: e.tensor_copy(out=screp.t[:], in_=sc.t[:, :].unsqueeze(2).to_broadcast([128, 8, 128])),
                  reads=[sc], writes=[screp])
            fw.op('dve', lambda e: e.memset(ones1.t[:], 1.0), writes=[ones1])
            pi = 0
            for l in range(nlayers):
                wv = w_ada[l].rearrange("(k p) n -> p k n", p=128)
                for piece in range(6):
                    w = wa[pi % 2]
                    pi += 1
                    fw.dma_group('sp', [(w.t[:, k, :], wv[:, k, piece * 1024:(piece + 1) * 1024]) for k in range(8)],
                                 writes=[w], sem=w)
                    if piece in (2, 5):
                        j = 0 if piece == 2 else 1
                        br = brow[j]
                        gp = gpb[j]
                        fw.dma('sp', br.t[:], b_ada[l:l + 1, piece * 1024:(piece + 1) * 1024], writes=[br], sem=br)
                        fw.dma('sp', gp.t[:], (g_post_mix if j == 0 else g_post_ffn)[l, :].partition_broadcast(128),
                               writes=[gp], sem=gp)
                        for nhf in range(2):
                            ps = bcps[nhf]
                            for k in range(8):
                                fw.op('pe', lambda e: e.matmul(ps.t[:], lhsT=screp.t[:, k, :], rhs=w.t[:, k, nhf * 512:(nhf + 1) * 512],
                                                               start=(k == 0), stop=False), reads=[screp, w], writes=[ps])
                            fw.op('pe', lambda e: e.matmul(ps.t[:], lhsT=ones1.t[0:1, :], rhs=br.t[0:1, nhf * 512:(nhf + 1) * 512],
                                                           start=False, stop=True), reads=[ones1, br], writes=[ps])
                            G = Gt[j]
                            fw.op('dve', lambda e: e.tensor_tensor(out=G.t[:, nhf * 512:(nhf + 1) * 512], in0=ps.t[:],
                                                                   in1=gp.t[:, nhf * 512:(nhf + 1) * 512], op=ALU.mult),
                                  reads=[ps, gp], writes=[G])
                        fw.dma('sp', gbc_d[l, j], Gt[j].t[:], reads=[Gt[j]], sem=Gt[j])
                    else:
                        for jj in range(8):
                            for k in range(8):
                                fw.op('pe', lambda e: e.matmul(colps.t[:, jj:jj + 1], lhsT=w.t[:, k, jj * 128:(jj + 1) * 128],
                                                               rhs=sc.t[:, k:k + 1], start=(k == 0), stop=(k == 7)),
                                      reads=[sc, w], writes=[colps])
                        fw.op('dve', lambda e: e.tensor_tensor(out=mcol.t[:], in0=colps.t[:, 0:8],
                                                               in1=bcol.t[:, l, piece * 8:(piece + 1) * 8], op=ALU.add),
                              reads=[colps, bcol], writes=[mcol])
                        if piece in (0, 3):
                            slot = 1 if piece == 0 else 3
                            fw.op('dve', lambda e: e.tensor_copy(out=AB.t[:, l, slot, :], in_=mcol.t[:]), reads=[mcol], writes=[AB])
                        else:
                            slot = 0 if piece == 1 else 2
                            gi = 0 if piece == 1 else 1
                            fw.op('dve', lambda e: e.scalar_tensor_tensor(out=AB.t[:, l, slot, :], in0=mcol.t[:], scalar=1.0,
                                                                          in1=gcol.t[:, l, gi, :], op0=ALU.add, op1=ALU.mult),
                                  reads=[mcol, gcol], writes=[AB])

        def norm_transpose(xb, l, slot, tp, hbuf, hview, sq, ss, rstd, xn):
            fw.op('dve', lambda e: e.memset(ss.t[:], 0.0), writes=[ss])
            fw.op('act', lambda e: e.activation(out=sq.t[:], in_=xb.t[:], func=AF.Square, accum_out=ss.t[:, 0:1]),
                  reads=[xb], writes=[sq, ss])
            _rms_rstd(fw, (ss, ss.t[:, 0:1]), (rstd, rstd.t[:, 0:1]), nh, D)
            fw.op('dve', lambda e: e.tensor_scalar(out=xn.t[:], in0=xb.t[:], scalar1=rstd.t[:, 0:1], scalar2=None, op0=ALU.mult),
                  reads=[xb, rstd], writes=[xn])
            for k in range(8):
                fw.op('pe', lambda e: e.transpose(out=tp.t[:, k, :], in_=xn.t[:, k * 128:(k + 1) * 128], identity=ident.t[:]),
                      reads=[xn, ident], writes=[tp])
            for k in range(8):
                fw.op('act', lambda e: e.activation(out=hview(k), in_=tp.t[:, k, :], func=AF.Identity,
                                                    scale=AB.t[:, l, slot, k:k + 1], bias=AB.t[:, l, slot + 1, k:k + 1]),
                      reads=[tp, AB], writes=[hbuf])

        for l in range(nlayers):
            xin = x if l == 0 else x2_d
            xout = out if l == nlayers - 1 else x2_d
            if "B" in phases:
              with fw.scope():
                W = fw.sb("winb", [128, 8, NCOLP], BF16, dma=True)
                fw.dma_group('pool', [(W.t[:, k, :], w_in_p[l, k * 128:(k + 1) * 128, :]) for k in range(8)], writes=[W], sem=W)
                rC = fw.sb("rC", [128, S], F32, dma=True)
                rS = fw.sb("rS", [128, S], F32, dma=True)
                fw.dma('sp', rC.t[:], ropeC[:, :], writes=[rC], sem=rC)
                fw.dma('sp', rS.t[:], ropeS[:, :], writes=[rS], sem=rS)
                xt = [fw.sb(f"xt{i}", [128, D], F32, dma=True) for i in range(2)]
                sq = fw.sb("sq", [128, D], BF16)
                ss = [fw.sb(f"ss{i}", [128, 1], F32) for i in range(2)]
                rstd = [fw.sb(f"rstd{i}", [128, 1], F32) for i in range(2)]
                xn = [fw.sb(f"xn{i}", [128, D], BF16) for i in range(2)]
                tps = [fw.ps(f"tp{i}", [128, 8, 128], BF16) for i in range(2)]
                hT = [fw.sb(f"hT{i}", [128, 8, 512], BF16) for i in range(2)]
                pm = [fw.ps(f"pm{i}", [128, 512], F32) for i in range(2)]
                pp = [fw.ps(f"pp{i}", [128, 512], F32) for i in range(2)]
                pv = [fw.ps(f"pv{i}", [128, 512], F32) for i in range(2)]
                t1 = [fw.sb(f"t1_{i}", [128, 512], F32) for i in range(2)]
                t2 = [fw.sb(f"t2_{i}", [128, 512], F32) for i in range(2)]
                t3 = [fw.sb(f"t3_{i}", [128, 512], F32) for i in range(2)]
                ob = [fw.sb(f"ob{i}", [128, 512], BF16, dma=True) for i in range(3)]
                vt = [fw.sb(f"vt{i}", [128, 8, 65], BF16, dma=True) for i in range(2)]
                for v in vt:
                    fw.op('dve', lambda e: e.memset(v.t[:], 1.0), writes=[v])
                fj = 0
                oj = 0
                vj = 0
                for c in range(NCH):
                    h = hT[c % 2]
                    cs = slice(c * 512, (c + 1) * 512)
                    for i in range(4):
                        tt = c * 4 + i
                        xb = xt[tt % 2]
                        fw.dma('sp', xb.t[:], xin[tt * 128:(tt + 1) * 128, :], writes=[xb], sem=xb)
                        norm_transpose(xb, l, 0, tps[tt % 2], h, lambda k: h.t[:, k, i * 128:(i + 1) * 128],
                                       sq, ss[tt % 2], rstd[tt % 2], xn[tt % 2])
                    tiles = [(g, ft, 128) for g in range(5) for ft in range(4)] + [(5, 0, 64)]
                    for (g, ft, M) in tiles:
                        cm = g * 1024 + ft * 128 if g < 5 else 5120
                        cp = cm + 512 if g < 5 else 5184
                        pmain, ppart = pm[fj % 2], pp[fj % 2]
                        a1, a2, a3 = t1[fj % 2], t2[fj % 2], t3[fj % 2]
                        fj += 1
                        for k in range(8):
                            fw.op('pe', lambda e: e.matmul(pmain.t[0:M, :], lhsT=W.t[:, k, cm:cm + M], rhs=h.t[:, k, :],
                                                           start=(k == 0), stop=(k == 7)), reads=[W, h], writes=[pmain])
                        for k in range(8):
                            fw.op('pe', lambda e: e.matmul(ppart.t[0:M, :], lhsT=W.t[:, k, cp:cp + M], rhs=h.t[:, k, :],
                                                           start=(k == 0), stop=(k == 7)), reads=[W, h], writes=[ppart])
                        fw.op('dve', lambda e: e.tensor_tensor(out=a1.t[0:M, :], in0=ppart.t[0:M, :], in1=rS.t[0:M, cs], op=ALU.mult),
                              reads=[ppart, rS], writes=[a1])
                        fw.op('dve', lambda e: e.tensor_tensor(out=a2.t[0:M, :], in0=pmain.t[0:M, :], in1=rC.t[0:M, cs], op=ALU.mult),
                              reads=[pmain, rC], writes=[a2])
                        o = ob[oj % 3]
                        oj += 1
                        if g == 1:
                            fw.op('pool', lambda e: e.tensor_tensor(out=a3.t[:], in0=a1.t[:], in1=a2.t[:], op=ALU.add),
                                  reads=[a1, a2], writes=[a3])
                            fw.op('act', lambda e: e.copy(out=o.t[:], in_=a3.t[:]), reads=[a3], writes=[o])
                            fw.op('dve', lambda e: e.reduce_sum(out=ksum.t[:, ft, 2 * c:2 * c + 2],
                                                                in_=a3.t[:, :].rearrange("p (b s) -> p b s", b=2), axis=AX.X),
                                  reads=[a3], writes=[ksum])
                        else:
                            fw.op('pool', lambda e: e.tensor_tensor(out=o.t[0:M, :], in0=a1.t[0:M, :], in1=a2.t[0:M, :], op=ALU.add),
                                  reads=[a1, a2], writes=[o])
                        dst = scrT[g][ft * 128:(ft + 1) * 128, cs] if g < 5 else kiT_d[:, cs]
                        fw.dma('sp', dst, o.t[0:M, :], reads=[o], sem=o)
                    for i in range(4):
                        tt = c * 4 + i
                        for (dst, c0) in ((mva, C_MV), (dva, C_DV)):
                            ps = pv[vj % 2]
                            v = vt[vj % 2]
                            vj += 1
                            for k in range(8):
                                fw.op('pe', lambda e: e.matmul(ps.t[:], lhsT=h.t[:, k, i * 128:(i + 1) * 128], rhs=W.t[:, k, c0:c0 + 512],
                                                               start=(k == 0), stop=(k == 7)), reads=[W, h], writes=[ps])
                            fw.op('act', lambda e: e.copy(out=v.t[:, :, 0:64], in_=ps.t[:, :].rearrange("p (h d) -> p h d", h=8)),
                                  reads=[ps], writes=[v])
                            fw.dma('sp', dst[tt * 128:(tt + 1) * 128, :], v.t[:, :, :].rearrange("p h d -> p (h d)"), reads=[v], sem=v)
                        ps = pv[vj % 2]
                        vj += 1
                        for k in range(8):
                            fw.op('pe', lambda e: e.matmul(ps.t[:, 0:8], lhsT=h.t[:, k, i * 128:(i + 1) * 128], rhs=W.t[:, k, C_WI:C_WI + 8],
                                                           start=(k == 0), stop=(k == 7)), reads=[W, h], writes=[ps])
                        fw.op('act', lambda e: e.copy(out=WI.t[:, tt, :], in_=ps.t[:, 0:8]), reads=[ps], writes=[WI])
            if "C" in phases:
              with fw.scope():
                V = fw.sb("mV", [128, NT, 520], BF16, dma=True)
                fw.dma('sp', V.t[:], mva.rearrange("(n p) c -> p n c", p=128), writes=[V], sem=V)
                Qt = [fw.sb(f"mQ{i}", [80, S], BF16) for i in range(2)]
                Kt = [fw.sb(f"mK{i}", [80, S], BF16) for i in range(2)]
                Qm = [fw.view(f"mQm{i}", Qt[i].t, dma=True) for i in range(2)]
                Qb = [fw.view(f"mQb{i}", Qt[i].t) for i in range(2)]
                Km = [fw.view(f"mKm{i}", Kt[i].t, dma=True) for i in range(2)]
                Kc = [fw.view(f"mKc{i}", Kt[i].t, dma=True) for i in range(2)]
                for i in range(2):
                    fw.dma('sp', Kt[i].t[64:80, :], onehot_d[:, :], writes=[Kc[i]], sem=Kc[i])
                kmb = fw.sb("kmb", [64, 8, 16], BF16)
                for h in range(8):
                    e_, ft = h % 2, h // 2
                    fw.op('act', lambda e: e.mul(out=kmb.t[0:64, h, :], in_=ksum.t[e_ * 64:(e_ + 1) * 64, ft, :], mul=1.0 / 256.0),
                          reads=[ksum], writes=[kmb])
                tri = fw.sb("tri", [128, 128], BF16, dma=True)
                fw.dma('sp', tri.t[:], tri_d[:, :], writes=[tri], sem=tri)
                cbs = fw.sb("cbs", [128, 3, 512], F32, dma=True)
                fw.dma_group('sp', [(cbs.t[:, 0, :], cbsel_d[:, :]), (cbs.t[:, 1, :], cblt_d[:, :]), (cbs.t[:, 2, :], cbfin_d[:, :])],
                             writes=[cbs], sem=cbs)
                gps = fw.ps("gps", [128, 512], F32)
                tb = fw.ps("tb", [128, 1024], BF16)
                sps = [fw.ps(f"sp{i}", [128, 512], F32) for i in range(3)]
                ops_ = [fw.ps(f"op{i}", [128, 512], F32) for i in range(2)]
                pt = [fw.sb(f"pt{i}", [128, 512], BF16) for i in range(3)]
                gm = fw.sb("gm", [128, 512], F32)
                ee = fw.sb("ee", [128, 512], F32)
                g2 = fw.sb("g2", [128, 512], F32)
                g3 = fw.sb("g3", [128, 512], F32)
                mx = fw.sb("mx", [128, 3, 32], F32)
                bt = fw.sb("bt", [128, 512], BF16)
                rec = [fw.sb(f"rec{i}", [128, 4, 1], F32) for i in range(2)]
                osb = [fw.sb(f"osb{i}", [128, 4, 64], F32, dma=True) for i in range(2)]
                v3 = lambda t: t[:, :].rearrange("p (a n) -> p a n", n=16)
                bc3 = lambda j: mx.t[:, j, :].unsqueeze(2).to_broadcast([128, 32, 16])
                si = 0
                oi = 0
                def gateA(h):
                    b = h % 2
                    fw.dma('sp', Qt[b].t[0:64, :], scrT[0][h * 64:(h + 1) * 64, :], writes=[Qm[b]], sem=Qm[b])
                    fw.dma('sp', Kt[b].t[0:64, :], scrT[1][h * 64:(h + 1) * 64, :], writes=[Km[b]], sem=Km[b])
                    for tt in range(NT):
                        fw.op('pe', lambda e: e.matmul(gps.t[:, tt * 16:(tt + 1) * 16], lhsT=Qt[b].t[0:64, tt * 128:(tt + 1) * 128],
                                                       rhs=kmb.t[0:64, h, :], start=True, stop=True), reads=[Qm[b], kmb], writes=[gps])
                    fw.op('dve', lambda e: e.tensor_tensor(out=gm.t[:], in0=gps.t[:], in1=cbs.t[:, 0, :], op=ALU.add),
                          reads=[gps, cbs], writes=[gm])
                    fw.op('dve', lambda e: e.reduce_max(out=mx.t[:, 0, :], in_=v3(gm.t), axis=AX.X), reads=[gm], writes=[mx])
                    fw.op('dve', lambda e: e.tensor_tensor(out=v3(ee.t), in0=v3(gm.t), in1=bc3(0), op=ALU.is_equal),
                          reads=[gm, mx], writes=[ee])
                    fw.op('dve', lambda e: e.scalar_tensor_tensor(out=g2.t[:], in0=ee.t[:], scalar=-1e9, in1=gm.t[:], op0=ALU.mult, op1=ALU.add),
                          reads=[ee, gm], writes=[g2])
                    fw.op('dve', lambda e: e.reduce_max(out=mx.t[:, 1, :], in_=v3(g2.t), axis=AX.X), reads=[g2], writes=[mx])
                    fw.op('dve', lambda e: e.tensor_tensor(out=v3(ee.t), in0=v3(g2.t), in1=bc3(1), op=ALU.is_equal),
                          reads=[g2, mx], writes=[ee])
                    fw.op('dve', lambda e: e.scalar_tensor_tensor(out=g3.t[:], in0=ee.t[:], scalar=-1e9, in1=g2.t[:], op0=ALU.mult, op1=ALU.add),
                          reads=[ee, g2], writes=[g3])
                    fw.op('dve', lambda e: e.reduce_max(out=mx.t[:, 2, :], in_=v3(g3.t), axis=AX.X), reads=[g3], writes=[mx])
                    fw.op('dve', lambda e: e.tensor_tensor(out=v3(ee.t), in0=v3(gm.t), in1=bc3(2), op=ALU.is_lt),
                          reads=[gm, mx], writes=[ee])
                    fw.op('dve', lambda e: e.tensor_tensor(out=g2.t[:], in0=ee.t[:], in1=cbs.t[:, 1, :], op=ALU.mult),
                          reads=[ee, cbs], writes=[g2])
                    fw.op('dve', lambda e: e.tensor_tensor(out=bt.t[:], in0=g2.t[:], in1=cbs.t[:, 2, :], op=ALU.add),
                          reads=[g2, cbs], writes=[bt])

                def gateB(h):
                    b = h % 2
                    for tt in range(NT):
                        fw.op('pe', lambda e: e.transpose(out=tb.t[0:16, (tt % 4) * 128:(tt % 4 + 1) * 128], in_=bt.t[:, tt * 16:(tt + 1) * 16],
                                                          identity=ident.t[:]), reads=[bt, ident], writes=[tb])
                        if tt % 4 == 3:
                            q4 = tt // 4
                            fw.op('act', lambda e: e.copy(out=Qt[b].t[64:80, q4 * 512:(q4 + 1) * 512], in_=tb.t[0:16, 0:512]),
                                  reads=[tb], writes=[Qb[b]])

                def attn(h):
                    nonlocal si, oi
                    b = h % 2
                    for qc in range(NCH):
                        O = ops_[oi % 2]
                        rc = rec[oi % 2]
                        ob_ = osb[oi % 2]
                        oi += 1
                        nst = 4 * qc + 4

                        def qk(st):
                            nonlocal si
                            sp_ = sps[si % 3]
                            Pt = pt[si % 3]
                            si += 1
                            fw.op('pe', lambda e: e.matmul(sp_.t[:], lhsT=Kt[b].t[0:80, st * 128:(st + 1) * 128],
                                                           rhs=Qt[b].t[0:80, qc * 512:(qc + 1) * 512], start=True, stop=True),
                                  reads=[Km[b], Kc[b], Qm[b], Qb[b]], writes=[sp_])
                            fw.op('act', lambda e: e.activation(out=Pt.t[:], in_=sp_.t[:], func=AF.Exp, scale=0.125),
                                  reads=[sp_], writes=[Pt])
                            j = st - 4 * qc
                            if j >= 0:
                                fw.op('pool', lambda e: e.tensor_tensor(out=Pt.t[:, j * 128:(j + 1) * 128], in0=Pt.t[:, j * 128:(j + 1) * 128],
                                                                        in1=tri.t[:], op=ALU.mult), reads=[Pt, tri], writes=[Pt])
                            return Pt, j

                        pend = qk(0)
                        first = True
                        for st in range(nst):
                            nxt = qk(st + 1) if st + 1 < nst else None
                            Pt, j = pend
                            for i in range(max(j, 0), 4):
                                fw.op('pe', lambda e: e.matmul(O.t[:, i * 65:(i + 1) * 65], lhsT=Pt.t[:, i * 128:(i + 1) * 128],
                                                               rhs=V.t[:, st, h * 65:(h + 1) * 65], start=first, stop=(st == 4 * qc + i)),
                                      reads=[Pt, V], writes=[O])
                                first = False
                            pend = nxt
                        Ov = O.t[:, 0:260].rearrange("p (i d) -> p i d", d=65)
                        fw.op('dve', lambda e: e.reciprocal(out=rc.t[:], in_=Ov[:, :, 64:65]), reads=[O], writes=[rc])
                        fw.op('dve', lambda e: e.tensor_tensor(out=ob_.t[:], in0=Ov[:, :, 0:64], in1=rc.t[:, :, :].to_broadcast([128, 4, 64]),
                                                               op=ALU.mult), reads=[O, rc], writes=[ob_])
                        fw.dma('sp', om_d[qc * 512:(qc + 1) * 512, h * 64:(h + 1) * 64].rearrange("(i p) d -> p i d", p=128), ob_.t[:],
                               reads=[ob_], sem=ob_)

                gateA(0)
                gateB(0)
                for h in range(8):
                    if h + 1 < 8:
                        gateA(h + 1)
                    attn(h)
                    if h + 1 < 8:
                        gateB(h + 1)

            if "D" in phases:
              with fw.scope():
                V = fw.sb("dV", [128, NT, 520], BF16, dma=True)
                fw.dma('sp', V.t[:], dva.rearrange("(n p) c -> p n c", p=128), writes=[V], sem=V)
                K2 = fw.sb("K2", [128, 4, S], BF16, dma=True)
                fw.dma('sp', K2.t[:], scrT[3].rearrange("(hp two d) t -> (two d) hp t", two=2, d=64), writes=[K2], sem=K2)
                ki = fw.sb("kiT", [64, S], BF16, dma=True)
                fw.dma('sp', ki.t[:], kiT_d[:, :], writes=[ki], sem=ki)
                negtri = fw.sb("negtri", [128, 128], F32, dma=True)
                fw.dma('sp', negtri.t[:], negtri_d[:, :], writes=[negtri], sem=negtri)
                cpow = fw.sb("cpow", [128, KBIS], F32, dma=True)
                fw.dma('sp', cpow.t[:], cpow_d[:, :], writes=[cpow], sem=cpow)
                QI = [fw.sb(f"QI{i}", [64, 8, 128], BF16, dma=True) for i in range(2)]
                Q2 = [fw.sb(f"Q2{i}", [128, 4, 128], BF16, dma=True) for i in range(3)]
                i4 = fw.sb("i4", [128, 4, 128], BF16)
                fw.op('dve', lambda e: e.tensor_copy(out=i4.t[:], in_=ident.t[:, :].unsqueeze(1).to_broadcast([128, 4, 128])), reads=[ident], writes=[i4])
                score = [fw.sb(f"score{i}", [128, S], F32) for i in range(2)]
                junk = fw.sb("junk", [128, S], BF16)
                mb = [fw.sb(f"mb{i}", [128, S], BF16) for i in range(2)]
                rlb = [fw.sb(f"rl{i}", [128, 512], BF16) for i in range(3)]
                dg = [fw.sb(f"dg{i}", [128, 8, 128], BF16) for i in range(2)]
                scps = fw.ps("scps", [128, 512], F32)
                lps = [fw.ps(f"lps{i}", [128, 512], F32) for i in range(2)]
                sp3 = [fw.ps(f"dsp{i}", [128, 512], F32) for i in range(3)]
                opsd = [fw.ps(f"dop{e_}", [128, 512], F32) for e_ in range(2)]
                ptd = [fw.sb(f"dpt{i}", [128, 2, 512], BF16) for i in range(2)]
                sm = [fw.sb(f"sm{i}", [128, 8], F32) for i in range(2)]
                dkt = [fw.sb(f"dk{i}", [128, KBIS], F32) for i in range(2)]
                recd = [fw.sb(f"drec{e_}", [128, 4, 1], F32) for e_ in range(2)]
                osbd = [fw.sb(f"dosb{i}", [128, 4, 2, 64], F32, dma=True) for i in range(2)]
                qiv = scrT[4].rearrange("(h d) t -> d h t", d=64)
                q2v = scrT[2].rearrange("(hp two d) t -> (two d) hp t", two=2, d=64)
                cnt_ = {"li": 0, "ti": 0, "ai": 0}

                def indexer(qt):
                    L = (qt + 1) * 128
                    sc_ = score[qt % 2]
                    qi_ = QI[qt % 2]
                    fw.dma('sp', qi_.t[:], qiv[:, :, qt * 128:(qt + 1) * 128], writes=[qi_], sem=qi_)
                    fw.dma('sp', Q2[qt % 3].t[:], q2v[:, :, qt * 128:(qt + 1) * 128], writes=[Q2[qt % 3]], sem=Q2[qt % 3])
                    dg_ = dg[qt % 2]
                    fw.op('dve', lambda e: e.tensor_tensor(out=dg_.t[:], in0=ident.t[:, :].unsqueeze(1).to_broadcast([128, 8, 128]),
                                                           in1=WI.t[:, qt, :].unsqueeze(2).to_broadcast([128, 8, 128]), op=ALU.mult),
                          reads=[ident, WI], writes=[dg_])
                    nch = (L + 511) // 512
                    for ch in range(nch):
                        ncol = min(512, L - ch * 512)
                        cs = slice(ch * 512, ch * 512 + ncol)

                        def logit(h):
                            lp = lps[cnt_["li"] % 2]
                            cnt_["li"] += 1
                            R = rlb[cnt_["ti"] % 3]
                            cnt_["ti"] += 1
                            fw.op('pe', lambda e: e.matmul(lp.t[:, 0:ncol], lhsT=qi_.t[0:64, h, :], rhs=ki.t[0:64, cs], start=True, stop=True),
                                  reads=[qi_, ki], writes=[lp])
                            fw.op('act', lambda e: e.activation(out=R.t[:, 0:ncol], in_=lp.t[:, 0:ncol], func=AF.Relu), reads=[lp], writes=[R])
                            return R

                        pend = logit(0)
                        for h in range(8):
                            nxt = logit(h + 1) if h + 1 < 8 else None
                            R = pend
                            fw.op('pe', lambda e: e.matmul(scps.t[:, 0:ncol], lhsT=dg_.t[:, h, :], rhs=R.t[:, 0:ncol], start=(h == 0), stop=(h == 7)),
                                  reads=[dg_, R], writes=[scps])
                            pend = nxt
                        fw.op('act', lambda e: e.copy(out=sc_.t[:, cs], in_=scps.t[:, 0:ncol]), reads=[scps], writes=[sc_])

                def select(qt):
                    L = (qt + 1) * 128
                    sc_ = score[qt % 2]
                    s_ = sm[qt % 2]
                    dk_ = dkt[qt % 2]
                    mb_ = mb[qt % 2]
                    if qt >= 2:
                        fw.op('dve', lambda e: e.reduce_max(out=s_.t[:, 0:1], in_=sc_.t[:, 0:L], axis=AX.X), reads=[sc_], writes=[s_])
                        fw.op('dve', lambda e: e.tensor_reduce(out=s_.t[:, 1:2], in_=sc_.t[:, 0:L], axis=AX.X, op=ALU.min), reads=[sc_], writes=[s_])
                        fw.op('dve', lambda e: e.scalar_tensor_tensor(out=s_.t[:, 2:3], in0=s_.t[:, 1:2], scalar=-1.0, in1=s_.t[:, 0:1],
                                                                      op0=ALU.mult, op1=ALU.max), reads=[s_], writes=[s_])
                    fw.op('pool', lambda e: e.tensor_tensor(out=sc_.t[:, qt * 128:L], in0=sc_.t[:, qt * 128:L], in1=negtri.t[:], op=ALU.add),
                          reads=[sc_, negtri], writes=[sc_])
                    if qt >= 2:
                        fw.op('dve', lambda e: e.tensor_scalar(out=dk_.t[:], in0=cpow.t[:], scalar1=s_.t[:, 2:3], scalar2=None, op0=ALU.mult),
                              reads=[cpow, s_], writes=[dk_])
                        fw.op('dve', lambda e: e.memset(s_.t[:, 3:4], 0.0), writes=[s_])
                        for k in range(KBIS):
                            fw.op('dve', lambda e: e.tensor_scalar(out=junk.t[:, 0:L], in0=sc_.t[:, 0:L], scalar1=s_.t[:, 3:4], scalar2=0.0,
                                                                   op0=ALU.is_ge, op1=ALU.add, accum_out=s_.t[:, 4:5]),
                                  reads=[sc_, s_], writes=[junk, s_])
                            last = (k == KBIS - 1)
                            fw.op('dve', lambda e: e.tensor_scalar(out=s_.t[:, 5:6], in0=s_.t[:, 4:5], scalar1=255.5,
                                                                   scalar2=(1.0 if last else 0.5), op0=ALU.is_ge, op1=ALU.subtract),
                                  reads=[s_], writes=[s_])
                            dst = s_.t[:, 6:7] if last else s_.t[:, 3:4]
                            fw.op('dve', lambda e: e.scalar_tensor_tensor(out=dst, in0=s_.t[:, 5:6], scalar=dk_.t[:, k:k + 1], in1=s_.t[:, 3:4],
                                                                          op0=ALU.mult, op1=ALU.add), reads=[s_, dk_], writes=[s_])
                        fw.op('dve', lambda e: e.tensor_scalar(out=mb_.t[:, 0:L], in0=sc_.t[:, 0:L], scalar1=s_.t[:, 6:7], scalar2=-BIG,
                                                               op0=ALU.is_lt, op1=ALU.mult), reads=[sc_, s_], writes=[mb_])
                    else:
                        fw.op('dve', lambda e: e.tensor_scalar(out=mb_.t[:, 0:L], in0=sc_.t[:, 0:L], scalar1=-1e29, scalar2=-BIG,
                                                               op0=ALU.is_lt, op1=ALU.mult), reads=[sc_], writes=[mb_])

                def attend(qt):
                    mb_ = mb[qt % 2]
                    q2_ = Q2[qt % 3]
                    def qk(st):
                        ai_ = cnt_["ai"]
                        PT = ptd[ai_ % 2]
                        cnt_["ai"] += 1
                        for e_ in range(2):
                            bank = sp3[(2 * ai_ + e_) % 3]
                            for hp in range(4):
                                fw.op('pe', lambda e: e.matmul(bank.t[:, hp * 128:(hp + 1) * 128],
                                                               lhsT=K2.t[64 * e_:64 * e_ + 64, hp, st * 128:(st + 1) * 128],
                                                               rhs=q2_.t[64 * e_:64 * e_ + 64, hp, :], start=(hp == 0), stop=False),
                                      reads=[K2, q2_], writes=[bank])
                            fw.op('pe', lambda e: e.matmul(bank.t[:], lhsT=mb_.t[:, st * 128:(st + 1) * 128],
                                                           rhs=i4.t[:, :, :].rearrange("p a t -> p (a t)"), start=False, stop=True),
                                  reads=[mb_, i4], writes=[bank])
                            fw.op('act', lambda e: e.activation(out=PT.t[:, e_, :], in_=bank.t[:], func=AF.Exp, scale=0.125),
                                  reads=[bank], writes=[PT])
                        return PT

                    pend = qk(0)
                    for st in range(qt + 1):
                        nxt = qk(st + 1) if st + 1 <= qt else None
                        PT = pend
                        for e_ in range(2):
                            for hp in range(4):
                                hh = 2 * hp + e_
                                fw.op('pe', lambda e: e.matmul(opsd[e_].t[:, hp * 65:(hp + 1) * 65], lhsT=PT.t[:, e_, hp * 128:(hp + 1) * 128],
                                                               rhs=V.t[:, st, hh * 65:(hh + 1) * 65], start=(st == 0 and hp == 0), stop=(st == qt)),
                                      reads=[PT, V], writes=[opsd[e_]])
                        pend = nxt
                    ob_ = osbd[qt % 2]
                    for e_ in range(2):
                        Ov = opsd[e_].t[:, 0:260].rearrange("p (i d) -> p i d", d=65)
                        fw.op('dve', lambda e: e.reciprocal(out=recd[e_].t[:], in_=Ov[:, :, 64:65]), reads=[opsd[e_]], writes=[recd[e_]])
                        fw.op('dve', lambda e: e.tensor_tensor(out=ob_.t[:, :, e_, :], in0=Ov[:, :, 0:64],
                                                               in1=recd[e_].t[:, :, :].to_broadcast([128, 4, 64]), op=ALU.mult),
                              reads=[opsd[e_], recd[e_]], writes=[ob_])
                    fw.dma('sp', od_d[qt * 128:(qt + 1) * 128, :], ob_.t[:, :, :, :].rearrange("p a e d -> p (a e d)"), reads=[ob_], sem=ob_)

                indexer(0)
                for qt in range(NT):
                    if qt + 1 < NT:
                        indexer(qt + 1)
                    select(qt)
                    if qt >= 1:
                        attend(qt - 1)
                attend(NT - 1)

            if "E" in phases:
              with fw.scope():
                Wo = fw.sb("Wo", [128, 8, D], BF16, dma=True)
                fw.dma_group('pool', [(Wo.t[:, k, :], w_out[l, k * 128:(k + 1) * 128, :]) for k in range(8)], writes=[Wo], sem=Wo)
                gmo = fw.sb("gmo", [128, D], F32, dma=True)
                fw.dma_group('sp', [(gmo.t[:, 0:512], g_moba_out[l, :].partition_broadcast(128)),
                                    (gmo.t[:, 512:1024], g_dsa_out[l, :].partition_broadcast(128))], writes=[gmo], sem=gmo)
                G = fw.sb("Gm", [128, D], F32, dma=True)
                fw.dma('sp', G.t[:], gbc_d[l, 0], writes=[G], sem=G)
                ot = [fw.sb(f"ot{i}", [128, D], F32, dma=True) for i in range(2)]
                xt = [fw.sb(f"ext{i}", [128, D], F32, dma=True) for i in range(2)]
                sq = fw.sb("esq", [128, D], BF16)
                ss = [fw.sb(f"ess{i}", [128, 4], F32) for i in range(2)]
                rs = [fw.sb(f"ers{i}", [128, 4], F32) for i in range(2)]
                on = [fw.sb(f"on{i}", [128, D], BF16) for i in range(2)]
                tps = [fw.ps(f"etp{i}", [128, 8, 128], BF16) for i in range(2)]
                oT = [fw.sb(f"oT{i}", [128, 8, 128], BF16) for i in range(2)]
                yps = [[fw.ps(f"yps{i}{j}", [128, 512], F32) for j in range(2)] for i in range(2)]
                ysb = [fw.sb(f"ysb{i}", [128, D], F32, dma=True) for i in range(2)]
                for tt in range(NT):
                    o_, x_, s_, r_, n_, tp, oT_, yp, y_ = ot[tt % 2], xt[tt % 2], ss[tt % 2], rs[tt % 2], on[tt % 2], tps[tt % 2], oT[tt % 2], yps[tt % 2], ysb[tt % 2]
                    rows = slice(tt * 128, (tt + 1) * 128)
                    fw.dma_group('sp', [(o_.t[:, 0:512], om_d[rows, :]), (o_.t[:, 512:1024], od_d[rows, :])], writes=[o_], sem=o_)
                    fw.dma('sp', x_.t[:], xin[rows, :], writes=[x_], sem=x_)
                    fw.op('dve', lambda e: e.memset(s_.t[:], 0.0), writes=[s_])
                    for j in range(2):
                        fw.op('act', lambda e: e.activation(out=sq.t[:, 0:512], in_=o_.t[:, j * 512:(j + 1) * 512], func=AF.Square,
                                                            accum_out=s_.t[:, j:j + 1]), reads=[o_], writes=[sq, s_])
                    fw.op('dve', lambda e: e.tensor_scalar(out=s_.t[:, 0:2], in0=s_.t[:, 0:2], scalar1=1.0 / 512, scalar2=EPS, op0=ALU.mult, op1=ALU.add),
                          reads=[s_], writes=[s_])
                    fw.op('pool', lambda e: e.tensor_tensor(out=r_.t[:, 0:2], in0=s_.t[:, 0:2], in1=nh.t[:, 0:1].to_broadcast([128, 2]), op=ALU.pow),
                          reads=[s_, nh], writes=[r_])
                    for j in range(2):
                        fw.op('dve', lambda e: e.scalar_tensor_tensor(out=n_.t[:, j * 512:(j + 1) * 512], in0=o_.t[:, j * 512:(j + 1) * 512],
                                                                      scalar=r_.t[:, j:j + 1], in1=gmo.t[:, j * 512:(j + 1) * 512],
                                                                      op0=ALU.mult, op1=ALU.mult), reads=[o_, r_, gmo], writes=[n_])
                    for k in range(8):
                        fw.op('pe', lambda e: e.transpose(out=tp.t[:, k, :], in_=n_.t[:, k * 128:(k + 1) * 128], identity=ident.t[:]),
                              reads=[n_, ident], writes=[tp])
                    fw.op('act', lambda e: e.copy(out=oT_.t[:], in_=tp.t[:]), reads=[tp], writes=[oT_])
                    for j in range(2):
                        for k in range(8):
                            fw.op('pe', lambda e: e.matmul(yp[j].t[:], lhsT=oT_.t[:, k, :], rhs=Wo.t[:, k, j * 512:(j + 1) * 512],
                                                           start=(k == 0), stop=(k == 7)), reads=[oT_, Wo], writes=[yp[j]])
                        fw.op('act', lambda e: e.activation(out=sq.t[:, 0:512], in_=yp[j].t[:], func=AF.Square, accum_out=s_.t[:, 2 + j:3 + j]),
                              reads=[yp[j]], writes=[sq, s_])
                    fw.op('dve', lambda e: e.tensor_tensor(out=s_.t[:, 2:3], in0=s_.t[:, 2:3], in1=s_.t[:, 3:4], op=ALU.add), reads=[s_], writes=[s_])
                    _rms_rstd(fw, (s_, s_.t[:, 2:3]), (r_, r_.t[:, 2:3]), nh, D)
                    for j in range(2):
                        fw.op('dve', lambda e: e.scalar_tensor_tensor(out=y_.t[:, j * 512:(j + 1) * 512], in0=yp[j].t[:], scalar=r_.t[:, 2:3],
                                                                      in1=G.t[:, j * 512:(j + 1) * 512], op0=ALU.mult, op1=ALU.mult),
                              reads=[yp[j], r_, G], writes=[y_])
                    fw.op('pool', lambda e: e.tensor_tensor(out=y_.t[:], in0=y_.t[:], in1=x_.t[:], op=ALU.add), reads=[y_, x_], writes=[y_])
                    fw.dma('sp', x1_d[rows, :], y_.t[:], reads=[y_], sem=y_)

            if "F" in phases:
              with fw.scope():
                Wa = fw.sb("Wa", [128, 8, DFF], BF16, dma=True)
                Wl = fw.sb("Wl", [128, 8, DFF], BF16, dma=True)
                Wd = fw.sb("Wd", [128, NFC, D], BF16, dma=True)
                fw.dma_group('pool', [(Wa.t[:, k, :], w_up_act[l, k * 128:(k + 1) * 128, :]) for k in range(8)], writes=[Wa], sem=Wa)
                fw.dma_group('pool', [(Wl.t[:, k, :], w_up_lin[l, k * 128:(k + 1) * 128, :]) for k in range(8)], writes=[Wl], sem=Wl)
                fw.dma_group('pool', [(Wd.t[:, f, :], w_down[l, f * 128:(f + 1) * 128, :]) for f in range(NFC)], writes=[Wd], sem=Wd)
                wc = fw.sb("wc", [128, NFC, 3], F32, dma=True)
                bcv = fw.sb("bcv", [128, NFC], F32, dma=True)
                fw.dma('sp', wc.t[:], w_conv_col[l], writes=[wc], sem=wc)
                fw.dma('sp', bcv.t[:], b_conv_col[l], writes=[bcv], sem=bcv)
                G = fw.sb("Gf", [128, D], F32, dma=True)
                fw.dma('sp', G.t[:], gbc_d[l, 1], writes=[G], sem=G)
                carry = fw.sb("carry", [128, NFC, 2], F32)
                fw.op('dve', lambda e: e.memset(carry.t[:], 0.0), writes=[carry])
                xt = fw.sb("fxt", [128, D], F32, dma=True)
                sq = fw.sb("fsq", [128, D], BF16)
                ss = fw.sb("fss", [128, 4], F32)
                rs = fw.sb("frs", [128, 4], F32)
                xn = fw.sb("fxn", [128, D], BF16)
                tps = [fw.ps(f"ftp{i}", [128, 8, 128], BF16) for i in range(2)]
                hT = fw.sb("fhT", [128, 8, 512], BF16)
                ups = [fw.ps(f"ups{i}", [128, 512], F32) for i in range(2)]
                lps = [fw.ps(f"flps{i}", [128, 512], F32) for i in range(2)]
                yps = [fw.ps(f"fyps{j}", [128, 512], F32) for j in range(2)]
                ubuf = [fw.sb(f"ubuf{i}", [128, 514], F32) for i in range(2)]
                av = [fw.sb(f"av{i}", [128, 512], F32) for i in range(2)]
                gT = fw.sb("gT", [128, NFC, 512], BF16)
                xr = fw.sb("xr", [128, D], F32, dma=True)
                ysb = fw.sb("fysb", [128, D], F32, dma=True)
                ssv = fw.view("fssv", ss.t)
                rsv = fw.view("frsv", rs.t)
                fi = 0
                for c in range(NCH):
                    for i in range(4):
                        tt = c * 4 + i
                        fw.dma('sp', xt.t[:], x1_d[tt * 128:(tt + 1) * 128, :], writes=[xt], sem=xt)
                        norm_transpose(xt, l, 2, tps[tt % 2], hT, lambda k: hT.t[:, k, i * 128:(i + 1) * 128], sq, ssv, rsv, xn)
                    for fc in range(NFC):
                        U, Lp, ub, a_ = ups[fi % 2], lps[fi % 2], ubuf[fi % 2], av[fi % 2]
                        fi += 1
                        fs = slice(fc * 128, (fc + 1) * 128)
                        for k in range(8):
                            fw.op('pe', lambda e: e.matmul(U.t[:], lhsT=Wa.t[:, k, fs], rhs=hT.t[:, k, :], start=(k == 0), stop=(k == 7)),
                                  reads=[Wa, hT], writes=[U])
                        for k in range(8):
                            fw.op('pe', lambda e: e.matmul(Lp.t[:], lhsT=Wl.t[:, k, fs], rhs=hT.t[:, k, :], start=(k == 0), stop=(k == 7)),
                                  reads=[Wl, hT], writes=[Lp])
                        fw.op('act', lambda e: e.copy(out=ub.t[:, 2:514], in_=U.t[:]), reads=[U], writes=[ub])
                        fw.op('act', lambda e: e.copy(out=ub.t[:, 0:2], in_=carry.t[:, fc, :]), reads=[carry], writes=[ub])
                        fw.op('dve', lambda e: e.tensor_scalar(out=a_.t[:], in0=ub.t[:, 2:514], scalar1=wc.t[:, fc, 2:3], scalar2=bcv.t[:, fc:fc + 1],
                                                               op0=ALU.mult, op1=ALU.add), reads=[ub, wc, bcv], writes=[a_])
                        fw.op('dve', lambda e: e.scalar_tensor_tensor(out=a_.t[:], in0=ub.t[:, 1:513], scalar=wc.t[:, fc, 1:2], in1=a_.t[:],
                                                                      op0=ALU.mult, op1=ALU.add), reads=[ub, wc, a_], writes=[a_])
                        fw.op('dve', lambda e: e.scalar_tensor_tensor(out=a_.t[:], in0=ub.t[:, 0:512], scalar=wc.t[:, fc, 0:1], in1=a_.t[:],
                                                                       op0=ALU.mult, op1=ALU.add), reads=[ub, wc, a_], writes=[a_])
                        fw.op('act', lambda e: e.copy(out=carry.t[:, fc, :], in_=ub.t[:, 512:514]), reads=[ub], writes=[carry])
                        fw.op('act', lambda e: e.activation(out=a_.t[:], in_=a_.t[:], func=AF.Gelu_apprx_tanh), reads=[a_], writes=[a_])
                        fw.op('dve', lambda e: e.tensor_tensor(out=gT.t[:, fc, :], in0=a_.t[:], in1=Lp.t[:], op=ALU.mult),
                              reads=[a_, Lp], writes=[gT])
                    for i in range(4):
                        tt = c * 4 + i
                        rows = slice(tt * 128, (tt + 1) * 128)
                        fw.dma('sp', xr.t[:], x1_d[rows, :], writes=[xr], sem=xr)
                        fw.op('dve', lambda e: e.memset(ss.t[:, 2:4], 0.0), writes=[ssv])
                        for j in range(2):
                            for fc in range(NFC):
                                fw.op('pe', lambda e: e.matmul(yps[j].t[:], lhsT=gT.t[:, fc, i * 128:(i + 1) * 128], rhs=Wd.t[:, fc, j * 512:(j + 1) * 512],
                                                               start=(fc == 0), stop=(fc == NFC - 1)), reads=[gT, Wd], writes=[yps[j]])
                            fw.op('act', lambda e: e.activation(out=sq.t[:, 0:512], in_=yps[j].t[:], func=AF.Square, accum_out=ss.t[:, 2 + j:3 + j]),
                                  reads=[yps[j]], writes=[sq, ssv])
                        fw.op('dve', lambda e: e.tensor_tensor(out=ss.t[:, 2:3], in0=ss.t[:, 2:3], in1=ss.t[:, 3:4], op=ALU.add), reads=[ssv], writes=[ssv])
                        _rms_rstd(fw, (ssv, ss.t[:, 2:3]), (rsv, rs.t[:, 2:3]), nh, D)
                        for j in range(2):
                            fw.op('dve', lambda e: e.scalar_tensor_tensor(out=ysb.t[:, j * 512:(j + 1) * 512], in0=yps[j].t[:], scalar=rs.t[:, 2:3],
                                                                          in1=G.t[:, j * 512:(j + 1) * 512], op0=ALU.mult, op1=ALU.mult),
                                  reads=[yps[j], rsv, G], writes=[ysb])
                        fw.op('pool', lambda e: e.tensor_tensor(out=ysb.t[:], in0=ysb.t[:], in1=xr.t[:], op=ALU.add), reads=[ysb, xr], writes=[ysb])
                        fw.dma('sp', xout[rows, :], ysb.t[:], reads=[ysb], sem=ysb)
        fw.barrier()
    return nc


def _consts():
    bf = ml_dtypes.bfloat16
    pos = np.arange(S, dtype=np.float32)
    inv = (np.float32(500000.0) ** (-np.arange(0, 16, 2, dtype=np.float32) / np.float32(16))).astype(np.float32)
    ang = (pos[None, :] * inv[:, None]).astype(np.float32)
    cos, sin = np.cos(ang).astype(np.float32), np.sin(ang).astype(np.float32)
    C = np.ones((128, S), np.float32)
    Sg = np.zeros((128, S), np.float32)
    for p_ in range(128):
        d = p_ % 64
        if d < 16:
            C[p_] = cos[d % 8]
            Sg[p_] = -sin[d % 8] if d < 8 else sin[d % 8]
    i = np.arange(128)
    tri = (i[None, :] >= i[:, None]).astype(np.float32).astype(bf)
    negtri = np.where(i[None, :] <= i[:, None], 0.0, -1e30).astype(np.float32)
    onehot = (np.arange(S)[None, :] // 256 == np.arange(16)[:, None]).astype(np.float32).astype(bf)
    tt = np.repeat(np.arange(NT), 16)
    n = np.tile(np.arange(16), NT)
    cur = tt // 2
    cbsel = np.where(n >= cur, -BIG, 0.0).astype(np.float32)
    cblt = np.where(n < cur, -BIG, 0.0).astype(np.float32)
    cbfin = np.where(n > cur, -BIG, 0.0).astype(np.float32)
    rep = lambda v: np.ascontiguousarray(np.broadcast_to(v[None, :], (128, v.shape[0])))
    cpow = (2.0 ** (-np.arange(KBIS, dtype=np.float64))).astype(np.float32)
    return {
        "ropeC": C, "ropeS": Sg, "ident": np.eye(128, dtype=np.float32).astype(bf), "tri": tri, "negtri": negtri,
        "onehot": onehot, "cbsel": rep(cbsel), "cblt": rep(cblt), "cbfin": rep(cbfin), "cpow": rep(cpow),
    }


def _perm_w_in(w_in):
    offs = {"mq": 0, "mk": 512, "mv": 1024, "dq": 1536, "dk": 2048, "dv": 2560, "qi": 3072, "ki": 3584, "wi": 3648}
    j = np.arange(64)
    perm = np.where(j < 8, j + 8, np.where(j < 16, j - 8, j))
    cols = []
    for g in ("mq", "mk", "dq", "dk", "qi"):
        base = offs[g]
        cols.append(base + np.arange(512))
        cols.append(base + (np.arange(512) // 64) * 64 + perm[np.arange(512) % 64])
    cols.append(offs["ki"] + np.arange(64))
    cols.append(offs["ki"] + perm)
    cols.append(offs["mv"] + np.arange(512))
    cols.append(offs["dv"] + np.arange(512))
    cols.append(offs["wi"] + np.arange(8))
    cols = np.concatenate(cols)
    assert cols.shape[0] == NCOLP
    return np.ascontiguousarray(w_in[:, :, cols])


def _shared_inputs(w_ada, b_ada, g_pre_mix, w_in, g_moba_out, g_dsa_out, w_out, g_post_mix, g_pre_ffn,
                   w_up_act, w_up_lin, w_conv, b_conv, w_down, g_post_ffn):
    f = lambda a: np.ascontiguousarray(np.asarray(a, dtype=np.float32))
    col8 = lambda g: np.ascontiguousarray(f(g).reshape(2, 8, 128).transpose(0, 2, 1))
    sh = {
        "w_ada": f(w_ada), "b_ada": f(b_ada),
        "b_ada_col": np.ascontiguousarray(f(b_ada).reshape(2, 48, 128).transpose(0, 2, 1)),
        "g_pre_mix_col": col8(g_pre_mix), "g_pre_ffn_col": col8(g_pre_ffn),
        "g_post_mix": f(g_post_mix), "g_post_ffn": f(g_post_ffn),
        "g_moba_out": f(g_moba_out), "g_dsa_out": f(g_dsa_out),
        "w_in_p": _perm_w_in(f(w_in)), "w_out": f(w_out), "w_up_act": f(w_up_act), "w_up_lin": f(w_up_lin),
        "w_conv_col": np.ascontiguousarray(f(w_conv).reshape(2, 3, NFC, 128).transpose(0, 3, 2, 1)),
        "b_conv_col": np.ascontiguousarray(f(b_conv).reshape(2, NFC, 128).transpose(0, 2, 1)),
        "w_down": f(w_down),
    }
    sh.update(_consts())
    return sh


def _core_inputs(x, c, b, shared):
    m = dict(shared)
    m["x"] = np.ascontiguousarray(np.asarray(x[b], dtype=np.float32))
    m["cT"] = np.ascontiguousarray(np.asarray(c[b], dtype=np.float32).reshape(8, 128).T)
    return m


def kernel(x, c, w_ada, b_ada, g_pre_mix, w_in, g_moba_out, g_dsa_out, w_out, g_post_mix,
           g_pre_ffn, w_up_act, w_up_lin, w_conv, b_conv, w_down, g_post_ffn):
    x = np.asarray(x)
    c = np.asarray(c)
    shared = _shared_inputs(w_ada, b_ada, g_pre_mix, w_in, g_moba_out, g_dsa_out, w_out, g_post_mix, g_pre_ffn,
                            w_up_act, w_up_lin, w_conv, b_conv, w_down, g_post_ffn)
    nc = build_nc()
    in_maps = [_core_inputs(x, c, b, shared) for b in range(8)]
    res = run_bass_kernel_spmd(nc, in_maps, core_ids=list(range(8)))
    return np.stack([np.asarray(r["out"], dtype=np.float32) for r in res.results], axis=0)
```

```python
import numpy as np
import ml_dtypes
from contextlib import ExitStack
import concourse.bass as bass
import concourse.mybir as mybir
from concourse.bass_utils import run_bass_kernel_spmd

F32 = mybir.dt.float32
BF16 = mybir.dt.bfloat16
ALU = mybir.AluOpType
AF = mybir.ActivationFunctionType
AX = mybir.AxisListType


class DSem:
    __slots__ = ("idx", "cnt")

    def __init__(self, idx):
        self.idx = idx
        self.cnt = 0


class Buf:
    __slots__ = ("name", "t", "w", "r", "dsem")

    def __init__(self, name, t=None):
        self.name = name
        self.t = t
        self.w = {}
        self.r = {}
        self.dsem = None


class _Scope:
    def __init__(self, fw):
        self.fw = fw

    def __enter__(self):
        fw = self.fw
        self.prev = (fw.es, fw.scope_dsems)
        self.stack = ExitStack()
        self.stack.__enter__()
        fw.es = self.stack
        fw.scope_dsems = []
        return self

    def __exit__(self, *a):
        fw = self.fw
        fw.barrier()
        fw.dpool.extend(fw.scope_dsems)
        fw.es, fw.scope_dsems = self.prev
        return self.stack.__exit__(*a)


class FW:
    SEM_MAX = 30000

    def __init__(self, nc, es):
        self.nc = nc
        self.es = es
        self.sem_es = es
        self.eng = {"pe": nc.tensor, "act": nc.scalar, "dve": nc.vector, "pool": nc.gpsimd, "sp": nc.sync}
        self.sems = []
        self.cur = {}
        self.own = {e: set() for e in self.eng}
        self.known = {e: {} for e in self.eng}
        self.issued = {}
        self.dpool = []
        self.scope_dsems = []
        self.nwaits = 0
        self.nops = 0
        for e in self.eng:
            self._newsem(e)

    def _alloc_sem(self, name):
        s = self.sem_es.enter_context(self.nc.semaphore(name))
        self.sems.append(s)
        return len(self.sems) - 1

    def _newsem(self, e):
        i = self._alloc_sem(f"s_{e}_{len(self.sems)}")
        self.cur[e] = [i, 0]
        self.own[e].add(i)

    def _get_dsem(self):
        if self.dpool:
            d = self.dpool.pop()
        else:
            d = DSem(self._alloc_sem(f"d_{len(self.sems)}"))
        self.scope_dsems.append(d)
        return d

    def scope(self):
        return _Scope(self)

    def sb(self, name, shape, dtype, dma=False):
        self.nuniq = getattr(self, "nuniq", 0) + 1
        t = self.es.enter_context(self.nc.sbuf_tensor(f"{name}_u{self.nuniq}", shape, dtype))
        b = Buf(name, t)
        if dma:
            b.dsem = self._get_dsem()
        return b

    def ps(self, name, shape, dtype):
        self.nuniq = getattr(self, "nuniq", 0) + 1
        t = self.es.enter_context(self.nc.psum_tensor(f"{name}_u{self.nuniq}", shape, dtype))
        return Buf(name, t)

    def view(self, name, t, dma=False):
        b = Buf(name, t)
        if dma:
            b.dsem = self._get_dsem()
        return b

    def _need(self, reads, writes):
        need = {}
        for b in reads:
            for s, v in b.w.items():
                if need.get(s, 0) < v:
                    need[s] = v
        for b in writes:
            for s, v in b.w.items():
                if need.get(s, 0) < v:
                    need[s] = v
            for s, v in b.r.items():
                if need.get(s, 0) < v:
                    need[s] = v
        return need

    def _waits(self, e, need, skip_own=False):
        k = self.known[e]
        eng = self.eng[e]
        for s, v in need.items():
            if skip_own and s in self.own[e]:
                continue
            if k.get(s, 0) >= v:
                continue
            eng.wait_ge(self.sems[s], v)
            self.nwaits += 1
            k[s] = v

    def _record(self, t, reads, writes):
        s, v = t
        self.issued[s] = v
        for b in reads:
            if b.r.get(s, 0) < v:
                b.r[s] = v
        for b in writes:
            b.w = {s: v}
            b.r = {}

    def op(self, e, fn, reads=(), writes=(), same=None):
        if same is None:
            same = (e != "pe")
        need = self._need(reads, writes)
        self._waits(e, need, skip_own=not same)
        ins = fn(self.eng[e])
        c = self.cur[e]
        c[1] += 1
        ins.then_inc(self.sems[c[0]], 1)
        self._record((c[0], c[1]), reads, writes)
        self.nops += 1
        if c[1] >= self.SEM_MAX:
            self._newsem(e)
        return ins

    def dma(self, q, out, in_, reads=(), writes=(), sem=None, **kw):
        return self.dma_group(q, [(out, in_)], reads, writes, sem, **kw)

    def dma_group(self, q, pairs, reads=(), writes=(), sem=None, **kw):
        d = sem.dsem
        need = self._need(reads, writes)
        if d.cnt:
            v = 16 * d.cnt
            if need.get(d.idx, 0) < v:
                need[d.idx] = v
        self._waits(q, need)
        for (out, in_) in pairs:
            ins = self.eng[q].dma_start(out=out, in_=in_, **kw)
            d.cnt += 1
            ins.then_inc(self.sems[d.idx], 16)
            self.nops += 1
        self._record((d.idx, 16 * d.cnt), reads, writes)

    def barrier(self):
        need = dict(self.issued)
        for e in self.eng:
            self._waits(e, need)


S = 4096
D = 1024
NT = 32
NCH = 8
DFF = 2816
NFC = 22
NCOLP = 6280
C_MV, C_DV, C_WI = 5248, 5760, 6272
BIG = 30000.0
KBIS = 18
EPS = 1e-6


class P:
    pass


def _rms_rstd(fw, ssb, rstd, nh, n):
    (ss_buf, ss_ap), (r_buf, r_ap) = ssb, rstd
    fw.op('dve', lambda e: e.tensor_scalar(out=ss_ap, in0=ss_ap, scalar1=1.0 / n, scalar2=EPS, op0=ALU.mult, op1=ALU.add),
          reads=[ss_buf], writes=[ss_buf])
    fw.op('pool', lambda e: e.tensor_tensor(out=r_ap, in0=ss_ap, in1=nh.t[:, 0:1], op=ALU.pow), reads=[ss_buf, nh], writes=[r_buf])


def build_nc(nlayers=2, phases="ABCDEF", dbg=False):
    nc = bass.Bass("TRN2", target_bir_lowering=False)
    p = P()

    def din(name, shape, dt=F32):
        return nc.dram_tensor(name, shape, dt, kind="ExternalInput").ap()

    def dscr(name, shape, dt):
        return nc.dram_tensor(name, shape, dt, kind=("ExternalOutput" if dbg else "Internal")).ap()

    x = din("x", [S, D])
    cT = din("cT", [128, 8])
    w_ada = din("w_ada", [2, D, 6 * D])
    b_ada = din("b_ada", [2, 6 * D])
    b_ada_col = din("b_ada_col", [2, 128, 48])
    g_pre_mix_col = din("g_pre_mix_col", [2, 128, 8])
    g_pre_ffn_col = din("g_pre_ffn_col", [2, 128, 8])
    g_post_mix = din("g_post_mix", [2, D])
    g_post_ffn = din("g_post_ffn", [2, D])
    g_moba_out = din("g_moba_out", [2, 512])
    g_dsa_out = din("g_dsa_out", [2, 512])
    w_in_p = din("w_in_p", [2, D, NCOLP])
    w_out = din("w_out", [2, D, D])
    w_up_act = din("w_up_act", [2, D, DFF])
    w_up_lin = din("w_up_lin", [2, D, DFF])
    w_conv_col = din("w_conv_col", [2, 128, NFC, 3])
    b_conv_col = din("b_conv_col", [2, 128, NFC])
    w_down = din("w_down", [2, DFF, D])
    ropeC = din("ropeC", [128, S])
    ropeS = din("ropeS", [128, S])
    ident_d = din("ident", [128, 128], BF16)
    tri_d = din("tri", [128, 128], BF16)
    negtri_d = din("negtri", [128, 128])
    onehot_d = din("onehot", [16, S], BF16)
    cbsel_d = din("cbsel", [128, 512])
    cblt_d = din("cblt", [128, 512])
    cbfin_d = din("cbfin", [128, 512])
    cpow_d = din("cpow", [128, KBIS])
    out = nc.dram_tensor("out", [S, D], F32, kind="ExternalOutput").ap()

    scrT = [dscr(n, [512, S], BF16) for n in ("mqT", "mkT", "dqT", "dkT", "qiT")]
    kiT_d = dscr("kiT", [64, S], BF16)
    mva = dscr("mva", [S, 520], BF16)
    dva = dscr("dva", [S, 520], BF16)
    om_d = dscr("om", [S, 512], F32)
    od_d = dscr("od", [S, 512], F32)
    x1_d = dscr("x1", [S, D], F32)
    x2_d = dscr("x2", [S, D], F32)
    gbc_d = dscr("gbc", [2, 2, 128, D], F32)

    with ExitStack() as es:
        fw = FW(nc, es)
        AB = fw.sb("AB", [128, 2, 4, 8], F32)
        WI = fw.sb("WI", [128, NT, 8], F32)
        ksum = fw.sb("ksum", [128, 4, 16], F32)
        ident = fw.sb("identb", [128, 128], BF16, dma=True)
        nh = fw.sb("neghalf", [128, 1], F32)
        fw.dma('sp', ident.t[:], ident_d[:, :], writes=[ident], sem=ident)
        fw.op('dve', lambda e: e.memset(nh.t[:], -0.5), writes=[nh])

        if "A" in phases:
          with fw.scope():
            ct = fw.sb("ct", [128, 8], F32, dma=True)
            sc = fw.sb("sc", [128, 8], F32)
            screp = fw.sb("screp", [128, 8, 128], F32)
            ones1 = fw.sb("ones1", [1, 128], F32)
            wa = [fw.sb(f"wa{i}", [128, 8, 1024], F32, dma=True) for i in range(2)]
            brow = [fw.sb(f"brow{i}", [1, 1024], F32, dma=True) for i in range(2)]
            bcol = fw.sb("bcol", [128, 2, 48], F32, dma=True)
            gcol = fw.sb("gcol", [128, 2, 2, 8], F32, dma=True)
            gpb = [fw.sb(f"gpb{i}", [128, 1024], F32, dma=True) for i in range(2)]
            colps = fw.ps("colps", [128, 512], F32)
            bcps = [fw.ps(f"bcps{i}", [128, 512], F32) for i in range(2)]
            mcol = fw.sb("mcol", [128, 8], F32)
            Gt = [fw.sb(f"Gt{i}", [128, D], F32, dma=True) for i in range(2)]
            fw.dma('sp', ct.t[:], cT[:, :], writes=[ct], sem=ct)
            fw.dma_group('sp', [(bcol.t[:, l, :], b_ada_col[l]) for l in range(2)], writes=[bcol], sem=bcol)
            fw.dma_group('sp', [(gcol.t[:, l, 0, :], g_pre_mix_col[l]) for l in range(2)]
                         + [(gcol.t[:, l, 1, :], g_pre_ffn_col[l]) for l in range(2)], writes=[gcol], sem=gcol)
            fw.op('act', lambda e: e.activation(out=sc.t[:], in_=ct.t[:], func=AF.Silu), reads=[ct], writes=[sc])
            fw.op('dve', lambda e: e.tensor_copy(out=screp.t[:], in_=sc.t[:, :].unsqueeze(2).to_broadcast([128, 8, 128])),
                  reads=[sc], writes=[screp])
            fw.op('dve', lambda e: e.memset(ones1.t[:], 1.0), writes=[ones1])
            pi = 0
            for l in range(nlayers):
                wv = w_ada[l].rearrange("(k p) n -> p k n", p=128)
                for piece in range(6):
                    w = wa[pi % 2]
                    pi += 1
                    fw.dma_group('sp', [(w.t[:, k, :], wv[:, k, piece * 1024:(piece + 1) * 1024]) for k in range(8)],
                                 writes=[w], sem=w)
                    if piece in (2, 5):
                        j = 0 if piece == 2 else 1
                        br = brow[j]
                        gp = gpb[j]
                        fw.dma('sp', br.t[:], b_ada[l:l + 1, piece * 1024:(piece + 1) * 1024], writes=[br], sem=br)
                        fw.dma('sp', gp.t[:], (g_post_mix if j == 0 else g_post_ffn)[l, :].partition_broadcast(128),
                               writes=[gp], sem=gp)
                        for nhf in range(2):
                            ps = bcps[nhf]
                            for k in range(8):
                                fw.op('pe', lambda e: e.matmul(ps.t[:], lhsT=screp.t[:, k, :], rhs=w.t[:, k, nhf * 512:(nhf + 1) * 512],
                                                               start=(k == 0), stop=False), reads=[screp, w], writes=[ps])
                            fw.op('pe', lambda e: e.matmul(ps.t[:], lhsT=ones1.t[0:1, :], rhs=br.t[0:1, nhf * 512:(nhf + 1) * 512],
                                                           start=False, stop=True), reads=[ones1, br], writes=[ps])
                            G = Gt[j]
                            fw.op('dve', lambda e: e.tensor_tensor(out=G.t[:, nhf * 512:(nhf + 1) * 512], in0=ps.t[:],
                                                                   in1=gp.t[:, nhf * 512:(nhf + 1) * 512], op=ALU.mult),
                                  reads=[ps, gp], writes=[G])
                        fw.dma('sp', gbc_d[l, j], Gt[j].t[:], reads=[Gt[j]], sem=Gt[j])
                    else:
                        for jj in range(8):
                            for k in range(8):
                                fw.op('pe', lambda e: e.matmul(colps.t[:, jj:jj + 1], lhsT=w.t[:, k, jj * 128:(jj + 1) * 128],
                                                               rhs=sc.t[:, k:k + 1], start=(k == 0), stop=(k == 7)),
                                      reads=[sc, w], writes=[colps])
                        fw.op('dve', lambda e: e.tensor_tensor(out=mcol.t[:], in0=colps.t[:, 0:8],
                                                               in1=bcol.t[:, l, piece * 8:(piece + 1) * 8], op=ALU.add),
                              reads=[colps, bcol], writes=[mcol])
                        if piece in (0, 3):
                            slot = 1 if piece == 0 else 3
                            fw.op('dve', lambda e: e.tensor_copy(out=AB.t[:, l, slot, :], in_=mcol.t[:]), reads=[mcol], writes=[AB])
                        else:
                            slot = 0 if piece == 1 else 2
                            gi = 0 if piece == 1 else 1
                            fw.op('dve', lambda e: e.scalar_tensor_tensor(out=AB.t[:, l, slot, :], in0=mcol.t[:], scalar=1.0,
                                                                          in1=gcol.t[:, l, gi, :], op0=ALU.add, op1=ALU.mult),
                                  reads=[mcol, gcol], writes=[AB])

        def norm_transpose(xb, l, slot, tp, hbuf, hview, sq, ss, rstd, xn):
            fw.op('dve', lambda e: e.memset(ss.t[:], 0.0), writes=[ss])
            fw.op('act', lambda e: e.activation(out=sq.t[:], in_=xb.t[:], func=AF.Square, accum_out=ss.t[:, 0:1]),
                  reads=[xb], writes=[sq, ss])
            _rms_rstd(fw, (ss, ss.t[:, 0:1]), (rstd, rstd.t[:, 0:1]), nh, D)
            fw.op('dve', lambda e: e.tensor_scalar(out=xn.t[:], in0=xb.t[:], scalar1=rstd.t[:, 0:1], scalar2=None, op0=ALU.mult),
                  reads=[xb, rstd], writes=[xn])
            for k in range(8):
                fw.op('pe', lambda e: e.transpose(out=tp.t[:, k, :], in_=xn.t[:, k * 128:(k + 1) * 128], identity=ident.t[:]),
                      reads=[xn, ident], writes=[tp])
            for k in range(8):
                fw.op('act', lambda e: e.activation(out=hview(k), in_=tp.t[:, k, :], func=AF.Identity,
                                                    scale=AB.t[:, l, slot, k:k + 1], bias=AB.t[:, l, slot + 1, k:k + 1]),
                      reads=[tp, AB], writes=[hbuf])

        for l in range(nlayers):
            xin = x if l == 0 else x2_d
            xout = out if l == nlayers - 1 else x2_d
            if "B" in phases:
              with fw.scope():
                W = fw.sb("winb", [128, 8, NCOLP], BF16, dma=True)
                fw.dma_group('pool', [(W.t[:, k, :], w_in_p[l, k * 128:(k + 1) * 128, :]) for k in range(8)], writes=[W], sem=W)
                rC = fw.sb("rC", [128, S], F32, dma=True)
                rS = fw.sb("rS", [128, S], F32, dma=True)
                fw.dma('sp', rC.t[:], ropeC[:, :], writes=[rC], sem=rC)
                fw.dma('sp', rS.t[:], ropeS[:, :], writes=[rS], sem=rS)
                xt = [fw.sb(f"xt{i}", [128, D], F32, dma=True) for i in range(2)]
                sq = fw.sb("sq", [128, D], BF16)
                ss = [fw.sb(f"ss{i}", [128, 1], F32) for i in range(2)]
                rstd = [fw.sb(f"rstd{i}", [128, 1], F32) for i in range(2)]
                xn = [fw.sb(f"xn{i}", [128, D], BF16) for i in range(2)]
                tps = [fw.ps(f"tp{i}", [128, 8, 128], BF16) for i in range(2)]
                hT = [fw.sb(f"hT{i}", [128, 8, 512], BF16) for i in range(2)]
                pm = [fw.ps(f"pm{i}", [128, 512], F32) for i in range(2)]
                pp = [fw.ps(f"pp{i}", [128, 512], F32) for i in range(2)]
                pv = [fw.ps(f"pv{i}", [128, 512], F32) for i in range(2)]
                t1 = [fw.sb(f"t1_{i}", [128, 512], F32) for i in range(2)]
                t2 = [fw.sb(f"t2_{i}", [128, 512], F32) for i in range(2)]
                t3 = [fw.sb(f"t3_{i}", [128, 512], F32) for i in range(2)]
                ob = [fw.sb(f"ob{i}", [128, 512], BF16, dma=True) for i in range(3)]
                vt = [fw.sb(f"vt{i}", [128, 8, 65], BF16, dma=True) for i in range(2)]
                for v in vt:
                    fw.op('dve', lambda e: e.memset(v.t[:], 1.0), writes=[v])
                fj = 0
                oj = 0
                vj = 0
                for c in range(NCH):
                    h = hT[c % 2]
                    cs = slice(c * 512, (c + 1) * 512)
                    for i in range(4):
                        tt = c * 4 + i
                        xb = xt[tt % 2]
                        fw.dma('sp', xb.t[:], xin[tt * 128:(tt + 1) * 128, :], writes=[xb], sem=xb)
                        norm_transpose(xb, l, 0, tps[tt % 2], h, lambda k: h.t[:, k, i * 128:(i + 1) * 128],
                                       sq, ss[tt % 2], rstd[tt % 2], xn[tt % 2])
                    tiles = [(g, ft, 128) for g in range(5) for ft in range(4)] + [(5, 0, 64)]
                    for (g, ft, M) in tiles:
                        cm = g * 1024 + ft * 128 if g < 5 else 5120
                        cp = cm + 512 if g < 5 else 5184
                        pmain, ppart = pm[fj % 2], pp[fj % 2]
                        a1, a2, a3 = t1[fj % 2], t2[fj % 2], t3[fj % 2]
                        fj += 1
                        for k in range(8):
                            fw.op('pe', lambda e: e.matmul(pmain.t[0:M, :], lhsT=W.t[:, k, cm:cm + M], rhs=h.t[:, k, :],
                                                           start=(k == 0), stop=(k == 7)), reads=[W, h], writes=[pmain])
                        for k in range(8):
                            fw.op('pe', lambda e: e.matmul(ppart.t[0:M, :], lhsT=W.t[:, k, cp:cp + M], rhs=h.t[:, k, :],
                                                           start=(k == 0), stop=(k == 7)), reads=[W, h], writes=[ppart])
                        fw.op('dve', lambda e: e.tensor_tensor(out=a1.t[0:M, :], in0=ppart.t[0:M, :], in1=rS.t[0:M, cs], op=ALU.mult),
                              reads=[ppart, rS], writes=[a1])
                        fw.op('dve', lambda e: e.tensor_tensor(out=a2.t[0:M, :], in0=pmain.t[0:M, :], in1=rC.t[0:M, cs], op=ALU.mult),
                              reads=[pmain, rC], writes=[a2])
                        o = ob[oj % 3]
                        oj += 1
                        if g == 1:
                            fw.op('pool', lambda e: e.tensor_tensor(out=a3.t[:], in0=a1.t[:], in1=a2.t[:], op=ALU.add),
                                  reads=[a1, a2], writes=[a3])
                            fw.op('act', lambda e: e.copy(out=o.t[:], in_=a3.t[:]), reads=[a3], writes=[o])
                            fw.op('dve', lambda e: e.reduce_sum(out=ksum.t[:, ft, 2 * c:2 * c + 2],
                                                                in_=a3.t[:, :].rearrange("p (b s) -> p b s", b=2), axis=AX.X),
                                  reads=[a3], writes=[ksum])
                        else:
                            fw.op('pool', lambda e: e.tensor_tensor(out=o.t[0:M, :], in0=a1.t[0:M, :], in1=a2.t[0:M, :], op=ALU.add),
                                  reads=[a1, a2], writes=[o])
                        dst = scrT[g][ft * 128:(ft + 1) * 128, cs] if g < 5 else kiT_d[:, cs]
                        fw.dma('sp', dst, o.t[0:M, :], reads=[o], sem=o)
                    for i in range(4):
                        tt = c * 4 + i
                        for (dst, c0) in ((mva, C_MV), (dva, C_DV)):
                            ps = pv[vj % 2]
                            v = vt[vj % 2]
                            vj += 1
                            for k in range(8):
                                fw.op('pe', lambda e: e.matmul(ps.t[:], lhsT=h.t[:, k, i * 128:(i + 1) * 128], rhs=W.t[:, k, c0:c0 + 512],
                                                               start=(k == 0), stop=(k == 7)), reads=[W, h], writes=[ps])
                            fw.op('act', lambda e: e.copy(out=v.t[:, :, 0:64], in_=ps.t[:, :].rearrange("p (h d) -> p h d", h=8)),
                                  reads=[ps], writes=[v])
                            fw.dma('sp', dst[tt * 128:(tt + 1) * 128, :], v.t[:, :, :].rearrange("p h d -> p (h d)"), reads=[v], sem=v)
                        ps = pv[vj % 2]
                        vj += 1
                        for k in range(8):
                            fw.op('pe', lambda e: e.matmul(ps.t[:, 0:8], lhsT=h.t[:, k, i * 128:(i + 1) * 128], rhs=W.t[:, k, C_WI:C_WI + 8],
                                                           start=(k == 0), stop=(k == 7)), reads=[W, h], writes=[ps])
                        fw.op('act', lambda e: e.copy(out=WI.t[:, tt, :], in_=ps.t[:, 0:8]), reads=[ps], writes=[WI])
            if "C" in phases:
              with fw.scope():
                V = fw.sb("mV", [128, NT, 520], BF16, dma=True)
                fw.dma('sp', V.t[:], mva.rearrange("(n p) c -> p n c", p=128), writes=[V], sem=V)
                Qt = [fw.sb(f"mQ{i}", [80, S], BF16) for i in range(2)]
                Kt = [fw.sb(f"mK{i}", [80, S], BF16) for i in range(2)]
                Qm = [fw.view(f"mQm{i}", Qt[i].t, dma=True) for i in range(2)]
                Qb = [fw.view(f"mQb{i}", Qt[i].t) for i in range(2)]
                Km = [fw.view(f"mKm{i}", Kt[i].t, dma=True) for i in range(2)]
                Kc = [fw.view(f"mKc{i}", Kt[i].t, dma=True) for i in range(2)]
                for i in range(2):
                    fw.dma('sp', Kt[i].t[64:80, :], onehot_d[:, :], writes=[Kc[i]], sem=Kc[i])
                kmb = fw.sb("kmb", [64, 8, 16], BF16)
                for h in range(8):
                    e_, ft = h % 2, h // 2
                    fw.op('act', lambda e: e.mul(out=kmb.t[0:64, h, :], in_=ksum.t[e_ * 64:(e_ + 1) * 64, ft, :], mul=1.0 / 256.0),
                          reads=[ksum], writes=[kmb])
                tri = fw.sb("tri", [128, 128], BF16, dma=True)
                fw.dma('sp', tri.t[:], tri_d[:, :], writes=[tri], sem=tri)
                cbs = fw.sb("cbs", [128, 3, 512], F32, dma=True)
                fw.dma_group('sp', [(cbs.t[:, 0, :], cbsel_d[:, :]), (cbs.t[:, 1, :], cblt_d[:, :]), (cbs.t[:, 2, :], cbfin_d[:, :])],
                             writes=[cbs], sem=cbs)
                gps = fw.ps("gps", [128, 512], F32)
                tb = fw.ps("tb", [128, 1024], BF16)
                sps = [fw.ps(f"sp{i}", [128, 512], F32) for i in range(3)]
                ops_ = [fw.ps(f"op{i}", [128, 512], F32) for i in range(2)]
                pt = [fw.sb(f"pt{i}", [128, 512], BF16) for i in range(3)]
                gm = fw.sb("gm", [128, 512], F32)
                ee = fw.sb("ee", [128, 512], F32)
                g2 = fw.sb("g2", [128, 512], F32)
                g3 = fw.sb("g3", [128, 512], F32)
                mx = fw.sb("mx", [128, 3, 32], F32)
                bt = fw.sb("bt", [128, 512], BF16)
                rec = [fw.sb(f"rec{i}", [128, 4, 1], F32) for i in range(2)]
                osb = [fw.sb(f"osb{i}", [128, 4, 64], F32, dma=True) for i in range(2)]
                v3 = lambda t: t[:, :].rearrange("p (a n) -> p a n", n=16)
                bc3 = lambda j: mx.t[:, j, :].unsqueeze(2).to_broadcast([128, 32, 16])
                si = 0
                oi = 0
                def gateA(h):
                    b = h % 2
                    fw.dma('sp', Qt[b].t[0:64, :], scrT[0][h * 64:(h + 1) * 64, :], writes=[Qm[b]], sem=Qm[b])
                    fw.dma('sp', Kt[b].t[0:64, :], scrT[1][h * 64:(h + 1) * 64, :], writes=[Km[b]], sem=Km[b])
                    for tt in range(NT):
                        fw.op('pe', lambda e: e.matmul(gps.t[:, tt * 16:(tt + 1) * 16], lhsT=Qt[b].t[0:64, tt * 128:(tt + 1) * 128],
                                                       rhs=kmb.t[0:64, h, :], start=True, stop=True), reads=[Qm[b], kmb], writes=[gps])
                    fw.op('dve', lambda e: e.tensor_tensor(out=gm.t[:], in0=gps.t[:], in1=cbs.t[:, 0, :], op=ALU.add),
                          reads=[gps, cbs], writes=[gm])
                    fw.op('dve', lambda e: e.reduce_max(out=mx.t[:, 0, :], in_=v3(gm.t), axis=AX.X), reads=[gm], writes=[mx])
                    fw.op('dve', lambda e: e.tensor_tensor(out=v3(ee.t), in0=v3(gm.t), in1=bc3(0), op=ALU.is_equal),
                          reads=[gm, mx], writes=[ee])
                    fw.op('dve', lambda e: e.scalar_tensor_tensor(out=g2.t[:], in0=ee.t[:], scalar=-1e9, in1=gm.t[:], op0=ALU.mult, op1=ALU.add),
                          reads=[ee, gm], writes=[g2])
                    fw.op('dve', lambda e: e.reduce_max(out=mx.t[:, 1, :], in_=v3(g2.t), axis=AX.X), reads=[g2], writes=[mx])
                    fw.op('dve', lambda e: e.tensor_tensor(out=v3(ee.t), in0=v3(g2.t), in1=bc3(1), op=ALU.is_equal),
                          reads=[g2, mx], writes=[ee])
                    fw.op('dve', lambda e: e.scalar_tensor_tensor(out=g3.t[:], in0=ee.t[:], scalar=-1e9, in1=g2.t[:], op0=ALU.mult, op1=ALU.add),
                          reads=[ee, g2], writes=[g3])
                    fw.op('dve', lambda e: e.reduce_max(out=mx.t[:, 2, :], in_=v3(g3.t), axis=AX.X), reads=[g3], writes=[mx])
                    fw.op('dve', lambda e: e.tensor_tensor(out=v3(ee.t), in0=v3(gm.t), in1=bc3(2), op=ALU.is_lt),
                          reads=[gm, mx], writes=[ee])
                    fw.op('dve', lambda e: e.tensor_tensor(out=g2.t[:], in0=ee.t[:], in1=cbs.t[:, 1, :], op=ALU.mult),
                          reads=[ee, cbs], writes=[g2])
                    fw.op('dve', lambda e: e.tensor_tensor(out=bt.t[:], in0=g2.t[:], in1=cbs.t[:, 2, :], op=ALU.add),
                          reads=[g2, cbs], writes=[bt])

                def gateB(h):
                    b = h % 2
                    for tt in range(NT):
                        fw.op('pe', lambda e: e.transpose(out=tb.t[0:16, (tt % 4) * 128:(tt % 4 + 1) * 128], in_=bt.t[:, tt * 16:(tt + 1) * 16],
                                                          identity=ident.t[:]), reads=[bt, ident], writes=[tb])
                        if tt % 4 == 3:
                            q4 = tt // 4
                            fw.op('act', lambda e: e.copy(out=Qt[b].t[64:80, q4 * 512:(q4 + 1) * 512], in_=tb.t[0:16, 0:512]),
                                  reads=[tb], writes=[Qb[b]])

                def attn(h):
                    nonlocal si, oi
                    b = h % 2
                    for qc in range(NCH):
                        O = ops_[oi % 2]
                        rc = rec[oi % 2]
                        ob_ = osb[oi % 2]
                        oi += 1
                        nst = 4 * qc + 4

                        def qk(st):
                            nonlocal si
                            sp_ = sps[si % 3]
                            Pt = pt[si % 3]
                            si += 1
                            fw.op('pe', lambda e: e.matmul(sp_.t[:], lhsT=Kt[b].t[0:80, st * 128:(st + 1) * 128],
                                                           rhs=Qt[b].t[0:80, qc * 512:(qc + 1) * 512], start=True, stop=True),
                                  reads=[Km[b], Kc[b], Qm[b], Qb[b]], writes=[sp_])
                            fw.op('act', lambda e: e.activation(out=Pt.t[:], in_=sp_.t[:], func=AF.Exp, scale=0.125),
                                  reads=[sp_], writes=[Pt])
                            j = st - 4 * qc
                            if j >= 0:
                                fw.op('pool', lambda e: e.tensor_tensor(out=Pt.t[:, j * 128:(j + 1) * 128], in0=Pt.t[:, j * 128:(j + 1) * 128],
                                                                        in1=tri.t[:], op=ALU.mult), reads=[Pt, tri], writes=[Pt])
                            return Pt, j

                        pend = qk(0)
                        first = True
                        for st in range(nst):
                            nxt = qk(st + 1) if st + 1 < nst else None
                            Pt, j = pend
                            for i in range(max(j, 0), 4):
                                fw.op('pe', lambda e: e.matmul(O.t[:, i * 65:(i + 1) * 65], lhsT=Pt.t[:, i * 128:(i + 1) * 128],
                                                               rhs=V.t[:, st, h * 65:(h + 1) * 65], start=first, stop=(st == 4 * qc + i)),
                                      reads=[Pt, V], writes=[O])
                                first = False
                            pend = nxt
                        Ov = O.t[:, 0:260].rearrange("p (i d) -> p i d", d=65)
                        fw.op('dve', lambda e: e.reciprocal(out=rc.t[:], in_=Ov[:, :, 64:65]), reads=[O], writes=[rc])
                        fw.op('dve', lambda e: e.tensor_tensor(out=ob_.t[:], in0=Ov[:, :, 0:64], in1=rc.t[:, :, :].to_broadcast([128, 4, 64]),
                                                               op=ALU.mult), reads=[O, rc], writes=[ob_])
                        fw.dma('sp', om_d[qc * 512:(qc + 1) * 512, h * 64:(h + 1) * 64].rearrange("(i p) d -> p i d", p=128), ob_.t[:],
                               reads=[ob_], sem=ob_)

                gateA(0)
                gateB(0)
                for h in range(8):
                    if h + 1 < 8:
                        gateA(h + 1)
                    attn(h)
                    if h + 1 < 8:
                        gateB(h + 1)

            if "D" in phases:
              with fw.scope():
                V = fw.sb("dV", [128, NT, 520], BF16, dma=True)
                fw.dma('sp', V.t[:], dva.rearrange("(n p) c -> p n c", p=128), writes=[V], sem=V)
                K2 = fw.sb("K2", [128, 4, S], BF16, dma=True)
                fw.dma('sp', K2.t[:], scrT[3].rearrange("(hp two d) t -> (two d) hp t", two=2, d=64), writes=[K2], sem=K2)
                ki = fw.sb("kiT", [64, S], BF16, dma=True)
                fw.dma('sp', ki.t[:], kiT_d[:, :], writes=[ki], sem=ki)
                negtri = fw.sb("negtri", [128, 128], F32, dma=True)
                fw.dma('sp', negtri.t[:], negtri_d[:, :], writes=[negtri], sem=negtri)
                cpow = fw.sb("cpow", [128, KBIS], F32, dma=True)
                fw.dma('sp', cpow.t[:], cpow_d[:, :], writes=[cpow], sem=cpow)
                QI = [fw.sb(f"QI{i}", [64, 8, 128], BF16, dma=True) for i in range(2)]
                Q2 = [fw.sb(f"Q2{i}", [128, 4, 128], BF16, dma=True) for i in range(3)]
                i4 = fw.sb("i4", [128, 4, 128], BF16)
                fw.op('dve', lambda e: e.tensor_copy(out=i4.t[:], in_=ident.t[:, :].unsqueeze(1).to_broadcast([128, 4, 128])), reads=[ident], writes=[i4])
                score = [fw.sb(f"score{i}", [128, S], F32) for i in range(2)]
                junk = fw.sb("junk", [128, S], BF16)
                mb = [fw.sb(f"mb{i}", [128, S], BF16) for i in range(2)]
                rlb = [fw.sb(f"rl{i}", [128, 512], BF16) for i in range(3)]
                dg = [fw.sb(f"dg{i}", [128, 8, 128], BF16) for i in range(2)]
                scps = fw.ps("scps", [128, 512], F32)
                lps = [fw.ps(f"lps{i}", [128, 512], F32) for i in range(2)]
                sp3 = [fw.ps(f"dsp{i}", [128, 512], F32) for i in range(3)]
                opsd = [fw.ps(f"dop{e_}", [128, 512], F32) for e_ in range(2)]
                ptd = [fw.sb(f"dpt{i}", [128, 2, 512], BF16) for i in range(2)]
                sm = [fw.sb(f"sm{i}", [128, 8], F32) for i in range(2)]
                dkt = [fw.sb(f"dk{i}", [128, KBIS], F32) for i in range(2)]
                recd = [fw.sb(f"drec{e_}", [128, 4, 1], F32) for e_ in range(2)]
                osbd = [fw.sb(f"dosb{i}", [128, 4, 2, 64], F32, dma=True) for i in range(2)]
                qiv = scrT[4].rearrange("(h d) t -> d h t", d=64)
                q2v = scrT[2].rearrange("(hp two d) t -> (two d) hp t", two=2, d=64)
                cnt_ = {"li": 0, "ti": 0, "ai": 0}

                def indexer(qt):
                    L = (qt + 1) * 128
                    sc_ = score[qt % 2]
                    qi_ = QI[qt % 2]
                    fw.dma('sp', qi_.t[:], qiv[:, :, qt * 128:(qt + 1) * 128], writes=[qi_], sem=qi_)
                    fw.dma('sp', Q2[qt % 3].t[:], q2v[:, :, qt * 128:(qt + 1) * 128], writes=[Q2[qt % 3]], sem=Q2[qt % 3])
                    dg_ = dg[qt % 2]
                    fw.op('dve', lambda e: e.tensor_tensor(out=dg_.t[:], in0=ident.t[:, :].unsqueeze(1).to_broadcast([128, 8, 128]),
                                                           in1=WI.t[:, qt, :].unsqueeze(2).to_broadcast([128, 8, 128]), op=ALU.mult),
                          reads=[ident, WI], writes=[dg_])
                    nch = (L + 511) // 512
                    for ch in range(nch):
                        ncol = min(512, L - ch * 512)
                        cs = slice(ch * 512, ch * 512 + ncol)

                        def logit(h):
                            lp = lps[cnt_["li"] % 2]
                            cnt_["li"] += 1
                            R = rlb[cnt_["ti"] % 3]
                            cnt_["ti"] += 1
                            fw.op('pe', lambda e: e.matmul(lp.t[:, 0:ncol], lhsT=qi_.t[0:64, h, :], rhs=ki.t[0:64, cs], start=True, stop=True),
                                  reads=[qi_, ki], writes=[lp])
                            fw.op('act', lambda e: e.activation(out=R.t[:, 0:ncol], in_=lp.t[:, 0:ncol], func=AF.Relu), reads=[lp], writes=[R])
                            return R

                        pend = logit(0)
                        for h in range(8):
                            nxt = logit(h + 1) if h + 1 < 8 else None
                            R = pend
                            fw.op('pe', lambda e: e.matmul(scps.t[:, 0:ncol], lhsT=dg_.t[:, h, :], rhs=R.t[:, 0:ncol], start=(h == 0), stop=(h == 7)),
                                  reads=[dg_, R], writes=[scps])
                            pend = nxt
                        fw.op('act', lambda e: e.copy(out=sc_.t[:, cs], in_=scps.t[:, 0:ncol]), reads=[scps], writes=[sc_])

                def select(qt):
                    L = (qt + 1) * 128
                    sc_ = score[qt % 2]
                    s_ = sm[qt % 2]
                    dk_ = dkt[qt % 2]
                    mb_ = mb[qt % 2]
                    if qt >= 2:
                        fw.op('dve', lambda e: e.reduce_max(out=s_.t[:, 0:1], in_=sc_.t[:, 0:L], axis=AX.X), reads=[sc_], writes=[s_])
                        fw.op('dve', lambda e: e.tensor_reduce(out=s_.t[:, 1:2], in_=sc_.t[:, 0:L], axis=AX.X, op=ALU.min), reads=[sc_], writes=[s_])
                        fw.op('dve', lambda e: e.scalar_tensor_tensor(out=s_.t[:, 2:3], in0=s_.t[:, 1:2], scalar=-1.0, in1=s_.t[:, 0:1],
                                                                      op0=ALU.mult, op1=ALU.max), reads=[s_], writes=[s_])
                    fw.op('pool', lambda e: e.tensor_tensor(out=sc_.t[:, qt * 128:L], in0=sc_.t[:, qt * 128:L], in1=negtri.t[:], op=ALU.add),
                          reads=[sc_, negtri], writes=[sc_])
                    if qt >= 2:
                        fw.op('dve', lambda e: e.tensor_scalar(out=dk_.t[:], in0=cpow.t[:], scalar1=s_.t[:, 2:3], scalar2=None, op0=ALU.mult),
                              reads=[cpow, s_], writes=[dk_])
                        fw.op('dve', lambda e: e.memset(s_.t[:, 3:4], 0.0), writes=[s_])
                        for k in range(KBIS):
                            fw.op('dve', lambda e: e.tensor_scalar(out=junk.t[:, 0:L], in0=sc_.t[:, 0:L], scalar1=s_.t[:, 3:4], scalar2=0.0,
                                                                   op0=ALU.is_ge, op1=ALU.add, accum_out=s_.t[:, 4:5]),
                                  reads=[sc_, s_], writes=[junk, s_])
                            last = (k == KBIS - 1)
                            fw.op('dve', lambda e: e.tensor_scalar(out=s_.t[:, 5:6], in0=s_.t[:, 4:5], scalar1=255.5,
                                                                   scalar2=(1.0 if last else 0.5), op0=ALU.is_ge, op1=ALU.subtract),
                                  reads=[s_], writes=[s_])
                            dst = s_.t[:, 6:7] if last else s_.t[:, 3:4]
                            fw.op('dve', lambda e: e.scalar_tensor_tensor(out=dst, in0=s_.t[:, 5:6], scalar=dk_.t[:, k:k + 1], in1=s_.t[:, 3:4],
                                                                          op0=ALU.mult, op1=ALU.add), reads=[s_, dk_], writes=[s_])
                        fw.op('dve', lambda e: e.tensor_scalar(out=mb_.t[:, 0:L], in0=sc_.t[:, 0:L], scalar1=s_.t[:, 6:7], scalar2=-BIG,
                                                               op0=ALU.is_lt, op1=ALU.mult), reads=[sc_, s_], writes=[mb_])
                    else:
                        fw.op('dve', lambda e: e.tensor_scalar(out=mb_.t[:, 0:L], in0=sc_.t[:, 0:L], scalar1=-1e29, scalar2=-BIG,
                                                               op0=ALU.is_lt, op1=ALU.mult), reads=[sc_], writes=[mb_])

                def attend(qt):
                    mb_ = mb[qt % 2]
                    q2_ = Q2[qt % 3]
                    def qk(st):
                        ai_ = cnt_["ai"]
                        PT = ptd[ai_ % 2]
                        cnt_["ai"] += 1
                        for e_ in range(2):
                            bank = sp3[(2 * ai_ + e_) % 3]
                            for hp in range(4):
                                fw.op('pe', lambda e: e.matmul(bank.t[:, hp * 128:(hp + 1) * 128],
                                                               lhsT=K2.t[64 * e_:64 * e_ + 64, hp, st * 128:(st + 1) * 128],
                                                               rhs=q2_.t[64 * e_:64 * e_ + 64, hp, :], start=(hp == 0), stop=False),
                                      reads=[K2, q2_], writes=[bank])
                            fw.op('pe', lambda e: e.matmul(bank.t[:], lhsT=mb_.t[:, st * 128:(st + 1) * 128],
                                                           rhs=i4.t[:, :, :].rearrange("p a t -> p (a t)"), start=False, stop=True),
                                  reads=[mb_, i4], writes=[bank])
                            fw.op('act', lambda e: e.activation(out=PT.t[:, e_, :], in_=bank.t[:], func=AF.Exp, scale=0.125),
                                  reads=[bank], writes=[PT])
                        return PT

                    pend = qk(0)
                    for st in range(qt + 1):
                        nxt = qk(st + 1) if st + 1 <= qt else None
                        PT = pend
                        for e_ in range(2):
                            for hp in range(4):
                                hh = 2 * hp + e_
                                fw.op('pe', lambda e: e.matmul(opsd[e_].t[:, hp * 65:(hp + 1) * 65], lhsT=PT.t[:, e_, hp * 128:(hp + 1) * 128],
                                                               rhs=V.t[:, st, hh * 65:(hh + 1) * 65], start=(st == 0 and hp == 0), stop=(st == qt)),
                                      reads=[PT, V], writes=[opsd[e_]])
                        pend = nxt
                    ob_ = osbd[qt % 2]
                    for e_ in range(2):
                        Ov = opsd[e_].t[:, 0:260].rearrange("p (i d) -> p i d", d=65)
                        fw.op('dve', lambda e: e.reciprocal(out=recd[e_].t[:], in_=Ov[:, :, 64:65]), reads=[opsd[e_]], writes=[recd[e_]])
                        fw.op('dve', lambda e: e.tensor_tensor(out=ob_.t[:, :, e_, :], in0=Ov[:, :, 0:64],
                                                               in1=recd[e_].t[:, :, :].to_broadcast([128, 4, 64]), op=ALU.mult),
                              reads=[opsd[e_], recd[e_]], writes=[ob_])
                    fw.dma('sp', od_d[qt * 128:(qt + 1) * 128, :], ob_.t[:, :, :, :].rearrange("p a e d -> p (a e d)"), reads=[ob_], sem=ob_)

                indexer(0)
                for qt in range(NT):
                    if qt + 1 < NT:
                        indexer(qt + 1)
                    select(qt)
                    if qt >= 1:
                        attend(qt - 1)
                attend(NT - 1)

            if "E" in phases:
              with fw.scope():
                Wo = fw.sb("Wo", [128, 8, D], BF16, dma=True)
                fw.dma_group('pool', [(Wo.t[:, k, :], w_out[l, k * 128:(k + 1) * 128, :]) for k in range(8)], writes=[Wo], sem=Wo)
                gmo = fw.sb("gmo", [128, D], F32, dma=True)
                fw.dma_group('sp', [(gmo.t[:, 0:512], g_moba_out[l, :].partition_broadcast(128)),
                                    (gmo.t[:, 512:1024], g_dsa_out[l, :].partition_broadcast(128))], writes=[gmo], sem=gmo)
                G = fw.sb("Gm", [128, D], F32, dma=True)
                fw.dma('sp', G.t[:], gbc_d[l, 0], writes=[G], sem=G)
                ot = [fw.sb(f"ot{i}", [128, D], F32, dma=True) for i in range(2)]
                xt = [fw.sb(f"ext{i}", [128, D], F32, dma=True) for i in range(2)]
                sq = fw.sb("esq", [128, D], BF16)
                ss = [fw.sb(f"ess{i}", [128, 4], F32) for i in range(2)]
                rs = [fw.sb(f"ers{i}", [128, 4], F32) for i in range(2)]
                on = [fw.sb(f"on{i}", [128, D], BF16) for i in range(2)]
                tps = [fw.ps(f"etp{i}", [128, 8, 128], BF16) for i in range(2)]
                oT = [fw.sb(f"oT{i}", [128, 8, 128], BF16) for i in range(2)]
                yps = [[fw.ps(f"yps{i}{j}", [128, 512], F32) for j in range(2)] for i in range(2)]
                ysb = [fw.sb(f"ysb{i}", [128, D], F32, dma=True) for i in range(2)]
                for tt in range(NT):
                    o_, x_, s_, r_, n_, tp, oT_, yp, y_ = ot[tt % 2], xt[tt % 2], ss[tt % 2], rs[tt % 2], on[tt % 2], tps[tt % 2], oT[tt % 2], yps[tt % 2], ysb[tt % 2]
                    rows = slice(tt * 128, (tt + 1) * 128)
                    fw.dma_group('sp', [(o_.t[:, 0:512], om_d[rows, :]), (o_.t[:, 512:1024], od_d[rows, :])], writes=[o_], sem=o_)
                    fw.dma('sp', x_.t[:], xin[rows, :], writes=[x_], sem=x_)
                    fw.op('dve', lambda e: e.memset(s_.t[:], 0.0), writes=[s_])
                    for j in range(2):
                        fw.op('act', lambda e: e.activation(out=sq.t[:, 0:512], in_=o_.t[:, j * 512:(j + 1) * 512], func=AF.Square,
                                                            accum_out=s_.t[:, j:j + 1]), reads=[o_], writes=[sq, s_])
                    fw.op('dve', lambda e: e.tensor_scalar(out=s_.t[:, 0:2], in0=s_.t[:, 0:2], scalar1=1.0 / 512, scalar2=EPS, op0=ALU.mult, op1=ALU.add),
                          reads=[s_], writes=[s_])
                    fw.op('pool', lambda e: e.tensor_tensor(out=r_.t[:, 0:2], in0=s_.t[:, 0:2], in1=nh.t[:, 0:1].to_broadcast([128, 2]), op=ALU.pow),
                          reads=[s_, nh], writes=[r_])
                    for j in range(2):
                        fw.op('dve', lambda e: e.scalar_tensor_tensor(out=n_.t[:, j * 512:(j + 1) * 512], in0=o_.t[:, j * 512:(j + 1) * 512],
                                                                      scalar=r_.t[:, j:j + 1], in1=gmo.t[:, j * 512:(j + 1) * 512],
                                                                      op0=ALU.mult, op1=ALU.mult), reads=[o_, r_, gmo], writes=[n_])
                    for k in range(8):
                        fw.op('pe', lambda e: e.transpose(out=tp.t[:, k, :], in_=n_.t[:, k * 128:(k + 1) * 128], identity=ident.t[:]),
                              reads=[n_, ident], writes=[tp])
                    fw.op('act', lambda e: e.copy(out=oT_.t[:], in_=tp.t[:]), reads=[tp], writes=[oT_])
                    for j in range(2):
                        for k in range(8):
                            fw.op('pe', lambda e: e.matmul(yp[j].t[:], lhsT=oT_.t[:, k, :], rhs=Wo.t[:, k, j * 512:(j + 1) * 512],
                                                           start=(k == 0), stop=(k == 7)), reads=[oT_, Wo], writes=[yp[j]])
                        fw.op('act', lambda e: e.activation(out=sq.t[:, 0:512], in_=yp[j].t[:], func=AF.Square, accum_out=s_.t[:, 2 + j:3 + j]),
                              reads=[yp[j]], writes=[sq, s_])
                    fw.op('dve', lambda e: e.tensor_tensor(out=s_.t[:, 2:3], in0=s_.t[:, 2:3], in1=s_.t[:, 3:4], op=ALU.add), reads=[s_], writes=[s_])
                    _rms_rstd(fw, (s_, s_.t[:, 2:3]), (r_, r_.t[:, 2:3]), nh, D)
                    for j in range(2):
                        fw.op('dve', lambda e: e.scalar_tensor_tensor(out=y_.t[:, j * 512:(j + 1) * 512], in0=yp[j].t[:], scalar=r_.t[:, 2:3],
                                                                      in1=G.t[:, j * 512:(j + 1) * 512], op0=ALU.mult, op1=ALU.mult),
                              reads=[yp[j], r_, G], writes=[y_])
                    fw.op('pool', lambda e: e.tensor_tensor(out=y_.t[:], in0=y_.t[:], in1=x_.t[:], op=ALU.add), reads=[y_, x_], writes=[y_])
                    fw.dma('sp', x1_d[rows, :], y_.t[:], reads=[y_], sem=y_)

            if "F" in phases:
              with fw.scope():
                Wa = fw.sb("Wa", [128, 8, DFF], BF16, dma=True)
                Wl = fw.sb("Wl", [128, 8, DFF], BF16, dma=True)
                Wd = fw.sb("Wd", [128, NFC, D], BF16, dma=True)
                fw.dma_group('pool', [(Wa.t[:, k, :], w_up_act[l, k * 128:(k + 1) * 128, :]) for k in range(8)], writes=[Wa], sem=Wa)
                fw.dma_group('pool', [(Wl.t[:, k, :], w_up_lin[l, k * 128:(k + 1) * 128, :]) for k in range(8)], writes=[Wl], sem=Wl)
                fw.dma_group('pool', [(Wd.t[:, f, :], w_down[l, f * 128:(f + 1) * 128, :]) for f in range(NFC)], writes=[Wd], sem=Wd)
                wc = fw.sb("wc", [128, NFC, 3], F32, dma=True)
                bcv = fw.sb("bcv", [128, NFC], F32, dma=True)
                fw.dma('sp', wc.t[:], w_conv_col[l], writes=[wc], sem=wc)
                fw.dma('sp', bcv.t[:], b_conv_col[l], writes=[bcv], sem=bcv)
                G = fw.sb("Gf", [128, D], F32, dma=True)
                fw.dma('sp', G.t[:], gbc_d[l, 1], writes=[G], sem=G)
                carry = fw.sb("carry", [128, NFC, 2], F32)
                fw.op('dve', lambda e: e.memset(carry.t[:], 0.0), writes=[carry])
                xt = fw.sb("fxt", [128, D], F32, dma=True)
                sq = fw.sb("fsq", [128, D], BF16)
                ss = fw.sb("fss", [128, 4], F32)
                rs = fw.sb("frs", [128, 4], F32)
                xn = fw.sb("fxn", [128, D], BF16)
                tps = [fw.ps(f"ftp{i}", [128, 8, 128], BF16) for i in range(2)]
                hT = fw.sb("fhT", [128, 8, 512], BF16)
                ups = [fw.ps(f"ups{i}", [128, 512], F32) for i in range(2)]
                lps = [fw.ps(f"flps{i}", [128, 512], F32) for i in range(2)]
                yps = [fw.ps(f"fyps{j}", [128, 512], F32) for j in range(2)]
                ubuf = [fw.sb(f"ubuf{i}", [128, 514], F32) for i in range(2)]
                av = [fw.sb(f"av{i}", [128, 512], F32) for i in range(2)]
                gT = fw.sb("gT", [128, NFC, 512], BF16)
                xr = fw.sb("xr", [128, D], F32, dma=True)
                ysb = fw.sb("fysb", [128, D], F32, dma=True)
                ssv = fw.view("fssv", ss.t)
                rsv = fw.view("frsv", rs.t)
                fi = 0
                for c in range(NCH):
                    for i in range(4):
                        tt = c * 4 + i
                        fw.dma('sp', xt.t[:], x1_d[tt * 128:(tt + 1) * 128, :], writes=[xt], sem=xt)
                        norm_transpose(xt, l, 2, tps[tt % 2], hT, lambda k: hT.t[:, k, i * 128:(i + 1) * 128], sq, ssv, rsv, xn)
                    for fc in range(NFC):
                        U, Lp, ub, a_ = ups[fi % 2], lps[fi % 2], ubuf[fi % 2], av[fi % 2]
                        fi += 1
                        fs = slice(fc * 128, (fc + 1) * 128)
                        for k in range(8):
                            fw.op('pe', lambda e: e.matmul(U.t[:], lhsT=Wa.t[:, k, fs], rhs=hT.t[:, k, :], start=(k == 0), stop=(k == 7)),
                                  reads=[Wa, hT], writes=[U])
                        for k in range(8):
                            fw.op('pe', lambda e: e.matmul(Lp.t[:], lhsT=Wl.t[:, k, fs], rhs=hT.t[:, k, :], start=(k == 0), stop=(k == 7)),
                                  reads=[Wl, hT], writes=[Lp])
                        fw.op('act', lambda e: e.copy(out=ub.t[:, 2:514], in_=U.t[:]), reads=[U], writes=[ub])
                        fw.op('act', lambda e: e.copy(out=ub.t[:, 0:2], in_=carry.t[:, fc, :]), reads=[carry], writes=[ub])
                        fw.op('dve', lambda e: e.tensor_scalar(out=a_.t[:], in0=ub.t[:, 2:514], scalar1=wc.t[:, fc, 2:3], scalar2=bcv.t[:, fc:fc + 1],
                                                               op0=ALU.mult, op1=ALU.add), reads=[ub, wc, bcv], writes=[a_])
                        fw.op('dve', lambda e: e.scalar_tensor_tensor(out=a_.t[:], in0=ub.t[:, 1:513], scalar=wc.t[:, fc, 1:2], in1=a_.t[:],
                                                                      op0=ALU.mult, op1=ALU.add), reads=[ub, wc, a_], writes=[a_])
                        fw.op('dve', lambda e: e.scalar_tensor_tensor(out=a_.t[:], in0=ub.t[:, 0:512], scalar=wc.t[:, fc, 0:1], in1=a_.t[:],
                                                                       op0=ALU.mult, op1=ALU.add), reads=[ub, wc, a_], writes=[a_])
                        fw.op('act', lambda e: e.copy(out=carry.t[:, fc, :], in_=ub.t[:, 512:514]), reads=[ub], writes=[carry])
                        fw.op('act', lambda e: e.activation(out=a_.t[:], in_=a_.t[:], func=AF.Gelu_apprx_tanh), reads=[a_], writes=[a_])
                        fw.op('dve', lambda e: e.tensor_tensor(out=gT.t[:, fc, :], in0=a_.t[:], in1=Lp.t[:], op=ALU.mult),
                              reads=[a_, Lp], writes=[gT])
                    for i in range(4):
                        tt = c * 4 + i
                        rows = slice(tt * 128, (tt + 1) * 128)
                        fw.dma('sp', xr.t[:], x1_d[rows, :], writes=[xr], sem=xr)
                        fw.op('dve', lambda e: e.memset(ss.t[:, 2:4], 0.0), writes=[ssv])
                        for j in range(2):
                            for fc in range(NFC):
                                fw.op('pe', lambda e: e.matmul(yps[j].t[:], lhsT=gT.t[:, fc, i * 128:(i + 1) * 128], rhs=Wd.t[:, fc, j * 512:(j + 1) * 512],
                                                               start=(fc == 0), stop=(fc == NFC - 1)), reads=[gT, Wd], writes=[yps[j]])
                            fw.op('act', lambda e: e.activation(out=sq.t[:, 0:512], in_=yps[j].t[:], func=AF.Square, accum_out=ss.t[:, 2 + j:3 + j]),
                                  reads=[yps[j]], writes=[sq, ssv])
                        fw.op('dve', lambda e: e.tensor_tensor(out=ss.t[:, 2:3], in0=ss.t[:, 2:3], in1=ss.t[:, 3:4], op=ALU.add), reads=[ssv], writes=[ssv])
                        _rms_rstd(fw, (ssv, ss.t[:, 2:3]), (rsv, rs.t[:, 2:3]), nh, D)
                        for j in range(2):
                            fw.op('dve', lambda e: e.scalar_tensor_tensor(out=ysb.t[:, j * 512:(j + 1) * 512], in0=yps[j].t[:], scalar=rs.t[:, 2:3],
                                                                          in1=G.t[:, j * 512:(j + 1) * 512], op0=ALU.mult, op1=ALU.mult),
                                  reads=[yps[j], rsv, G], writes=[ysb])
                        fw.op('pool', lambda e: e.tensor_tensor(out=ysb.t[:], in0=ysb.t[:], in1=xr.t[:], op=ALU.add), reads=[ysb, xr], writes=[ysb])
                        fw.dma('sp', xout[rows, :], ysb.t[:], reads=[ysb], sem=ysb)
        fw.barrier()
    return nc


def _consts():
    bf = ml_dtypes.bfloat16
    pos = np.arange(S, dtype=np.float32)
    inv = (np.float32(500000.0) ** (-np.arange(0, 16, 2, dtype=np.float32) / np.float32(16))).astype(np.float32)
    ang = (pos[None, :] * inv[:, None]).astype(np.float32)
    cos, sin = np.cos(ang).astype(np.float32), np.sin(ang).astype(np.float32)
    C = np.ones((128, S), np.float32)
    Sg = np.zeros((128, S), np.float32)
    for p_ in range(128):
        d = p_ % 64
        if d < 16:
            C[p_] = cos[d % 8]
            Sg[p_] = -sin[d % 8] if d < 8 else sin[d % 8]
    i = np.arange(128)
    tri = (i[None, :] >= i[:, None]).astype(np.float32).astype(bf)
    negtri = np.where(i[None, :] <= i[:, None], 0.0, -1e30).astype(np.float32)
    onehot = (np.arange(S)[None, :] // 256 == np.arange(16)[:, None]).astype(np.float32).astype(bf)
    tt = np.repeat(np.arange(NT), 16)
    n = np.tile(np.arange(16), NT)
    cur = tt // 2
    cbsel = np.where(n >= cur, -BIG, 0.0).astype(np.float32)
    cblt = np.where(n < cur, -BIG, 0.0).astype(np.float32)
    cbfin = np.where(n > cur, -BIG, 0.0).astype(np.float32)
    rep = lambda v: np.ascontiguousarray(np.broadcast_to(v[None, :], (128, v.shape[0])))
    cpow = (2.0 ** (-np.arange(KBIS, dtype=np.float64))).astype(np.float32)
    return {
        "ropeC": C, "ropeS": Sg, "ident": np.eye(128, dtype=np.float32).astype(bf), "tri": tri, "negtri": negtri,
        "onehot": onehot, "cbsel": rep(cbsel), "cblt": rep(cblt), "cbfin": rep(cbfin), "cpow": rep(cpow),
    }


def _perm_w_in(w_in):
    offs = {"mq": 0, "mk": 512, "mv": 1024, "dq": 1536, "dk": 2048, "dv": 2560, "qi": 3072, "ki": 3584, "wi": 3648}
    j = np.arange(64)
    perm = np.where(j < 8, j + 8, np.where(j < 16, j - 8, j))
    cols = []
    for g in ("mq", "mk", "dq", "dk", "qi"):
        base = offs[g]
        cols.append(base + np.arange(512))
        cols.append(base + (np.arange(512) // 64) * 64 + perm[np.arange(512) % 64])
    cols.append(offs["ki"] + np.arange(64))
    cols.append(offs["ki"] + perm)
    cols.append(offs["mv"] + np.arange(512))
    cols.append(offs["dv"] + np.arange(512))
    cols.append(offs["wi"] + np.arange(8))
    cols = np.concatenate(cols)
    assert cols.shape[0] == NCOLP
    return np.ascontiguousarray(w_in[:, :, cols])


def _shared_inputs(w_ada, b_ada, g_pre_mix, w_in, g_moba_out, g_dsa_out, w_out, g_post_mix, g_pre_ffn,
                   w_up_act, w_up_lin, w_conv, b_conv, w_down, g_post_ffn):
    f = lambda a: np.ascontiguousarray(np.asarray(a, dtype=np.float32))
    col8 = lambda g: np.ascontiguousarray(f(g).reshape(2, 8, 128).transpose(0, 2, 1))
    sh = {
        "w_ada": f(w_ada), "b_ada": f(b_ada),
        "b_ada_col": np.ascontiguousarray(f(b_ada).reshape(2, 48, 128).transpose(0, 2, 1)),
        "g_pre_mix_col": col8(g_pre_mix), "g_pre_ffn_col": col8(g_pre_ffn),
        "g_post_mix": f(g_post_mix), "g_post_ffn": f(g_post_ffn),
        "g_moba_out": f(g_moba_out), "g_dsa_out": f(g_dsa_out),
        "w_in_p": _perm_w_in(f(w_in)), "w_out": f(w_out), "w_up_act": f(w_up_act), "w_up_lin": f(w_up_lin),
        "w_conv_col": np.ascontiguousarray(f(w_conv).reshape(2, 3, NFC, 128).transpose(0, 3, 2, 1)),
        "b_conv_col": np.ascontiguousarray(f(b_conv).reshape(2, NFC, 128).transpose(0, 2, 1)),
        "w_down": f(w_down),
    }
    sh.update(_consts())
    return sh


def _core_inputs(x, c, b, shared):
    m = dict(shared)
    m["x"] = np.ascontiguousarray(np.asarray(x[b], dtype=np.float32))
    m["cT"] = np.ascontiguousarray(np.asarray(c[b], dtype=np.float32).reshape(8, 128).T)
    return m


def kernel(x, c, w_ada, b_ada, g_pre_mix, w_in, g_moba_out, g_dsa_out, w_out, g_post_mix,
           g_pre_ffn, w_up_act, w_up_lin, w_conv, b_conv, w_down, g_post_ffn):
    x = np.asarray(x)
    c = np.asarray(c)
    shared = _shared_inputs(w_ada, b_ada, g_pre_mix, w_in, g_moba_out, g_dsa_out, w_out, g_post_mix, g_pre_ffn,
                            w_up_act, w_up_lin, w_conv, b_conv, w_down, g_post_ffn)
    nc = build_nc()
    in_maps = [_core_inputs(x, c, b, shared) for b in range(8)]
    res = run_bass_kernel_spmd(nc, in_maps, core_ids=list(range(8)))
    return np.stack([np.asarray(r["out"], dtype=np.float32) for r in res.results], axis=0)
```

```python
import numpy as np
import ml_dtypes
from contextlib import ExitStack
import concourse.bass as bass
import concourse.mybir as mybir
from concourse.bass_utils import run_bass_kernel_spmd

F32 = mybir.dt.float32
BF16 = mybir.dt.bfloat16
ALU = mybir.AluOpType
AF = mybir.ActivationFunctionType
AX = mybir.AxisListType


class DSem:
    __slots__ = ("idx", "cnt")

    def __init__(self, idx):
        self.idx = idx
        self.cnt = 0


class Buf:
    __slots__ = ("name", "t", "w", "r", "dsem")

    def __init__(self, name, t=None):
        self.name = name
        self.t = t
        self.w = {}
        self.r = {}
        self.dsem = None


class _Scope:
    def __init__(self, fw):
        self.fw = fw

    def __enter__(self):
        fw = self.fw
        self.prev = (fw.es, fw.scope_dsems)
        self.stack = ExitStack()
        self.stack.__enter__()
        fw.es = self.stack
        fw.scope_dsems = []
        return self

    def __exit__(self, *a):
        fw = self.fw
        fw.barrier()
        fw.dpool.extend(fw.scope_dsems)
        fw.es, fw.scope_dsems = self.prev
        return self.stack.__exit__(*a)


class FW:
    SEM_MAX = 30000

    def __init__(self, nc, es):
        self.nc = nc
        self.es = es
        self.sem_es = es
        self.eng = {"pe": nc.tensor, "act": nc.scalar, "dve": nc.vector, "pool": nc.gpsimd, "sp": nc.sync}
        self.sems = []
        self.cur = {}
        self.own = {e: set() for e in self.eng}
        self.known = {e: {} for e in self.eng}
        self.issued = {}
        self.dpool = []
        self.scope_dsems = []
        self.nwaits = 0
        self.nops = 0
        for e in self.eng:
            self._newsem(e)

    def _alloc_sem(self, name):
        s = self.sem_es.enter_context(self.nc.semaphore(name))
        self.sems.append(s)
        return len(self.sems) - 1

    def _newsem(self, e):
        i = self._alloc_sem(f"s_{e}_{len(self.sems)}")
        self.cur[e] = [i, 0]
        self.own[e].add(i)

    def _get_dsem(self):
        if self.dpool:
            d = self.dpool.pop()
        else:
            d = DSem(self._alloc_sem(f"d_{len(self.sems)}"))
        self.scope_dsems.append(d)
        return d

    def scope(self):
        return _Scope(self)

    def sb(self, name, shape, dtype, dma=False):
        self.nuniq = getattr(self, "nuniq", 0) + 1
        t = self.es.enter_context(self.nc.sbuf_tensor(f"{name}_u{self.nuniq}", shape, dtype))
        b = Buf(name, t)
        if dma:
            b.dsem = self._get_dsem()
        return b

    def ps(self, name, shape, dtype):
        self.nuniq = getattr(self, "nuniq", 0) + 1
        t = self.es.enter_context(self.nc.psum_tensor(f"{name}_u{self.nuniq}", shape, dtype))
        return Buf(name, t)

    def view(self, name, t, dma=False):
        b = Buf(name, t)
        if dma:
            b.dsem = self._get_dsem()
        return b

    def _need(self, reads, writes):
        need = {}
        for b in reads:
            for s, v in b.w.items():
                if need.get(s, 0) < v:
                    need[s] = v
        for b in writes:
            for s, v in b.w.items():
                if need.get(s, 0) < v:
                    need[s] = v
            for s, v in b.r.items():
                if need.get(s, 0) < v:
                    need[s] = v
        return need

    def _waits(self, e, need, skip_own=False):
        k = self.known[e]
        eng = self.eng[e]
        for s, v in need.items():
            if skip_own and s in self.own[e]:
                continue
            if k.get(s, 0) >= v:
                continue
            eng.wait_ge(self.sems[s], v)
            self.nwaits += 1
            k[s] = v

    def _record(self, t, reads, writes):
        s, v = t
        self.issued[s] = v
        for b in reads:
            if b.r.get(s, 0) < v:
                b.r[s] = v
        for b in writes:
            b.w = {s: v}
            b.r = {}

    def op(self, e, fn, reads=(), writes=(), same=None):
        if same is None:
            same = (e != "pe")
        need = self._need(reads, writes)
        self._waits(e, need, skip_own=not same)
        ins = fn(self.eng[e])
        c = self.cur[e]
        c[1] += 1
        ins.then_inc(self.sems[c[0]], 1)
        self._record((c[0], c[1]), reads, writes)
        self.nops += 1
        if c[1] >= self.SEM_MAX:
            self._newsem(e)
        return ins

    def dma(self, q, out, in_, reads=(), writes=(), sem=None, **kw):
        return self.dma_group(q, [(out, in_)], reads, writes, sem, **kw)

    def dma_group(self, q, pairs, reads=(), writes=(), sem=None, **kw):
        d = sem.dsem
        need = self._need(reads, writes)
        if d.cnt:
            v = 16 * d.cnt
            if need.get(d.idx, 0) < v:
                need[d.idx] = v
        self._waits(q, need)
        for (out, in_) in pairs:
            ins = self.eng[q].dma_start(out=out, in_=in_, **kw)
            d.cnt += 1
            ins.then_inc(self.sems[d.idx], 16)
            self.nops += 1
        self._record((d.idx, 16 * d.cnt), reads, writes)

    def barrier(self):
        need = dict(self.issued)
        for e in self.eng:
            self._waits(e, need)


S = 4096
D = 1024
NT = 32
NCH = 8
DFF = 2816
NFC = 22
NCOLP = 6280
C_MV, C_DV, C_WI = 5248, 5760, 6272
BIG = 30000.0
KBIS = 18
EPS = 1e-6


class P:
    pass


def _rms_rstd(fw, ssb, rstd, nh, n):
    (ss_buf, ss_ap), (r_buf, r_ap) = ssb, rstd
    fw.op('dve', lambda e: e.tensor_scalar(out=ss_ap, in0=ss_ap, scalar1=1.0 / n, scalar2=EPS, op0=ALU.mult, op1=ALU.add),
          reads=[ss_buf], writes=[ss_buf])
    fw.op('pool', lambda e: e.tensor_tensor(out=r_ap, in0=ss_ap, in1=nh.t[:, 0:1], op=ALU.pow), reads=[ss_buf, nh], writes=[r_buf])


def build_nc(nlayers=2, phases="ABCDEF", dbg=False):
    nc = bass.Bass("TRN2", target_bir_lowering=False)
    p = P()

    def din(name, shape, dt=F32):
        return nc.dram_tensor(name, shape, dt, kind="ExternalInput").ap()

    def dscr(name, shape, dt):
        return nc.dram_tensor(name, shape, dt, kind=("ExternalOutput" if dbg else "Internal")).ap()

    x = din("x", [S, D])
    cT = din("cT", [128, 8])
    w_ada = din("w_ada", [2, D, 6 * D])
    b_ada = din("b_ada", [2, 6 * D])
    b_ada_col = din("b_ada_col", [2, 128, 48])
    g_pre_mix_col = din("g_pre_mix_col", [2, 128, 8])
    g_pre_ffn_col = din("g_pre_ffn_col", [2, 128, 8])
    g_post_mix = din("g_post_mix", [2, D])
    g_post_ffn = din("g_post_ffn", [2, D])
    g_moba_out = din("g_moba_out", [2, 512])
    g_dsa_out = din("g_dsa_out", [2, 512])
    w_in_p = din("w_in_p", [2, D, NCOLP])
    w_out = din("w_out", [2, D, D])
    w_up_act = din("w_up_act", [2, D, DFF])
    w_up_lin = din("w_up_lin", [2, D, DFF])
    w_conv_col = din("w_conv_col", [2, 128, NFC, 3])
    b_conv_col = din("b_conv_col", [2, 128, NFC])
    w_down = din("w_down", [2, DFF, D])
    ropeC = din("ropeC", [128, S])
    ropeS = din("ropeS", [128, S])
    ident_d = din("ident", [128, 128], BF16)
    tri_d = din("tri", [128, 128], BF16)
    negtri_d = din("negtri", [128, 128])
    onehot_d = din("onehot", [16, S], BF16)
    cbsel_d = din("cbsel", [128, 512])
    cblt_d = din("cblt", [128, 512])
    cbfin_d = din("cbfin", [128, 512])
    cpow_d = din("cpow", [128, KBIS])
    out = nc.dram_tensor("out", [S, D], F32, kind="ExternalOutput").ap()

    scrT = [dscr(n, [512, S], BF16) for n in ("mqT", "mkT", "dqT", "dkT", "qiT")]
    kiT_d = dscr("kiT", [64, S], BF16)
    mva = dscr("mva", [S, 520], BF16)
    dva = dscr("dva", [S, 520], BF16)
    om_d = dscr("om", [S, 512], F32)
    od_d = dscr("od", [S, 512], F32)
    x1_d = dscr("x1", [S, D], F32)
    x2_d = dscr("x2", [S, D], F32)
    gbc_d = dscr("gbc", [2, 2, 128, D], F32)

    with ExitStack() as es:
        fw = FW(nc, es)
        AB = fw.sb("AB", [128, 2, 4, 8], F32)
        WI = fw.sb("WI", [128, NT, 8], F32)
        ksum = fw.sb("ksum", [128, 4, 16], F32)
        ident = fw.sb("identb", [128, 128], BF16, dma=True)
        nh = fw.sb("neghalf", [128, 1], F32)
        fw.dma('sp', ident.t[:], ident_d[:, :], writes=[ident], sem=ident)
        fw.op('dve', lambda e: e.memset(nh.t[:], -0.5), writes=[nh])

        if "A" in phases:
          with fw.scope():
            ct = fw.sb("ct", [128, 8], F32, dma=True)
            sc = fw.sb("sc", [128, 8], F32)
            screp = fw.sb("screp", [128, 8, 128], F32)
            ones1 = fw.sb("ones1", [1, 128], F32)
            wa = [fw.sb(f"wa{i}", [128, 8, 1024], F32, dma=True) for i in range(2)]
            brow = [fw.sb(f"brow{i}", [1, 1024], F32, dma=True) for i in range(2)]
            bcol = fw.sb("bcol", [128, 2, 48], F32, dma=True)
            gcol = fw.sb("gcol", [128, 2, 2, 8], F32, dma=True)
            gpb = [fw.sb(f"gpb{i}", [128, 1024], F32, dma=True) for i in range(2)]
            colps = fw.ps("colps", [128, 512], F32)
            bcps = [fw.ps(f"bcps{i}", [128, 512], F32) for i in range(2)]
            mcol = fw.sb("mcol", [128, 8], F32)
            Gt = [fw.sb(f"Gt{i}", [128, D], F32, dma=True) for i in range(2)]
            fw.dma('sp', ct.t[:], cT[:, :], writes=[ct], sem=ct)
            fw.dma_group('sp', [(bcol.t[:, l, :], b_ada_col[l]) for l in range(2)], writes=[bcol], sem=bcol)
            fw.dma_group('sp', [(gcol.t[:, l, 0, :], g_pre_mix_col[l]) for l in range(2)]
                         + [(gcol.t[:, l, 1, :], g_pre_ffn_col[l]) for l in range(2)], writes=[gcol], sem=gcol)
            fw.op('act', lambda e: e.activation(out=sc.t[:], in_=ct.t[:], func=AF.Silu), reads=[ct], writes=[sc])
            fw.op('dve', lambda e: e.tensor_copy(out=screp.t[:], in_=sc.t[:, :].unsqueeze(2).to_broadcast([128, 8, 128])),
                  reads=[sc], writes=[screp])
            fw.op('dve', lambda e: e.memset(ones1.t[:], 1.0), writes=[ones1])
            pi = 0
            for l in range(nlayers):
                wv = w_ada[l].rearrange("(k p) n -> p k n", p=128)
                for piece in range(6):
                    w = wa[pi % 2]
                    pi += 1
                    fw.dma_group('sp', [(w.t[:, k, :], wv[:, k, piece * 1024:(piece + 1) * 1024]) for k in range(8)],
                                 writes=[w], sem=w)
                    if piece in (2, 5):
                        j = 0 if piece == 2 else 1
                        br = brow[j]
                        gp = gpb[j]
                        fw.dma('sp', br.t[:], b_ada[l:l + 1, piece * 1024:(piece + 1) * 1024], writes=[br], sem=br)
                        fw.dma('sp', gp.t[:], (g_post_mix if j == 0 else g_post_ffn)[l, :].partition_broadcast(128),
                               writes=[gp], sem=gp)
                        for nhf in range(2):
                            ps = bcps[nhf]
                            for k in range(8):
                                fw.op('pe', lambda e: e.matmul(ps.t[:], lhsT=screp.t[:, k, :], rhs=w.t[:, k, nhf * 512:(nhf + 1) * 512],
                                                               start=(k == 0), stop=False), reads=[screp, w], writes=[ps])
                            fw.op('pe', lambda e: e.matmul(ps.t[:], lhsT=ones1.t[0:1, :], rhs=br.t[0:1, nhf * 512:(nhf + 1) * 512],
                                                           start=False, stop=True), reads=[ones1, br], writes=[ps])
                            G = Gt[j]
                            fw.op('dve', lambda e: e.tensor_tensor(out=G.t[:, nhf * 512:(nhf + 1) * 512], in0=ps.t[:],
                                                                   in1=gp.t[:, nhf * 512:(nhf + 1) * 512], op=ALU.mult),
                                  reads=[ps, gp], writes=[G])
                        fw.dma('sp', gbc_d[l, j], Gt[j].t[:], reads=[Gt[j]], sem=Gt[j])
                    else:
                        for jj in range(8):
                            for k in range(8):
                                fw.op('pe', lambda e: e.matmul(colps.t[:, jj:jj + 1], lhsT=w.t[:, k, jj * 128:(jj + 1) * 128],
                                                               rhs=sc.t[:, k:k + 1], start=(k == 0), stop=(k == 7)),
                                      reads=[sc, w], writes=[colps])
                        fw.op('dve', lambda e: e.tensor_tensor(out=mcol.t[:], in0=colps.t[:, 0:8],
                                                               in1=bcol.t[:, l, piece * 8:(piece + 1) * 8], op=ALU.add),
                              reads=[colps, bcol], writes=[mcol])
                        if piece in (0, 3):
                            slot = 1 if piece == 0 else 3
                            fw.op('dve', lambda e: e.tensor_copy(out=AB.t[:, l, slot, :], in_=mcol.t[:]), reads=[mcol], writes=[AB])
                        else:
                            slot = 0 if piece == 1 else 2
                            gi = 0 if piece == 1 else 1
                            fw.op('dve', lambda e: e.scalar_tensor_tensor(out=AB.t[:, l, slot, :], in0=mcol.t[:], scalar=1.0,
                                                                          in1=gcol.t[:, l, gi, :], op0=ALU.add, op1=ALU.mult),
                                  reads=[mcol, gcol], writes=[AB])

        def norm_transpose(xb, l, slot, tp, hbuf, hview, sq, ss, rstd, xn):
            fw.op('dve', lambda e: e.memset(ss.t[:], 0.0), writes=[ss])
            fw.op('act', lambda e: e.activation(out=sq.t[:], in_=xb.t[:], func=AF.Square, accum_out=ss.t[:, 0:1]),
                  reads=[xb], writes=[sq, ss])
            _rms_rstd(fw, (ss, ss.t[:, 0:1]), (rstd, rstd.t[:, 0:1]), nh, D)
            fw.op('dve', lambda e: e.tensor_scalar(out=xn.t[:], in0=xb.t[:], scalar1=rstd.t[:, 0:1], scalar2=None, op0=ALU.mult),
                  reads=[xb, rstd], writes=[xn])
            for k in range(8):
                fw.op('pe', lambda e: e.transpose(out=tp.t[:, k, :], in_=xn.t[:, k * 128:(k + 1) * 128], identity=ident.t[:]),
                      reads=[xn, ident], writes=[tp])
            for k in range(8):
                fw.op('act', lambda e: e.activation(out=hview(k), in_=tp.t[:, k, :], func=AF.Identity,
                                                    scale=AB.t[:, l, slot, k:k + 1], bias=AB.t[:, l, slot + 1, k:k + 1]),
                      reads=[tp, AB], writes=[hbuf])

        for l in range(nlayers):
            xin = x if l == 0 else x2_d
            xout = out if l == nlayers - 1 else x2_d
            if "B" in phases:
              with fw.scope():
                W = fw.sb("winb", [128, 8, NCOLP], BF16, dma=True)
                fw.dma_group('pool', [(W.t[:, k, :], w_in_p[l, k * 128:(k + 1) * 128, :]) for k in range(8)], writes=[W], sem=W)
                rC = fw.sb("rC", [128, S], F32, dma=True)
                rS = fw.sb("rS", [128, S], F32, dma=True)
                fw.dma('sp', rC.t[:], ropeC[:, :], writes=[rC], sem=rC)
                fw.dma('sp', rS.t[:], ropeS[:, :], writes=[rS], sem=rS)
                xt = [fw.sb(f"xt{i}", [128, D], F32, dma=True) for i in range(2)]
                sq = fw.sb("sq", [128, D], BF16)
                ss = [fw.sb(f"ss{i}", [128, 1], F32) for i in range(2)]
                rstd = [fw.sb(f"rstd{i}", [128, 1], F32) for i in range(2)]
                xn = [fw.sb(f"xn{i}", [128, D], BF16) for i in range(2)]
                tps = [fw.ps(f"tp{i}", [128, 8, 128], BF16) for i in range(2)]
                hT = [fw.sb(f"hT{i}", [128, 8, 512], BF16) for i in range(2)]
                pm = [fw.ps(f"pm{i}", [128, 512], F32) for i in range(2)]
                pp = [fw.ps(f"pp{i}", [128, 512], F32) for i in range(2)]
                pv = [fw.ps(f"pv{i}", [128, 512], F32) for i in range(2)]
                t1 = [fw.sb(f"t1_{i}", [128, 512], F32) for i in range(2)]
                t2 = [fw.sb(f"t2_{i}", [128, 512], F32) for i in range(2)]
                t3 = [fw.sb(f"t3_{i}", [128, 512], F32) for i in range(2)]
                ob = [fw.sb(f"ob{i}", [128, 512], BF16, dma=True) for i in range(3)]
                vt = [fw.sb(f"vt{i}", [128, 8, 65], BF16, dma=True) for i in range(2)]
                for v in vt:
                    fw.op('dve', lambda e: e.memset(v.t[:], 1.0), writes=[v])
                fj = 0
                oj = 0
                vj = 0
                for c in range(NCH):
                    h = hT[c % 2]
                    cs = slice(c * 512, (c + 1) * 512)
                    for i in range(4):
                        tt = c * 4 + i
                        xb = xt[tt % 2]
                        fw.dma('sp', xb.t[:], xin[tt * 128:(tt + 1) * 128, :], writes=[xb], sem=xb)
                        norm_transpose(xb, l, 0, tps[tt % 2], h, lambda k: h.t[:, k, i * 128:(i + 1) * 128],
                                       sq, ss[tt % 2], rstd[tt % 2], xn[tt % 2])
                    tiles = [(g, ft, 128) for g in range(5) for ft in range(4)] + [(5, 0, 64)]
                    for (g, ft, M) in tiles:
                        cm = g * 1024 + ft * 128 if g < 5 else 5120
                        cp = cm + 512 if g < 5 else 5184
                        pmain, ppart = pm[fj % 2], pp[fj % 2]
                        a1, a2, a3 = t1[fj % 2], t2[fj % 2], t3[fj % 2]
                        fj += 1
                        for k in range(8):
                            fw.op('pe', lambda e: e.matmul(pmain.t[0:M, :], lhsT=W.t[:, k, cm:cm + M], rhs=h.t[:, k, :],
                                                           start=(k == 0), stop=(k == 7)), reads=[W, h], writes=[pmain])
                        for k in range(8):
                            fw.op('pe', lambda e: e.matmul(ppart.t[0:M, :], lhsT=W.t[:, k, cp:cp + M], rhs=h.t[:, k, :],
                                                           start=(k == 0), stop=(k == 7)), reads=[W, h], writes=[ppart])
                        fw.op('dve', lambda e: e.tensor_tensor(out=a1.t[0:M, :], in0=ppart.t[0:M, :], in1=rS.t[0:M, cs], op=ALU.mult),
                              reads=[ppart, rS], writes=[a1])
                        fw.op('dve', lambda e: e.tensor_tensor(out=a2.t[0:M, :], in0=pmain.t[0:M, :], in1=rC.t[0:M, cs], op=ALU.mult),
                              reads=[pmain, rC], writes=[a2])
                        o = ob[oj % 3]
                        oj += 1
                        if g == 1:
                            fw.op('pool', lambda e: e.tensor_tensor(out=a3.t[:], in0=a1.t[:], in1=a2.t[:], op=ALU.add),
                                  reads=[a1, a2], writes=[a3])
                            fw.op('act', lambda e: e.copy(out=o.t[:], in_=a3.t[:]), reads=[a3], writes=[o])
                            fw.op('dve', lambda e: e.reduce_sum(out=ksum.t[:, ft, 2 * c:2 * c + 2],
                                                                in_=a3.t[:, :].rearrange("p (b s) -> p b s", b=2), axis=AX.X),
                                  reads=[a3], writes=[ksum])
                        else:
                            fw.op('pool', lambda e: e.tensor_tensor(out=o.t[0:M, :], in0=a1.t[0:M, :], in1=a2.t[0:M, :], op=ALU.add),
                                  reads=[a1, a2], writes=[o])
                        dst = scrT[g][ft * 128:(ft + 1) * 128, cs] if g < 5 else kiT_d[:, cs]
                        fw.dma('sp', dst, o.t[0:M, :], reads=[o], sem=o)
                    for i in range(4):
                        tt = c * 4 + i
                        for (dst, c0) in ((mva, C_MV), (dva, C_DV)):
                            ps = pv[vj % 2]
                            v = vt[vj % 2]
                            vj += 1
                            for k in range(8):
                                fw.op('pe', lambda e: e.matmul(ps.t[:], lhsT=h.t[:, k, i * 128:(i + 1) * 128], rhs=W.t[:, k, c0:c0 + 512],
                                                               start=(k == 0), stop=(k == 7)), reads=[W, h], writes=[ps])
                            fw.op('act', lambda e: e.copy(out=v.t[:, :, 0:64], in_=ps.t[:, :].rearrange("p (h d) -> p h d", h=8)),
                                  reads=[ps], writes=[v])
                            fw.dma('sp', dst[tt * 128:(tt + 1) * 128, :], v.t[:, :, :].rearrange("p h d -> p (h d)"), reads=[v], sem=v)
                        ps = pv[vj % 2]
                        vj += 1
                        for k in range(8):
                            fw.op('pe', lambda e: e.matmul(ps.t[:, 0:8], lhsT=h.t[:, k, i * 128:(i + 1) * 128], rhs=W.t[:, k, C_WI:C_WI + 8],
                                                           start=(k == 0), stop=(k == 7)), reads=[W, h], writes=[ps])
                        fw.op('act', lambda e: e.copy(out=WI.t[:, tt, :], in_=ps.t[:, 0:8]), reads=[ps], writes=[WI])
            if "C" in phases:
              with fw.scope():
                V = fw.sb("mV", [128, NT, 520], BF16, dma=True)
                fw.dma('sp', V.t[:], mva.rearrange("(n p) c -> p n c", p=128), writes=[V], sem=V)
                Qt = [fw.sb(f"mQ{i}", [80, S], BF16) for i in range(2)]
                Kt = [fw.sb(f"mK{i}", [80, S], BF16) for i in range(2)]
                Qm = [fw.view(f"mQm{i}", Qt[i].t, dma=True) for i in range(2)]
                Qb = [fw.view(f"mQb{i}", Qt[i].t) for i in range(2)]
                Km = [fw.view(f"mKm{i}", Kt[i].t, dma=True) for i in range(2)]
                Kc = [fw.view(f"mKc{i}", Kt[i].t, dma=True) for i in range(2)]
                for i in range(2):
                    fw.dma('sp', Kt[i].t[64:80, :], onehot_d[:, :], writes=[Kc[i]], sem=Kc[i])
                kmb = fw.sb("kmb", [64, 8, 16], BF16)
                for h in range(8):
                    e_, ft = h % 2, h // 2
                    fw.op('act', lambda e: e.mul(out=kmb.t[0:64, h, :], in_=ksum.t[e_ * 64:(e_ + 1) * 64, ft, :], mul=1.0 / 256.0),
                          reads=[ksum], writes=[kmb])
                tri = fw.sb("tri", [128, 128], BF16, dma=True)
                fw.dma('sp', tri.t[:], tri_d[:, :], writes=[tri], sem=tri)
                cbs = fw.sb("cbs", [128, 3, 512], F32, dma=True)
                fw.dma_group('sp', [(cbs.t[:, 0, :], cbsel_d[:, :]), (cbs.t[:, 1, :], cblt_d[:, :]), (cbs.t[:, 2, :], cbfin_d[:, :])],
                             writes=[cbs], sem=cbs)
                gps = fw.ps("gps", [128, 512], F32)
                tb = fw.ps("tb", [128, 1024], BF16)
                sps = [fw.ps(f"sp{i}", [128, 512], F32) for i in range(3)]
                ops_ = [fw.ps(f"op{i}", [128, 512], F32) for i in range(2)]
                pt = [fw.sb(f"pt{i}", [128, 512], BF16) for i in range(3)]
                gm = fw.sb("gm", [128, 512], F32)
                ee = fw.sb("ee", [128, 512], F32)
                g2 = fw.sb("g2", [128, 512], F32)
                g3 = fw.sb("g3", [128, 512], F32)
                mx = fw.sb("mx", [128, 3, 32], F32)
                bt = fw.sb("bt", [128, 512], BF16)
                rec = [fw.sb(f"rec{i}", [128, 4, 1], F32) for i in range(2)]
                osb = [fw.sb(f"osb{i}", [128, 4, 64], F32, dma=True) for i in range(2)]
                v3 = lambda t: t[:, :].rearrange("p (a n) -> p a n", n=16)
                bc3 = lambda j: mx.t[:, j, :].unsqueeze(2).to_broadcast([128, 32, 16])
                si = 0
                oi = 0
                def gateA(h):
                    b = h % 2
                    fw.dma('sp', Qt[b].t[0:64, :], scrT[0][h * 64:(h + 1) * 64, :], writes=[Qm[b]], sem=Qm[b])
                    fw.dma('sp', Kt[b].t[0:64, :], scrT[1][h * 64:(h + 1) * 64, :], writes=[Km[b]], sem=Km[b])
                    for tt in range(NT):
                        fw.op('pe', lambda e: e.matmul(gps.t[:, tt * 16:(tt + 1) * 16], lhsT=Qt[b].t[0:64, tt * 128:(tt + 1) * 128],
                                                       rhs=kmb.t[0:64, h, :], start=True, stop=True), reads=[Qm[b], kmb], writes=[gps])
                    fw.op('dve', lambda e: e.tensor_tensor(out=gm.t[:], in0=gps.t[:], in1=cbs.t[:, 0, :], op=ALU.add),
                          reads=[gps, cbs], writes=[gm])
                    fw.op('dve', lambda e: e.reduce_max(out=mx.t[:, 0, :], in_=v3(gm.t), axis=AX.X), reads=[gm], writes=[mx])
                    fw.op('dve', lambda e: e.tensor_tensor(out=v3(ee.t), in0=v3(gm.t), in1=bc3(0), op=ALU.is_equal),
                          reads=[gm, mx], writes=[ee])
                    fw.op('dve', lambda e: e.scalar_tensor_tensor(out=g2.t[:], in0=ee.t[:], scalar=-1e9, in1=gm.t[:], op0=ALU.mult, op1=ALU.add),
                          reads=[ee, gm], writes=[g2])
                    fw.op('dve', lambda e: e.reduce_max(out=mx.t[:, 1, :], in_=v3(g2.t), axis=AX.X), reads=[g2], writes=[mx])
                    fw.op('dve', lambda e: e.tensor_tensor(out=v3(ee.t), in0=v3(g2.t), in1=bc3(1), op=ALU.is_equal),
                          reads=[g2, mx], writes=[ee])
                    fw.op('dve', lambda e: e.scalar_tensor_tensor(out=g3.t[:], in0=ee.t[:], scalar=-1e9, in1=g2.t[:], op0=ALU.mult, op1=ALU.add),
                          reads=[ee, g2], writes=[g3])
                    fw.op('dve', lambda e: e.reduce_max(out=mx.t[:, 2, :], in_=v3(g3.t), axis=AX.X), reads=[g3], writes=[mx])
                    fw.op('dve', lambda e: e.tensor_tensor(out=v3(ee.t), in0=v3(gm.t), in1=bc3(2), op=ALU.is_lt),
                          reads=[gm, mx], writes=[ee])
                    fw.op('dve', lambda e: e.tensor_tensor(out=g2.t[:], in0=ee.t[:], in1=cbs.t[:, 1, :], op=ALU.mult),
                          reads=[ee, cbs], writes=[g2])
                    fw.op('dve', lambda e: e.tensor_tensor(out=bt.t[:], in0=g2.t[:], in1=cbs.t[:, 2, :], op=ALU.add),
                          reads=[g2, cbs], writes=[bt])

                def gateB(h):
                    b = h % 2
                    for tt in range(NT):
                        fw.op('pe', lambda e: e.transpose(out=tb.t[0:16, (tt % 4) * 128:(tt % 4 + 1) * 128], in_=bt.t[:, tt * 16:(tt + 1) * 16],
                                                          identity=ident.t[:]), reads=[bt, ident], writes=[tb])
                        if tt % 4 == 3:
                            q4 = tt // 4
                            fw.op('act', lambda e: e.copy(out=Qt[b].t[64:80, q4 * 512:(q4 + 1) * 512], in_=tb.t[0:16, 0:512]),
                                  reads=[tb], writes=[Qb[b]])

                def attn(h):
                    nonlocal si, oi
                    b = h % 2
                    for qc in range(NCH):
                        O = ops_[oi % 2]
                        rc = rec[oi % 2]
                        ob_ = osb[oi % 2]
                        oi += 1
                        nst = 4 * qc + 4

                        def qk(st):
                            nonlocal si
                            sp_ = sps[si % 3]
                            Pt = pt[si % 3]
                            si += 1
                            fw.op('pe', lambda e: e.matmul(sp_.t[:], lhsT=Kt[b].t[0:80, st * 128:(st + 1) * 128],
                                                           rhs=Qt[b].t[0:80, qc * 512:(qc + 1) * 512], start=True, stop=True),
                                  reads=[Km[b], Kc[b], Qm[b], Qb[b]], writes=[sp_])
                            fw.op('act', lambda e: e.activation(out=Pt.t[:], in_=sp_.t[:], func=AF.Exp, scale=0.125),
                                  reads=[sp_], writes=[Pt])
                            j = st - 4 * qc
                            if j >= 0:
                                fw.op('pool', lambda e: e.tensor_tensor(out=Pt.t[:, j * 128:(j + 1) * 128], in0=Pt.t[:, j * 128:(j + 1) * 128],
                                                                        in1=tri.t[:], op=ALU.mult), reads=[Pt, tri], writes=[Pt])
                            return Pt, j

                        pend = qk(0)
                        first = True
                        for st in range(nst):
                            nxt = qk(st + 1) if st + 1 < nst else None
                            Pt, j = pend
                            for i in range(max(j, 0), 4):
                                fw.op('pe', lambda e: e.matmul(O.t[:, i * 65:(i + 1) * 65], lhsT=Pt.t[:, i * 128:(i + 1) * 128],
                                                               rhs=V.t[:, st, h * 65:(h + 1) * 65], start=first, stop=(st == 4 * qc + i)),
                                      reads=[Pt, V], writes=[O])
                                first = False
                            pend = nxt
                        Ov = O.t[:, 0:260].rearrange("p (i d) -> p i d", d=65)
                        fw.op('dve', lambda e: e.reciprocal(out=rc.t[:], in_=Ov[:, :, 64:65]), reads=[O], writes=[rc])
                        fw.op('dve', lambda e: e.tensor_tensor(out=ob_.t[:], in0=Ov[:, :, 0:64], in1=rc.t[:, :, :].to_broadcast([128, 4, 64]),
                                                               op=ALU.mult), reads=[O, rc], writes=[ob_])
                        fw.dma('sp', om_d[qc * 512:(qc + 1) * 512, h * 64:(h + 1) * 64].rearrange("(i p) d -> p i d", p=128), ob_.t[:],
                               reads=[ob_], sem=ob_)

                gateA(0)
                gateB(0)
                for h in range(8):
                    if h + 1 < 8:
                        gateA(h + 1)
                    attn(h)
                    if h + 1 < 8:
                        gateB(h + 1)

            if "D" in phases:
              with fw.scope():
                V = fw.sb("dV", [128, NT, 520], BF16, dma=True)
                fw.dma('sp', V.t[:], dva.rearrange("(n p) c -> p n c", p=128), writes=[V], sem=V)
                K2 = fw.sb("K2", [128, 4, S], BF16, dma=True)
                fw.dma('sp', K2.t[:], scrT[3].rearrange("(hp two d) t -> (two d) hp t", two=2, d=64), writes=[K2], sem=K2)
                ki = fw.sb("kiT", [64, S], BF16, dma=True)
                fw.dma('sp', ki.t[:], kiT_d[:, :], writes=[ki], sem=ki)
                negtri = fw.sb("negtri", [128, 128], F32, dma=True)
                fw.dma('sp', negtri.t[:], negtri_d[:, :], writes=[negtri], sem=negtri)
                cpow = fw.sb("cpow", [128, KBIS], F32, dma=True)
                fw.dma('sp', cpow.t[:], cpow_d[:, :], writes=[cpow], sem=cpow)
                QI = [fw.sb(f"QI{i}", [64, 8, 128], BF16, dma=True) for i in range(2)]
                Q2 = [fw.sb(f"Q2{i}", [128, 4, 128], BF16, dma=True) for i in range(3)]
                i4 = fw.sb("i4", [128, 4, 128], BF16)
                fw.op('dve', lambda e: e.tensor_copy(out=i4.t[:], in_=ident.t[:, :].unsqueeze(1).to_broadcast([128, 4, 128])), reads=[ident], writes=[i4])
                score = [fw.sb(f"score{i}", [128, S], F32) for i in range(2)]
                junk = fw.sb("junk", [128, S], BF16)
                mb = [fw.sb(f"mb{i}", [128, S], BF16) for i in range(2)]
                rlb = [fw.sb(f"rl{i}", [128, 512], BF16) for i in range(3)]
                dg = [fw.sb(f"dg{i}", [128, 8, 128], BF16) for i in range(2)]
                scps = fw.ps("scps", [128, 512], F32)
                lps = [fw.ps(f"lps{i}", [128, 512], F32) for i in range(2)]
                sp3 = [fw.ps(f"dsp{i}", [128, 512], F32) for i in range(3)]
                opsd = [fw.ps(f"dop{e_}", [128, 512], F32) for e_ in range(2)]
                ptd = [fw.sb(f"dpt{i}", [128, 2, 512], BF16) for i in range(2)]
                sm = [fw.sb(f"sm{i}", [128, 8], F32) for i in range(2)]
                dkt = [fw.sb(f"dk{i}", [128, KBIS], F32) for i in range(2)]
                recd = [fw.sb(f"drec{e_}", [128, 4, 1], F32) for e_ in range(2)]
                osbd = [fw.sb(f"dosb{i}", [128, 4, 2, 64], F32, dma=True) for i in range(2)]
                qiv = scrT[4].rearrange("(h d) t -> d h t", d=64)
                q2v = scrT[2].rearrange("(hp two d) t -> (two d) hp t", two=2, d=64)
                cnt_ = {"li": 0, "ti": 0, "ai": 0}

                def indexer(qt):
                    L = (qt + 1) * 128
                    sc_ = score[qt % 2]
                    qi_ = QI[qt % 2]
                    fw.dma('sp', qi_.t[:], qiv[:, :, qt * 128:(qt + 1) * 128], writes=[qi_], sem=qi_)
                    fw.dma('sp', Q2[qt % 3].t[:], q2v[:, :, qt * 128:(qt + 1) * 128], writes=[Q2[qt % 3]], sem=Q2[qt % 3])
                    dg_ = dg[qt % 2]
                    fw.op('dve', lambda e: e.tensor_tensor(out=dg_.t[:], in0=ident.t[:, :].unsqueeze(1).to_broadcast([128, 8, 128]),
                                                           in1=WI.t[:, qt, :].unsqueeze(2).to_broadcast([128, 8, 128]), op=ALU.mult),
                          reads=[ident, WI], writes=[dg_])
                    nch = (L + 511) // 512
                    for ch in range(nch):
                        ncol = min(512, L - ch * 512)
                        cs = slice(ch * 512, ch * 512 + ncol)

                        def logit(h):
                            lp = lps[cnt_["li"] % 2]
                            cnt_["li"] += 1
                            R = rlb[cnt_["ti"] % 3]
                            cnt_["ti"] += 1
                            fw.op('pe', lambda e: e.matmul(lp.t[:, 0:ncol], lhsT=qi_.t[0:64, h, :], rhs=ki.t[0:64, cs], start=True, stop=True),
                                  reads=[qi_, ki], writes=[lp])
                            fw.op('act', lambda e: e.activation(out=R.t[:, 0:ncol], in_=lp.t[:, 0:ncol], func=AF.Relu), reads=[lp], writes=[R])
                            return R

                        pend = logit(0)
                        for h in range(8):
                            nxt = logit(h + 1) if h + 1 < 8 else None
                            R = pend
                            fw.op('pe', lambda e: e.matmul(scps.t[:, 0:ncol], lhsT=dg_.t[:, h, :], rhs=R.t[:, 0:ncol], start=(h == 0), stop=(h == 7)),
                                  reads=[dg_, R], writes=[scps])
                            pend = nxt
                        fw.op('act', lambda e: e.copy(out=sc_.t[:, cs], in_=scps.t[:, 0:ncol]), reads=[scps], writes=[sc_])

                def select(qt):
                    L = (qt + 1) * 128
                    sc_ = score[qt % 2]
                    s_ = sm[qt % 2]
                    dk_ = dkt[qt % 2]
                    mb_ = mb[qt % 2]
                    if qt >= 2:
                        fw.op('dve', lambda e: e.reduce_max(out=s_.t[:, 0:1], in_=sc_.t[:, 0:L], axis=AX.X), reads=[sc_], writes=[s_])
                        fw.op('dve', lambda e: e.tensor_reduce(out=s_.t[:, 1:2], in_=sc_.t[:, 0:L], axis=AX.X, op=ALU.min), reads=[sc_], writes=[s_])
                        fw.op('dve', lambda e: e.scalar_tensor_tensor(out=s_.t[:, 2:3], in0=s_.t[:, 1:2], scalar=-1.0, in1=s_.t[:, 0:1],
                                                                      op0=ALU.mult, op1=ALU.max), reads=[s_], writes=[s_])
                    fw.op('pool', lambda e: e.tensor_tensor(out=sc_.t[:, qt * 128:L], in0=sc_.t[:, qt * 128:L], in1=negtri.t[:], op=ALU.add),
                          reads=[sc_, negtri], writes=[sc_])
                    if qt >= 2:
                        fw.op('dve', lambda e: e.tensor_scalar(out=dk_.t[:], in0=cpow.t[:], scalar1=s_.t[:, 2:3], scalar2=None, op0=ALU.mult),
                              reads=[cpow, s_], writes=[dk_])
                        fw.op('dve', lambda e: e.memset(s_.t[:, 3:4], 0.0), writes=[s_])
                        for k in range(KBIS):
                            fw.op('dve', lambda e: e.tensor_scalar(out=junk.t[:, 0:L], in0=sc_.t[:, 0:L], scalar1=s_.t[:, 3:4], scalar2=0.0,
                                                                   op0=ALU.is_ge, op1=ALU.add, accum_out=s_.t[:, 4:5]),
                                  reads=[sc_, s_], writes=[junk, s_])
                            last = (k == KBIS - 1)
                            fw.op('dve', lambda e: e.tensor_scalar(out=s_.t[:, 5:6], in0=s_.t[:, 4:5], scalar1=255.5,
                                                                   scalar2=(1.0 if last else 0.5), op0=ALU.is_ge, op1=ALU.subtract),
                                  reads=[s_], writes=[s_])
                            dst = s_.t[:, 6:7] if last else s_.t[:, 3:4]
                            fw.op('dve', lambda e: e.scalar_tensor_tensor(out=dst, in0=s_.t[:, 5:6], scalar=dk_.t[:, k:k + 1], in1=s_.t[:, 3:4],
                                                                          op0=ALU.mult, op1=ALU.add), reads=[s_, dk_], writes=[s_])
                        fw.op('dve', lambda e: e.tensor_scalar(out=mb_.t[:, 0:L], in0=sc_.t[:, 0:L], scalar1=s_.t[:, 6:7], scalar2=-BIG,
                                                               op0=ALU.is_lt, op1=ALU.mult), reads=[sc_, s_], writes=[mb_])
                    else:
                        fw.op('dve', lambda e: e.tensor_scalar(out=mb_.t[:, 0:L], in0=sc_.t[:, 0:L], scalar1=-1e29, scalar2=-BIG,
                                                               op0=ALU.is_lt, op1=ALU.mult), reads=[sc_], writes=[mb_])

                def attend(qt):
                    mb_ = mb[qt % 2]
                    q2_ = Q2[qt % 3]
                    def qk(st):
                        ai_ = cnt_["ai"]
                        PT = ptd[ai_ % 2]
                        cnt_["ai"] += 1
                        for e_ in range(2):
                            bank = sp3[(2 * ai_ + e_) % 3]
                            for hp in range(4):
                                fw.op('pe', lambda e: e.matmul(bank.t[:, hp * 128:(hp + 1) * 128],
                                                               lhsT=K2.t[64 * e_:64 * e_ + 64, hp, st * 128:(st + 1) * 128],
                                                               rhs=q2_.t[64 * e_:64 * e_ + 64, hp, :], start=(hp == 0), stop=False),
                                      reads=[K2, q2_], writes=[bank])
                            fw.op('pe', lambda e: e.matmul(bank.t[:], lhsT=mb_.t[:, st * 128:(st + 1) * 128],
                                                           rhs=i4.t[:, :, :].rearrange("p a t -> p (a t)"), start=False, stop=True),
                                  reads=[mb_, i4], writes=[bank])
                            fw.op('act', lambda e: e.activation(out=PT.t[:, e_, :], in_=bank.t[:], func=AF.Exp, scale=0.125),
                                  reads=[bank], writes=[PT])
                        return PT

                    pend = qk(0)
                    for st in range(qt + 1):
                        nxt = qk(st + 1) if st + 1 <= qt else None
                        PT = pend
                        for e_ in range(2):
                            for hp in range(4):
                                hh = 2 * hp + e_
                                fw.op('pe', lambda e: e.matmul(opsd[e_].t[:, hp * 65:(hp + 1) * 65], lhsT=PT.t[:, e_, hp * 128:(hp + 1) * 128],
                                                               rhs=V.t[:, st, hh * 65:(hh + 1) * 65], start=(st == 0 and hp == 0), stop=(st == qt)),
                                      reads=[PT, V], writes=[opsd[e_]])
                        pend = nxt
                    ob_ = osbd[qt % 2]
                    for e_ in range(2):
                        Ov = opsd[e_].t[:, 0:260].rearrange("p (i d) -> p i d", d=65)
                        fw.op('dve', lambda e: e.reciprocal(out=recd[e_].t[:], in_=Ov[:, :, 64:65]), reads=[opsd[e_]], writes=[recd[e_]])
                        fw.op('dve', lambda e: e.tensor_tensor(out=ob_.t[:, :, e_, :], in0=Ov[:, :, 0:64],
                                                               in1=recd[e_].t[:, :, :].to_broadcast([128, 4, 64]), op=ALU.mult),
                              reads=[opsd[e_], recd[e_]], writes=[ob_])
                    fw.dma('sp', od_d[qt * 128:(qt + 1) * 128, :], ob_.t[:, :, :, :].rearrange("p a e d -> p (a e d)"), reads=[ob_], sem=ob_)

                indexer(0)
                for qt in range(NT):
                    if qt + 1 < NT:
                        indexer(qt + 1)
                    select(qt)
                    if qt >= 1:
                        attend(qt - 1)
                attend(NT - 1)

            if "E" in phases:
              with fw.scope():
                Wo = fw.sb("Wo", [128, 8, D], BF16, dma=True)
                fw.dma_group('pool', [(Wo.t[:, k, :], w_out[l, k * 128:(k + 1) * 128, :]) for k in range(8)], writes=[Wo], sem=Wo)
                gmo = fw.sb("gmo", [128, D], F32, dma=True)
                fw.dma_group('sp', [(gmo.t[:, 0:512], g_moba_out[l, :].partition_broadcast(128)),
                                    (gmo.t[:, 512:1024], g_dsa_out[l, :].partition_broadcast(128))], writes=[gmo], sem=gmo)
                G = fw.sb("Gm", [128, D], F32, dma=True)
                fw.dma('sp', G.t[:], gbc_d[l, 0], writes=[G], sem=G)
                ot = [fw.sb(f"ot{i}", [128, D], F32, dma=True) for i in range(2)]
                xt = [fw.sb(f"ext{i}", [128, D], F32, dma=True) for i in range(3)]
                sq = fw.sb("esq", [128, D], BF16)
                ss = [fw.sb(f"ess{i}", [128, 4], F32) for i in range(3)]
                rs = [fw.sb(f"ers{i}", [128, 4], F32) for i in range(3)]
                on = [fw.sb(f"on{i}", [128, D], BF16) for i in range(2)]
                tps = [fw.ps(f"etp{i}", [128, 8, 128], BF16) for i in range(2)]
                oT = [fw.sb(f"oT{i}", [128, 8, 128], BF16) for i in range(3)]
                yps = [[fw.ps(f"yps{i}{j}", [128, 512], F32) for j in range(2)] for i in range(2)]
                ysb = [fw.sb(f"ysb{i}", [128, D], F32, dma=True) for i in range(2)]

                def s1a(tt):
                    o_, x_, s_, r_, n_ = ot[tt % 2], xt[tt % 3], ss[tt % 3], rs[tt % 3], on[tt % 2]
                    rows = slice(tt * 128, (tt + 1) * 128)
                    fw.dma_group('sp', [(o_.t[:, 0:512], om_d[rows, :]), (o_.t[:, 512:1024], od_d[rows, :])], writes=[o_], sem=o_)
                    fw.dma('sp', x_.t[:], xin[rows, :], writes=[x_], sem=x_)
                    fw.op('dve', lambda e: e.memset(s_.t[:], 0.0), writes=[s_])
                    for j in range(2):
                        fw.op('act', lambda e: e.activation(out=sq.t[:, 0:512], in_=o_.t[:, j * 512:(j + 1) * 512], func=AF.Square,
                                                            accum_out=s_.t[:, j:j + 1]), reads=[o_], writes=[sq, s_])
                    fw.op('dve', lambda e: e.tensor_scalar(out=s_.t[:, 0:2], in0=s_.t[:, 0:2], scalar1=1.0 / 512, scalar2=EPS, op0=ALU.mult, op1=ALU.add),
                          reads=[s_], writes=[s_])
                    fw.op('pool', lambda e: e.tensor_tensor(out=r_.t[:, 0:2], in0=s_.t[:, 0:2], in1=nh.t[:, 0:1].to_broadcast([128, 2]), op=ALU.pow),
                          reads=[s_, nh], writes=[r_])
                    for j in range(2):
                        fw.op('dve', lambda e: e.scalar_tensor_tensor(out=n_.t[:, j * 512:(j + 1) * 512], in0=o_.t[:, j * 512:(j + 1) * 512],
                                                                      scalar=r_.t[:, j:j + 1], in1=gmo.t[:, j * 512:(j + 1) * 512],
                                                                      op0=ALU.mult, op1=ALU.mult), reads=[o_, r_, gmo], writes=[n_])

                def s1b(tt):
                    n_, tp, oT_ = on[tt % 2], tps[tt % 2], oT[tt % 3]
                    for k in range(8):
                        fw.op('pe', lambda e: e.transpose(out=tp.t[:, k, :], in_=n_.t[:, k * 128:(k + 1) * 128], identity=ident.t[:]),
                              reads=[n_, ident], writes=[tp])
                    fw.op('act', lambda e: e.copy(out=oT_.t[:], in_=tp.t[:]), reads=[tp], writes=[oT_])

                def s2a(tt):
                    oT_, yp = oT[tt % 3], yps[tt % 2]
                    for j in range(2):
                        for k in range(8):
                            fw.op('pe', lambda e: e.matmul(yp[j].t[:], lhsT=oT_.t[:, k, :], rhs=Wo.t[:, k, j * 512:(j + 1) * 512],
                                                           start=(k == 0), stop=(k == 7)), reads=[oT_, Wo], writes=[yp[j]])

                def s2b(tt):
                    x_, s_, r_, yp, y_ = xt[tt % 3], ss[tt % 3], rs[tt % 3], yps[tt % 2], ysb[tt % 2]
                    rows = slice(tt * 128, (tt + 1) * 128)
                    for j in range(2):
                        fw.op('act', lambda e: e.activation(out=sq.t[:, 512:1024], in_=yp[j].t[:], func=AF.Square, accum_out=s_.t[:, 2 + j:3 + j]),
                              reads=[yp[j]], writes=[sq, s_])
                    fw.op('dve', lambda e: e.tensor_tensor(out=s_.t[:, 2:3], in0=s_.t[:, 2:3], in1=s_.t[:, 3:4], op=ALU.add), reads=[s_], writes=[s_])
                    _rms_rstd(fw, (s_, s_.t[:, 2:3]), (r_, r_.t[:, 2:3]), nh, D)
                    for j in range(2):
                        fw.op('dve', lambda e: e.scalar_tensor_tensor(out=y_.t[:, j * 512:(j + 1) * 512], in0=yp[j].t[:], scalar=r_.t[:, 2:3],
                                                                      in1=G.t[:, j * 512:(j + 1) * 512], op0=ALU.mult, op1=ALU.mult),
                              reads=[yp[j], r_, G], writes=[y_])
                    fw.op('pool', lambda e: e.tensor_tensor(out=y_.t[:], in0=y_.t[:], in1=x_.t[:], op=ALU.add), reads=[y_, x_], writes=[y_])
                    fw.dma('sp', x1_d[rows, :], y_.t[:], reads=[y_], sem=y_)

                s1a(0)
                s1b(0)
                s1a(1)
                s1b(1)
                for tt in range(NT):
                    s2a(tt)
                    if tt + 2 < NT:
                        s1a(tt + 2)
                        s1b(tt + 2)
                    s2b(tt)

            if "F" in phases:
              with fw.scope():
                Wa = fw.sb("Wa", [128, 8, DFF], BF16, dma=True)
                Wl = fw.sb("Wl", [128, 8, DFF], BF16, dma=True)
                Wd = fw.sb("Wd", [128, NFC, D], BF16, dma=True)
                fw.dma_group('pool', [(Wa.t[:, k, :], w_up_act[l, k * 128:(k + 1) * 128, :]) for k in range(8)], writes=[Wa], sem=Wa)
                fw.dma_group('pool', [(Wl.t[:, k, :], w_up_lin[l, k * 128:(k + 1) * 128, :]) for k in range(8)], writes=[Wl], sem=Wl)
                fw.dma_group('pool', [(Wd.t[:, f, :], w_down[l, f * 128:(f + 1) * 128, :]) for f in range(NFC)], writes=[Wd], sem=Wd)
                wc = fw.sb("wc", [128, NFC, 3], F32, dma=True)
                bcv = fw.sb("bcv", [128, NFC], F32, dma=True)
                fw.dma('sp', wc.t[:], w_conv_col[l], writes=[wc], sem=wc)
                fw.dma('sp', bcv.t[:], b_conv_col[l], writes=[bcv], sem=bcv)
                G = fw.sb("Gf", [128, D], F32, dma=True)
                fw.dma('sp', G.t[:], gbc_d[l, 1], writes=[G], sem=G)
                carry = fw.sb("carry", [128, NFC, 2], F32)
                fw.op('dve', lambda e: e.memset(carry.t[:], 0.0), writes=[carry])
                xt = fw.sb("fxt", [128, D], F32, dma=True)
                sq = fw.sb("fsq", [128, D], BF16)
                ss = fw.sb("fss", [128, 4], F32)
                rs = fw.sb("frs", [128, 4], F32)
                xn = fw.sb("fxn", [128, D], BF16)
                tps = [fw.ps(f"ftp{i}", [128, 8, 128], BF16) for i in range(2)]
                hT = fw.sb("fhT", [128, 8, 512], BF16)
                ups = [fw.ps(f"ups{i}", [128, 512], F32) for i in range(2)]
                lps = [fw.ps(f"flps{i}", [128, 512], F32) for i in range(2)]
                yps = [fw.ps(f"fyps{j}", [128, 512], F32) for j in range(2)]
                ubuf = [fw.sb(f"ubuf{i}", [128, 514], F32) for i in range(2)]
                av = [fw.sb(f"av{i}", [128, 512], F32) for i in range(2)]
                gT = fw.sb("gT", [128, NFC, 512], BF16)
                xr = fw.sb("xr", [128, D], F32, dma=True)
                ysb = fw.sb("fysb", [128, D], F32, dma=True)
                ssv = fw.view("fssv", ss.t)
                rsv = fw.view("frsv", rs.t)
                fi = 0
                for c in range(NCH):
                    for i in range(4):
                        tt = c * 4 + i
                        fw.dma('sp', xt.t[:], x1_d[tt * 128:(tt + 1) * 128, :], writes=[xt], sem=xt)
                        norm_transpose(xt, l, 2, tps[tt % 2], hT, lambda k: hT.t[:, k, i * 128:(i + 1) * 128], sq, ssv, rsv, xn)
                    for fc in range(NFC):
                        U, Lp, ub, a_ = ups[fi % 2], lps[fi % 2], ubuf[fi % 2], av[fi % 2]
                        fi += 1
                        fs = slice(fc * 128, (fc + 1) * 128)
                        for k in range(8):
                            fw.op('pe', lambda e: e.matmul(U.t[:], lhsT=Wa.t[:, k, fs], rhs=hT.t[:, k, :], start=(k == 0), stop=(k == 7)),
                                  reads=[Wa, hT], writes=[U])
                        for k in range(8):
                            fw.op('pe', lambda e: e.matmul(Lp.t[:], lhsT=Wl.t[:, k, fs], rhs=hT.t[:, k, :], start=(k == 0), stop=(k == 7)),
                                  reads=[Wl, hT], writes=[Lp])
                        fw.op('act', lambda e: e.copy(out=ub.t[:, 2:514], in_=U.t[:]), reads=[U], writes=[ub])
                        fw.op('act', lambda e: e.copy(out=ub.t[:, 0:2], in_=carry.t[:, fc, :]), reads=[carry], writes=[ub])
                        fw.op('dve', lambda e: e.tensor_scalar(out=a_.t[:], in0=ub.t[:, 2:514], scalar1=wc.t[:, fc, 2:3], scalar2=bcv.t[:, fc:fc + 1],
                                                               op0=ALU.mult, op1=ALU.add), reads=[ub, wc, bcv], writes=[a_])
                        fw.op('dve', lambda e: e.scalar_tensor_tensor(out=a_.t[:], in0=ub.t[:, 1:513], scalar=wc.t[:, fc, 1:2], in1=a_.t[:],
                                                                      op0=ALU.mult, op1=ALU.add), reads=[ub, wc, a_], writes=[a_])
                        fw.op('dve', lambda e: e.scalar_tensor_tensor(out=a_.t[:], in0=ub.t[:, 0:512], scalar=wc.t[:, fc, 0:1], in1=a_.t[:],
                                                                       op0=ALU.mult, op1=ALU.add), reads=[ub, wc, a_], writes=[a_])
                        fw.op('act', lambda e: e.copy(out=carry.t[:, fc, :], in_=ub.t[:, 512:514]), reads=[ub], writes=[carry])
                        fw.op('act', lambda e: e.activation(out=a_.t[:], in_=a_.t[:], func=AF.Gelu_apprx_tanh), reads=[a_], writes=[a_])
                        fw.op('dve', lambda e: e.tensor_tensor(out=gT.t[:, fc, :], in0=a_.t[:], in1=Lp.t[:], op=ALU.mult),
                              reads=[a_, Lp], writes=[gT])
                    for i in range(4):
                        tt = c * 4 + i
                        rows = slice(tt * 128, (tt + 1) * 128)
                        fw.dma('sp', xr.t[:], x1_d[rows, :], writes=[xr], sem=xr)
                        fw.op('dve', lambda e: e.memset(ss.t[:, 2:4], 0.0), writes=[ssv])
                        for j in range(2):
                            for fc in range(NFC):
                                fw.op('pe', lambda e: e.matmul(yps[j].t[:], lhsT=gT.t[:, fc, i * 128:(i + 1) * 128], rhs=Wd.t[:, fc, j * 512:(j + 1) * 512],
                                                               start=(fc == 0), stop=(fc == NFC - 1)), reads=[gT, Wd], writes=[yps[j]])
                            fw.op('act', lambda e: e.activation(out=sq.t[:, 0:512], in_=yps[j].t[:], func=AF.Square, accum_out=ss.t[:, 2 + j:3 + j]),
                                  reads=[yps[j]], writes=[sq, ssv])
                        fw.op('dve', lambda e: e.tensor_tensor(out=ss.t[:, 2:3], in0=ss.t[:, 2:3], in1=ss.t[:, 3:4], op=ALU.add), reads=[ssv], writes=[ssv])
                        _rms_rstd(fw, (ssv, ss.t[:, 2:3]), (rsv, rs.t[:, 2:3]), nh, D)
                        for j in range(2):
                            fw.op('dve', lambda e: e.scalar_tensor_tensor(out=ysb.t[:, j * 512:(j + 1) * 512], in0=yps[j].t[:], scalar=rs.t[:, 2:3],
                                                                          in1=G.t[:, j * 512:(j + 1) * 512], op0=ALU.mult, op1=ALU.mult),
                                  reads=[yps[j], rsv, G], writes=[ysb])
                        fw.op('pool', lambda e: e.tensor_tensor(out=ysb.t[:], in0=ysb.t[:], in1=xr.t[:], op=ALU.add), reads=[ysb, xr], writes=[ysb])
                        fw.dma('sp', xout[rows, :], ysb.t[:], reads=[ysb], sem=ysb)
        fw.barrier()
    return nc


def _consts():
    bf = ml_dtypes.bfloat16
    pos = np.arange(S, dtype=np.float32)
    inv = (np.float32(500000.0) ** (-np.arange(0, 16, 2, dtype=np.float32) / np.float32(16))).astype(np.float32)
    ang = (pos[None, :] * inv[:, None]).astype(np.float32)
    cos, sin = np.cos(ang).astype(np.float32), np.sin(ang).astype(np.float32)
    C = np.ones((128, S), np.float32)
    Sg = np.zeros((128, S), np.float32)
    for p_ in range(128):
        d = p_ % 64
        if d < 16:
            C[p_] = cos[d % 8]
            Sg[p_] = -sin[d % 8] if d < 8 else sin[d % 8]
    i = np.arange(128)
    tri = (i[None, :] >= i[:, None]).astype(np.float32).astype(bf)
    negtri = np.where(i[None, :] <= i[:, None], 0.0, -1e30).astype(np.float32)
    onehot = (np.arange(S)[None, :] // 256 == np.arange(16)[:, None]).astype(np.float32).astype(bf)
    tt = np.repeat(np.arange(NT), 16)
    n = np.tile(np.arange(16), NT)
    cur = tt // 2
    cbsel = np.where(n >= cur, -BIG, 0.0).astype(np.float32)
    cblt = np.where(n < cur, -BIG, 0.0).astype(np.float32)
    cbfin = np.where(n > cur, -BIG, 0.0).astype(np.float32)
    rep = lambda v: np.ascontiguousarray(np.broadcast_to(v[None, :], (128, v.shape[0])))
    cpow = (2.0 ** (-np.arange(KBIS, dtype=np.float64))).astype(np.float32)
    return {
        "ropeC": C, "ropeS": Sg, "ident": np.eye(128, dtype=np.float32).astype(bf), "tri": tri, "negtri": negtri,
        "onehot": onehot, "cbsel": rep(cbsel), "cblt": rep(cblt), "cbfin": rep(cbfin), "cpow": rep(cpow),
    }


def _perm_w_in(w_in):
    offs = {"mq": 0, "mk": 512, "mv": 1024, "dq": 1536, "dk": 2048, "dv": 2560, "qi": 3072, "ki": 3584, "wi": 3648}
    j = np.arange(64)
    perm = np.where(j < 8, j + 8, np.where(j < 16, j - 8, j))
    cols = []
    for g in ("mq", "mk", "dq", "dk", "qi"):
        base = offs[g]
        cols.append(base + np.arange(512))
        cols.append(base + (np.arange(512) // 64) * 64 + perm[np.arange(512) % 64])
    cols.append(offs["ki"] + np.arange(64))
    cols.append(offs["ki"] + perm)
    cols.append(offs["mv"] + np.arange(512))
    cols.append(offs["dv"] + np.arange(512))
    cols.append(offs["wi"] + np.arange(8))
    cols = np.concatenate(cols)
    assert cols.shape[0] == NCOLP
    return np.ascontiguousarray(w_in[:, :, cols])


def _shared_inputs(w_ada, b_ada, g_pre_mix, w_in, g_moba_out, g_dsa_out, w_out, g_post_mix, g_pre_ffn,
                   w_up_act, w_up_lin, w_conv, b_conv, w_down, g_post_ffn):
    f = lambda a: np.ascontiguousarray(np.asarray(a, dtype=np.float32))
    col8 = lambda g: np.ascontiguousarray(f(g).reshape(2, 8, 128).transpose(0, 2, 1))
    sh = {
        "w_ada": f(w_ada), "b_ada": f(b_ada),
        "b_ada_col": np.ascontiguousarray(f(b_ada).reshape(2, 48, 128).transpose(0, 2, 1)),
        "g_pre_mix_col": col8(g_pre_mix), "g_pre_ffn_col": col8(g_pre_ffn),
        "g_post_mix": f(g_post_mix), "g_post_ffn": f(g_post_ffn),
        "g_moba_out": f(g_moba_out), "g_dsa_out": f(g_dsa_out),
        "w_in_p": _perm_w_in(f(w_in)), "w_out": f(w_out), "w_up_act": f(w_up_act), "w_up_lin": f(w_up_lin),
        "w_conv_col": np.ascontiguousarray(f(w_conv).reshape(2, 3, NFC, 128).transpose(0, 3, 2, 1)),
        "b_conv_col": np.ascontiguousarray(f(b_conv).reshape(2, NFC, 128).transpose(0, 2, 1)),
        "w_down": f(w_down),
    }
    sh.update(_consts())
    return sh


def _core_inputs(x, c, b, shared):
    m = dict(shared)
    m["x"] = np.ascontiguousarray(np.asarray(x[b], dtype=np.float32))
    m["cT"] = np.ascontiguousarray(np.asarray(c[b], dtype=np.float32).reshape(8, 128).T)
    return m


def kernel(x, c, w_ada, b_ada, g_pre_mix, w_in, g_moba_out, g_dsa_out, w_out, g_post_mix,
           g_pre_ffn, w_up_act, w_up_lin, w_conv, b_conv, w_down, g_post_ffn):
    x = np.asarray(x)
    c = np.asarray(c)
    shared = _shared_inputs(w_ada, b_ada, g_pre_mix, w_in, g_moba_out, g_dsa_out, w_out, g_post_mix, g_pre_ffn,
                            w_up_act, w_up_lin, w_conv, b_conv, w_down, g_post_ffn)
    nc = build_nc()
    in_maps = [_core_inputs(x, c, b, shared) for b in range(8)]
    res = run_bass_kernel_spmd(nc, in_maps, core_ids=list(range(8)))
    return np.stack([np.asarray(r["out"], dtype=np.float32) for r in res.results], axis=0)
```

```python
import numpy as np
import ml_dtypes
from contextlib import ExitStack
import concourse.bass as bass
import concourse.mybir as mybir
from concourse.bass_utils import run_bass_kernel_spmd

F32 = mybir.dt.float32
BF16 = mybir.dt.bfloat16
ALU = mybir.AluOpType
AF = mybir.ActivationFunctionType
AX = mybir.AxisListType


class DSem:
    __slots__ = ("idx", "cnt")

    def __init__(self, idx):
        self.idx = idx
        self.cnt = 0


class Buf:
    __slots__ = ("name", "t", "w", "r", "dsem")

    def __init__(self, name, t=None):
        self.name = name
        self.t = t
        self.w = {}
        self.r = {}
        self.dsem = None


class _Scope:
    def __init__(self, fw):
        self.fw = fw

    def __enter__(self):
        fw = self.fw
        self.prev = (fw.es, fw.scope_dsems)
        self.stack = ExitStack()
        self.stack.__enter__()
        fw.es = self.stack
        fw.scope_dsems = []
        return self

    def __exit__(self, *a):
        fw = self.fw
        fw.barrier()
        fw.dpool.extend(fw.scope_dsems)
        fw.es, fw.scope_dsems = self.prev
        return self.stack.__exit__(*a)


class FW:
    SEM_MAX = 30000

    def __init__(self, nc, es):
        self.nc = nc
        self.es = es
        self.sem_es = es
        self.eng = {"pe": nc.tensor, "act": nc.scalar, "dve": nc.vector, "pool": nc.gpsimd, "sp": nc.sync}
        self.sems = []
        self.cur = {}
        self.own = {e: set() for e in self.eng}
        self.known = {e: {} for e in self.eng}
        self.issued = {}
        self.dpool = []
        self.scope_dsems = []
        self.nwaits = 0
        self.nops = 0
        for e in self.eng:
            self._newsem(e)

    def _alloc_sem(self, name):
        s = self.sem_es.enter_context(self.nc.semaphore(name))
        self.sems.append(s)
        return len(self.sems) - 1

    def _newsem(self, e):
        i = self._alloc_sem(f"s_{e}_{len(self.sems)}")
        self.cur[e] = [i, 0]
        self.own[e].add(i)

    def _get_dsem(self):
        if self.dpool:
            d = self.dpool.pop()
        else:
            d = DSem(self._alloc_sem(f"d_{len(self.sems)}"))
        self.scope_dsems.append(d)
        return d

    def scope(self):
        return _Scope(self)

    def sb(self, name, shape, dtype, dma=False):
        self.nuniq = getattr(self, "nuniq", 0) + 1
        t = self.es.enter_context(self.nc.sbuf_tensor(f"{name}_u{self.nuniq}", shape, dtype))
        b = Buf(name, t)
        if dma:
            b.dsem = self._get_dsem()
        return b

    def ps(self, name, shape, dtype):
        self.nuniq = getattr(self, "nuniq", 0) + 1
        t = self.es.enter_context(self.nc.psum_tensor(f"{name}_u{self.nuniq}", shape, dtype))
        return Buf(name, t)

    def view(self, name, t, dma=False):
        b = Buf(name, t)
        if dma:
            b.dsem = self._get_dsem()
        return b

    def _need(self, reads, writes):
        need = {}
        for b in reads:
            for s, v in b.w.items():
                if need.get(s, 0) < v:
                    need[s] = v
        for b in writes:
            for s, v in b.w.items():
                if need.get(s, 0) < v:
                    need[s] = v
            for s, v in b.r.items():
                if need.get(s, 0) < v:
                    need[s] = v
        return need

    def _waits(self, e, need, skip_own=False):
        k = self.known[e]
        eng = self.eng[e]
        for s, v in need.items():
            if skip_own and s in self.own[e]:
                continue
            if k.get(s, 0) >= v:
                continue
            eng.wait_ge(self.sems[s], v)
            self.nwaits += 1
            k[s] = v

    def _record(self, t, reads, writes):
        s, v = t
        self.issued[s] = v
        for b in reads:
            if b.r.get(s, 0) < v:
                b.r[s] = v
        for b in writes:
            b.w = {s: v}
            b.r = {}

    def op(self, e, fn, reads=(), writes=(), same=None):
        if same is None:
            same = (e != "pe")
        need = self._need(reads, writes)
        self._waits(e, need, skip_own=not same)
        ins = fn(self.eng[e])
        c = self.cur[e]
        c[1] += 1
        ins.then_inc(self.sems[c[0]], 1)
        self._record((c[0], c[1]), reads, writes)
        self.nops += 1
        if c[1] >= self.SEM_MAX:
            self._newsem(e)
        return ins

    def dma(self, q, out, in_, reads=(), writes=(), sem=None, **kw):
        return self.dma_group(q, [(out, in_)], reads, writes, sem, **kw)

    def dma_group(self, q, pairs, reads=(), writes=(), sem=None, **kw):
        d = sem.dsem
        need = self._need(reads, writes)
        if d.cnt:
            v = 16 * d.cnt
            if need.get(d.idx, 0) < v:
                need[d.idx] = v
        self._waits(q, need)
        for (out, in_) in pairs:
            ins = self.eng[q].dma_start(out=out, in_=in_, **kw)
            d.cnt += 1
            ins.then_inc(self.sems[d.idx], 16)
            self.nops += 1
        self._record((d.idx, 16 * d.cnt), reads, writes)

    def barrier(self):
        need = dict(self.issued)
        for e in self.eng:
            self._waits(e, need)


S = 4096
D = 1024
NT = 32
NCH = 8
DFF = 2816
NFC = 22
NCOLP = 6280
C_MV, C_DV, C_WI = 5248, 5760, 6272
BIG = 30000.0
KBIS = 18
EPS = 1e-6


class P:
    pass


def _rms_rstd(fw, ssb, rstd, nh, n):
    (ss_buf, ss_ap), (r_buf, r_ap) = ssb, rstd
    fw.op('dve', lambda e: e.tensor_scalar(out=ss_ap, in0=ss_ap, scalar1=1.0 / n, scalar2=EPS, op0=ALU.mult, op1=ALU.add),
          reads=[ss_buf], writes=[ss_buf])
    fw.op('pool', lambda e: e.tensor_tensor(out=r_ap, in0=ss_ap, in1=nh.t[:, 0:1], op=ALU.pow), reads=[ss_buf, nh], writes=[r_buf])


def build_nc(nlayers=2, phases="ABCDEF", dbg=False):
    nc = bass.Bass("TRN2", target_bir_lowering=False)
    p = P()

    def din(name, shape, dt=F32):
        return nc.dram_tensor(name, shape, dt, kind="ExternalInput").ap()

    def dscr(name, shape, dt):
        return nc.dram_tensor(name, shape, dt, kind=("ExternalOutput" if dbg else "Internal")).ap()

    x = din("x", [S, D])
    cT = din("cT", [128, 8])
    w_ada = din("w_ada", [2, D, 6 * D])
    b_ada = din("b_ada", [2, 6 * D])
    b_ada_col = din("b_ada_col", [2, 128, 48])
    g_pre_mix_col = din("g_pre_mix_col", [2, 128, 8])
    g_pre_ffn_col = din("g_pre_ffn_col", [2, 128, 8])
    g_post_mix = din("g_post_mix", [2, D])
    g_post_ffn = din("g_post_ffn", [2, D])
    g_moba_out = din("g_moba_out", [2, 512])
    g_dsa_out = din("g_dsa_out", [2, 512])
    w_in_p = din("w_in_p", [2, D, NCOLP])
    w_out = din("w_out", [2, D, D])
    w_up_act = din("w_up_act", [2, D, DFF])
    w_up_lin = din("w_up_lin", [2, D, DFF])
    w_conv_col = din("w_conv_col", [2, 128, NFC, 3])
    b_conv_col = din("b_conv_col", [2, 128, NFC])
    w_down = din("w_down", [2, DFF, D])
    ropeC = din("ropeC", [128, S])
    ropeS = din("ropeS", [128, S])
    ident_d = din("ident", [128, 128], BF16)
    tri_d = din("tri", [128, 128], BF16)
    negtri_d = din("negtri", [128, 128])
    onehot_d = din("onehot", [16, S], BF16)
    cbsel_d = din("cbsel", [128, 512])
    cblt_d = din("cblt", [128, 512])
    cbfin_d = din("cbfin", [128, 512])
    cpow_d = din("cpow", [128, KBIS])
    out = nc.dram_tensor("out", [S, D], F32, kind="ExternalOutput").ap()

    scrT = [dscr(n, [512, S], BF16) for n in ("mqT", "mkT", "dqT", "dkT", "qiT")]
    kiT_d = dscr("kiT", [64, S], BF16)
    mva = dscr("mva", [S, 520], BF16)
    dva = dscr("dva", [S, 520], BF16)
    om_d = dscr("om", [S, 512], F32)
    od_d = dscr("od", [S, 512], F32)
    x1_d = dscr("x1", [S, D], F32)
    x2_d = dscr("x2", [S, D], F32)
    gbc_d = dscr("gbc", [2, 2, 128, D], F32)

    with ExitStack() as es:
        fw = FW(nc, es)
        AB = fw.sb("AB", [128, 2, 4, 8], F32)
        WI = fw.sb("WI", [128, NT, 8], F32)
        ksum = fw.sb("ksum", [128, 4, 16], F32)
        ident = fw.sb("identb", [128, 128], BF16, dma=True)
        nh = fw.sb("neghalf", [128, 1], F32)
        fw.dma('sp', ident.t[:], ident_d[:, :], writes=[ident], sem=ident)
        fw.op('dve', lambda e: e.memset(nh.t[:], -0.5), writes=[nh])

        if "A" in phases:
          with fw.scope():
            ct = fw.sb("ct", [128, 8], F32, dma=True)
            sc = fw.sb("sc", [128, 8], F32)
            screp = fw.sb("screp", [128, 8, 128], F32)
            ones1 = fw.sb("ones1", [1, 128], F32)
            wa = [fw.sb(f"wa{i}", [128, 8, 1024], F32, dma=True) for i in range(2)]
            brow = [fw.sb(f"brow{i}", [1, 1024], F32, dma=True) for i in range(2)]
            bcol = fw.sb("bcol", [128, 2, 48], F32, dma=True)
            gcol = fw.sb("gcol", [128, 2, 2, 8], F32, dma=True)
            gpb = [fw.sb(f"gpb{i}", [128, 1024], F32, dma=True) for i in range(2)]
            colps = fw.ps("colps", [128, 512], F32)
            bcps = [fw.ps(f"bcps{i}", [128, 512], F32) for i in range(2)]
            mcol = fw.sb("mcol", [128, 8], F32)
            Gt = [fw.sb(f"Gt{i}", [128, D], F32, dma=True) for i in range(2)]
            fw.dma('sp', ct.t[:], cT[:, :], writes=[ct], sem=ct)
            fw.dma_group('sp', [(bcol.t[:, l, :], b_ada_col[l]) for l in range(2)], writes=[bcol], sem=bcol)
            fw.dma_group('sp', [(gcol.t[:, l, 0, :], g_pre_mix_col[l]) for l in range(2)]
                         + [(gcol.t[:, l, 1, :], g_pre_ffn_col[l]) for l in range(2)], writes=[gcol], sem=gcol)
            fw.op('act', lambda e: e.activation(out=sc.t[:], in_=ct.t[:], func=AF.Silu), reads=[ct], writes=[sc])
            fw.op('dve', lambda e: e.tensor_copy(out=screp.t[:], in_=sc.t[:, :].unsqueeze(2).to_broadcast([128, 8, 128])),
                  reads=[sc], writes=[screp])
            fw.op('dve', lambda e: e.memset(ones1.t[:], 1.0), writes=[ones1])
            pi = 0
            for l in range(nlayers):
                wv = w_ada[l].rearrange("(k p) n -> p k n", p=128)
                for piece in range(6):
                    w = wa[pi % 2]
                    pi += 1
                    fw.dma_group('sp', [(w.t[:, k, :], wv[:, k, piece * 1024:(piece + 1) * 1024]) for k in range(8)],
                                 writes=[w], sem=w)
                    if piece in (2, 5):
                        j = 0 if piece == 2 else 1
                        br = brow[j]
                        gp = gpb[j]
                        fw.dma('sp', br.t[:], b_ada[l:l + 1, piece * 1024:(piece + 1) * 1024], writes=[br], sem=br)
                        fw.dma('sp', gp.t[:], (g_post_mix if j == 0 else g_post_ffn)[l, :].partition_broadcast(128),
                               writes=[gp], sem=gp)
                        for nhf in range(2):
                            ps = bcps[nhf]
                            for k in range(8):
                                fw.op('pe', lambda e: e.matmul(ps.t[:], lhsT=screp.t[:, k, :], rhs=w.t[:, k, nhf * 512:(nhf + 1) * 512],
                                                               start=(k == 0), stop=False), reads=[screp, w], writes=[ps])
                            fw.op('pe', lambda e: e.matmul(ps.t[:], lhsT=ones1.t[0:1, :], rhs=br.t[0:1, nhf * 512:(nhf + 1) * 512],
                                                           start=False, stop=True), reads=[ones1, br], writes=[ps])
                            G = Gt[j]
                            fw.op('dve', lambda e: e.tensor_tensor(out=G.t[:, nhf * 512:(nhf + 1) * 512], in0=ps.t[:],
                                                                   in1=gp.t[:, nhf * 512:(nhf + 1) * 512], op=ALU.mult),
                                  reads=[ps, gp], writes=[G])
                        fw.dma('sp', gbc_d[l, j], Gt[j].t[:], reads=[Gt[j]], sem=Gt[j])
                    else:
                        for jj in range(8):
                            for k in range(8):
                                fw.op('pe', lambda e: e.matmul(colps.t[:, jj:jj + 1], lhsT=w.t[:, k, jj * 128:(jj + 1) * 128],
                                                               rhs=sc.t[:, k:k + 1], start=(k == 0), stop=(k == 7)),
                                      reads=[sc, w], writes=[colps])
                        fw.op('dve', lambda e: e.tensor_tensor(out=mcol.t[:], in0=colps.t[:, 0:8],
                                                               in1=bcol.t[:, l, piece * 8:(piece + 1) * 8], op=ALU.add),
                              reads=[colps, bcol], writes=[mcol])
                        if piece in (0, 3):
                            slot = 1 if piece == 0 else 3
                            fw.op('dve', lambda e: e.tensor_copy(out=AB.t[:, l, slot, :], in_=mcol.t[:]), reads=[mcol], writes=[AB])
                        else:
                            slot = 0 if piece == 1 else 2
                            gi = 0 if piece == 1 else 1
                            fw.op('dve', lambda e: e.scalar_tensor_tensor(out=AB.t[:, l, slot, :], in0=mcol.t[:], scalar=1.0,
                                                                          in1=gcol.t[:, l, gi, :], op0=ALU.add, op1=ALU.mult),
                                  reads=[mcol, gcol], writes=[AB])

        def norm_transpose(xb, l, slot, tp, hbuf, hview, sq, ss, rstd, xn):
            fw.op('dve', lambda e: e.memset(ss.t[:], 0.0), writes=[ss])
            fw.op('act', lambda e: e.activation(out=sq.t[:], in_=xb.t[:], func=AF.Square, accum_out=ss.t[:, 0:1]),
                  reads=[xb], writes=[sq, ss])
            _rms_rstd(fw, (ss, ss.t[:, 0:1]), (rstd, rstd.t[:, 0:1]), nh, D)
            fw.op('dve', lambda e: e.tensor_scalar(out=xn.t[:], in0=xb.t[:], scalar1=rstd.t[:, 0:1], scalar2=None, op0=ALU.mult),
                  reads=[xb, rstd], writes=[xn])
            for k in range(8):
                fw.op('pe', lambda e: e.transpose(out=tp.t[:, k, :], in_=xn.t[:, k * 128:(k + 1) * 128], identity=ident.t[:]),
                      reads=[xn, ident], writes=[tp])
            for k in range(8):
                fw.op('act', lambda e: e.activation(out=hview(k), in_=tp.t[:, k, :], func=AF.Identity,
                                                    scale=AB.t[:, l, slot, k:k + 1], bias=AB.t[:, l, slot + 1, k:k + 1]),
                      reads=[tp, AB], writes=[hbuf])

        for l in range(nlayers):
            xin = x if l == 0 else x2_d
            xout = out if l == nlayers - 1 else x2_d
            if "B" in phases:
              with fw.scope():
                W = fw.sb("winb", [128, 8, NCOLP], BF16, dma=True)
                fw.dma_group('pool', [(W.t[:, k, :], w_in_p[l, k * 128:(k + 1) * 128, :]) for k in range(8)], writes=[W], sem=W)
                rC = fw.sb("rC", [128, S], F32, dma=True)
                rS = fw.sb("rS", [128, S], F32, dma=True)
                fw.dma('sp', rC.t[:], ropeC[:, :], writes=[rC], sem=rC)
                fw.dma('sp', rS.t[:], ropeS[:, :], writes=[rS], sem=rS)
                xt = [fw.sb(f"xt{i}", [128, D], F32, dma=True) for i in range(2)]
                sq = fw.sb("sq", [128, D], BF16)
                ss = [fw.sb(f"ss{i}", [128, 1], F32) for i in range(4)]
                rstd = [fw.sb(f"rstd{i}", [128, 1], F32) for i in range(4)]
                xn = [fw.sb(f"xn{i}", [128, D], BF16) for i in range(4)]
                tps = [fw.ps(f"tp{i}", [128, 8, 128], BF16) for i in range(2)]
                hT = [fw.sb(f"hT{i}", [128, 8, 512], BF16) for i in range(2)]
                pm = [fw.ps(f"pm{i}", [128, 512], F32) for i in range(2)]
                pp = [fw.ps(f"pp{i}", [128, 512], F32) for i in range(2)]
                pv = [fw.ps(f"pv{i}", [128, 512], F32) for i in range(2)]
                t1 = [fw.sb(f"t1_{i}", [128, 512], F32) for i in range(2)]
                t2 = [fw.sb(f"t2_{i}", [128, 512], F32) for i in range(2)]
                t3 = [fw.sb(f"t3_{i}", [128, 512], F32) for i in range(2)]
                ob = [fw.sb(f"ob{i}", [128, 512], BF16, dma=True) for i in range(3)]
                vt = [fw.sb(f"vt{i}", [128, 8, 65], BF16, dma=True) for i in range(2)]
                for v in vt:
                    fw.op('dve', lambda e: e.memset(v.t[:], 1.0), writes=[v])
                fj = 0
                oj = 0
                vj = 0
                def normA(c):
                    for i in range(4):
                        tt = c * 4 + i
                        xb, s_, r_, n_ = xt[tt % 2], ss[i], rstd[i], xn[i]
                        fw.dma('sp', xb.t[:], xin[tt * 128:(tt + 1) * 128, :], writes=[xb], sem=xb)
                        fw.op('dve', lambda e: e.memset(s_.t[:], 0.0), writes=[s_])
                        fw.op('act', lambda e: e.activation(out=sq.t[:], in_=xb.t[:], func=AF.Square, accum_out=s_.t[:, 0:1]),
                              reads=[xb], writes=[sq, s_])
                        _rms_rstd(fw, (s_, s_.t[:, 0:1]), (r_, r_.t[:, 0:1]), nh, D)
                        fw.op('dve', lambda e: e.tensor_scalar(out=n_.t[:], in0=xb.t[:], scalar1=r_.t[:, 0:1], scalar2=None, op0=ALU.mult),
                              reads=[xb, r_], writes=[n_])

                def normB(c):
                    hh = hT[c % 2]
                    for i in range(4):
                        tt = c * 4 + i
                        tp, n_ = tps[tt % 2], xn[i]
                        for k in range(8):
                            fw.op('pe', lambda e: e.transpose(out=tp.t[:, k, :], in_=n_.t[:, k * 128:(k + 1) * 128], identity=ident.t[:]),
                                  reads=[n_, ident], writes=[tp])
                        for k in range(8):
                            fw.op('act', lambda e: e.activation(out=hh.t[:, k, i * 128:(i + 1) * 128], in_=tp.t[:, k, :], func=AF.Identity,
                                                                scale=AB.t[:, l, 0, k:k + 1], bias=AB.t[:, l, 1, k:k + 1]),
                                  reads=[tp, AB], writes=[hh])

                normA(0)
                normB(0)
                for c in range(NCH):
                    h = hT[c % 2]
                    cs = slice(c * 512, (c + 1) * 512)
                    if c + 1 < NCH:
                        normA(c + 1)
                    tiles = [(g, ft, 128) for g in range(5) for ft in range(4)] + [(5, 0, 64)]
                    for ti_, (g, ft, M) in enumerate(tiles):
                        if ti_ == 10 and c + 1 < NCH:
                            normB(c + 1)
                        cm = g * 1024 + ft * 128 if g < 5 else 5120
                        cp = cm + 512 if g < 5 else 5184
                        pmain, ppart = pm[fj % 2], pp[fj % 2]
                        a1, a2, a3 = t1[fj % 2], t2[fj % 2], t3[fj % 2]
                        fj += 1
                        for k in range(8):
                            fw.op('pe', lambda e: e.matmul(pmain.t[0:M, :], lhsT=W.t[:, k, cm:cm + M], rhs=h.t[:, k, :],
                                                           start=(k == 0), stop=(k == 7)), reads=[W, h], writes=[pmain])
                        for k in range(8):
                            fw.op('pe', lambda e: e.matmul(ppart.t[0:M, :], lhsT=W.t[:, k, cp:cp + M], rhs=h.t[:, k, :],
                                                           start=(k == 0), stop=(k == 7)), reads=[W, h], writes=[ppart])
                        fw.op('dve', lambda e: e.tensor_tensor(out=a1.t[0:M, :], in0=ppart.t[0:M, :], in1=rS.t[0:M, cs], op=ALU.mult),
                              reads=[ppart, rS], writes=[a1])
                        fw.op('dve', lambda e: e.tensor_tensor(out=a2.t[0:M, :], in0=pmain.t[0:M, :], in1=rC.t[0:M, cs], op=ALU.mult),
                              reads=[pmain, rC], writes=[a2])
                        o = ob[oj % 3]
                        oj += 1
                        if g == 1:
                            fw.op('pool', lambda e: e.tensor_tensor(out=a3.t[:], in0=a1.t[:], in1=a2.t[:], op=ALU.add),
                                  reads=[a1, a2], writes=[a3])
                            fw.op('act', lambda e: e.copy(out=o.t[:], in_=a3.t[:]), reads=[a3], writes=[o])
                            fw.op('dve', lambda e: e.reduce_sum(out=ksum.t[:, ft, 2 * c:2 * c + 2],
                                                                in_=a3.t[:, :].rearrange("p (b s) -> p b s", b=2), axis=AX.X),
                                  reads=[a3], writes=[ksum])
                        else:
                            fw.op('pool', lambda e: e.tensor_tensor(out=o.t[0:M, :], in0=a1.t[0:M, :], in1=a2.t[0:M, :], op=ALU.add),
                                  reads=[a1, a2], writes=[o])
                        dst = scrT[g][ft * 128:(ft + 1) * 128, cs] if g < 5 else kiT_d[:, cs]
                        fw.dma('sp', dst, o.t[0:M, :], reads=[o], sem=o)
                    for i in range(4):
                        tt = c * 4 + i
                        for (dst, c0) in ((mva, C_MV), (dva, C_DV)):
                            ps = pv[vj % 2]
                            v = vt[vj % 2]
                            vj += 1
                            for k in range(8):
                                fw.op('pe', lambda e: e.matmul(ps.t[:], lhsT=h.t[:, k, i * 128:(i + 1) * 128], rhs=W.t[:, k, c0:c0 + 512],
                                                               start=(k == 0), stop=(k == 7)), reads=[W, h], writes=[ps])
                            fw.op('act', lambda e: e.copy(out=v.t[:, :, 0:64], in_=ps.t[:, :].rearrange("p (h d) -> p h d", h=8)),
                                  reads=[ps], writes=[v])
                            fw.dma('sp', dst[tt * 128:(tt + 1) * 128, :], v.t[:, :, :].rearrange("p h d -> p (h d)"), reads=[v], sem=v)
                        ps = pv[vj % 2]
                        vj += 1
                        for k in range(8):
                            fw.op('pe', lambda e: e.matmul(ps.t[:, 0:8], lhsT=h.t[:, k, i * 128:(i + 1) * 128], rhs=W.t[:, k, C_WI:C_WI + 8],
                                                           start=(k == 0), stop=(k == 7)), reads=[W, h], writes=[ps])
                        fw.op('act', lambda e: e.copy(out=WI.t[:, tt, :], in_=ps.t[:, 0:8]), reads=[ps], writes=[WI])
            if "C" in phases:
              with fw.scope():
                V = fw.sb("mV", [128, NT, 520], BF16, dma=True)
                fw.dma('sp', V.t[:], mva.rearrange("(n p) c -> p n c", p=128), writes=[V], sem=V)
                Qt = [fw.sb(f"mQ{i}", [80, S], BF16) for i in range(2)]
                Kt = [fw.sb(f"mK{i}", [80, S], BF16) for i in range(2)]
                Qm = [fw.view(f"mQm{i}", Qt[i].t, dma=True) for i in range(2)]
                Qb = [fw.view(f"mQb{i}", Qt[i].t) for i in range(2)]
                Km = [fw.view(f"mKm{i}", Kt[i].t, dma=True) for i in range(2)]
                Kc = [fw.view(f"mKc{i}", Kt[i].t, dma=True) for i in range(2)]
                for i in range(2):
                    fw.dma('sp', Kt[i].t[64:80, :], onehot_d[:, :], writes=[Kc[i]], sem=Kc[i])
                kmb = fw.sb("kmb", [64, 8, 16], BF16)
                for h in range(8):
                    e_, ft = h % 2, h // 2
                    fw.op('act', lambda e: e.mul(out=kmb.t[0:64, h, :], in_=ksum.t[e_ * 64:(e_ + 1) * 64, ft, :], mul=1.0 / 256.0),
                          reads=[ksum], writes=[kmb])
                tri = fw.sb("tri", [128, 128], BF16, dma=True)
                fw.dma('sp', tri.t[:], tri_d[:, :], writes=[tri], sem=tri)
                cbs = fw.sb("cbs", [128, 3, 512], F32, dma=True)
                fw.dma_group('sp', [(cbs.t[:, 0, :], cbsel_d[:, :]), (cbs.t[:, 1, :], cblt_d[:, :]), (cbs.t[:, 2, :], cbfin_d[:, :])],
                             writes=[cbs], sem=cbs)
                gps = fw.ps("gps", [128, 512], F32)
                tb = fw.ps("tb", [128, 1024], BF16)
                sps = [fw.ps(f"sp{i}", [128, 512], F32) for i in range(3)]
                ops_ = [fw.ps(f"op{i}", [128, 512], F32) for i in range(2)]
                pt = [fw.sb(f"pt{i}", [128, 512], BF16) for i in range(3)]
                gm = fw.sb("gm", [128, 512], F32)
                ee = fw.sb("ee", [128, 512], F32)
                g2 = fw.sb("g2", [128, 512], F32)
                g3 = fw.sb("g3", [128, 512], F32)
                mx = fw.sb("mx", [128, 3, 32], F32)
                bt = fw.sb("bt", [128, 512], BF16)
                rec = [fw.sb(f"rec{i}", [128, 4, 1], F32) for i in range(2)]
                osb = [fw.sb(f"osb{i}", [128, 4, 64], F32, dma=True) for i in range(2)]
                v3 = lambda t: t[:, :].rearrange("p (a n) -> p a n", n=16)
                bc3 = lambda j: mx.t[:, j, :].unsqueeze(2).to_broadcast([128, 32, 16])
                si = 0
                oi = 0
                def gateA(h):
                    b = h % 2
                    fw.dma('sp', Qt[b].t[0:64, :], scrT[0][h * 64:(h + 1) * 64, :], writes=[Qm[b]], sem=Qm[b])
                    fw.dma('sp', Kt[b].t[0:64, :], scrT[1][h * 64:(h + 1) * 64, :], writes=[Km[b]], sem=Km[b])
                    for tt in range(NT):
                        fw.op('pe', lambda e: e.matmul(gps.t[:, tt * 16:(tt + 1) * 16], lhsT=Qt[b].t[0:64, tt * 128:(tt + 1) * 128],
                                                       rhs=kmb.t[0:64, h, :], start=True, stop=True), reads=[Qm[b], kmb], writes=[gps])
                    fw.op('dve', lambda e: e.tensor_tensor(out=gm.t[:], in0=gps.t[:], in1=cbs.t[:, 0, :], op=ALU.add),
                          reads=[gps, cbs], writes=[gm])
                    fw.op('dve', lambda e: e.reduce_max(out=mx.t[:, 0, :], in_=v3(gm.t), axis=AX.X), reads=[gm], writes=[mx])
                    fw.op('dve', lambda e: e.tensor_tensor(out=v3(ee.t), in0=v3(gm.t), in1=bc3(0), op=ALU.is_equal),
                          reads=[gm, mx], writes=[ee])
                    fw.op('dve', lambda e: e.scalar_tensor_tensor(out=g2.t[:], in0=ee.t[:], scalar=-1e9, in1=gm.t[:], op0=ALU.mult, op1=ALU.add),
                          reads=[ee, gm], writes=[g2])
                    fw.op('dve', lambda e: e.reduce_max(out=mx.t[:, 1, :], in_=v3(g2.t), axis=AX.X), reads=[g2], writes=[mx])
                    fw.op('dve', lambda e: e.tensor_tensor(out=v3(ee.t), in0=v3(g2.t), in1=bc3(1), op=ALU.is_equal),
                          reads=[g2, mx], writes=[ee])
                    fw.op('dve', lambda e: e.scalar_tensor_tensor(out=g3.t[:], in0=ee.t[:], scalar=-1e9, in1=g2.t[:], op0=ALU.mult, op1=ALU.add),
                          reads=[ee, g2], writes=[g3])
                    fw.op('dve', lambda e: e.reduce_max(out=mx.t[:, 2, :], in_=v3(g3.t), axis=AX.X), reads=[g3], writes=[mx])
                    fw.op('dve', lambda e: e.tensor_tensor(out=v3(ee.t), in0=v3(gm.t), in1=bc3(2), op=ALU.is_lt),
                          reads=[gm, mx], writes=[ee])
                    fw.op('dve', lambda e: e.tensor_tensor(out=g2.t[:], in0=ee.t[:], in1=cbs.t[:, 1, :], op=ALU.mult),
                          reads=[ee, cbs], writes=[g2])
                    fw.op('dve', lambda e: e.tensor_tensor(out=bt.t[:], in0=g2.t[:], in1=cbs.t[:, 2, :], op=ALU.add),
                          reads=[g2, cbs], writes=[bt])

                def gateB(h):
                    b = h % 2
                    for tt in range(NT):
                        fw.op('pe', lambda e: e.transpose(out=tb.t[0:16, (tt % 4) * 128:(tt % 4 + 1) * 128], in_=bt.t[:, tt * 16:(tt + 1) * 16],
                                                          identity=ident.t[:]), reads=[bt, ident], writes=[tb])
                        if tt % 4 == 3:
                            q4 = tt // 4
                            fw.op('act', lambda e: e.copy(out=Qt[b].t[64:80, q4 * 512:(q4 + 1) * 512], in_=tb.t[0:16, 0:512]),
                                  reads=[tb], writes=[Qb[b]])

                def attn(h):
                    nonlocal si, oi
                    b = h % 2
                    for qc in range(NCH):
                        O = ops_[oi % 2]
                        rc = rec[oi % 2]
                        ob_ = osb[oi % 2]
                        oi += 1
                        nst = 4 * qc + 4

                        def qk(st):
                            nonlocal si
                            sp_ = sps[si % 3]
                            Pt = pt[si % 3]
                            si += 1
                            fw.op('pe', lambda e: e.matmul(sp_.t[:], lhsT=Kt[b].t[0:80, st * 128:(st + 1) * 128],
                                                           rhs=Qt[b].t[0:80, qc * 512:(qc + 1) * 512], start=True, stop=True),
                                  reads=[Km[b], Kc[b], Qm[b], Qb[b]], writes=[sp_])
                            fw.op('act', lambda e: e.activation(out=Pt.t[:], in_=sp_.t[:], func=AF.Exp, scale=0.125),
                                  reads=[sp_], writes=[Pt])
                            j = st - 4 * qc
                            if j >= 0:
                                fw.op('pool', lambda e: e.tensor_tensor(out=Pt.t[:, j * 128:(j + 1) * 128], in0=Pt.t[:, j * 128:(j + 1) * 128],
                                                                        in1=tri.t[:], op=ALU.mult), reads=[Pt, tri], writes=[Pt])
                            return Pt, j

                        pend = qk(0)
                        first = True
                        for st in range(nst):
                            nxt = qk(st + 1) if st + 1 < nst else None
                            Pt, j = pend
                            for i in range(max(j, 0), 4):
                                fw.op('pe', lambda e: e.matmul(O.t[:, i * 65:(i + 1) * 65], lhsT=Pt.t[:, i * 128:(i + 1) * 128],
                                                               rhs=V.t[:, st, h * 65:(h + 1) * 65], start=first, stop=(st == 4 * qc + i)),
                                      reads=[Pt, V], writes=[O])
                                first = False
                            pend = nxt
                        Ov = O.t[:, 0:260].rearrange("p (i d) -> p i d", d=65)
                        fw.op('dve', lambda e: e.reciprocal(out=rc.t[:], in_=Ov[:, :, 64:65]), reads=[O], writes=[rc])
                        fw.op('dve', lambda e: e.tensor_tensor(out=ob_.t[:], in0=Ov[:, :, 0:64], in1=rc.t[:, :, :].to_broadcast([128, 4, 64]),
                                                               op=ALU.mult), reads=[O, rc], writes=[ob_])
                        fw.dma('sp', om_d[qc * 512:(qc + 1) * 512, h * 64:(h + 1) * 64].rearrange("(i p) d -> p i d", p=128), ob_.t[:],
                               reads=[ob_], sem=ob_)

                gateA(0)
                gateB(0)
                for h in range(8):
                    if h + 1 < 8:
                        gateA(h + 1)
                    attn(h)
                    if h + 1 < 8:
                        gateB(h + 1)

            if "D" in phases:
              with fw.scope():
                V = fw.sb("dV", [128, NT, 520], BF16, dma=True)
                fw.dma('sp', V.t[:], dva.rearrange("(n p) c -> p n c", p=128), writes=[V], sem=V)
                K2 = fw.sb("K2", [128, 4, S], BF16, dma=True)
                fw.dma('sp', K2.t[:], scrT[3].rearrange("(hp two d) t -> (two d) hp t", two=2, d=64), writes=[K2], sem=K2)
                ki = fw.sb("kiT", [64, S], BF16, dma=True)
                fw.dma('sp', ki.t[:], kiT_d[:, :], writes=[ki], sem=ki)
                negtri = fw.sb("negtri", [128, 128], F32, dma=True)
                fw.dma('sp', negtri.t[:], negtri_d[:, :], writes=[negtri], sem=negtri)
                cpow = fw.sb("cpow", [128, KBIS], F32, dma=True)
                fw.dma('sp', cpow.t[:], cpow_d[:, :], writes=[cpow], sem=cpow)
                QI = [fw.sb(f"QI{i}", [64, 8, 128], BF16, dma=True) for i in range(2)]
                Q2 = [fw.sb(f"Q2{i}", [128, 4, 128], BF16, dma=True) for i in range(3)]
                i4 = fw.sb("i4", [128, 4, 128], BF16)
                fw.op('dve', lambda e: e.tensor_copy(out=i4.t[:], in_=ident.t[:, :].unsqueeze(1).to_broadcast([128, 4, 128])), reads=[ident], writes=[i4])
                score = [fw.sb(f"score{i}", [128, S], F32) for i in range(2)]
                junk = fw.sb("junk", [128, S], BF16)
                mb = [fw.sb(f"mb{i}", [128, S], BF16) for i in range(2)]
                rlb = [fw.sb(f"rl{i}", [128, 512], BF16) for i in range(3)]
                dg = [fw.sb(f"dg{i}", [128, 8, 128], BF16) for i in range(2)]
                scps = fw.ps("scps", [128, 512], F32)
                lps = [fw.ps(f"lps{i}", [128, 512], F32) for i in range(2)]
                sp3 = [fw.ps(f"dsp{i}", [128, 512], F32) for i in range(3)]
                opsd = [fw.ps(f"dop{e_}", [128, 512], F32) for e_ in range(2)]
                ptd = [fw.sb(f"dpt{i}", [128, 2, 512], BF16) for i in range(2)]
                sm = [fw.sb(f"sm{i}", [128, 8], F32) for i in range(2)]
                dkt = [fw.sb(f"dk{i}", [128, KBIS], F32) for i in range(2)]
                recd = [fw.sb(f"drec{e_}", [128, 4, 1], F32) for e_ in range(2)]
                osbd = [fw.sb(f"dosb{i}", [128, 4, 2, 64], F32, dma=True) for i in range(2)]
                qiv = scrT[4].rearrange("(h d) t -> d h t", d=64)
                q2v = scrT[2].rearrange("(hp two d) t -> (two d) hp t", two=2, d=64)
                cnt_ = {"li": 0, "ti": 0, "ai": 0}

                def indexer(qt):
                    L = (qt + 1) * 128
                    sc_ = score[qt % 2]
                    qi_ = QI[qt % 2]
                    fw.dma('sp', qi_.t[:], qiv[:, :, qt * 128:(qt + 1) * 128], writes=[qi_], sem=qi_)
                    fw.dma('sp', Q2[qt % 3].t[:], q2v[:, :, qt * 128:(qt + 1) * 128], writes=[Q2[qt % 3]], sem=Q2[qt % 3])
                    dg_ = dg[qt % 2]
                    fw.op('dve', lambda e: e.tensor_tensor(out=dg_.t[:], in0=ident.t[:, :].unsqueeze(1).to_broadcast([128, 8, 128]),
                                                           in1=WI.t[:, qt, :].unsqueeze(2).to_broadcast([128, 8, 128]), op=ALU.mult),
                          reads=[ident, WI], writes=[dg_])
                    nch = (L + 511) // 512
                    for ch in range(nch):
                        ncol = min(512, L - ch * 512)
                        cs = slice(ch * 512, ch * 512 + ncol)

                        def logit(h):
                            lp = lps[cnt_["li"] % 2]
                            cnt_["li"] += 1
                            R = rlb[cnt_["ti"] % 3]
                            cnt_["ti"] += 1
                            fw.op('pe', lambda e: e.matmul(lp.t[:, 0:ncol], lhsT=qi_.t[0:64, h, :], rhs=ki.t[0:64, cs], start=True, stop=True),
                                  reads=[qi_, ki], writes=[lp])
                            fw.op('act', lambda e: e.activation(out=R.t[:, 0:ncol], in_=lp.t[:, 0:ncol], func=AF.Relu), reads=[lp], writes=[R])
                            return R

                        pend = logit(0)
                        for h in range(8):
                            nxt = logit(h + 1) if h + 1 < 8 else None
                            R = pend
                            fw.op('pe', lambda e: e.matmul(scps.t[:, 0:ncol], lhsT=dg_.t[:, h, :], rhs=R.t[:, 0:ncol], start=(h == 0), stop=(h == 7)),
                                  reads=[dg_, R], writes=[scps])
                            pend = nxt
                        fw.op('act', lambda e: e.copy(out=sc_.t[:, cs], in_=scps.t[:, 0:ncol]), reads=[scps], writes=[sc_])

                def select(qt):
                    L = (qt + 1) * 128
                    sc_ = score[qt % 2]
                    s_ = sm[qt % 2]
                    dk_ = dkt[qt % 2]
                    mb_ = mb[qt % 2]
                    if qt >= 2:
                        fw.op('dve', lambda e: e.reduce_max(out=s_.t[:, 0:1], in_=sc_.t[:, 0:L], axis=AX.X), reads=[sc_], writes=[s_])
                        fw.op('dve', lambda e: e.tensor_reduce(out=s_.t[:, 1:2], in_=sc_.t[:, 0:L], axis=AX.X, op=ALU.min), reads=[sc_], writes=[s_])
                        fw.op('dve', lambda e: e.scalar_tensor_tensor(out=s_.t[:, 2:3], in0=s_.t[:, 1:2], scalar=-1.0, in1=s_.t[:, 0:1],
                                                                      op0=ALU.mult, op1=ALU.max), reads=[s_], writes=[s_])
                    fw.op('pool', lambda e: e.tensor_tensor(out=sc_.t[:, qt * 128:L], in0=sc_.t[:, qt * 128:L], in1=negtri.t[:], op=ALU.add),
                          reads=[sc_, negtri], writes=[sc_])
                    if qt >= 2:
                        fw.op('dve', lambda e: e.tensor_scalar(out=dk_.t[:], in0=cpow.t[:], scalar1=s_.t[:, 2:3], scalar2=None, op0=ALU.mult),
                              reads=[cpow, s_], writes=[dk_])
                        fw.op('dve', lambda e: e.memset(s_.t[:, 3:4], 0.0), writes=[s_])
                        for k in range(KBIS):
                            fw.op('dve', lambda e: e.tensor_scalar(out=junk.t[:, 0:L], in0=sc_.t[:, 0:L], scalar1=s_.t[:, 3:4], scalar2=0.0,
                                                                   op0=ALU.is_ge, op1=ALU.add, accum_out=s_.t[:, 4:5]),
                                  reads=[sc_, s_], writes=[junk, s_])
                            last = (k == KBIS - 1)
                            fw.op('dve', lambda e: e.tensor_scalar(out=s_.t[:, 5:6], in0=s_.t[:, 4:5], scalar1=255.5,
                                                                   scalar2=(1.0 if last else 0.5), op0=ALU.is_ge, op1=ALU.subtract),
                                  reads=[s_], writes=[s_])
                            dst = s_.t[:, 6:7] if last else s_.t[:, 3:4]
                            fw.op('dve', lambda e: e.scalar_tensor_tensor(out=dst, in0=s_.t[:, 5:6], scalar=dk_.t[:, k:k + 1], in1=s_.t[:, 3:4],
                                                                          op0=ALU.mult, op1=ALU.add), reads=[s_, dk_], writes=[s_])
                        fw.op('dve', lambda e: e.tensor_scalar(out=mb_.t[:, 0:L], in0=sc_.t[:, 0:L], scalar1=s_.t[:, 6:7], scalar2=-BIG,
                                                               op0=ALU.is_lt, op1=ALU.mult), reads=[sc_, s_], writes=[mb_])
                    else:
                        fw.op('dve', lambda e: e.tensor_scalar(out=mb_.t[:, 0:L], in0=sc_.t[:, 0:L], scalar1=-1e29, scalar2=-BIG,
                                                               op0=ALU.is_lt, op1=ALU.mult), reads=[sc_], writes=[mb_])

                def attend(qt):
                    mb_ = mb[qt % 2]
                    q2_ = Q2[qt % 3]
                    def qk(st):
                        ai_ = cnt_["ai"]
                        PT = ptd[ai_ % 2]
                        cnt_["ai"] += 1
                        for e_ in range(2):
                            bank = sp3[(2 * ai_ + e_) % 3]
                            for hp in range(4):
                                fw.op('pe', lambda e: e.matmul(bank.t[:, hp * 128:(hp + 1) * 128],
                                                               lhsT=K2.t[64 * e_:64 * e_ + 64, hp, st * 128:(st + 1) * 128],
                                                               rhs=q2_.t[64 * e_:64 * e_ + 64, hp, :], start=(hp == 0), stop=False),
                                      reads=[K2, q2_], writes=[bank])
                            fw.op('pe', lambda e: e.matmul(bank.t[:], lhsT=mb_.t[:, st * 128:(st + 1) * 128],
                                                           rhs=i4.t[:, :, :].rearrange("p a t -> p (a t)"), start=False, stop=True),
                                  reads=[mb_, i4], writes=[bank])
                            fw.op('act', lambda e: e.activation(out=PT.t[:, e_, :], in_=bank.t[:], func=AF.Exp, scale=0.125),
                                  reads=[bank], writes=[PT])
                        return PT

                    pend = qk(0)
                    for st in range(qt + 1):
                        nxt = qk(st + 1) if st + 1 <= qt else None
                        PT = pend
                        for e_ in range(2):
                            for hp in range(4):
                                hh = 2 * hp + e_
                                fw.op('pe', lambda e: e.matmul(opsd[e_].t[:, hp * 65:(hp + 1) * 65], lhsT=PT.t[:, e_, hp * 128:(hp + 1) * 128],
                                                               rhs=V.t[:, st, hh * 65:(hh + 1) * 65], start=(st == 0 and hp == 0), stop=(st == qt)),
                                      reads=[PT, V], writes=[opsd[e_]])
                        pend = nxt
                    ob_ = osbd[qt % 2]
                    for e_ in range(2):
                        Ov = opsd[e_].t[:, 0:260].rearrange("p (i d) -> p i d", d=65)
                        fw.op('dve', lambda e: e.reciprocal(out=recd[e_].t[:], in_=Ov[:, :, 64:65]), reads=[opsd[e_]], writes=[recd[e_]])
                        fw.op('dve', lambda e: e.tensor_tensor(out=ob_.t[:, :, e_, :], in0=Ov[:, :, 0:64],
                                                               in1=recd[e_].t[:, :, :].to_broadcast([128, 4, 64]), op=ALU.mult),
                              reads=[opsd[e_], recd[e_]], writes=[ob_])
                    fw.dma('sp', od_d[qt * 128:(qt + 1) * 128, :], ob_.t[:, :, :, :].rearrange("p a e d -> p (a e d)"), reads=[ob_], sem=ob_)

                indexer(0)
                for qt in range(NT):
                    if qt + 1 < NT:
                        indexer(qt + 1)
                    select(qt)
                    if qt >= 1:
                        attend(qt - 1)
                attend(NT - 1)

            if "E" in phases:
              with fw.scope():
                Wo = fw.sb("Wo", [128, 8, D], BF16, dma=True)
                fw.dma_group('pool', [(Wo.t[:, k, :], w_out[l, k * 128:(k + 1) * 128, :]) for k in range(8)], writes=[Wo], sem=Wo)
                gmo = fw.sb("gmo", [128, D], F32, dma=True)
                fw.dma_group('sp', [(gmo.t[:, 0:512], g_moba_out[l, :].partition_broadcast(128)),
                                    (gmo.t[:, 512:1024], g_dsa_out[l, :].partition_broadcast(128))], writes=[gmo], sem=gmo)
                G = fw.sb("Gm", [128, D], F32, dma=True)
                fw.dma('sp', G.t[:], gbc_d[l, 0], writes=[G], sem=G)
                ot = [fw.sb(f"ot{i}", [128, D], F32, dma=True) for i in range(2)]
                xt = [fw.sb(f"ext{i}", [128, D], F32, dma=True) for i in range(3)]
                sq = fw.sb("esq", [128, D], BF16)
                ss = [fw.sb(f"ess{i}", [128, 4], F32) for i in range(3)]
                rs = [fw.sb(f"ers{i}", [128, 4], F32) for i in range(3)]
                on = [fw.sb(f"on{i}", [128, D], BF16) for i in range(2)]
                tps = [fw.ps(f"etp{i}", [128, 8, 128], BF16) for i in range(2)]
                oT = [fw.sb(f"oT{i}", [128, 8, 128], BF16) for i in range(3)]
                yps = [[fw.ps(f"yps{i}{j}", [128, 512], F32) for j in range(2)] for i in range(2)]
                ysb = [fw.sb(f"ysb{i}", [128, D], F32, dma=True) for i in range(2)]

                def s1a(tt):
                    o_, x_, s_, r_, n_ = ot[tt % 2], xt[tt % 3], ss[tt % 3], rs[tt % 3], on[tt % 2]
                    rows = slice(tt * 128, (tt + 1) * 128)
                    fw.dma_group('sp', [(o_.t[:, 0:512], om_d[rows, :]), (o_.t[:, 512:1024], od_d[rows, :])], writes=[o_], sem=o_)
                    fw.dma('sp', x_.t[:], xin[rows, :], writes=[x_], sem=x_)
                    fw.op('dve', lambda e: e.memset(s_.t[:], 0.0), writes=[s_])
                    for j in range(2):
                        fw.op('act', lambda e: e.activation(out=sq.t[:, 0:512], in_=o_.t[:, j * 512:(j + 1) * 512], func=AF.Square,
                                                            accum_out=s_.t[:, j:j + 1]), reads=[o_], writes=[sq, s_])
                    fw.op('dve', lambda e: e.tensor_scalar(out=s_.t[:, 0:2], in0=s_.t[:, 0:2], scalar1=1.0 / 512, scalar2=EPS, op0=ALU.mult, op1=ALU.add),
                          reads=[s_], writes=[s_])
                    fw.op('pool', lambda e: e.tensor_tensor(out=r_.t[:, 0:2], in0=s_.t[:, 0:2], in1=nh.t[:, 0:1].to_broadcast([128, 2]), op=ALU.pow),
                          reads=[s_, nh], writes=[r_])
                    for j in range(2):
                        fw.op('dve', lambda e: e.scalar_tensor_tensor(out=n_.t[:, j * 512:(j + 1) * 512], in0=o_.t[:, j * 512:(j + 1) * 512],
                                                                      scalar=r_.t[:, j:j + 1], in1=gmo.t[:, j * 512:(j + 1) * 512],
                                                                      op0=ALU.mult, op1=ALU.mult), reads=[o_, r_, gmo], writes=[n_])

                def s1b(tt):
                    n_, tp, oT_ = on[tt % 2], tps[tt % 2], oT[tt % 3]
                    for k in range(8):
                        fw.op('pe', lambda e: e.transpose(out=tp.t[:, k, :], in_=n_.t[:, k * 128:(k + 1) * 128], identity=ident.t[:]),
                              reads=[n_, ident], writes=[tp])
                    fw.op('act', lambda e: e.copy(out=oT_.t[:], in_=tp.t[:]), reads=[tp], writes=[oT_])

                def s2a(tt):
                    oT_, yp = oT[tt % 3], yps[tt % 2]
                    for j in range(2):
                        for k in range(8):
                            fw.op('pe', lambda e: e.matmul(yp[j].t[:], lhsT=oT_.t[:, k, :], rhs=Wo.t[:, k, j * 512:(j + 1) * 512],
                                                           start=(k == 0), stop=(k == 7)), reads=[oT_, Wo], writes=[yp[j]])

                def s2b(tt):
                    x_, s_, r_, yp, y_ = xt[tt % 3], ss[tt % 3], rs[tt % 3], yps[tt % 2], ysb[tt % 2]
                    rows = slice(tt * 128, (tt + 1) * 128)
                    for j in range(2):
                        fw.op('act', lambda e: e.activation(out=sq.t[:, 512:1024], in_=yp[j].t[:], func=AF.Square, accum_out=s_.t[:, 2 + j:3 + j]),
                              reads=[yp[j]], writes=[sq, s_])
                    fw.op('dve', lambda e: e.tensor_tensor(out=s_.t[:, 2:3], in0=s_.t[:, 2:3], in1=s_.t[:, 3:4], op=ALU.add), reads=[s_], writes=[s_])
                    _rms_rstd(fw, (s_, s_.t[:, 2:3]), (r_, r_.t[:, 2:3]), nh, D)
                    for j in range(2):
                        fw.op('dve', lambda e: e.scalar_tensor_tensor(out=y_.t[:, j * 512:(j + 1) * 512], in0=yp[j].t[:], scalar=r_.t[:, 2:3],
                                                                      in1=G.t[:, j * 512:(j + 1) * 512], op0=ALU.mult, op1=ALU.mult),
                              reads=[yp[j], r_, G], writes=[y_])
                    fw.op('pool', lambda e: e.tensor_tensor(out=y_.t[:], in0=y_.t[:], in1=x_.t[:], op=ALU.add), reads=[y_, x_], writes=[y_])
                    fw.dma('sp', x1_d[rows, :], y_.t[:], reads=[y_], sem=y_)

                s1a(0)
                s1b(0)
                s1a(1)
                s1b(1)
                for tt in range(NT):
                    s2a(tt)
                    if tt + 2 < NT:
                        s1a(tt + 2)
                        s1b(tt + 2)
                    s2b(tt)

            if "F" in phases:
              with fw.scope():
                Wa = fw.sb("Wa", [128, 8, DFF], BF16, dma=True)
                Wl = fw.sb("Wl", [128, 8, DFF], BF16, dma=True)
                Wd = fw.sb("Wd", [128, NFC, D], BF16, dma=True)
                fw.dma_group('pool', [(Wa.t[:, k, :], w_up_act[l, k * 128:(k + 1) * 128, :]) for k in range(8)], writes=[Wa], sem=Wa)
                fw.dma_group('pool', [(Wl.t[:, k, :], w_up_lin[l, k * 128:(k + 1) * 128, :]) for k in range(8)], writes=[Wl], sem=Wl)
                fw.dma_group('pool', [(Wd.t[:, f, :], w_down[l, f * 128:(f + 1) * 128, :]) for f in range(NFC)], writes=[Wd], sem=Wd)
                wc = fw.sb("wc", [128, NFC, 3], F32, dma=True)
                bcv = fw.sb("bcv", [128, NFC], F32, dma=True)
                fw.dma('sp', wc.t[:], w_conv_col[l], writes=[wc], sem=wc)
                fw.dma('sp', bcv.t[:], b_conv_col[l], writes=[bcv], sem=bcv)
                G = fw.sb("Gf", [128, D], F32, dma=True)
                fw.dma('sp', G.t[:], gbc_d[l, 1], writes=[G], sem=G)
                carry = fw.sb("carry", [128, NFC, 2], F32)
                fw.op('dve', lambda e: e.memset(carry.t[:], 0.0), writes=[carry])
                xt = fw.sb("fxt", [128, D], F32, dma=True)
                sq = fw.sb("fsq", [128, D], BF16)
                ss = fw.sb("fss", [128, 4], F32)
                rs = fw.sb("frs", [128, 4], F32)
                xn = fw.sb("fxn", [128, D], BF16)
                tps = [fw.ps(f"ftp{i}", [128, 8, 128], BF16) for i in range(2)]
                hT = fw.sb("fhT", [128, 8, 512], BF16)
                ups = [fw.ps(f"ups{i}", [128, 512], F32) for i in range(2)]
                lps = [fw.ps(f"flps{i}", [128, 512], F32) for i in range(2)]
                yps = [fw.ps(f"fyps{j}", [128, 512], F32) for j in range(2)]
                ubuf = [fw.sb(f"ubuf{i}", [128, 514], F32) for i in range(2)]
                av = [fw.sb(f"av{i}", [128, 512], F32) for i in range(2)]
                gT = fw.sb("gT", [128, NFC, 512], BF16)
                xr = fw.sb("xr", [128, D], F32, dma=True)
                ysb = fw.sb("fysb", [128, D], F32, dma=True)
                ssv = fw.view("fssv", ss.t)
                rsv = fw.view("frsv", rs.t)
                fi = 0
                for c in range(NCH):
                    for i in range(4):
                        tt = c * 4 + i
                        fw.dma('sp', xt.t[:], x1_d[tt * 128:(tt + 1) * 128, :], writes=[xt], sem=xt)
                        norm_transpose(xt, l, 2, tps[tt % 2], hT, lambda k: hT.t[:, k, i * 128:(i + 1) * 128], sq, ssv, rsv, xn)
                    for fc in range(NFC):
                        U, Lp, ub, a_ = ups[fi % 2], lps[fi % 2], ubuf[fi % 2], av[fi % 2]
                        fi += 1
                        fs = slice(fc * 128, (fc + 1) * 128)
                        for k in range(8):
                            fw.op('pe', lambda e: e.matmul(U.t[:], lhsT=Wa.t[:, k, fs], rhs=hT.t[:, k, :], start=(k == 0), stop=(k == 7)),
                                  reads=[Wa, hT], writes=[U])
                        for k in range(8):
                            fw.op('pe', lambda e: e.matmul(Lp.t[:], lhsT=Wl.t[:, k, fs], rhs=hT.t[:, k, :], start=(k == 0), stop=(k == 7)),
                                  reads=[Wl, hT], writes=[Lp])
                        fw.op('act', lambda e: e.copy(out=ub.t[:, 2:514], in_=U.t[:]), reads=[U], writes=[ub])
                        fw.op('act', lambda e: e.copy(out=ub.t[:, 0:2], in_=carry.t[:, fc, :]), reads=[carry], writes=[ub])
                        fw.op('dve', lambda e: e.tensor_scalar(out=a_.t[:], in0=ub.t[:, 2:514], scalar1=wc.t[:, fc, 2:3], scalar2=bcv.t[:, fc:fc + 1],
                                                               op0=ALU.mult, op1=ALU.add), reads=[ub, wc, bcv], writes=[a_])
                        fw.op('dve', lambda e: e.scalar_tensor_tensor(out=a_.t[:], in0=ub.t[:, 1:513], scalar=wc.t[:, fc, 1:2], in1=a_.t[:],
                                                                      op0=ALU.mult, op1=ALU.add), reads=[ub, wc, a_], writes=[a_])
                        fw.op('dve', lambda e: e.scalar_tensor_tensor(out=a_.t[:], in0=ub.t[:, 0:512], scalar=wc.t[:, fc, 0:1], in1=a_.t[:],
                                                                       op0=ALU.mult, op1=ALU.add), reads=[ub, wc, a_], writes=[a_])
                        fw.op('act', lambda e: e.copy(out=carry.t[:, fc, :], in_=ub.t[:, 512:514]), reads=[ub], writes=[carry])
                        fw.op('act', lambda e: e.activation(out=a_.t[:], in_=a_.t[:], func=AF.Gelu_apprx_tanh), reads=[a_], writes=[a_])
                        fw.op('dve', lambda e: e.tensor_tensor(out=gT.t[:, fc, :], in0=a_.t[:], in1=Lp.t[:], op=ALU.mult),
                              reads=[a_, Lp], writes=[gT])
                    for i in range(4):
                        tt = c * 4 + i
                        rows = slice(tt * 128, (tt + 1) * 128)
                        fw.dma('sp', xr.t[:], x1_d[rows, :], writes=[xr], sem=xr)
                        fw.op('dve', lambda e: e.memset(ss.t[:, 2:4], 0.0), writes=[ssv])
                        for j in range(2):
                            for fc in range(NFC):
                                fw.op('pe', lambda e: e.matmul(yps[j].t[:], lhsT=gT.t[:, fc, i * 128:(i + 1) * 128], rhs=Wd.t[:, fc, j * 512:(j + 1) * 512],
                                                               start=(fc == 0), stop=(fc == NFC - 1)), reads=[gT, Wd], writes=[yps[j]])
                            fw.op('act', lambda e: e.activation(out=sq.t[:, 0:512], in_=yps[j].t[:], func=AF.Square, accum_out=ss.t[:, 2 + j:3 + j]),
                                  reads=[yps[j]], writes=[sq, ssv])
                        fw.op('dve', lambda e: e.tensor_tensor(out=ss.t[:, 2:3], in0=ss.t[:, 2:3], in1=ss.t[:, 3:4], op=ALU.add), reads=[ssv], writes=[ssv])
                        _rms_rstd(fw, (ssv, ss.t[:, 2:3]), (rsv, rs.t[:, 2:3]), nh, D)
                        for j in range(2):
                            fw.op('dve', lambda e: e.scalar_tensor_tensor(out=ysb.t[:, j * 512:(j + 1) * 512], in0=yps[j].t[:], scalar=rs.t[:, 2:3],
                                                                          in1=G.t[:, j * 512:(j + 1) * 512], op0=ALU.mult, op1=ALU.mult),
                                  reads=[yps[j], rsv, G], writes=[ysb])
                        fw.op('pool', lambda e: e.tensor_tensor(out=ysb.t[:], in0=ysb.t[:], in1=xr.t[:], op=ALU.add), reads=[ysb, xr], writes=[ysb])
                        fw.dma('sp', xout[rows, :], ysb.t[:], reads=[ysb], sem=ysb)
        fw.barrier()
    return nc


def _consts():
    bf = ml_dtypes.bfloat16
    pos = np.arange(S, dtype=np.float32)
    inv = (np.float32(500000.0) ** (-np.arange(0, 16, 2, dtype=np.float32) / np.float32(16))).astype(np.float32)
    ang = (pos[None, :] * inv[:, None]).astype(np.float32)
    cos, sin = np.cos(ang).astype(np.float32), np.sin(ang).astype(np.float32)
    C = np.ones((128, S), np.float32)
    Sg = np.zeros((128, S), np.float32)
    for p_ in range(128):
        d = p_ % 64
        if d < 16:
            C[p_] = cos[d % 8]
            Sg[p_] = -sin[d % 8] if d < 8 else sin[d % 8]
    i = np.arange(128)
    tri = (i[None, :] >= i[:, None]).astype(np.float32).astype(bf)
    negtri = np.where(i[None, :] <= i[:, None], 0.0, -1e30).astype(np.float32)
    onehot = (np.arange(S)[None, :] // 256 == np.arange(16)[:, None]).astype(np.float32).astype(bf)
    tt = np.repeat(np.arange(NT), 16)
    n = np.tile(np.arange(16), NT)
    cur = tt // 2
    cbsel = np.where(n >= cur, -BIG, 0.0).astype(np.float32)
    cblt = np.where(n < cur, -BIG, 0.0).astype(np.float32)
    cbfin = np.where(n > cur, -BIG, 0.0).astype(np.float32)
    rep = lambda v: np.ascontiguousarray(np.broadcast_to(v[None, :], (128, v.shape[0])))
    cpow = (2.0 ** (-np.arange(KBIS, dtype=np.float64))).astype(np.float32)
    return {
        "ropeC": C, "ropeS": Sg, "ident": np.eye(128, dtype=np.float32).astype(bf), "tri": tri, "negtri": negtri,
        "onehot": onehot, "cbsel": rep(cbsel), "cblt": rep(cblt), "cbfin": rep(cbfin), "cpow": rep(cpow),
    }


def _perm_w_in(w_in):
    offs = {"mq": 0, "mk": 512, "mv": 1024, "dq": 1536, "dk": 2048, "dv": 2560, "qi": 3072, "ki": 3584, "wi": 3648}
    j = np.arange(64)
    perm = np.where(j < 8, j + 8, np.where(j < 16, j - 8, j))
    cols = []
    for g in ("mq", "mk", "dq", "dk", "qi"):
        base = offs[g]
        cols.append(base + np.arange(512))
        cols.append(base + (np.arange(512) // 64) * 64 + perm[np.arange(512) % 64])
    cols.append(offs["ki"] + np.arange(64))
    cols.append(offs["ki"] + perm)
    cols.append(offs["mv"] + np.arange(512))
    cols.append(offs["dv"] + np.arange(512))
    cols.append(offs["wi"] + np.arange(8))
    cols = np.concatenate(cols)
    assert cols.shape[0] == NCOLP
    return np.ascontiguousarray(w_in[:, :, cols])


def _shared_inputs(w_ada, b_ada, g_pre_mix, w_in, g_moba_out, g_dsa_out, w_out, g_post_mix, g_pre_ffn,
                   w_up_act, w_up_lin, w_conv, b_conv, w_down, g_post_ffn):
    f = lambda a: np.ascontiguousarray(np.asarray(a, dtype=np.float32))
    col8 = lambda g: np.ascontiguousarray(f(g).reshape(2, 8, 128).transpose(0, 2, 1))
    sh = {
        "w_ada": f(w_ada), "b_ada": f(b_ada),
        "b_ada_col": np.ascontiguousarray(f(b_ada).reshape(2, 48, 128).transpose(0, 2, 1)),
        "g_pre_mix_col": col8(g_pre_mix), "g_pre_ffn_col": col8(g_pre_ffn),
        "g_post_mix": f(g_post_mix), "g_post_ffn": f(g_post_ffn),
        "g_moba_out": f(g_moba_out), "g_dsa_out": f(g_dsa_out),
        "w_in_p": _perm_w_in(f(w_in)), "w_out": f(w_out), "w_up_act": f(w_up_act), "w_up_lin": f(w_up_lin),
        "w_conv_col": np.ascontiguousarray(f(w_conv).reshape(2, 3, NFC, 128).transpose(0, 3, 2, 1)),
        "b_conv_col": np.ascontiguousarray(f(b_conv).reshape(2, NFC, 128).transpose(0, 2, 1)),
        "w_down": f(w_down),
    }
    sh.update(_consts())
    return sh


def _core_inputs(x, c, b, shared):
    m = dict(shared)
    m["x"] = np.ascontiguousarray(np.asarray(x[b], dtype=np.float32))
    m["cT"] = np.ascontiguousarray(np.asarray(c[b], dtype=np.float32).reshape(8, 128).T)
    return m


def kernel(x, c, w_ada, b_ada, g_pre_mix, w_in, g_moba_out, g_dsa_out, w_out, g_post_mix,
           g_pre_ffn, w_up_act, w_up_lin, w_conv, b_conv, w_down, g_post_ffn):
    x = np.asarray(x)
    c = np.asarray(c)
    shared = _shared_inputs(w_ada, b_ada, g_pre_mix, w_in, g_moba_out, g_dsa_out, w_out, g_post_mix, g_pre_ffn,
                            w_up_act, w_up_lin, w_conv, b_conv, w_down, g_post_ffn)
    nc = build_nc()
    in_maps = [_core_inputs(x, c, b, shared) for b in range(8)]
    res = run_bass_kernel_spmd(nc, in_maps, core_ids=list(range(8)))
    return np.stack([np.asarray(r["out"], dtype=np.float32) for r in res.results], axis=0)
```

```python
import numpy as np
import ml_dtypes
from contextlib import ExitStack
import concourse.bass as bass
import concourse.mybir as mybir
from concourse.bass_utils import run_bass_kernel_spmd

F32 = mybir.dt.float32
BF16 = mybir.dt.bfloat16
ALU = mybir.AluOpType
AF = mybir.ActivationFunctionType
AX = mybir.AxisListType


class DSem:
    __slots__ = ("idx", "cnt")

    def __init__(self, idx):
        self.idx = idx
        self.cnt = 0


class Buf:
    __slots__ = ("name", "t", "w", "r", "dsem")

    def __init__(self, name, t=None):
        self.name = name
        self.t = t
        self.w = {}
        self.r = {}
        self.dsem = None


class _Scope:
    def __init__(self, fw):
        self.fw = fw

    def __enter__(self):
        fw = self.fw
        self.prev = (fw.es, fw.scope_dsems)
        self.stack = ExitStack()
        self.stack.__enter__()
        fw.es = self.stack
        fw.scope_dsems = []
        return self

    def __exit__(self, *a):
        fw = self.fw
        fw.barrier()
        fw.dpool.extend(fw.scope_dsems)
        fw.es, fw.scope_dsems = self.prev
        return self.stack.__exit__(*a)


class FW:
    SEM_MAX = 30000

    def __init__(self, nc, es):
        self.nc = nc
        self.es = es
        self.sem_es = es
        self.eng = {"pe": nc.tensor, "act": nc.scalar, "dve": nc.vector, "pool": nc.gpsimd, "sp": nc.sync}
        self.sems = []
        self.cur = {}
        self.own = {e: set() for e in self.eng}
        self.known = {e: {} for e in self.eng}
        self.issued = {}
        self.dpool = []
        self.scope_dsems = []
        self.nwaits = 0
        self.nops = 0
        for e in self.eng:
            self._newsem(e)

    def _alloc_sem(self, name):
        s = self.sem_es.enter_context(self.nc.semaphore(name))
        self.sems.append(s)
        return len(self.sems) - 1

    def _newsem(self, e):
        i = self._alloc_sem(f"s_{e}_{len(self.sems)}")
        self.cur[e] = [i, 0]
        self.own[e].add(i)

    def _get_dsem(self):
        if self.dpool:
            d = self.dpool.pop()
        else:
            d = DSem(self._alloc_sem(f"d_{len(self.sems)}"))
        self.scope_dsems.append(d)
        return d

    def scope(self):
        return _Scope(self)

    def sb(self, name, shape, dtype, dma=False):
        self.nuniq = getattr(self, "nuniq", 0) + 1
        t = self.es.enter_context(self.nc.sbuf_tensor(f"{name}_u{self.nuniq}", shape, dtype))
        b = Buf(name, t)
        if dma:
            b.dsem = self._get_dsem()
        return b

    def ps(self, name, shape, dtype):
        self.nuniq = getattr(self, "nuniq", 0) + 1
        t = self.es.enter_context(self.nc.psum_tensor(f"{name}_u{self.nuniq}", shape, dtype))
        return Buf(name, t)

    def view(self, name, t, dma=False):
        b = Buf(name, t)
        if dma:
            b.dsem = self._get_dsem()
        return b

    def _need(self, reads, writes):
        need = {}
        for b in reads:
            for s, v in b.w.items():
                if need.get(s, 0) < v:
                    need[s] = v
        for b in writes:
            for s, v in b.w.items():
                if need.get(s, 0) < v:
                    need[s] = v
            for s, v in b.r.items():
                if need.get(s, 0) < v:
                    need[s] = v
        return need

    def _waits(self, e, need, skip_own=False):
        k = self.known[e]
        eng = self.eng[e]
        for s, v in need.items():
            if skip_own and s in self.own[e]:
                continue
            if k.get(s, 0) >= v:
                continue
            eng.wait_ge(self.sems[s], v)
            self.nwaits += 1
            k[s] = v

    def _record(self, t, reads, writes):
        s, v = t
        self.issued[s] = v
        for b in reads:
            if b.r.get(s, 0) < v:
                b.r[s] = v
        for b in writes:
            b.w = {s: v}
            b.r = {}

    def op(self, e, fn, reads=(), writes=(), same=None):
        if same is None:
            same = (e != "pe")
        need = self._need(reads, writes)
        self._waits(e, need, skip_own=not same)
        ins = fn(self.eng[e])
        c = self.cur[e]
        c[1] += 1
        ins.then_inc(self.sems[c[0]], 1)
        self._record((c[0], c[1]), reads, writes)
        self.nops += 1
        if c[1] >= self.SEM_MAX:
            self._newsem(e)
        return ins

    def dma(self, q, out, in_, reads=(), writes=(), sem=None, **kw):
        return self.dma_group(q, [(out, in_)], reads, writes, sem, **kw)

    def dma_group(self, q, pairs, reads=(), writes=(), sem=None, **kw):
        d = sem.dsem
        need = self._need(reads, writes)
        if d.cnt:
            v = 16 * d.cnt
            if need.get(d.idx, 0) < v:
                need[d.idx] = v
        self._waits(q, need)
        for (out, in_) in pairs:
            ins = self.eng[q].dma_start(out=out, in_=in_, **kw)
            d.cnt += 1
            ins.then_inc(self.sems[d.idx], 16)
            self.nops += 1
        self._record((d.idx, 16 * d.cnt), reads, writes)

    def barrier(self):
        need = dict(self.issued)
        for e in self.eng:
            self._waits(e, need)


S = 4096
D = 1024
NT = 32
NCH = 8
DFF = 2816
NFC = 22
NCOLP = 6280
C_MV, C_DV, C_WI = 5248, 5760, 6272
BIG = 30000.0
KBIS = 18
EPS = 1e-6


class P:
    pass


def _rms_rstd(fw, ssb, rstd, nh, n):
    (ss_buf, ss_ap), (r_buf, r_ap) = ssb, rstd
    fw.op('dve', lambda e: e.tensor_scalar(out=ss_ap, in0=ss_ap, scalar1=1.0 / n, scalar2=EPS, op0=ALU.mult, op1=ALU.add),
          reads=[ss_buf], writes=[ss_buf])
    fw.op('pool', lambda e: e.tensor_tensor(out=r_ap, in0=ss_ap, in1=nh.t[:, 0:1], op=ALU.pow), reads=[ss_buf, nh], writes=[r_buf])


def build_nc(nlayers=2, phases="ABCDEF", dbg=False):
    nc = bass.Bass("TRN2", target_bir_lowering=False)
    p = P()

    def din(name, shape, dt=F32):
        return nc.dram_tensor(name, shape, dt, kind="ExternalInput").ap()

    def dscr(name, shape, dt):
        return nc.dram_tensor(name, shape, dt, kind=("ExternalOutput" if dbg else "Internal")).ap()

    x = din("x", [S, D])
    cT = din("cT", [128, 8])
    w_ada = din("w_ada", [2, D, 6 * D])
    b_ada = din("b_ada", [2, 6 * D])
    b_ada_col = din("b_ada_col", [2, 128, 48])
    g_pre_mix_col = din("g_pre_mix_col", [2, 128, 8])
    g_pre_ffn_col = din("g_pre_ffn_col", [2, 128, 8])
    g_post_mix = din("g_post_mix", [2, D])
    g_post_ffn = din("g_post_ffn", [2, D])
    g_moba_out = din("g_moba_out", [2, 512])
    g_dsa_out = din("g_dsa_out", [2, 512])
    w_in_p = din("w_in_p", [2, D, NCOLP])
    w_out = din("w_out", [2, D, D])
    w_up_act = din("w_up_act", [2, D, DFF])
    w_up_lin = din("w_up_lin", [2, D, DFF])
    w_conv_col = din("w_conv_col", [2, 128, NFC, 3])
    b_conv_col = din("b_conv_col", [2, 128, NFC])
    w_down = din("w_down", [2, DFF, D])
    ropeC = din("ropeC", [128, S])
    ropeS = din("ropeS", [128, S])
    ident_d = din("ident", [128, 128], BF16)
    tri_d = din("tri", [128, 128], BF16)
    negtri_d = din("negtri", [128, 128])
    onehot_d = din("onehot", [16, S], BF16)
    cbsel_d = din("cbsel", [128, 512])
    cblt_d = din("cblt", [128, 512])
    cbfin_d = din("cbfin", [128, 512])
    cpow_d = din("cpow", [128, KBIS])
    out = nc.dram_tensor("out", [S, D], F32, kind="ExternalOutput").ap()

    scrT = [dscr(n, [512, S], BF16) for n in ("mqT", "mkT", "dqT", "dkT", "qiT")]
    kiT_d = dscr("kiT", [64, S], BF16)
    mva = dscr("mva", [S, 520], BF16)
    dva = dscr("dva", [S, 520], BF16)
    om_d = dscr("om", [S, 512], F32)
    od_d = dscr("od", [S, 512], F32)
    x1_d = dscr("x1", [S, D], F32)
    x2_d = dscr("x2", [S, D], F32)
    gbc_d = dscr("gbc", [2, 2, 128, D], F32)

    with ExitStack() as es:
        fw = FW(nc, es)
        AB = fw.sb("AB", [128, 2, 4, 8], F32)
        WI = fw.sb("WI", [128, NT, 8], F32)
        ksum = fw.sb("ksum", [128, 4, 16], F32)
        ident = fw.sb("identb", [128, 128], BF16, dma=True)
        nh = fw.sb("neghalf", [128, 1], F32)
        fw.dma('sp', ident.t[:], ident_d[:, :], writes=[ident], sem=ident)
        fw.op('dve', lambda e: e.memset(nh.t[:], -0.5), writes=[nh])

        if "A" in phases:
          with fw.scope():
            ct = fw.sb("ct", [128, 8], F32, dma=True)
            sc = fw.sb("sc", [128, 8], F32)
            screp = fw.sb("screp", [128, 8, 128], F32)
            ones1 = fw.sb("ones1", [1, 128], F32)
            wa = [fw.sb(f"wa{i}", [128, 8, 1024], F32, dma=True) for i in range(2)]
            brow = [fw.sb(f"brow{i}", [1, 1024], F32, dma=True) for i in range(2)]
            bcol = fw.sb("bcol", [128, 2, 48], F32, dma=True)
            gcol = fw.sb("gcol", [128, 2, 2, 8], F32, dma=True)
            gpb = [fw.sb(f"gpb{i}", [128, 1024], F32, dma=True) for i in range(2)]
            colps = fw.ps("colps", [128, 512], F32)
            bcps = [fw.ps(f"bcps{i}", [128, 512], F32) for i in range(2)]
            mcol = fw.sb("mcol", [128, 8], F32)
            Gt = [fw.sb(f"Gt{i}", [128, D], F32, dma=True) for i in range(2)]
            fw.dma('sp', ct.t[:], cT[:, :], writes=[ct], sem=ct)
            fw.dma_group('sp', [(bcol.t[:, l, :], b_ada_col[l]) for l in range(2)], writes=[bcol], sem=bcol)
            fw.dma_group('sp', [(gcol.t[:, l, 0, :], g_pre_mix_col[l]) for l in range(2)]
                         + [(gcol.t[:, l, 1, :], g_pre_ffn_col[l]) for l in range(2)], writes=[gcol], sem=gcol)
            fw.op('act', lambda e: e.activation(out=sc.t[:], in_=ct.t[:], func=AF.Silu), reads=[ct], writes=[sc])
            fw.op('dve', lambda e: e.tensor_copy(out=screp.t[:], in_=sc.t[:, :].unsqueeze(2).to_broadcast([128, 8, 128])),
                  reads=[sc], writes=[screp])
            fw.op('dve', lambda e: e.memset(ones1.t[:], 1.0), writes=[ones1])
            pi = 0
            for l in range(nlayers):
                wv = w_ada[l].rearrange("(k p) n -> p k n", p=128)
                for piece in range(6):
                    w = wa[pi % 2]
                    pi += 1
                    fw.dma_group('sp', [(w.t[:, k, :], wv[:, k, piece * 1024:(piece + 1) * 1024]) for k in range(8)],
                                 writes=[w], sem=w)
                    if piece in (2, 5):
                        j = 0 if piece == 2 else 1
                        br = brow[j]
                        gp = gpb[j]
                        fw.dma('sp', br.t[:], b_ada[l:l + 1, piece * 1024:(piece + 1) * 1024], writes=[br], sem=br)
                        fw.dma('sp', gp.t[:], (g_post_mix if j == 0 else g_post_ffn)[l, :].partition_broadcast(128),
                               writes=[gp], sem=gp)
                        for nhf in range(2):
                            ps = bcps[nhf]
                            for k in range(8):
                                fw.op('pe', lambda e: e.matmul(ps.t[:], lhsT=screp.t[:, k, :], rhs=w.t[:, k, nhf * 512:(nhf + 1) * 512],
                                                               start=(k == 0), stop=False), reads=[screp, w], writes=[ps])
                            fw.op('pe', lambda e: e.matmul(ps.t[:], lhsT=ones1.t[0:1, :], rhs=br.t[0:1, nhf * 512:(nhf + 1) * 512],
                                                           start=False, stop=True), reads=[ones1, br], writes=[ps])
                            G = Gt[j]
                            fw.op('dve', lambda e: e.tensor_tensor(out=G.t[:, nhf * 512:(nhf + 1) * 512], in0=ps.t[:],
                                                                   in1=gp.t[:, nhf * 512:(nhf + 1) * 512], op=ALU.mult),
                                  reads=[ps, gp], writes=[G])
                        fw.dma('sp', gbc_d[l, j], Gt[j].t[:], reads=[Gt[j]], sem=Gt[j])
                    else:
                        for jj in range(8):
                            for k in range(8):
                                fw.op('pe', lambda e: e.matmul(colps.t[:, jj:jj + 1], lhsT=w.t[:, k, jj * 128:(jj + 1) * 128],
                                                               rhs=sc.t[:, k:k + 1], start=(k == 0), stop=(k == 7)),
                                      reads=[sc, w], writes=[colps])
                        fw.op('dve', lambda e: e.tensor_tensor(out=mcol.t[:], in0=colps.t[:, 0:8],
                                                               in1=bcol.t[:, l, piece * 8:(piece + 1) * 8], op=ALU.add),
                              reads=[colps, bcol], writes=[mcol])
                        if piece in (0, 3):
                            slot = 1 if piece == 0 else 3
                            fw.op('dve', lambda e: e.tensor_copy(out=AB.t[:, l, slot, :], in_=mcol.t[:]), reads=[mcol], writes=[AB])
                        else:
                            slot = 0 if piece == 1 else 2
                            gi = 0 if piece == 1 else 1
                            fw.op('dve', lambda e: e.scalar_tensor_tensor(out=AB.t[:, l, slot, :], in0=mcol.t[:], scalar=1.0,
                                                                          in1=gcol.t[:, l, gi, :], op0=ALU.add, op1=ALU.mult),
                                  reads=[mcol, gcol], writes=[AB])

        def norm_transpose(xb, l, slot, tp, hbuf, hview, sq, ss, rstd, xn):
            fw.op('dve', lambda e: e.memset(ss.t[:], 0.0), writes=[ss])
            fw.op('act', lambda e: e.activation(out=sq.t[:], in_=xb.t[:], func=AF.Square, accum_out=ss.t[:, 0:1]),
                  reads=[xb], writes=[sq, ss])
            _rms_rstd(fw, (ss, ss.t[:, 0:1]), (rstd, rstd.t[:, 0:1]), nh, D)
            fw.op('dve', lambda e: e.tensor_scalar(out=xn.t[:], in0=xb.t[:], scalar1=rstd.t[:, 0:1], scalar2=None, op0=ALU.mult),
                  reads=[xb, rstd], writes=[xn])
            for k in range(8):
                fw.op('pe', lambda e: e.transpose(out=tp.t[:, k, :], in_=xn.t[:, k * 128:(k + 1) * 128], identity=ident.t[:]),
                      reads=[xn, ident], writes=[tp])
            for k in range(8):
                fw.op('act', lambda e: e.activation(out=hview(k), in_=tp.t[:, k, :], func=AF.Identity,
                                                    scale=AB.t[:, l, slot, k:k + 1], bias=AB.t[:, l, slot + 1, k:k + 1]),
                      reads=[tp, AB], writes=[hbuf])

        for l in range(nlayers):
            xin = x if l == 0 else x2_d
            xout = out if l == nlayers - 1 else x2_d
            if "B" in phases:
              with fw.scope():
                W = fw.sb("winb", [128, 8, NCOLP], BF16, dma=True)
                fw.dma_group('pool', [(W.t[:, k, :], w_in_p[l, k * 128:(k + 1) * 128, :]) for k in range(8)], writes=[W], sem=W)
                rC = fw.sb("rC", [128, S], F32, dma=True)
                rS = fw.sb("rS", [128, S], F32, dma=True)
                fw.dma('sp', rC.t[:], ropeC[:, :], writes=[rC], sem=rC)
                fw.dma('sp', rS.t[:], ropeS[:, :], writes=[rS], sem=rS)
                xt = [fw.sb(f"xt{i}", [128, D], F32, dma=True) for i in range(2)]
                sq = fw.sb("sq", [128, D], BF16)
                ss = [fw.sb(f"ss{i}", [128, 1], F32) for i in range(4)]
                rstd = [fw.sb(f"rstd{i}", [128, 1], F32) for i in range(4)]
                xn = [fw.sb(f"xn{i}", [128, D], BF16) for i in range(4)]
                tps = [fw.ps(f"tp{i}", [128, 8, 128], BF16) for i in range(2)]
                hT = [fw.sb(f"hT{i}", [128, 8, 512], BF16) for i in range(2)]
                pm = [fw.ps(f"pm{i}", [128, 512], F32) for i in range(2)]
                pp = [fw.ps(f"pp{i}", [128, 512], F32) for i in range(2)]
                pv = [fw.ps(f"pv{i}", [128, 512], F32) for i in range(2)]
                t1 = [fw.sb(f"t1_{i}", [128, 512], F32) for i in range(2)]
                t2 = [fw.sb(f"t2_{i}", [128, 512], F32) for i in range(2)]
                t3 = [fw.sb(f"t3_{i}", [128, 512], F32) for i in range(2)]
                ob = [fw.sb(f"ob{i}", [128, 512], BF16, dma=True) for i in range(3)]
                vt = [fw.sb(f"vt{i}", [128, 8, 65], BF16, dma=True) for i in range(2)]
                for v in vt:
                    fw.op('dve', lambda e: e.memset(v.t[:], 1.0), writes=[v])
                fj = 0
                oj = 0
                vj = 0
                def normA(c):
                    for i in range(4):
                        tt = c * 4 + i
                        xb, s_, r_, n_ = xt[tt % 2], ss[i], rstd[i], xn[i]
                        fw.dma('sp', xb.t[:], xin[tt * 128:(tt + 1) * 128, :], writes=[xb], sem=xb)
                        fw.op('dve', lambda e: e.memset(s_.t[:], 0.0), writes=[s_])
                        fw.op('act', lambda e: e.activation(out=sq.t[:], in_=xb.t[:], func=AF.Square, accum_out=s_.t[:, 0:1]),
                              reads=[xb], writes=[sq, s_])
                        _rms_rstd(fw, (s_, s_.t[:, 0:1]), (r_, r_.t[:, 0:1]), nh, D)
                        fw.op('dve', lambda e: e.tensor_scalar(out=n_.t[:], in0=xb.t[:], scalar1=r_.t[:, 0:1], scalar2=None, op0=ALU.mult),
                              reads=[xb, r_], writes=[n_])

                def normB(c):
                    hh = hT[c % 2]
                    for i in range(4):
                        tt = c * 4 + i
                        tp, n_ = tps[tt % 2], xn[i]
                        for k in range(8):
                            fw.op('pe', lambda e: e.transpose(out=tp.t[:, k, :], in_=n_.t[:, k * 128:(k + 1) * 128], identity=ident.t[:]),
                                  reads=[n_, ident], writes=[tp])
                        for k in range(8):
                            fw.op('act', lambda e: e.activation(out=hh.t[:, k, i * 128:(i + 1) * 128], in_=tp.t[:, k, :], func=AF.Identity,
                                                                scale=AB.t[:, l, 0, k:k + 1], bias=AB.t[:, l, 1, k:k + 1]),
                                  reads=[tp, AB], writes=[hh])

                normA(0)
                normB(0)
                for c in range(NCH):
                    h = hT[c % 2]
                    cs = slice(c * 512, (c + 1) * 512)
                    if c + 1 < NCH:
                        normA(c + 1)
                    tiles = [(g, ft, 128) for g in range(5) for ft in range(4)] + [(5, 0, 64)]
                    for ti_, (g, ft, M) in enumerate(tiles):
                        if ti_ == 10 and c + 1 < NCH:
                            normB(c + 1)
                        cm = g * 1024 + ft * 128 if g < 5 else 5120
                        cp = cm + 512 if g < 5 else 5184
                        pmain, ppart = pm[fj % 2], pp[fj % 2]
                        a1, a2, a3 = t1[fj % 2], t2[fj % 2], t3[fj % 2]
                        fj += 1
                        for k in range(8):
                            fw.op('pe', lambda e: e.matmul(pmain.t[0:M, :], lhsT=W.t[:, k, cm:cm + M], rhs=h.t[:, k, :],
                                                           start=(k == 0), stop=(k == 7)), reads=[W, h], writes=[pmain])
                        for k in range(8):
                            fw.op('pe', lambda e: e.matmul(ppart.t[0:M, :], lhsT=W.t[:, k, cp:cp + M], rhs=h.t[:, k, :],
                                                           start=(k == 0), stop=(k == 7)), reads=[W, h], writes=[ppart])
                        fw.op('dve', lambda e: e.tensor_tensor(out=a1.t[0:M, :], in0=ppart.t[0:M, :], in1=rS.t[0:M, cs], op=ALU.mult),
                              reads=[ppart, rS], writes=[a1])
                        fw.op('dve', lambda e: e.tensor_tensor(out=a2.t[0:M, :], in0=pmain.t[0:M, :], in1=rC.t[0:M, cs], op=ALU.mult),
                              reads=[pmain, rC], writes=[a2])
                        o = ob[oj % 3]
                        oj += 1
                        if g == 1:
                            fw.op('pool', lambda e: e.tensor_tensor(out=a3.t[:], in0=a1.t[:], in1=a2.t[:], op=ALU.add),
                                  reads=[a1, a2], writes=[a3])
                            fw.op('act', lambda e: e.copy(out=o.t[:], in_=a3.t[:]), reads=[a3], writes=[o])
                            fw.op('dve', lambda e: e.reduce_sum(out=ksum.t[:, ft, 2 * c:2 * c + 2],
                                                                in_=a3.t[:, :].rearrange("p (b s) -> p b s", b=2), axis=AX.X),
                                  reads=[a3], writes=[ksum])
                        else:
                            fw.op('pool', lambda e: e.tensor_tensor(out=o.t[0:M, :], in0=a1.t[0:M, :], in1=a2.t[0:M, :], op=ALU.add),
                                  reads=[a1, a2], writes=[o])
                        dst = scrT[g][ft * 128:(ft + 1) * 128, cs] if g < 5 else kiT_d[:, cs]
                        fw.dma('sp', dst, o.t[0:M, :], reads=[o], sem=o)
                    for i in range(4):
                        tt = c * 4 + i
                        for (dst, c0) in ((mva, C_MV), (dva, C_DV)):
                            ps = pv[vj % 2]
                            v = vt[vj % 2]
                            vj += 1
                            for k in range(8):
                                fw.op('pe', lambda e: e.matmul(ps.t[:], lhsT=h.t[:, k, i * 128:(i + 1) * 128], rhs=W.t[:, k, c0:c0 + 512],
                                                               start=(k == 0), stop=(k == 7)), reads=[W, h], writes=[ps])
                            fw.op('act', lambda e: e.copy(out=v.t[:, :, 0:64], in_=ps.t[:, :].rearrange("p (h d) -> p h d", h=8)),
                                  reads=[ps], writes=[v])
                            fw.dma('sp', dst[tt * 128:(tt + 1) * 128, :], v.t[:, :, :].rearrange("p h d -> p (h d)"), reads=[v], sem=v)
                        ps = pv[vj % 2]
                        vj += 1
                        for k in range(8):
                            fw.op('pe', lambda e: e.matmul(ps.t[:, 0:8], lhsT=h.t[:, k, i * 128:(i + 1) * 128], rhs=W.t[:, k, C_WI:C_WI + 8],
                                                           start=(k == 0), stop=(k == 7)), reads=[W, h], writes=[ps])
                        fw.op('act', lambda e: e.copy(out=WI.t[:, tt, :], in_=ps.t[:, 0:8]), reads=[ps], writes=[WI])
            if "C" in phases:
              with fw.scope():
                V = fw.sb("mV", [128, NT, 520], BF16, dma=True)
                fw.dma('sp', V.t[:], mva.rearrange("(n p) c -> p n c", p=128), writes=[V], sem=V)
                Qt = [fw.sb(f"mQ{i}", [80, S], BF16) for i in range(2)]
                Kt = [fw.sb(f"mK{i}", [80, S], BF16) for i in range(2)]
                Qm = [fw.view(f"mQm{i}", Qt[i].t, dma=True) for i in range(2)]
                Qb = [fw.view(f"mQb{i}", Qt[i].t) for i in range(2)]
                Km = [fw.view(f"mKm{i}", Kt[i].t, dma=True) for i in range(2)]
                Kc = [fw.view(f"mKc{i}", Kt[i].t, dma=True) for i in range(2)]
                for i in range(2):
                    fw.dma('sp', Kt[i].t[64:80, :], onehot_d[:, :], writes=[Kc[i]], sem=Kc[i])
                kmb = fw.sb("kmb", [64, 8, 16], BF16)
                for h in range(8):
                    e_, ft = h % 2, h // 2
                    fw.op('act', lambda e: e.mul(out=kmb.t[0:64, h, :], in_=ksum.t[e_ * 64:(e_ + 1) * 64, ft, :], mul=1.0 / 256.0),
                          reads=[ksum], writes=[kmb])
                tri = fw.sb("tri", [128, 128], BF16, dma=True)
                fw.dma('sp', tri.t[:], tri_d[:, :], writes=[tri], sem=tri)
                cbs = fw.sb("cbs", [128, 3, 512], F32, dma=True)
                fw.dma_group('sp', [(cbs.t[:, 0, :], cbsel_d[:, :]), (cbs.t[:, 1, :], cblt_d[:, :]), (cbs.t[:, 2, :], cbfin_d[:, :])],
                             writes=[cbs], sem=cbs)
                gps = fw.ps("gps", [128, 512], F32)
                tb = fw.ps("tb", [128, 1024], BF16)
                sps = [fw.ps(f"sp{i}", [128, 512], F32) for i in range(3)]
                ops_ = [fw.ps(f"op{i}", [128, 512], F32) for i in range(2)]
                pt = [fw.sb(f"pt{i}", [128, 512], BF16) for i in range(3)]
                gm = fw.sb("gm", [128, 512], F32)
                ee = fw.sb("ee", [128, 512], F32)
                g2 = fw.sb("g2", [128, 512], F32)
                g3 = fw.sb("g3", [128, 512], F32)
                mx = fw.sb("mx", [128, 3, 32], F32)
                bt = fw.sb("bt", [128, 512], BF16)
                rec = [fw.sb(f"rec{i}", [128, 4, 1], F32) for i in range(2)]
                osb = [fw.sb(f"osb{i}", [128, 4, 64], F32, dma=True) for i in range(2)]
                v3 = lambda t: t[:, :].rearrange("p (a n) -> p a n", n=16)
                bc3 = lambda j: mx.t[:, j, :].unsqueeze(2).to_broadcast([128, 32, 16])
                si = 0
                oi = 0
                def gateA(h):
                    b = h % 2
                    fw.dma('sp', Qt[b].t[0:64, :], scrT[0][h * 64:(h + 1) * 64, :], writes=[Qm[b]], sem=Qm[b])
                    fw.dma('sp', Kt[b].t[0:64, :], scrT[1][h * 64:(h + 1) * 64, :], writes=[Km[b]], sem=Km[b])
                    for tt in range(NT):
                        fw.op('pe', lambda e: e.matmul(gps.t[:, tt * 16:(tt + 1) * 16], lhsT=Qt[b].t[0:64, tt * 128:(tt + 1) * 128],
                                                       rhs=kmb.t[0:64, h, :], start=True, stop=True), reads=[Qm[b], kmb], writes=[gps])
                    fw.op('dve', lambda e: e.tensor_tensor(out=gm.t[:], in0=gps.t[:], in1=cbs.t[:, 0, :], op=ALU.add),
                          reads=[gps, cbs], writes=[gm])
                    fw.op('dve', lambda e: e.reduce_max(out=mx.t[:, 0, :], in_=v3(gm.t), axis=AX.X), reads=[gm], writes=[mx])
                    fw.op('dve', lambda e: e.tensor_tensor(out=v3(ee.t), in0=v3(gm.t), in1=bc3(0), op=ALU.is_equal),
                          reads=[gm, mx], writes=[ee])
                    fw.op('dve', lambda e: e.scalar_tensor_tensor(out=g2.t[:], in0=ee.t[:], scalar=-1e9, in1=gm.t[:], op0=ALU.mult, op1=ALU.add),
                          reads=[ee, gm], writes=[g2])
                    fw.op('dve', lambda e: e.reduce_max(out=mx.t[:, 1, :], in_=v3(g2.t), axis=AX.X), reads=[g2], writes=[mx])
                    fw.op('dve', lambda e: e.tensor_tensor(out=v3(ee.t), in0=v3(g2.t), in1=bc3(1), op=ALU.is_equal),
                          reads=[g2, mx], writes=[ee])
                    fw.op('dve', lambda e: e.scalar_tensor_tensor(out=g3.t[:], in0=ee.t[:], scalar=-1e9, in1=g2.t[:], op0=ALU.mult, op1=ALU.add),
                          reads=[ee, g2], writes=[g3])
                    fw.op('dve', lambda e: e.reduce_max(out=mx.t[:, 2, :], in_=v3(g3.t), axis=AX.X), reads=[g3], writes=[mx])
                    fw.op('dve', lambda e: e.tensor_tensor(out=v3(ee.t), in0=v3(gm.t), in1=bc3(2), op=ALU.is_lt),
                          reads=[gm, mx], writes=[ee])
                    fw.op('dve', lambda e: e.tensor_tensor(out=g2.t[:], in0=ee.t[:], in1=cbs.t[:, 1, :], op=ALU.mult),
                          reads=[ee, cbs], writes=[g2])
                    fw.op('dve', lambda e: e.tensor_tensor(out=bt.t[:], in0=g2.t[:], in1=cbs.t[:, 2, :], op=ALU.add),
                          reads=[g2, cbs], writes=[bt])

                def gateB(h):
                    b = h % 2
                    for tt in range(NT):
                        fw.op('pe', lambda e: e.transpose(out=tb.t[0:16, (tt % 4) * 128:(tt % 4 + 1) * 128], in_=bt.t[:, tt * 16:(tt + 1) * 16],
                                                          identity=ident.t[:]), reads=[bt, ident], writes=[tb])
                        if tt % 4 == 3:
                            q4 = tt // 4
                            fw.op('act', lambda e: e.copy(out=Qt[b].t[64:80, q4 * 512:(q4 + 1) * 512], in_=tb.t[0:16, 0:512]),
                                  reads=[tb], writes=[Qb[b]])

                def attn(h):
                    nonlocal si, oi
                    b = h % 2
                    for qc in range(NCH):
                        O = ops_[oi % 2]
                        rc = rec[oi % 2]
                        ob_ = osb[oi % 2]
                        oi += 1
                        nst = 4 * qc + 4

                        def qk(st):
                            nonlocal si
                            sp_ = sps[si % 3]
                            Pt = pt[si % 3]
                            si += 1
                            fw.op('pe', lambda e: e.matmul(sp_.t[:], lhsT=Kt[b].t[0:80, st * 128:(st + 1) * 128],
                                                           rhs=Qt[b].t[0:80, qc * 512:(qc + 1) * 512], start=True, stop=True),
                                  reads=[Km[b], Kc[b], Qm[b], Qb[b]], writes=[sp_])
                            fw.op('act', lambda e: e.activation(out=Pt.t[:], in_=sp_.t[:], func=AF.Exp, scale=0.125),
                                  reads=[sp_], writes=[Pt])
                            j = st - 4 * qc
                            if j >= 0:
                                fw.op('pool', lambda e: e.tensor_tensor(out=Pt.t[:, j * 128:(j + 1) * 128], in0=Pt.t[:, j * 128:(j + 1) * 128],
                                                                        in1=tri.t[:], op=ALU.mult), reads=[Pt, tri], writes=[Pt])
                            return Pt, j

                        pend = qk(0)
                        first = True
                        for st in range(nst):
                            nxt = qk(st + 1) if st + 1 < nst else None
                            Pt, j = pend
                            for i in range(max(j, 0), 4):
                                fw.op('pe', lambda e: e.matmul(O.t[:, i * 65:(i + 1) * 65], lhsT=Pt.t[:, i * 128:(i + 1) * 128],
                                                               rhs=V.t[:, st, h * 65:(h + 1) * 65], start=first, stop=(st == 4 * qc + i)),
                                      reads=[Pt, V], writes=[O])
                                first = False
                            pend = nxt
                        Ov = O.t[:, 0:260].rearrange("p (i d) -> p i d", d=65)
                        fw.op('dve', lambda e: e.reciprocal(out=rc.t[:], in_=Ov[:, :, 64:65]), reads=[O], writes=[rc])
                        fw.op('dve', lambda e: e.tensor_tensor(out=ob_.t[:], in0=Ov[:, :, 0:64], in1=rc.t[:, :, :].to_broadcast([128, 4, 64]),
                                                               op=ALU.mult), reads=[O, rc], writes=[ob_])
                        fw.dma('sp', om_d[qc * 512:(qc + 1) * 512, h * 64:(h + 1) * 64].rearrange("(i p) d -> p i d", p=128), ob_.t[:],
                               reads=[ob_], sem=ob_)

                gateA(0)
                gateB(0)
                for h in range(8):
                    if h + 1 < 8:
                        gateA(h + 1)
                    attn(h)
                    if h + 1 < 8:
                        gateB(h + 1)

            if "D" in phases:
              with fw.scope():
                V = fw.sb("dV", [128, NT, 520], BF16, dma=True)
                fw.dma('sp', V.t[:], dva.rearrange("(n p) c -> p n c", p=128), writes=[V], sem=V)
                K2 = fw.sb("K2", [128, 4, S], BF16, dma=True)
                fw.dma('sp', K2.t[:], scrT[3].rearrange("(hp two d) t -> (two d) hp t", two=2, d=64), writes=[K2], sem=K2)
                ki = fw.sb("kiT", [64, S], BF16, dma=True)
                fw.dma('sp', ki.t[:], kiT_d[:, :], writes=[ki], sem=ki)
                negtri = fw.sb("negtri", [128, 128], F32, dma=True)
                fw.dma('sp', negtri.t[:], negtri_d[:, :], writes=[negtri], sem=negtri)
                cpow = fw.sb("cpow", [128, KBIS], F32, dma=True)
                fw.dma('sp', cpow.t[:], cpow_d[:, :], writes=[cpow], sem=cpow)
                QI = [fw.sb(f"QI{i}", [64, 8, 128], BF16, dma=True) for i in range(2)]
                Q2 = [fw.sb(f"Q2{i}", [128, 4, 128], BF16, dma=True) for i in range(3)]
                i4 = fw.sb("i4", [128, 4, 128], BF16)
                fw.op('dve', lambda e: e.tensor_copy(out=i4.t[:], in_=ident.t[:, :].unsqueeze(1).to_broadcast([128, 4, 128])), reads=[ident], writes=[i4])
                score = [fw.sb(f"score{i}", [128, S], F32) for i in range(2)]
                junk = fw.sb("junk", [128, S], BF16)
                mb = [fw.sb(f"mb{i}", [128, S], BF16) for i in range(2)]
                rlb = [fw.sb(f"rl{i}", [128, 512], BF16) for i in range(3)]
                dg = [fw.sb(f"dg{i}", [128, 8, 128], BF16) for i in range(2)]
                scps = fw.ps("scps", [128, 512], F32)
                lps = [fw.ps(f"lps{i}", [128, 512], F32) for i in range(2)]
                sp3 = [fw.ps(f"dsp{i}", [128, 512], F32) for i in range(3)]
                opsd = [fw.ps(f"dop{e_}", [128, 512], F32) for e_ in range(2)]
                ptd = [fw.sb(f"dpt{i}", [128, 2, 512], BF16) for i in range(2)]
                sm = [fw.sb(f"sm{i}", [128, 8], F32) for i in range(2)]
                dkt = [fw.sb(f"dk{i}", [128, KBIS], F32) for i in range(2)]
                recd = [fw.sb(f"drec{e_}", [128, 4, 1], F32) for e_ in range(2)]
                osbd = [fw.sb(f"dosb{i}", [128, 4, 2, 64], F32, dma=True) for i in range(2)]
                qiv = scrT[4].rearrange("(h d) t -> d h t", d=64)
                q2v = scrT[2].rearrange("(hp two d) t -> (two d) hp t", two=2, d=64)
                cnt_ = {"li": 0, "ti": 0, "ai": 0}

                def indexer(qt):
                    L = (qt + 1) * 128
                    sc_ = score[qt % 2]
                    qi_ = QI[qt % 2]
                    fw.dma('sp', qi_.t[:], qiv[:, :, qt * 128:(qt + 1) * 128], writes=[qi_], sem=qi_)
                    fw.dma('sp', Q2[qt % 3].t[:], q2v[:, :, qt * 128:(qt + 1) * 128], writes=[Q2[qt % 3]], sem=Q2[qt % 3])
                    dg_ = dg[qt % 2]
                    fw.op('dve', lambda e: e.tensor_tensor(out=dg_.t[:], in0=ident.t[:, :].unsqueeze(1).to_broadcast([128, 8, 128]),
                                                           in1=WI.t[:, qt, :].unsqueeze(2).to_broadcast([128, 8, 128]), op=ALU.mult),
                          reads=[ident, WI], writes=[dg_])
                    nch = (L + 511) // 512
                    for ch in range(nch):
                        ncol = min(512, L - ch * 512)
                        cs = slice(ch * 512, ch * 512 + ncol)

                        def logit(h):
                            lp = lps[cnt_["li"] % 2]
                            cnt_["li"] += 1
                            R = rlb[cnt_["ti"] % 3]
                            cnt_["ti"] += 1
                            fw.op('pe', lambda e: e.matmul(lp.t[:, 0:ncol], lhsT=qi_.t[0:64, h, :], rhs=ki.t[0:64, cs], start=True, stop=True),
                                  reads=[qi_, ki], writes=[lp])
                            fw.op('act', lambda e: e.activation(out=R.t[:, 0:ncol], in_=lp.t[:, 0:ncol], func=AF.Relu), reads=[lp], writes=[R])
                            return R

                        pend = logit(0)
                        for h in range(8):
                            nxt = logit(h + 1) if h + 1 < 8 else None
                            R = pend
                            fw.op('pe', lambda e: e.matmul(scps.t[:, 0:ncol], lhsT=dg_.t[:, h, :], rhs=R.t[:, 0:ncol], start=(h == 0), stop=(h == 7)),
                                  reads=[dg_, R], writes=[scps])
                            pend = nxt
                        fw.op('act', lambda e: e.copy(out=sc_.t[:, cs], in_=scps.t[:, 0:ncol]), reads=[scps], writes=[sc_])

                def select(qt):
                    L = (qt + 1) * 128
                    sc_ = score[qt % 2]
                    s_ = sm[qt % 2]
                    dk_ = dkt[qt % 2]
                    mb_ = mb[qt % 2]
                    if qt >= 2:
                        fw.op('dve', lambda e: e.reduce_max(out=s_.t[:, 0:1], in_=sc_.t[:, 0:L], axis=AX.X), reads=[sc_], writes=[s_])
                        fw.op('dve', lambda e: e.tensor_reduce(out=s_.t[:, 1:2], in_=sc_.t[:, 0:L], axis=AX.X, op=ALU.min), reads=[sc_], writes=[s_])
                        fw.op('dve', lambda e: e.scalar_tensor_tensor(out=s_.t[:, 2:3], in0=s_.t[:, 1:2], scalar=-1.0, in1=s_.t[:, 0:1],
                                                                      op0=ALU.mult, op1=ALU.max), reads=[s_], writes=[s_])
                    fw.op('pool', lambda e: e.tensor_tensor(out=sc_.t[:, qt * 128:L], in0=sc_.t[:, qt * 128:L], in1=negtri.t[:], op=ALU.add),
                          reads=[sc_, negtri], writes=[sc_])
                    if qt >= 2:
                        fw.op('dve', lambda e: e.tensor_scalar(out=dk_.t[:], in0=cpow.t[:], scalar1=s_.t[:, 2:3], scalar2=None, op0=ALU.mult),
                              reads=[cpow, s_], writes=[dk_])
                        fw.op('dve', lambda e: e.memset(s_.t[:, 3:4], 0.0), writes=[s_])
                        for k in range(KBIS):
                            fw.op('dve', lambda e: e.tensor_scalar(out=junk.t[:, 0:L], in0=sc_.t[:, 0:L], scalar1=s_.t[:, 3:4], scalar2=0.0,
                                                                   op0=ALU.is_ge, op1=ALU.add, accum_out=s_.t[:, 4:5]),
                                  reads=[sc_, s_], writes=[junk, s_])
                            last = (k == KBIS - 1)
                            fw.op('dve', lambda e: e.tensor_scalar(out=s_.t[:, 5:6], in0=s_.t[:, 4:5], scalar1=255.5,
                                                                   scalar2=(1.0 if last else 0.5), op0=ALU.is_ge, op1=ALU.subtract),
                                  reads=[s_], writes=[s_])
                            dst = s_.t[:, 6:7] if last else s_.t[:, 3:4]
                            fw.op('dve', lambda e: e.scalar_tensor_tensor(out=dst, in0=s_.t[:, 5:6], scalar=dk_.t[:, k:k + 1], in1=s_.t[:, 3:4],
                                                                          op0=ALU.mult, op1=ALU.add), reads=[s_, dk_], writes=[s_])
                        fw.op('dve', lambda e: e.tensor_scalar(out=mb_.t[:, 0:L], in0=sc_.t[:, 0:L], scalar1=s_.t[:, 6:7], scalar2=-BIG,
                                                               op0=ALU.is_lt, op1=ALU.mult), reads=[sc_, s_], writes=[mb_])
                    else:
                        fw.op('dve', lambda e: e.tensor_scalar(out=mb_.t[:, 0:L], in0=sc_.t[:, 0:L], scalar1=-1e29, scalar2=-BIG,
                                                               op0=ALU.is_lt, op1=ALU.mult), reads=[sc_], writes=[mb_])

                def attend(qt):
                    mb_ = mb[qt % 2]
                    q2_ = Q2[qt % 3]
                    def qk(st):
                        ai_ = cnt_["ai"]
                        PT = ptd[ai_ % 2]
                        cnt_["ai"] += 1
                        for e_ in range(2):
                            bank = sp3[(2 * ai_ + e_) % 3]
                            for hp in range(4):
                                fw.op('pe', lambda e: e.matmul(bank.t[:, hp * 128:(hp + 1) * 128],
                                                               lhsT=K2.t[64 * e_:64 * e_ + 64, hp, st * 128:(st + 1) * 128],
                                                               rhs=q2_.t[64 * e_:64 * e_ + 64, hp, :], start=(hp == 0), stop=False),
                                      reads=[K2, q2_], writes=[bank])
                            fw.op('pe', lambda e: e.matmul(bank.t[:], lhsT=mb_.t[:, st * 128:(st + 1) * 128],
                                                           rhs=i4.t[:, :, :].rearrange("p a t -> p (a t)"), start=False, stop=True),
                                  reads=[mb_, i4], writes=[bank])
                            fw.op('act', lambda e: e.activation(out=PT.t[:, e_, :], in_=bank.t[:], func=AF.Exp, scale=0.125),
                                  reads=[bank], writes=[PT])
                        return PT

                    pend = qk(0)
                    for st in range(qt + 1):
                        nxt = qk(st + 1) if st + 1 <= qt else None
                        PT = pend
                        for e_ in range(2):
                            for hp in range(4):
                                hh = 2 * hp + e_
                                fw.op('pe', lambda e: e.matmul(opsd[e_].t[:, hp * 65:(hp + 1) * 65], lhsT=PT.t[:, e_, hp * 128:(hp + 1) * 128],
                                                               rhs=V.t[:, st, hh * 65:(hh + 1) * 65], start=(st == 0 and hp == 0), stop=(st == qt)),
                                      reads=[PT, V], writes=[opsd[e_]])
                        pend = nxt
                    ob_ = osbd[qt % 2]
                    for e_ in range(2):
                        Ov = opsd[e_].t[:, 0:260].rearrange("p (i d) -> p i d", d=65)
                        fw.op('dve', lambda e: e.reciprocal(out=recd[e_].t[:], in_=Ov[:, :, 64:65]), reads=[opsd[e_]], writes=[recd[e_]])
                        fw.op('dve', lambda e: e.tensor_tensor(out=ob_.t[:, :, e_, :], in0=Ov[:, :, 0:64],
                                                               in1=recd[e_].t[:, :, :].to_broadcast([128, 4, 64]), op=ALU.mult),
                              reads=[opsd[e_], recd[e_]], writes=[ob_])
                    fw.dma('sp', od_d[qt * 128:(qt + 1) * 128, :], ob_.t[:, :, :, :].rearrange("p a e d -> p (a e d)"), reads=[ob_], sem=ob_)

                indexer(0)
                for qt in range(NT):
                    if qt + 1 < NT:
                        indexer(qt + 1)
                    select(qt)
                    if qt >= 1:
                        attend(qt - 1)
                attend(NT - 1)

            if "E" in phases:
              with fw.scope():
                Wo = fw.sb("Wo", [128, 8, D], BF16, dma=True)
                fw.dma_group('pool', [(Wo.t[:, k, :], w_out[l, k * 128:(k + 1) * 128, :]) for k in range(8)], writes=[Wo], sem=Wo)
                gmo = fw.sb("gmo", [128, D], F32, dma=True)
                fw.dma_group('sp', [(gmo.t[:, 0:512], g_moba_out[l, :].partition_broadcast(128)),
                                    (gmo.t[:, 512:1024], g_dsa_out[l, :].partition_broadcast(128))], writes=[gmo], sem=gmo)
                G = fw.sb("Gm", [128, D], F32, dma=True)
                fw.dma('sp', G.t[:], gbc_d[l, 0], writes=[G], sem=G)
                ot = [fw.sb(f"ot{i}", [128, D], F32, dma=True) for i in range(2)]
                xt = [fw.sb(f"ext{i}", [128, D], F32, dma=True) for i in range(3)]
                sq = fw.sb("esq", [128, D], BF16)
                ss = [fw.sb(f"ess{i}", [128, 4], F32) for i in range(3)]
                rs = [fw.sb(f"ers{i}", [128, 4], F32) for i in range(3)]
                on = [fw.sb(f"on{i}", [128, D], BF16) for i in range(2)]
                tps = [fw.ps(f"etp{i}", [128, 8, 128], BF16) for i in range(2)]
                oT = [fw.sb(f"oT{i}", [128, 8, 128], BF16) for i in range(3)]
                yps = [[fw.ps(f"yps{i}{j}", [128, 512], F32) for j in range(2)] for i in range(2)]
                ysb = [fw.sb(f"ysb{i}", [128, D], F32, dma=True) for i in range(2)]

                def s1a(tt):
                    o_, x_, s_, r_, n_ = ot[tt % 2], xt[tt % 3], ss[tt % 3], rs[tt % 3], on[tt % 2]
                    rows = slice(tt * 128, (tt + 1) * 128)
                    fw.dma_group('sp', [(o_.t[:, 0:512], om_d[rows, :]), (o_.t[:, 512:1024], od_d[rows, :])], writes=[o_], sem=o_)
                    fw.dma('sp', x_.t[:], xin[rows, :], writes=[x_], sem=x_)
                    fw.op('dve', lambda e: e.memset(s_.t[:], 0.0), writes=[s_])
                    for j in range(2):
                        fw.op('act', lambda e: e.activation(out=sq.t[:, 0:512], in_=o_.t[:, j * 512:(j + 1) * 512], func=AF.Square,
                                                            accum_out=s_.t[:, j:j + 1]), reads=[o_], writes=[sq, s_])
                    fw.op('dve', lambda e: e.tensor_scalar(out=s_.t[:, 0:2], in0=s_.t[:, 0:2], scalar1=1.0 / 512, scalar2=EPS, op0=ALU.mult, op1=ALU.add),
                          reads=[s_], writes=[s_])
                    fw.op('pool', lambda e: e.tensor_tensor(out=r_.t[:, 0:2], in0=s_.t[:, 0:2], in1=nh.t[:, 0:1].to_broadcast([128, 2]), op=ALU.pow),
                          reads=[s_, nh], writes=[r_])
                    for j in range(2):
                        fw.op('dve', lambda e: e.scalar_tensor_tensor(out=n_.t[:, j * 512:(j + 1) * 512], in0=o_.t[:, j * 512:(j + 1) * 512],
                                                                      scalar=r_.t[:, j:j + 1], in1=gmo.t[:, j * 512:(j + 1) * 512],
                                                                      op0=ALU.mult, op1=ALU.mult), reads=[o_, r_, gmo], writes=[n_])

                def s1b(tt):
                    n_, tp, oT_ = on[tt % 2], tps[tt % 2], oT[tt % 3]
                    for k in range(8):
                        fw.op('pe', lambda e: e.transpose(out=tp.t[:, k, :], in_=n_.t[:, k * 128:(k + 1) * 128], identity=ident.t[:]),
                              reads=[n_, ident], writes=[tp])
                    fw.op('act', lambda e: e.copy(out=oT_.t[:], in_=tp.t[:]), reads=[tp], writes=[oT_])

                def s2a(tt):
                    oT_, yp = oT[tt % 3], yps[tt % 2]
                    for j in range(2):
                        for k in range(8):
                            fw.op('pe', lambda e: e.matmul(yp[j].t[:], lhsT=oT_.t[:, k, :], rhs=Wo.t[:, k, j * 512:(j + 1) * 512],
                                                           start=(k == 0), stop=(k == 7)), reads=[oT_, Wo], writes=[yp[j]])

                def s2b(tt):
                    x_, s_, r_, yp, y_ = xt[tt % 3], ss[tt % 3], rs[tt % 3], yps[tt % 2], ysb[tt % 2]
                    rows = slice(tt * 128, (tt + 1) * 128)
                    for j in range(2):
                        fw.op('act', lambda e: e.activation(out=sq.t[:, 512:1024], in_=yp[j].t[:], func=AF.Square, accum_out=s_.t[:, 2 + j:3 + j]),
                              reads=[yp[j]], writes=[sq, s_])
                    fw.op('dve', lambda e: e.tensor_tensor(out=s_.t[:, 2:3], in0=s_.t[:, 2:3], in1=s_.t[:, 3:4], op=ALU.add), reads=[s_], writes=[s_])
                    _rms_rstd(fw, (s_, s_.t[:, 2:3]), (r_, r_.t[:, 2:3]), nh, D)
                    for j in range(2):
                        fw.op('dve', lambda e: e.scalar_tensor_tensor(out=y_.t[:, j * 512:(j + 1) * 512], in0=yp[j].t[:], scalar=r_.t[:, 2:3],
                                                                      in1=G.t[:, j * 512:(j + 1) * 512], op0=ALU.mult, op1=ALU.mult),
                              reads=[yp[j], r_, G], writes=[y_])
                    fw.op('pool', lambda e: e.tensor_tensor(out=y_.t[:], in0=y_.t[:], in1=x_.t[:], op=ALU.add), reads=[y_, x_], writes=[y_])
                    fw.dma('sp', x1_d[rows, :], y_.t[:], reads=[y_], sem=y_)

                s1a(0)
                s1b(0)
                s1a(1)
                s1b(1)
                for tt in range(NT):
                    s2a(tt)
                    if tt + 2 < NT:
                        s1a(tt + 2)
                        s1b(tt + 2)
                    s2b(tt)

            if "F" in phases:
              with fw.scope():
                Wa = fw.sb("Wa", [128, 8, DFF], BF16, dma=True)
                Wl = fw.sb("Wl", [128, 8, DFF], BF16, dma=True)
                Wd = fw.sb("Wd", [128, NFC, D], BF16, dma=True)
                fw.dma_group('pool', [(Wa.t[:, k, :], w_up_act[l, k * 128:(k + 1) * 128, :]) for k in range(8)], writes=[Wa], sem=Wa)
                fw.dma_group('pool', [(Wl.t[:, k, :], w_up_lin[l, k * 128:(k + 1) * 128, :]) for k in range(8)], writes=[Wl], sem=Wl)
                fw.dma_group('pool', [(Wd.t[:, f, :], w_down[l, f * 128:(f + 1) * 128, :]) for f in range(NFC)], writes=[Wd], sem=Wd)
                wc = fw.sb("wc", [128, NFC, 3], F32, dma=True)
                bcv = fw.sb("bcv", [128, NFC], F32, dma=True)
                fw.dma('sp', wc.t[:], w_conv_col[l], writes=[wc], sem=wc)
                fw.dma('sp', bcv.t[:], b_conv_col[l], writes=[bcv], sem=bcv)
                G = fw.sb("Gf", [128, D], F32, dma=True)
                fw.dma('sp', G.t[:], gbc_d[l, 1], writes=[G], sem=G)
                carry = fw.sb("carry", [128, NFC, 2], F32)
                fw.op('dve', lambda e: e.memset(carry.t[:], 0.0), writes=[carry])
                xt = fw.sb("fxt", [128, D], F32, dma=True)
                sq = fw.sb("fsq", [128, D], BF16)
                ss = fw.sb("fss", [128, 4], F32)
                rs = fw.sb("frs", [128, 4], F32)
                fxn = [fw.sb(f"fxn{i}", [128, D], BF16) for i in range(4)]
                fss = [fw.sb(f"fss4{i}", [128, 1], F32) for i in range(4)]
                frs = [fw.sb(f"frs4{i}", [128, 1], F32) for i in range(4)]
                tps = [fw.ps(f"ftp{i}", [128, 8, 128], BF16) for i in range(2)]
                hT = fw.sb("fhT", [128, 8, 512], BF16)
                ups = [fw.ps(f"ups{i}", [128, 512], F32) for i in range(2)]
                lps = [fw.ps(f"flps{i}", [128, 512], F32) for i in range(2)]
                yps = [fw.ps(f"fyps{j}", [128, 512], F32) for j in range(2)]
                ubuf = [fw.sb(f"ubuf{i}", [128, 514], F32) for i in range(2)]
                av = [fw.sb(f"av{i}", [128, 512], F32) for i in range(2)]
                gT = fw.sb("gT", [128, NFC, 512], BF16)
                xr = fw.sb("xr", [128, D], F32, dma=True)
                ysb = fw.sb("fysb", [128, D], F32, dma=True)
                ssv = fw.view("fssv", ss.t)
                rsv = fw.view("frsv", rs.t)
                fi = 0
                def fnormA(c):
                    for i in range(4):
                        tt = c * 4 + i
                        s_, r_, n_ = fss[i], frs[i], fxn[i]
                        fw.dma('sp', xt.t[:], x1_d[tt * 128:(tt + 1) * 128, :], writes=[xt], sem=xt)
                        fw.op('dve', lambda e: e.memset(s_.t[:], 0.0), writes=[s_])
                        fw.op('act', lambda e: e.activation(out=sq.t[:], in_=xt.t[:], func=AF.Square, accum_out=s_.t[:, 0:1]),
                              reads=[xt], writes=[sq, s_])
                        _rms_rstd(fw, (s_, s_.t[:, 0:1]), (r_, r_.t[:, 0:1]), nh, D)
                        fw.op('dve', lambda e: e.tensor_scalar(out=n_.t[:], in0=xt.t[:], scalar1=r_.t[:, 0:1], scalar2=None, op0=ALU.mult),
                              reads=[xt, r_], writes=[n_])

                def fnormB(c):
                    for i in range(4):
                        tt = c * 4 + i
                        tp, n_ = tps[tt % 2], fxn[i]
                        for k in range(8):
                            fw.op('pe', lambda e: e.transpose(out=tp.t[:, k, :], in_=n_.t[:, k * 128:(k + 1) * 128], identity=ident.t[:]),
                                  reads=[n_, ident], writes=[tp])
                        for k in range(8):
                            fw.op('act', lambda e: e.activation(out=hT.t[:, k, i * 128:(i + 1) * 128], in_=tp.t[:, k, :], func=AF.Identity,
                                                                scale=AB.t[:, l, 2, k:k + 1], bias=AB.t[:, l, 3, k:k + 1]),
                                  reads=[tp, AB], writes=[hT])

                fnormA(0)
                fnormB(0)
                for c in range(NCH):
                    if c + 1 < NCH:
                        fnormA(c + 1)
                    for fc in range(NFC):
                        U, Lp, ub, a_ = ups[fi % 2], lps[fi % 2], ubuf[fi % 2], av[fi % 2]
                        fi += 1
                        fs = slice(fc * 128, (fc + 1) * 128)
                        for k in range(8):
                            fw.op('pe', lambda e: e.matmul(U.t[:], lhsT=Wa.t[:, k, fs], rhs=hT.t[:, k, :], start=(k == 0), stop=(k == 7)),
                                  reads=[Wa, hT], writes=[U])
                        for k in range(8):
                            fw.op('pe', lambda e: e.matmul(Lp.t[:], lhsT=Wl.t[:, k, fs], rhs=hT.t[:, k, :], start=(k == 0), stop=(k == 7)),
                                  reads=[Wl, hT], writes=[Lp])
                        fw.op('act', lambda e: e.copy(out=ub.t[:, 2:514], in_=U.t[:]), reads=[U], writes=[ub])
                        fw.op('act', lambda e: e.copy(out=ub.t[:, 0:2], in_=carry.t[:, fc, :]), reads=[carry], writes=[ub])
                        fw.op('dve', lambda e: e.tensor_scalar(out=a_.t[:], in0=ub.t[:, 2:514], scalar1=wc.t[:, fc, 2:3], scalar2=bcv.t[:, fc:fc + 1],
                                                               op0=ALU.mult, op1=ALU.add), reads=[ub, wc, bcv], writes=[a_])
                        fw.op('dve', lambda e: e.scalar_tensor_tensor(out=a_.t[:], in0=ub.t[:, 1:513], scalar=wc.t[:, fc, 1:2], in1=a_.t[:],
                                                                      op0=ALU.mult, op1=ALU.add), reads=[ub, wc, a_], writes=[a_])
                        fw.op('dve', lambda e: e.scalar_tensor_tensor(out=a_.t[:], in0=ub.t[:, 0:512], scalar=wc.t[:, fc, 0:1], in1=a_.t[:],
                                                                       op0=ALU.mult, op1=ALU.add), reads=[ub, wc, a_], writes=[a_])
                        fw.op('act', lambda e: e.copy(out=carry.t[:, fc, :], in_=ub.t[:, 512:514]), reads=[ub], writes=[carry])
                        fw.op('act', lambda e: e.activation(out=a_.t[:], in_=a_.t[:], func=AF.Gelu_apprx_tanh), reads=[a_], writes=[a_])
                        fw.op('dve', lambda e: e.tensor_tensor(out=gT.t[:, fc, :], in0=a_.t[:], in1=Lp.t[:], op=ALU.mult),
                              reads=[a_, Lp], writes=[gT])
                    if c + 1 < NCH:
                        fnormB(c + 1)
                    for i in range(4):
                        tt = c * 4 + i
                        rows = slice(tt * 128, (tt + 1) * 128)
                        fw.dma('sp', xr.t[:], x1_d[rows, :], writes=[xr], sem=xr)
                        fw.op('dve', lambda e: e.memset(ss.t[:, 2:4], 0.0), writes=[ssv])
                        for j in range(2):
                            for fc in range(NFC):
                                fw.op('pe', lambda e: e.matmul(yps[j].t[:], lhsT=gT.t[:, fc, i * 128:(i + 1) * 128], rhs=Wd.t[:, fc, j * 512:(j + 1) * 512],
                                                               start=(fc == 0), stop=(fc == NFC - 1)), reads=[gT, Wd], writes=[yps[j]])
                            fw.op('act', lambda e: e.activation(out=sq.t[:, 0:512], in_=yps[j].t[:], func=AF.Square, accum_out=ss.t[:, 2 + j:3 + j]),
                                  reads=[yps[j]], writes=[sq, ssv])
                        fw.op('dve', lambda e: e.tensor_tensor(out=ss.t[:, 2:3], in0=ss.t[:, 2:3], in1=ss.t[:, 3:4], op=ALU.add), reads=[ssv], writes=[ssv])
                        _rms_rstd(fw, (ssv, ss.t[:, 2:3]), (rsv, rs.t[:, 2:3]), nh, D)
                        for j in range(2):
                            fw.op('dve', lambda e: e.scalar_tensor_tensor(out=ysb.t[:, j * 512:(j + 1) * 512], in0=yps[j].t[:], scalar=rs.t[:, 2:3],
                                                                          in1=G.t[:, j * 512:(j + 1) * 512], op0=ALU.mult, op1=ALU.mult),
                                  reads=[yps[j], rsv, G], writes=[ysb])
                        fw.op('pool', lambda e: e.tensor_tensor(out=ysb.t[:], in0=ysb.t[:], in1=xr.t[:], op=ALU.add), reads=[ysb, xr], writes=[ysb])
                        fw.dma('sp', xout[rows, :], ysb.t[:], reads=[ysb], sem=ysb)
        fw.barrier()
    return nc


def _consts():
    bf = ml_dtypes.bfloat16
    pos = np.arange(S, dtype=np.float32)
    inv = (np.float32(500000.0) ** (-np.arange(0, 16, 2, dtype=np.float32) / np.float32(16))).astype(np.float32)
    ang = (pos[None, :] * inv[:, None]).astype(np.float32)
    cos, sin = np.cos(ang).astype(np.float32), np.sin(ang).astype(np.float32)
    C = np.ones((128, S), np.float32)
    Sg = np.zeros((128, S), np.float32)
    for p_ in range(128):
        d = p_ % 64
        if d < 16:
            C[p_] = cos[d % 8]
            Sg[p_] = -sin[d % 8] if d < 8 else sin[d % 8]
    i = np.arange(128)
    tri = (i[None, :] >= i[:, None]).astype(np.float32).astype(bf)
    negtri = np.where(i[None, :] <= i[:, None], 0.0, -1e30).astype(np.float32)
    onehot = (np.arange(S)[None, :] // 256 == np.arange(16)[:, None]).astype(np.float32).astype(bf)
    tt = np.repeat(np.arange(NT), 16)
    n = np.tile(np.arange(16), NT)
    cur = tt // 2
    cbsel = np.where(n >= cur, -BIG, 0.0).astype(np.float32)
    cblt = np.where(n < cur, -BIG, 0.0).astype(np.float32)
    cbfin = np.where(n > cur, -BIG, 0.0).astype(np.float32)
    rep = lambda v: np.ascontiguousarray(np.broadcast_to(v[None, :], (128, v.shape[0])))
    cpow = (2.0 ** (-np.arange(KBIS, dtype=np.float64))).astype(np.float32)
    return {
        "ropeC": C, "ropeS": Sg, "ident": np.eye(128, dtype=np.float32).astype(bf), "tri": tri, "negtri": negtri,
        "onehot": onehot, "cbsel": rep(cbsel), "cblt": rep(cblt), "cbfin": rep(cbfin), "cpow": rep(cpow),
    }


def _perm_w_in(w_in):
    offs = {"mq": 0, "mk": 512, "mv": 1024, "dq": 1536, "dk": 2048, "dv": 2560, "qi": 3072, "ki": 3584, "wi": 3648}
    j = np.arange(64)
    perm = np.where(j < 8, j + 8, np.where(j < 16, j - 8, j))
    cols = []
    for g in ("mq", "mk", "dq", "dk", "qi"):
        base = offs[g]
        cols.append(base + np.arange(512))
        cols.append(base + (np.arange(512) // 64) * 64 + perm[np.arange(512) % 64])
    cols.append(offs["ki"] + np.arange(64))
    cols.append(offs["ki"] + perm)
    cols.append(offs["mv"] + np.arange(512))
    cols.append(offs["dv"] + np.arange(512))
    cols.append(offs["wi"] + np.arange(8))
    cols = np.concatenate(cols)
    assert cols.shape[0] == NCOLP
    return np.ascontiguousarray(w_in[:, :, cols])


def _shared_inputs(w_ada, b_ada, g_pre_mix, w_in, g_moba_out, g_dsa_out, w_out, g_post_mix, g_pre_ffn,
                   w_up_act, w_up_lin, w_conv, b_conv, w_down, g_post_ffn):
    f = lambda a: np.ascontiguousarray(np.asarray(a, dtype=np.float32))
    col8 = lambda g: np.ascontiguousarray(f(g).reshape(2, 8, 128).transpose(0, 2, 1))
    sh = {
        "w_ada": f(w_ada), "b_ada": f(b_ada),
        "b_ada_col": np.ascontiguousarray(f(b_ada).reshape(2, 48, 128).transpose(0, 2, 1)),
        "g_pre_mix_col": col8(g_pre_mix), "g_pre_ffn_col": col8(g_pre_ffn),
        "g_post_mix": f(g_post_mix), "g_post_ffn": f(g_post_ffn),
        "g_moba_out": f(g_moba_out), "g_dsa_out": f(g_dsa_out),
        "w_in_p": _perm_w_in(f(w_in)), "w_out": f(w_out), "w_up_act": f(w_up_act), "w_up_lin": f(w_up_lin),
        "w_conv_col": np.ascontiguousarray(f(w_conv).reshape(2, 3, NFC, 128).transpose(0, 3, 2, 1)),
        "b_conv_col": np.ascontiguousarray(f(b_conv).reshape(2, NFC, 128).transpose(0, 2, 1)),
        "w_down": f(w_down),
    }
    sh.update(_consts())
    return sh


def _core_inputs(x, c, b, shared):
    m = dict(shared)
    m["x"] = np.ascontiguousarray(np.asarray(x[b], dtype=np.float32))
    m["cT"] = np.ascontiguousarray(np.asarray(c[b], dtype=np.float32).reshape(8, 128).T)
    return m


def kernel(x, c, w_ada, b_ada, g_pre_mix, w_in, g_moba_out, g_dsa_out, w_out, g_post_mix,
           g_pre_ffn, w_up_act, w_up_lin, w_conv, b_conv, w_down, g_post_ffn):
    x = np.asarray(x)
    c = np.asarray(c)
    shared = _shared_inputs(w_ada, b_ada, g_pre_mix, w_in, g_moba_out, g_dsa_out, w_out, g_post_mix, g_pre_ffn,
                            w_up_act, w_up_lin, w_conv, b_conv, w_down, g_post_ffn)
    nc = build_nc()
    in_maps = [_core_inputs(x, c, b, shared) for b in range(8)]
    res = run_bass_kernel_spmd(nc, in_maps, core_ids=list(range(8)))
    return np.stack([np.asarray(r["out"], dtype=np.float32) for r in res.results], axis=0)
```
